# Optimizing a Trainium2 kernel written in Bass

```python
import math
import jax, jax.numpy as jnp
from jax import lax
import numpy as np

D_MODEL = 1024
BATCH = 4
SEQ = 4096
DEPTH = 1

GRID_W = 64
CTX_LEN = 256
NORM_EPS = 1e-6
ATT_HEADS = 8
ATT_DK = 64
ATT_DV = 2 * ATT_DK
ATT_W = ATT_HEADS * ATT_DV
ROPE_BASE = 10000.0
Q_BLOCK = 128
SSM_GROUP = 16
SSM_W = D_MODEL // 2
SSM_GROUPS = SSM_W // SSM_GROUP
SSM_STATE = 64
DT_MIN = 1e-3
DT_MAX = 1e-1
PEER_HEADS = 8
PEER_NKEYS = 128
PEER_EXPERTS = PEER_NKEYS * PEER_NKEYS
PEER_TOPK = 16
PEER_DKEY = 128
PEER_BLOCK = 128
N_Q = 2 * ATT_HEADS * ATT_DK
N_K = 2 * ATT_HEADS * ATT_DK
N_V = ATT_W
N_IN_MIX = N_Q + N_K + N_V + SSM_W
N_IN = N_IN_MIX + 2 * D_MODEL

kernel_name = 'hybrid_diffattn_s5_peer_dit_block'


def _rmsnorm(h, g):
    hf = h.astype(jnp.float32)
    hf = hf * lax.rsqrt(jnp.mean(hf * hf, axis=-1, keepdims=True) + NORM_EPS)
    return (hf * g.astype(jnp.float32)).astype(h.dtype)


def _modulate(h, shift, scale):
    return h * (1 + scale) + shift


def _axial_angles(pos):
    n_freq = ATT_DK // 4
    inv = ROPE_BASE ** (-2.0 * jnp.arange(n_freq, dtype=jnp.float32) / (ATT_DK // 2))
    return pos.astype(jnp.float32)[:, None] * inv[None, :]


def _rope_1d(h, ang):
    f = ang.shape[-1]
    cos = jnp.cos(ang)[:, None, None, :]
    sin = jnp.sin(ang)[:, None, None, :]
    h1, h2 = h[..., :f], h[..., f:]
    return jnp.concatenate([h1 * cos - h2 * sin, h2 * cos + h1 * sin], axis=-1)


def _axial_rope(h, ang_r, ang_c):
    half = ATT_DK // 2
    hf = h.astype(jnp.float32)
    out = jnp.concatenate([_rope_1d(hf[..., :half], ang_r), _rope_1d(hf[..., half:], ang_c)], axis=-1)
    return out.astype(h.dtype)


def _diff_attend(q, k, v, lam):
    s = jnp.einsum('bqhmd,bkhmd->mbhqk', q, k, preferred_element_type=jnp.float32) * (ATT_DK ** -0.5)
    p = jax.nn.softmax(s, axis=-1)
    w = p[0] - lam * p[1]
    return jnp.einsum('bhqk,bkhe->bqhe', w.astype(v.dtype), v)


def _diff_attend_blocked(q, k, v, lam):
    b, l = q.shape[:2]
    nb = l // Q_BLOCK
    qb = jnp.moveaxis(q.reshape(b, nb, Q_BLOCK, ATT_HEADS, 2, ATT_DK), 1, 0)
    o = lax.map(lambda qblk: _diff_attend(qblk, k, v, lam), qb)
    return jnp.moveaxis(o, 0, 1).reshape(b, l, ATT_HEADS, ATT_DV)


def _diff_post(o, g, lam_init):
    b, l = o.shape[:2]
    return (_rmsnorm(o, g) * (1.0 - lam_init)).reshape(b, l, ATT_W)


def _s5_discretize(a_re, a_im, log_dt, b_re, b_im):
    ar = a_re.astype(jnp.float32)
    ai = a_im.astype(jnp.float32)
    dt = jnp.exp(log_dt.astype(jnp.float32))[:, None]
    mag = jnp.exp(ar * dt)
    abar_re = mag * jnp.cos(ai * dt)
    abar_im = mag * jnp.sin(ai * dt)
    den = ar * ar + ai * ai
    nr = abar_re - 1.0
    ni = abar_im
    fr = (nr * ar + ni * ai) / den
    fi = (ni * ar - nr * ai) / den
    br = b_re.astype(jnp.float32)
    bi = b_im.astype(jnp.float32)
    bbar_re = fr[..., None] * br - fi[..., None] * bi
    bbar_im = fr[..., None] * bi + fi[..., None] * br
    return abar_re, abar_im, bbar_re, bbar_im


def _cplx_combine(e1, e2):
    a1r, a1i, b1r, b1i = e1
    a2r, a2i, b2r, b2i = e2
    return (a2r * a1r - a2i * a1i,
            a2r * a1i + a2i * a1r,
            a2r * b1r - a2i * b1i + b2r,
            a2r * b1i + a2i * b1r + b2i)


def _s5_states(u, disc, h0, reverse):
    abar_re, abar_im, bbar_re, bbar_im = disc
    bu_re = jnp.einsum('gnp,blgp->blgn', bbar_re, u)
    bu_im = jnp.einsum('gnp,blgp->blgn', bbar_im, u)
    if h0 is not None:
        h0_re, h0_im = h0
        first = -1 if reverse else 0
        bu_re = bu_re.at[:, first].add(abar_re * h0_re - abar_im * h0_im)
        bu_im = bu_im.at[:, first].add(abar_re * h0_im + abar_im * h0_re)
    a_re = jnp.broadcast_to(abar_re, bu_re.shape)
    a_im = jnp.broadcast_to(abar_im, bu_im.shape)
    _, _, h_re, h_im = lax.associative_scan(_cplx_combine, (a_re, a_im, bu_re, bu_im), reverse=reverse, axis=1)
    return h_re, h_im


def _s5_readout(c_re, c_im, h):
    return (jnp.einsum('gpn,blgn->blgp', c_re.astype(jnp.float32), h[0])
            - jnp.einsum('gpn,blgn->blgp', c_im.astype(jnp.float32), h[1]))


def _s5_output(u, h_f, h_b, c_re, c_im, d_skip, w_glu, b_glu, dtype):
    b, l = u.shape[:2]
    y = (_s5_readout(c_re[0], c_im[0], h_f) + _s5_readout(c_re[1], c_im[1], h_b)
         + d_skip.astype(jnp.float32).reshape(SSM_GROUPS, SSM_GROUP) * u)
    y = jax.nn.gelu(y.reshape(b, l, SSM_W)).astype(dtype)
    return y * jax.nn.sigmoid(y @ w_glu + b_glu)


def _merge(attn, ssm, gates, w_attn_up, w_ssm_up, w_out):
    g_a, g_s = jnp.split(jax.nn.sigmoid(gates), 2, axis=-1)
    return (g_a * (attn @ w_attn_up) + g_s * (ssm @ w_ssm_up)) @ w_out


def _peer(h, w_query, sub_k1, sub_k2, table_u, table_v):
    b, l, d = h.shape
    half = PEER_DKEY // 2
    n_cand = PEER_TOPK * PEER_TOPK

    def block(tb):
        q = (tb @ w_query).reshape(PEER_BLOCK, PEER_HEADS, PEER_DKEY)
        s1 = jnp.einsum('thd,kd->thk', q[..., :half], sub_k1, preferred_element_type=jnp.float32)
        s2 = jnp.einsum('thd,kd->thk', q[..., half:], sub_k2, preferred_element_type=jnp.float32)
        v1, i1 = lax.top_k(s1, PEER_TOPK)
        v2, i2 = lax.top_k(s2, PEER_TOPK)
        cand_s = (v1[..., :, None] + v2[..., None, :]).reshape(PEER_BLOCK, PEER_HEADS, n_cand)
        cand_i = (i1[..., :, None] * PEER_NKEYS + i2[..., None, :]).reshape(PEER_BLOCK, PEER_HEADS, n_cand)
        top_s, top_j = lax.top_k(cand_s, PEER_TOPK)
        idx = jnp.take_along_axis(cand_i, top_j, axis=-1)
        gate = jax.nn.softmax(top_s, axis=-1)
        u_sel = jnp.take(table_u, idx, axis=0)
        act = jax.nn.gelu(jnp.einsum('thkd,td->thk', u_sel, tb, preferred_element_type=jnp.float32))
        v_sel = jnp.take(table_v, idx, axis=0)
        return jnp.einsum('thk,thkd->td', (gate * act).astype(tb.dtype), v_sel)

    out = lax.map(block, h.reshape(b * l // PEER_BLOCK, PEER_BLOCK, d))
    return out.reshape(b, l, d)


def setup_inputs(seed: int = 0) -> dict:
    key = jax.random.key(seed)
    keys = iter(jax.random.split(key, 48))

    def nrm(shape, scale):
        return jax.random.normal(next(keys), shape, jnp.float32) * scale

    D = D_MODEL
    G, N, P = SSM_GROUPS, SSM_STATE, SSM_GROUP
    half = PEER_DKEY // 2
    a_im_init = math.pi * jnp.arange(N, dtype=jnp.float32)
    return {
        'x': nrm((BATCH, SEQ, D), 1.0),
        'c': nrm((BATCH, D), 1.0),
        'ctx': nrm((BATCH, CTX_LEN, D), 1.0),
        'c_ctx': nrm((D,), 1.0),
        'ada_w': nrm((DEPTH, D, 6 * D), 0.5 * D ** -0.5),
        'ada_b': nrm((DEPTH, 6 * D), 0.02),
        'norm1_g': 1.0 + nrm((DEPTH, D), 0.02),
        'norm2_g': 1.0 + nrm((DEPTH, D), 0.02),
        'w_in': nrm((DEPTH, D, N_IN), D ** -0.5),
        'lambda_q1': nrm((DEPTH, ATT_DK), 0.1),
        'lambda_k1': nrm((DEPTH, ATT_DK), 0.1),
        'lambda_q2': nrm((DEPTH, ATT_DK), 0.1),
        'lambda_k2': nrm((DEPTH, ATT_DK), 0.1),
        'subln_g': 1.0 + nrm((DEPTH, ATT_DV), 0.02),
        'w_attn_up': nrm((DEPTH, ATT_W, D), ATT_W ** -0.5),
        'ssm_a_re': -0.5 + nrm((DEPTH, 2, G, N), 0.01),
        'ssm_a_im': a_im_init + nrm((DEPTH, 2, G, N), 0.01),
        'ssm_log_dt': jax.random.uniform(next(keys), (DEPTH, 2, G), jnp.float32, math.log(DT_MIN), math.log(DT_MAX)),
        'ssm_b_re': nrm((DEPTH, 2, G, N, P), (2 * P) ** -0.5),
        'ssm_b_im': nrm((DEPTH, 2, G, N, P), (2 * P) ** -0.5),
        'ssm_c_re': nrm((DEPTH, 2, G, P, N), (2 * N) ** -0.5),
        'ssm_c_im': nrm((DEPTH, 2, G, P, N), (2 * N) ** -0.5),
        'ssm_d': nrm((DEPTH, SSM_W), 1.0),
        'w_glu': nrm((DEPTH, SSM_W, SSM_W), SSM_W ** -0.5),
        'b_glu': nrm((DEPTH, SSM_W), 0.01),
        'w_ssm_up': nrm((DEPTH, SSM_W, D), SSM_W ** -0.5),
        'w_out': nrm((DEPTH, D, D), D ** -0.5),
        'peer_w_query': nrm((DEPTH, D, PEER_HEADS * PEER_DKEY), D ** -0.5),
        'peer_sub_k1': nrm((DEPTH, PEER_NKEYS, half), half ** -0.5),
        'peer_sub_k2': nrm((DEPTH, PEER_NKEYS, half), half ** -0.5),
        'peer_u': nrm((DEPTH, PEER_EXPERTS, D), D ** -0.5),
        'peer_v': nrm((DEPTH, PEER_EXPERTS, D), 1.0),
        'final_norm_g': 1.0 + nrm((D,), 0.02),
    }


def reference(x, c, ctx, c_ctx, ada_w, ada_b, norm1_g, norm2_g, w_in,
              lambda_q1, lambda_k1, lambda_q2, lambda_k2, subln_g, w_attn_up,
              ssm_a_re, ssm_a_im, ssm_log_dt, ssm_b_re, ssm_b_im, ssm_c_re, ssm_c_im,
              ssm_d, w_glu, b_glu, w_ssm_up, w_out,
              peer_w_query, peer_sub_k1, peer_sub_k2, peer_u, peer_v, final_norm_g):
    b, l, _ = x.shape
    n_ctx = ctx.shape[1]
    ROWS = l // GRID_W
    pos_row = jnp.repeat(jnp.arange(ROWS, dtype=jnp.int32), GRID_W)
    pos_col = jnp.tile(jnp.arange(GRID_W, dtype=jnp.int32), ROWS)
    ang_r = _axial_angles(pos_row)
    ang_c = _axial_angles(pos_col)
    c_act = jax.nn.silu(c)
    cc_act = jax.nn.silu(c_ctx)
    s_q, s_k, s_v = N_Q, N_Q + N_K, N_Q + N_K + N_V

    for i in range(DEPTH):
        ctx_out = i + 1 < DEPTH
        sh1, sc1, g1, sh2, sc2, g2 = jnp.split((c_act @ ada_w[i] + ada_b[i])[:, None, :], 6, axis=-1)
        sh1c, sc1c, g1c, sh2c, sc2c, g2c = jnp.split(cc_act @ ada_w[i] + ada_b[i], 6, axis=-1)
        lam_init = 0.8 - 0.6 * math.exp(-0.3 * i)
        lam = (jnp.exp(jnp.sum(lambda_q1[i].astype(jnp.float32) * lambda_k1[i].astype(jnp.float32)))
               - jnp.exp(jnp.sum(lambda_q2[i].astype(jnp.float32) * lambda_k2[i].astype(jnp.float32)))
               + lam_init)

        xn = _modulate(_rmsnorm(x, norm1_g[i]), sh1, sc1)
        cn = _modulate(_rmsnorm(ctx, norm1_g[i]), sh1c, sc1c)
        px = xn @ w_in[i]
        pc = cn @ w_in[i][:, s_q:N_IN_MIX]

        qx = _axial_rope(px[..., :s_q].reshape(b, l, ATT_HEADS, 2, ATT_DK), ang_r, ang_c)
        kx = _axial_rope(px[..., s_q:s_k].reshape(b, l, ATT_HEADS, 2, ATT_DK), ang_r, ang_c)
        vx = px[..., s_k:s_v].reshape(b, l, ATT_HEADS, ATT_DV)
        kc = pc[..., :N_K].reshape(b, n_ctx, ATT_HEADS, 2, ATT_DK)
        vc = pc[..., N_K:N_K + N_V].reshape(b, n_ctx, ATT_HEADS, ATT_DV)
        k_all = jnp.concatenate([kc, kx], axis=1)
        v_all = jnp.concatenate([vc, vx], axis=1)
        attn_x = _diff_post(_diff_attend_blocked(qx, k_all, v_all, lam), subln_g[i], lam_init)

        ux = px[..., s_v:N_IN_MIX].astype(jnp.float32).reshape(b, l, SSM_GROUPS, SSM_GROUP)
        uc = pc[..., N_K + N_V:].astype(jnp.float32).reshape(b, n_ctx, SSM_GROUPS, SSM_GROUP)
        disc_f = _s5_discretize(ssm_a_re[i, 0], ssm_a_im[i, 0], ssm_log_dt[i, 0], ssm_b_re[i, 0], ssm_b_im[i, 0])
        disc_b = _s5_discretize(ssm_a_re[i, 1], ssm_a_im[i, 1], ssm_log_dt[i, 1], ssm_b_re[i, 1], ssm_b_im[i, 1])
        hcf = _s5_states(uc, disc_f, None, False)
        hcb = _s5_states(uc, disc_b, None, True)
        hxf = _s5_states(ux, disc_f, (hcf[0][:, -1], hcf[1][:, -1]), False)
        hxb = _s5_states(ux, disc_b, (hcb[0][:, 0], hcb[1][:, 0]), True)
        ssm_x = _s5_output(ux, hxf, hxb, ssm_c_re[i], ssm_c_im[i], ssm_d[i], w_glu[i], b_glu[i], x.dtype)

        mix_x = _merge(attn_x, ssm_x, px[..., N_IN_MIX:], w_attn_up[i], w_ssm_up[i], w_out[i])

        if ctx_out:
            qc = (cn @ w_in[i][:, :s_q]).reshape(b, n_ctx, ATT_HEADS, 2, ATT_DK)
            attn_c = _diff_post(_diff_attend(qc, kc, vc, lam), subln_g[i], lam_init)
            ssm_cx = _s5_output(uc, hcf, hcb, ssm_c_re[i], ssm_c_im[i], ssm_d[i], w_glu[i], b_glu[i], ctx.dtype)
            mix_c = _merge(attn_c, ssm_cx, cn @ w_in[i][:, N_IN_MIX:], w_attn_up[i], w_ssm_up[i], w_out[i])

        x = x + g1 * mix_x

        xn2 = _modulate(_rmsnorm(x, norm2_g[i]), sh2, sc2)
        x = x + g2 * _peer(xn2, peer_w_query[i], peer_sub_k1[i], peer_sub_k2[i], peer_u[i], peer_v[i])

        if ctx_out:
            ctx = ctx + g1c * mix_c
            cn2 = _modulate(_rmsnorm(ctx, norm2_g[i]), sh2c, sc2c)
            ctx = ctx + g2c * _peer(cn2, peer_w_query[i], peer_sub_k1[i], peer_sub_k2[i], peer_u[i], peer_v[i])

    return _rmsnorm(x, final_norm_g)
```

```python
import math
from contextlib import ExitStack

import numpy as np
import concourse.bass as bass
import concourse.mybir as mybir
from concourse.bass_utils import run_bass_kernel_spmd

F32 = mybir.dt.float32
BF16 = mybir.dt.bfloat16
I32 = mybir.dt.int32
U32 = mybir.dt.uint32
AF = mybir.ActivationFunctionType
ALU = mybir.AluOpType
AX = mybir.AxisListType

ENG = ("pe", "dve", "act", "pool", "sp")
D = 1024
NT = 4096
NOWN = 2048
NCTX = 256
NKEY = NCTX + NT
EPS = 1e-6
LAM_INIT = 0.2
PI = math.pi


class Sched:
    def __init__(self, nc, stack, n_dma_sems=32):
        self.nc = nc
        self.eobj = {"pe": nc.tensor, "dve": nc.vector, "act": nc.scalar, "pool": nc.gpsimd, "sp": nc.sync}
        self.sem = {e: stack.enter_context(nc.semaphore("sem_" + e)) for e in ENG if e != "sp"}
        self.cnt = {e: 0 for e in ENG}
        self.dsem = [stack.enter_context(nc.semaphore("dsem%d" % i)) for i in range(n_dma_sems)]
        self.dval = [0] * n_dma_sems
        self.dnext = 0
        self.waited = {e: {} for e in ENG}
        self.ops = {e: [] for e in ENG}
        self.lastw = {}
        self.readers = {}
        self.ninstr = 0
        self.excl = set()
        sems = list(self.sem.values()) + self.dsem
        with nc.Block() as block:
            @block.sync
            def _(eng):
                for h in sems:
                    nc.sync.sem_clear(h)

    def _semh(self, semkey):
        return self.sem[semkey] if isinstance(semkey, str) else self.dsem[semkey[1]]

    def _wait(self, e, tok):
        semkey, val, teng = tok
        if self.waited[e].get(semkey, 0) >= val:
            return
        self.waited[e][semkey] = val
        h = self._semh(semkey)
        eo = self.eobj[e]
        self.ops[e].append(lambda: eo.wait_ge(h, val))

    def _deps(self, e, r, w):
        toks = []
        for k in r:
            t = self.lastw.get(k)
            if t is not None and not (t[2] == e and e == "pe"):
                toks.append(t)
            if k in self.excl:
                for t in self.readers.get(k, ()):
                    if t[2] != e:
                        toks.append(t)
        for k in w:
            t = self.lastw.get(k)
            if t is not None and not (t[2] == e and t[0] == e):
                toks.append(t)
            for t in self.readers.get(k, ()):
                if t[2] == e and t[0] == e:
                    continue
                toks.append(t)
        return toks

    def _record(self, tok, r, w):
        for k in r:
            self.readers.setdefault(k, []).append(tok)
        for k in w:
            self.lastw[k] = tok
            self.readers[k] = []
        self.ninstr += 1

    def I(self, e, fn, r=(), w=()):
        for t in self._deps(e, r, w):
            self._wait(e, t)
        self.cnt[e] += 1
        val = self.cnt[e]
        h = self.sem[e]
        self.ops[e].append(lambda: fn().then_inc(h, 1))
        tok = (e, val, e)
        self._record(tok, r, w)
        return tok

    def D(self, e, fn, r=(), w=()):
        for t in self._deps(e, r, w):
            self._wait(e, t)
        i = self.dnext
        self.dnext = (self.dnext + 1) % len(self.dsem)
        if self.dval[i] > 0:
            self._wait(e, (("d", i), self.dval[i], "dma"))
        self.dval[i] += 16
        val = self.dval[i]
        h = self.dsem[i]
        self.ops[e].append(lambda: fn().then_inc(h, 16))
        tok = (("d", i), val, "dma")
        self._record(tok, r, w)
        return tok

    def wait_all(self, e="sp"):
        for i in range(len(self.dsem)):
            if self.dval[i] > 0:
                self._wait(e, (("d", i), self.dval[i], "dma"))
        for e2 in ENG:
            if e2 != "sp" and e2 != e and self.cnt[e2] > 0:
                self._wait(e, (e2, self.cnt[e2], e2))

    def flush(self):
        self.wait_all("sp")
        ops = self.ops
        self.ops = {e: [] for e in ENG}
        with self.nc.Block() as block:
            @block.tensor
            def _(eng):
                for f in ops["pe"]:
                    f()

            @block.vector
            def _(eng):
                for f in ops["dve"]:
                    f()

            @block.scalar
            def _(eng):
                for f in ops["act"]:
                    f()

            @block.gpsimd
            def _(eng):
                for f in ops["pool"]:
                    f()

            @block.sync
            def _(eng):
                for f in ops["sp"]:
                    f()
        self.lastw = {}
        self.readers = {}


class Bld:
    def __init__(self, nc, S):
        self.nc = nc
        self.S = S
        self.e = {"dve": nc.vector, "pool": nc.gpsimd, "act": nc.scalar, "sp": nc.sync, "pe": nc.tensor}

    def mm(self, out, lhsT, rhs, start=True, stop=True, r=(), w=()):
        nc = self.nc
        return self.S.I("pe", lambda: nc.tensor.matmul(out, lhsT=lhsT, rhs=rhs, start=start, stop=stop), r=r, w=w)

    def tr(self, out, in_, ident, r=(), w=()):
        nc = self.nc
        return self.S.I("pe", lambda: nc.tensor.transpose(out=out, in_=in_, identity=ident), r=r, w=w)

    def act(self, out, in_, func, r=(), w=(), scale=None, bias=None, accum=None):
        nc = self.nc
        kw = {}
        if scale is not None:
            kw["scale"] = scale
        if bias is not None:
            kw["bias"] = bias
        if accum is not None:
            kw["accum_out"] = accum
        return self.S.I("act", lambda: nc.scalar.activation(out=out, in_=in_, func=func, **kw), r=r, w=w)

    def tt(self, e, out, in0, in1, op, r=(), w=()):
        eo = self.e[e]
        return self.S.I(e, lambda: eo.tensor_tensor(out=out, in0=in0, in1=in1, op=op), r=r, w=w)

    def ts(self, e, out, in0, s1, s2, op0, op1=None, r=(), w=(), accum=None):
        eo = self.e[e]
        kw = {}
        if op1 is not None:
            kw["op1"] = op1
        if accum is not None:
            kw["accum_out"] = accum
        return self.S.I(e, lambda: eo.tensor_scalar(out=out, in0=in0, scalar1=s1, scalar2=s2, op0=op0, **kw), r=r, w=w)

    def stt(self, out, in0, scalar, in1, op0, op1, r=(), w=(), accum=None):
        nc = self.nc
        kw = {}
        if accum is not None:
            kw["accum_out"] = accum
        return self.S.I("dve", lambda: nc.vector.scalar_tensor_tensor(out=out, in0=in0, scalar=scalar, in1=in1, op0=op0, op1=op1, **kw), r=r, w=w)

    def cp(self, e, out, in_, r=(), w=()):
        if e == "act":
            nc = self.nc
            return self.S.I("act", lambda: nc.scalar.copy(out=out, in_=in_), r=r, w=w)
        eo = self.e[e]
        return self.S.I(e, lambda: eo.tensor_copy(out=out, in_=in_), r=r, w=w)

    def memset(self, e, ap, val, w=()):
        eo = self.e[e]
        return self.S.I(e, lambda: eo.memset(ap, val), w=w)

    def dma(self, e, out, in_, r=(), w=(), slow=False):
        eo = self.e[e]
        if slow:
            return self.S.D(e, lambda: eo.dma_start(out=out, in_=in_, allow_slow_non_contiguous=True), r=r, w=w)
        return self.S.D(e, lambda: eo.dma_start(out=out, in_=in_), r=r, w=w)

    def red(self, out, in_, op, axis=AX.X, r=(), w=()):
        nc = self.nc
        return self.S.I("dve", lambda: nc.vector.tensor_reduce(out=out, in_=in_, axis=axis, op=op), r=r, w=w)

    def recip(self, out, in_, r=(), w=()):
        nc = self.nc
        return self.S.I("dve", lambda: nc.vector.reciprocal(out=out, in_=in_), r=r, w=w)


GELU_C = 2.0 * math.sqrt(2.0 / math.pi)


def gelu_tanh(b, out, x, t, kx, kt, ko):
    b.tt("dve", t, x, x, ALU.mult, r=[kx], w=[kt])
    b.ts("dve", t, t, 0.044715, 1.0, ALU.mult, ALU.add, r=[kt], w=[kt])
    b.tt("dve", t, t, x, ALU.mult, r=[kt, kx], w=[kt])
    b.act(t, t, AF.Sigmoid, r=[kt], w=[kt], scale=GELU_C)
    b.tt("dve", out, x, t, ALU.mult, r=[kx, kt], w=[ko])


def bcl(ap, n):
    sh = list(ap.shape)
    return ap.unsqueeze(len(sh)).to_broadcast(sh + [n])


def build_program(debug=()):
    nc = bass.Bass("TRN2", target_bir_lowering=False)
    dbg = set(debug)

    def din(name, shape, dt=F32):
        return nc.dram_tensor(name, list(shape), dt, kind="ExternalInput").ap()

    def dscr(name, shape, dt=F32):
        kind = "ExternalOutput" if name in dbg else "Internal"
        return nc.dram_tensor(name, list(shape), dt, kind=kind).ap()

    io = dict(
        x_seq=din("x_seq", [NT, D]), ctx_seq=din("ctx_seq", [NCTX, D]), c_vec=din("c_vec", [D]), c_ctx=din("c_ctx", [D]),
        ada_w=din("ada_w", [D, 6 * D]), ada_b=din("ada_b", [6 * D]), norm1_g=din("norm1_g", [D]), norm2_g=din("norm2_g", [D]),
        w_in=din("w_in", [D, 5632]), lam4=din("lam4", [4, 64]), subln_g=din("subln_g", [128]),
        w_attn_up=din("w_attn_up", [D, D]), a_re=din("a_re", [2, 32, 64]), a_im=din("a_im", [2, 32, 64]),
        log_dt=din("log_dt", [2, 32]), b_re=din("b_re", [2, 32, 64, 16]), b_im=din("b_im", [2, 32, 64, 16]),
        c_re=din("c_re", [2, 32, 16, 64]), c_im=din("c_im", [2, 32, 16, 64]), ssm_d=din("ssm_d", [512]),
        w_glu=din("w_glu", [512, 512]), b_glu=din("b_glu", [512]), w_ssm_up=din("w_ssm_up", [512, D]),
        w_out=din("w_out", [D, D]), w_query=din("w_query", [D, D]), sub_k1=din("sub_k1", [128, 64]),
        sub_k2=din("sub_k2", [128, 64]), peer_u=din("peer_u", [16384, D]), peer_v=din("peer_v", [16384, D]),
        final_g=din("final_g", [D]), posT=din("posT", [128, NT]), fidx=din("fidx", [128, 1]), prot=din("prot", [128, 128]),
        selc=din("selc", [128, 8, 240]), cst=din("cst", [128, 512]),
    )
    out = nc.dram_tensor("out", [NOWN, D], F32, kind="ExternalOutput").ap()
    scr = dict(
        vec_s=dscr("vec_s", [4, D]),
        qT_s=dscr("qT_s", [8, 2, 65, NOWN], BF16),
        kT_s=dscr("kT_s", [8, 2, 65, NKEY], BF16),
        v_s=dscr("v_s", [34, 128, 8, 130], BF16),
        uT_s=dscr("uT_s", [4, 128, NKEY]),
        xnT_s=dscr("xnT_s", [128, 8, NOWN], BF16),
        attnT_s=dscr("attnT_s", [128, 8, NOWN], BF16),
        ssmT_s=dscr("ssmT_s", [128, 4, NOWN], BF16),
        x1_s=dscr("x1_s", [NOWN, D]),
        puv_b=dscr("puv_b", [16384, 2, D], BF16),
    )

    with ExitStack() as st:
        S = Sched(nc, st)
        b = Bld(nc, S)
        T = lambda name, shape, dt=F32: st.enter_context(nc.sbuf_tensor("s_" + name, list(shape), dt))
        G = {}
        G["ident_f"] = T("ident_f", [128, 128])
        G["ident_b"] = T("ident_b", [128, 128], BF16)
        G["vecT"] = T("vecT", [128, 10, 8])
        G["lam"] = T("lam", [128, 4])
        G["dbg"] = dbg
        if "stop5a" in dbg:
            G["idx_d"] = nc.dram_tensor("idx_d", [128, 128], I32, kind="ExternalOutput").ap()
            G["gate_d"] = nc.dram_tensor("gate_d", [128, 128], F32, kind="ExternalOutput").ap()
            G["sc_d"] = nc.dram_tensor("sc_d", [128, 2048], F32, kind="ExternalOutput").ap()
        if "ygT_d" in dbg:
            G["ygT_d"] = nc.dram_tensor("ygT_d", [128, 4, NOWN], F32, kind="ExternalOutput").ap()
        phase0(nc, S, b, io, scr, G)
        if "stop0" in dbg:
            return nc
        if "only5" in dbg:
            x1_in = nc.dram_tensor("x1_in", [NOWN, D], F32, kind="ExternalInput").ap()
            with nc.sbuf_tensor("s_cpy", [128, 16, D], F32) as cpy:
                b.dma("sp", cpy[:], x1_in.rearrange("(t p) d -> p t d", p=128), w=["cpy"])
                b.dma("sp", scr["x1_s"].rearrange("(t p) d -> p t d", p=128), cpy[:], r=["cpy"], w=["x1_s"])
                S.flush()
            phase5(nc, S, b, io, scr, G, out)
            return nc
        phase1(nc, S, b, io, scr, G)
        if "stop1" in dbg:
            return nc
        if "skip2" not in dbg:
            phase2(nc, S, b, io, scr, G)
        if "stop2" in dbg:
            return nc
        if "skip3" not in dbg:
            phase3(nc, S, b, io, scr, G)
        if "stop3" in dbg:
            return nc
        phase4(nc, S, b, io, scr, G)
        if "stop4" in dbg:
            return nc
        phase5(nc, S, b, io, scr, G, out)
    return nc


V_SCALE1, V_SHIFT1, V_SCALE1C, V_SHIFT1C, V_SCALE2, V_SHIFT2, V_G1, V_G2, V_N1G, V_N2G = range(10)
R_G1, R_G2, R_SCALE2, R_SHIFT2, R_FING = range(5)


def phase0(nc, S, b, io, scr, G):
    with ExitStack() as st:
        T = lambda name, shape, dt=F32: st.enter_context(nc.sbuf_tensor("s_" + name, list(shape), dt))
        P = lambda name, shape, dt=F32: (S.excl.add(name), st.enter_context(nc.psum_tensor("p_" + name, list(shape), dt)))[1]
        ident_f, ident_b, vecT, lam = G["ident_f"], G["ident_b"], G["vecT"], G["lam"]
        b.memset("pool", ident_f[:], 0.0, w=["ident_f"])
        S.I("pool", lambda: nc.gpsimd.affine_select(out=ident_f[:], in_=ident_f[:], pattern=[[-1, 128]], compare_op=ALU.not_equal,
                                                    fill=1.0, base=0, channel_multiplier=1), r=["ident_f"], w=["ident_f"])
        b.cp("pool", ident_b[:], ident_f[:], r=["ident_f"], w=["ident_b"])

        cT = T("cT", [128, 8, 2])
        b.dma("sp", cT[:, :, 0], io["c_vec"].rearrange("(j p) -> p j", p=128), w=["cT"], slow=True)
        b.dma("sp", cT[:, :, 1], io["c_ctx"].rearrange("(j p) -> p j", p=128), w=["cT"], slow=True)
        b.act(cT[:], cT[:], AF.Silu, r=["cT"], w=["cT"])
        adabT = T("adabT", [128, 48])
        b.dma("sp", adabT[:], io["ada_b"].rearrange("(c p) -> p c", p=128), w=["adabT"], slow=True)
        b.dma("sp", vecT[:, V_N1G, :], io["norm1_g"].rearrange("(j p) -> p j", p=128), w=["n1g"], slow=True)
        b.dma("sp", vecT[:, V_N2G, :], io["norm2_g"].rearrange("(j p) -> p j", p=128), w=["n2g"], slow=True)
        aw = [T("aw%d" % i, [128, 8, D]) for i in range(2)]
        modps = P("modps", [128, 48, 2])
        modT = T("modT", [128, 48, 2])
        for pc in range(6):
            sl = pc % 2
            b.dma("sp" if pc % 2 == 0 else "act", aw[sl][:], io["ada_w"][:, pc * D:(pc + 1) * D].rearrange("(j p) n -> p j n", p=128), w=["aw%d" % sl])
            for cc in range(8):
                for j in range(8):
                    b.mm(modps[:, pc * 8 + cc, :], aw[sl][:, j, cc * 128:(cc + 1) * 128], cT[:, j, :], start=(j == 0), stop=(j == 7),
                         r=["aw%d" % sl, "cT"], w=["modps"])
        b.tt("dve", modT[:], modps[:], bcl(adabT[:], 2), ALU.add, r=["modps", "adabT"], w=["modT"])
        b.stt(vecT[:, V_SCALE1, :], modT[:, 8:16, 0], 1.0, vecT[:, V_N1G, :], ALU.add, ALU.mult, r=["modT", "n1g"], w=["v_scale1"])
        b.stt(vecT[:, V_SCALE1C, :], modT[:, 8:16, 1], 1.0, vecT[:, V_N1G, :], ALU.add, ALU.mult, r=["modT", "n1g"], w=["v_scale1c"])
        b.stt(vecT[:, V_SCALE2, :], modT[:, 32:40, 0], 1.0, vecT[:, V_N2G, :], ALU.add, ALU.mult, r=["modT", "n2g"], w=["v_scale2"])
        b.cp("dve", vecT[:, V_SHIFT1, :], modT[:, 0:8, 0], r=["modT"], w=["v_shift1"])
        b.cp("dve", vecT[:, V_SHIFT1C, :], modT[:, 0:8, 1], r=["modT"], w=["v_shift1c"])
        b.cp("dve", vecT[:, V_SHIFT2, :], modT[:, 24:32, 0], r=["modT"], w=["v_shift2"])
        b.cp("dve", vecT[:, V_G1, :], modT[:, 16:24, 0], r=["modT"], w=["v_g1"])
        b.cp("dve", vecT[:, V_G2, :], modT[:, 40:48, 0], r=["modT"], w=["v_g2"])
        for i, (slot, key) in enumerate([(V_G1, "v_g1"), (V_G2, "v_g2"), (V_SCALE2, "v_scale2"), (V_SHIFT2, "v_shift2")]):
            b.dma("sp", scr["vec_s"][i].rearrange("(j p) -> p j", p=128), vecT[:, slot, :], r=[key], w=["vec_s%d" % i], slow=True)
        l4 = T("l4", [128, 4, 64])
        b.dma("sp", l4[:], io["lam4"].rearrange("a k -> (a k)").partition_broadcast(128).rearrange("p (a k) -> p a k", a=4), w=["l4"])
        lt = T("lt", [128, 2, 64])
        b.tt("dve", lt[:, 0, :], l4[:, 0, :], l4[:, 1, :], ALU.mult, r=["l4"], w=["lt"])
        b.tt("dve", lt[:, 1, :], l4[:, 2, :], l4[:, 3, :], ALU.mult, r=["l4"], w=["lt"])
        b.red(lam[:, 0:2], lt[:], ALU.add, r=["lt"], w=["lam"])
        b.act(lam[:, 0:2], lam[:, 0:2], AF.Exp, r=["lam"], w=["lam"])
        b.stt(lam[:, 2:3], lam[:, 0:1], LAM_INIT, lam[:, 1:2], ALU.add, ALU.subtract, r=["lam"], w=["lam2"])
        b.ts("dve", lam[:, 3:4], lam[:, 2:3], -1.0, None, ALU.mult, r=["lam2"], w=["lam3"])
        S.flush()


def phase1(nc, S, b, io, scr, G):
    ident_f, ident_b, vecT = G["ident_f"], G["ident_b"], G["vecT"]
    with ExitStack() as st:
        T = lambda name, shape, dt=F32: st.enter_context(nc.sbuf_tensor("s_" + name, list(shape), dt))
        P = lambda name, shape, dt=F32: (S.excl.add(name), st.enter_context(nc.psum_tensor("p_" + name, list(shape), dt)))[1]
        xnT = T("xnT", [128, 8, NKEY], BF16)
        with ExitStack() as st2:
            T2 = lambda name, shape, dt=F32: st2.enter_context(nc.sbuf_tensor("s_" + name, list(shape), dt))
            P2 = lambda name, shape, dt=F32: (S.excl.add(name), st2.enter_context(nc.psum_tensor("p_" + name, list(shape), dt)))[1]
            xt = [T2("xt%d" % i, [128, D]) for i in range(2)]
            xs = [T2("xs%d" % i, [128, D]) for i in range(2)]
            junk = T2("junk", [128, D])
            ss = [T2("ss%d" % i, [128, 4]) for i in range(2)]
            pT = [P2("pT%d" % i, [128, 8, 128]) for i in range(2)]
            tmp = [T2("tmp%d" % i, [128, 8, 128]) for i in range(2)]
            for ti in range(34):
                sl = ti % 2
                src = io["ctx_seq"][ti * 128:(ti + 1) * 128, :] if ti < 2 else io["x_seq"][(ti - 2) * 128:(ti - 1) * 128, :]
                vs, vh = (V_SCALE1C, V_SHIFT1C) if ti < 2 else (V_SCALE1, V_SHIFT1)
                ks, kh = ("v_scale1c", "v_shift1c") if ti < 2 else ("v_scale1", "v_shift1")
                b.dma("sp" if sl == 0 else "act", xt[sl][:], src, w=["xt%d" % sl])
                b.act(junk[:], xt[sl][:], AF.Square, r=["xt%d" % sl], w=["junk", "ss%d" % sl], accum=ss[sl][:, 0:1])
                b.ts("dve", ss[sl][:, 1:2], ss[sl][:, 0:1], 1.0 / D, EPS, ALU.mult, ALU.add, r=["ss%d" % sl], w=["ssb%d" % sl])
                b.act(ss[sl][:, 2:3], ss[sl][:, 1:2], AF.Sqrt, r=["ssb%d" % sl], w=["ssc%d" % sl])
                b.recip(ss[sl][:, 3:4], ss[sl][:, 2:3], r=["ssc%d" % sl], w=["ssd%d" % sl])
                b.act(xs[sl][:], xt[sl][:], AF.Copy, r=["xt%d" % sl, "ssd%d" % sl], w=["xs%d" % sl], scale=ss[sl][:, 3:4])
                for j in range(8):
                    b.tr(pT[sl][:, j, :], xs[sl][:, j * 128:(j + 1) * 128], ident_f[:], r=["xs%d" % sl], w=["pT%d" % sl])
                b.tt("dve", tmp[sl][:], pT[sl][:], bcl(vecT[:, vs, :], 128), ALU.mult, r=["pT%d" % sl, ks], w=["tmp%d" % sl])
                b.tt("pool", xnT[:, :, ti * 128:(ti + 1) * 128], tmp[sl][:], bcl(vecT[:, vh, :], 128), ALU.add, r=["tmp%d" % sl, kh], w=["xnT"])
            b.dma("sp", scr["xnT_s"][:, :, :], xnT[:, :, NCTX:NCTX + NOWN], r=["xnT"], w=["xnT_s"])
            S.flush()
        if "stop1a" in G["dbg"]:
            return
        cosT = T("cosT", [128, NT])
        sinT = T("sinT", [128, NT])
        with ExitStack() as st2:
            T2 = lambda name, shape, dt=F32: st2.enter_context(nc.sbuf_tensor("s_" + name, list(shape), dt))
            ang = T2("ang", [128, NT])
            y = T2("y", [128, NT])
            yi = T2("yi", [128, NT], I32)
            fi = T2("fi", [128, 2])
            b.dma("sp", ang[:], io["posT"][:, :], w=["ang"])
            b.dma("sp", fi[:, 0:1], io["fidx"][:, :], w=["fi"])
            b.act(fi[:, 1:2], fi[:, 0:1], AF.Exp, r=["fi"], w=["inv"], scale=-math.log(10000.0) / 16.0)
            b.ts("dve", ang[:], ang[:], fi[:, 1:2], None, ALU.mult, r=["ang", "inv"], w=["ang"])
            for tab, off, key in ((sinT, 0.5, "sinT"), (cosT, 0.75, "cosT")):
                b.ts("dve", y[:], ang[:], 1.0 / (2 * PI), off, ALU.mult, ALU.add, r=["ang"], w=["y"])
                b.cp("dve", yi[:], y[:], r=["y"], w=["yi"])
                b.cp("dve", tab[:], yi[:], r=["yi"], w=[key])
                b.tt("dve", y[:], y[:], tab[:], ALU.subtract, r=["y", key], w=["y"])
                b.ts("dve", tab[:], y[:], 0.0, None, ALU.is_lt, r=["y"], w=[key])
                b.tt("dve", y[:], y[:], tab[:], ALU.add, r=["y", key], w=["y"])
                b.ts("dve", y[:], y[:], 2 * PI, -PI, ALU.mult, ALU.add, r=["y"], w=["y"])
                b.ts("dve", y[:], y[:], 3.1415925, -3.1415925, ALU.min, ALU.max, r=["y"], w=["y"])
                b.act(tab[:], y[:], AF.Sin, r=["y"], w=[key])
            S.flush()
        if "stop1r" in G["dbg"]:
            return
        prot = T("prot", [128, 128], BF16)
        protf = T("protf", [128, 128])
        b.dma("sp", protf[:], io["prot"][:, :], w=["protf"])
        b.cp("dve", prot[:], protf[:], r=["protf"], w=["prot"])
        bones = T("bones", [128, 2], BF16)
        b.memset("pool", bones[:], 0.0, w=["bones"])
        b.memset("pool", bones[0:64, 0:1], 1.0, w=["bones"])
        b.memset("pool", bones[64:128, 1:2], 1.0, w=["bones"])
        onesrow = T("onesrow", [16, NKEY], BF16)
        b.memset("pool", onesrow[:], 1.0, w=["onesrow"])
        if "no_ones" not in G["dbg"]:
            b.dma("sp", scr["kT_s"][:, :, 64, :].rearrange("h m c -> (h m) c"), onesrow[:], r=["onesrow"], w=["kT_s64"])
        wf = [T("wf%d" % i, [128, 8, 512]) for i in range(2)]
        wb = [T("wb%d" % i, [128, 8, 512], BF16) for i in range(2)]
        pA = [P("pA%d" % i, [128, 512]) for i in range(2)]
        pB = [P("pB%d" % i, [128, 512]) for i in range(2)]
        pN = [P("pN%d" % i, [2, 512]) for i in range(2)]
        asb = [T("asb%d" % i, [128, 512], BF16) for i in range(2)]
        t1 = [T("t1_%d" % i, [128, 512]) for i in range(2)]
        t2 = [T("t2_%d" % i, [128, 512]) for i in range(2)]
        kr = [T("kr%d" % i, [128, 512], BF16) for i in range(3)]
        sq = [T("sq%d" % i, [128, 512], BF16) for i in range(2)]
        kmx = T("kmx", [2, 8, 10])
        negk = T("negk", [2, 8])
        nrow = [T("nrow%d" % i, [2, 512]) for i in range(2)]
        nrowb = [T("nrowb%d" % i, [2, 512], BF16) for i in range(2)]
        vsb = [T("vsb%d" % i, [128, 8, 130], BF16) for i in range(2)]
        usb = [T("usb%d" % i, [128, 512]) for i in range(2)]
        for i in range(2):
            b.memset("pool", vsb[i][:], 0.0, w=["vsb%d" % i])
            b.memset("pool", vsb[i][:, :, 128:129], 1.0, w=["vsb%d" % i])
        b.memset("pool", kmx[:], 0.0, w=["kmx"])
        cnt = {"w": 0, "t": 0, "v": 0, "u": 0, "kr": 0}
        if "stop1s" in G["dbg"]:
            S.flush()
            return

        def load_w(cb):
            sl = cnt["w"] % 2
            cnt["w"] += 1
            b.dma("sp", wf[sl][:], io["w_in"][:, cb * 512:(cb + 1) * 512].rearrange("(j p) n -> p j n", p=128), w=["wf%d" % sl])
            b.cp("dve" if "w_cast_dve" in G["dbg"] else "pool", wb[sl][:], wf[sl][:], r=["wf%d" % sl], w=["wb%d" % sl])
            return sl

        def qk_tile(wsl, ch, col0, ncol, tok0, rope, is_q, h):
            i = cnt["t"] % 2
            cnt["t"] += 1
            for j in range(8):
                b.mm(pA[i][:, :ncol], wb[wsl][:, j, ch * 128:(ch + 1) * 128], xnT[:, j, col0:col0 + ncol], start=(j == 0), stop=(j == 7),
                     r=["wb%d" % wsl, "xnT"], w=["pA%d" % i])
            ki = cnt["kr"] % 3
            cnt["kr"] += 1
            if rope and "no_rope_ops" not in G["dbg"]:
                b.cp("act", asb[i][:, :ncol], pA[i][:, :ncol], r=["pA%d" % i], w=["asb%d" % i])
                b.mm(pB[i][:, :ncol], prot[:], asb[i][:, :ncol], r=["prot", "asb%d" % i], w=["pB%d" % i])
                b.tt("dve", t1[i][:, :ncol], pA[i][:, :ncol], cosT[:, tok0:tok0 + ncol], ALU.mult, r=["pA%d" % i, "cosT"], w=["t1_%d" % i])
                b.tt("dve", t2[i][:, :ncol], pB[i][:, :ncol], sinT[:, tok0:tok0 + ncol], ALU.mult, r=["pB%d" % i, "sinT"], w=["t2_%d" % i])
                b.tt("pool", kr[ki][:, :ncol], t1[i][:, :ncol], t2[i][:, :ncol], ALU.add, r=["t1_%d" % i, "t2_%d" % i], w=["kr%d" % ki])
            else:
                b.cp("act", kr[ki][:, :ncol], pA[i][:, :ncol], r=["pA%d" % i], w=["kr%d" % ki])
            if "no_norm" not in G["dbg"]:
                b.act(sq[i][:, :ncol], kr[ki][:, :ncol], AF.Square, r=["kr%d" % ki], w=["sq%d" % i])
                b.mm(pN[i][:, :ncol], bones[:], sq[i][:, :ncol], r=["bones", "sq%d" % i], w=["pN%d" % i])
            return i, ki

        for cb in (2, 3):
            wsl = load_w(cb)
            for ch in range(4):
                h = (cb - 2) * 4 + ch
                blocks = [(0, NCTX, 0, False)] + [(NCTX + tb * 512, 512, tb * 512, True) for tb in range(8)]
                for bi, (col0, ncol, tok0, rope) in enumerate(blocks):
                    i, ki = qk_tile(wsl, ch, col0, ncol, tok0, rope, False, h)
                    if "no_norm" not in G["dbg"]:
                        b.red(kmx[:, h, bi:bi + 1], pN[i][:, :ncol], ALU.max, r=["pN%d" % i], w=["kmx"])
                    for m in range(2 if "no_kstore" not in G["dbg"] else 0):
                        b.dma("sp" if m == 0 else "act", scr["kT_s"][h, m, 0:64, col0:col0 + ncol], kr[ki][m * 64:(m + 1) * 64, :ncol], r=["kr%d" % ki], w=["kT_s"])
        if "stop1k" in G["dbg"]:
            S.flush()
            return
        b.red(negk[:], kmx[:], ALU.max, r=["kmx"], w=["negk"])
        b.act(negk[:], negk[:], AF.Sqrt, r=["negk"], w=["negk"])
        b.ts("dve", negk[:], negk[:], -1.0, None, ALU.mult, r=["negk"], w=["negk"])
        for cb in (0, 1):
            wsl = load_w(cb)
            for ch in range(4):
                h = cb * 4 + ch
                for tb in range(4):
                    i, ki = qk_tile(wsl, ch, NCTX + tb * 512, 512, tb * 512, True, True, h)
                    b.act(nrow[i][:], pN[i][:], AF.Sqrt, r=["pN%d" % i], w=["nrow%d" % i])
                    b.ts("dve", nrowb[i][:], nrow[i][:], negk[:, h:h + 1], None, ALU.mult, r=["nrow%d" % i, "negk"], w=["nrowb%d" % i])
                    b.dma("sp", scr["qT_s"][h, :, 64, tb * 512:(tb + 1) * 512], nrowb[i][:], r=["nrowb%d" % i], w=["qT_s"])
                    for m in range(2):
                        b.dma("sp" if m == 0 else "act", scr["qT_s"][h, m, 0:64, tb * 512:(tb + 1) * 512], kr[ki][m * 64:(m + 1) * 64, :], r=["kr%d" % ki], w=["qT_s"])
        if "stop1q" in G["dbg"]:
            S.flush()
            return
        wv = [load_w(4), load_w(5)]
        for ti in range(34):
            i = cnt["v"] % 2
            cnt["v"] += 1
            for half in range(2):
                pt = pA[half]
                for j in range(8):
                    b.mm(pt[:], xnT[:, j, ti * 128:(ti + 1) * 128], wb[wv[half]][:, j, :], start=(j == 0), stop=(j == 7),
                         r=["xnT", "wb%d" % wv[half]], w=["pA%d" % half])
                b.cp("act" if half == 0 else "dve", vsb[i][:, half * 4:(half + 1) * 4, 0:128], pt[:].rearrange("p (h e) -> p h e", h=4),
                     r=["pA%d" % half], w=["vsb%d" % i])
            b.dma("sp", scr["v_s"][ti], vsb[i][:], r=["vsb%d" % i], w=["v_s"])
        if "stop1v" in G["dbg"]:
            S.flush()
            return
        wsl = load_w(6)
        blocks = [(0, NCTX)] + [(NCTX + tb * 512, 512) for tb in range(8)]
        for ch in range(4):
            for (col0, ncol) in blocks:
                i = cnt["u"] % 2
                cnt["u"] += 1
                for j in range(8):
                    b.mm(pB[i][:, :ncol], wb[wsl][:, j, ch * 128:(ch + 1) * 128], xnT[:, j, col0:col0 + ncol], start=(j == 0), stop=(j == 7),
                         r=["wb%d" % wsl, "xnT"], w=["pB%d" % i])
                b.cp("act", usb[i][:, :ncol], pB[i][:, :ncol], r=["pB%d" % i], w=["usb%d" % i])
                b.dma("sp", scr["uT_s"][ch, :, col0:col0 + ncol], usb[i][:, :ncol], r=["usb%d" % i], w=["uT_s"])
        S.flush()


def phase2(nc, S, b, io, scr, G):
    ident_b, lam = G["ident_b"], G["lam"]
    with ExitStack() as st:
        T = lambda name, shape, dt=F32: st.enter_context(nc.sbuf_tensor("s_" + name, list(shape), dt))
        P = lambda name, shape, dt=F32: (S.excl.add(name), st.enter_context(nc.psum_tensor("p_" + name, list(shape), dt)))[1]
        kT = [T("kT%d" % i, [65, 2, NKEY], BF16) for i in range(2)]
        qT = [T("qT%d" % i, [65, 2, NOWN], BF16) for i in range(2)]
        vv = [T("vv%d" % i, [128, 34, 130], BF16) for i in range(2)]
        E = [T("E%d" % i, [128, 512], BF16) for i in range(4)]
        pS = [P("pS%d" % i, [128, 512]) for i in range(3)]
        pO = [P("pO%d" % i, [128, 512]) for i in range(4)]
        pTr = P("pTr", [128, 1024], BF16)
        osb = [T("osb%d" % i, [128, 130]) for i in range(4)]
        attnT = T("attnT", [128, 8, NOWN], BF16)
        gsub = T("gsub", [128, 128])
        sm = [T("sm%d" % i, [128, 8]) for i in range(2)]
        ot = [T("ot%d" % i, [128, 128]) for i in range(2)]
        ob = [T("ob%d" % i, [128, 128], BF16) for i in range(2)]
        junk = T("junk2", [128, 128])
        cvf = [T("cvf%d" % i, [128, 4, D]) for i in range(2)]
        cvb = [T("cvb%d" % i, [128, 4, D], BF16) for i in range(2)]
        cvc = [0]

        def convert_chunk():
            c = cvc[0]
            cvc[0] += 1
            if c >= 64:
                return
            i = c % 2
            src = io["peer_u"] if c < 32 else io["peer_v"]
            dst = scr["puv_b"][:, 0 if c < 32 else 1, :]
            r0 = (c % 32) * 512
            b.dma("sp", cvf[i][:], src[r0:r0 + 512, :].rearrange("(p j) d -> p j d", j=4), w=["cvf%d" % i])
            b.cp("pool", cvb[i][:], cvf[i][:], r=["cvf%d" % i], w=["cvb%d" % i])
            b.dma("act", dst[r0:r0 + 512, :].rearrange("(p j) d -> p j d", j=4), cvb[i][:], r=["cvb%d" % i], w=["pub"])

        b.dma("sp", gsub[:], io["subln_g"].partition_broadcast(128), w=["gsub"])
        b.ts("dve", gsub[:], gsub[:], 1.0 - LAM_INIT, None, ALU.mult, r=["gsub"], w=["gsub"])
        ecnt = 0

        def load_head(h):
            sl = h % 2
            b.dma("sp", kT[sl][:], scr["kT_s"][h].rearrange("m r c -> r m c"), w=["kT%d" % sl])
            b.dma("act", qT[sl][:], scr["qT_s"][h].rearrange("m r c -> r m c"), w=["qT%d" % sl])
            b.dma("sp", vv[sl][:], scr["v_s"][:, :, h, :].rearrange("k p e -> p k e"), w=["vv%d" % sl])

        pend_epi = [None]
        epic = [0]

        def epilogue(h, qg):
            for qb in range(2):
                e2 = epic[0] % 2
                epic[0] += 1
                o1, o2 = osb[qb], osb[2 + qb]
                k1, k2 = "osb%d" % qb, "osb%d" % (2 + qb)
                s_ = sm[e2]
                ks = "sm%d" % e2
                b.recip(s_[:, 0:1], o1[:, 128:129], r=[k1], w=[ks + "a"])
                b.recip(s_[:, 1:2], o2[:, 128:129], r=[k2], w=[ks + "b"])
                b.tt("dve", s_[:, 2:3], s_[:, 1:2], lam[:, 3:4], ALU.mult, r=[ks + "b", "lam3"], w=[ks + "c"])
                b.ts("dve", ot[e2][:], o1[:, 0:128], s_[:, 0:1], None, ALU.mult, r=[k1, ks + "a"], w=["ot%d" % e2])
                b.stt(ot[e2][:], o2[:, 0:128], s_[:, 2:3], ot[e2][:], ALU.mult, ALU.add, r=[k2, ks + "c", "ot%d" % e2], w=["ot%d" % e2])
                b.stt(junk[:], ot[e2][:], 1.0, ot[e2][:], ALU.mult, ALU.mult, r=["ot%d" % e2], w=["junk2", ks + "d"], accum=s_[:, 3:4])
                b.ts("dve", s_[:, 4:5], s_[:, 3:4], 1.0 / 128.0, EPS, ALU.mult, ALU.add, r=[ks + "d"], w=[ks + "e"])
                b.act(s_[:, 5:6], s_[:, 4:5], AF.Sqrt, r=[ks + "e"], w=[ks + "f"])
                b.recip(s_[:, 6:7], s_[:, 5:6], r=[ks + "f"], w=[ks + "g"])
                b.stt(ob[e2][:], ot[e2][:], s_[:, 6:7], gsub[:], ALU.mult, ALU.mult, r=["ot%d" % e2, ks + "g", "gsub"], w=["ob%d" % e2])

        def epilogue_b(h, qg):
            for qb in range(2):
                b.tr(pTr[:, qb * 128:(qb + 1) * 128], ob[qb][:], ident_b[:], r=["ob%d" % qb, "ident_b"], w=["pTr"])
            q0 = qg * 256
            b.cp("dve", attnT[:, h, q0:q0 + 256], pTr[:, 0:256], r=["pTr"], w=["attnT"])

        load_head(0)
        for h in range(8):
            sl = h % 2
            if h + 1 < 8:
                load_head(h + 1)
            for qg in range(8):
                eidx = {}
                for step in range(36):
                    kb = step
                    if kb < 34:
                        i = kb % 3
                        ei = ecnt % 4
                        ecnt += 1
                        eidx[kb] = ei
                        for m in range(2):
                            b.mm(pS[i][:, m * 256:(m + 1) * 256], kT[sl][:, m, kb * 128:(kb + 1) * 128], qT[sl][:, m, qg * 256:(qg + 1) * 256],
                                 r=["kT%d" % sl, "qT%d" % sl], w=["pS%d" % i])
                        b.act(E[ei][:], pS[i][:], AF.Exp, r=["pS%d" % i], w=["E%d" % ei], scale=0.125)
                    pk = step - 2
                    if pk >= 0:
                        pei = eidx[pk]
                        for m in range(2):
                            for qb in range(2):
                                a = m * 2 + qb
                                b.mm(pO[a][:, 0:129], E[pei][:, m * 256 + qb * 128: m * 256 + (qb + 1) * 128], vv[sl][:, pk, 0:129],
                                     start=(pk == 0), stop=(pk == 33), r=["E%d" % pei, "vv%d" % sl], w=["pO%d" % a])
                    if step == 10:
                        convert_chunk()
                    if step == 4 and pend_epi[0] is not None:
                        epilogue(*pend_epi[0])
                    if step == 20 and pend_epi[0] is not None:
                        epilogue_b(*pend_epi[0])
                        pend_epi[0] = None
                for a in range(4):
                    b.cp("act" if a % 2 == 0 else "dve", osb[a][:, 0:129], pO[a][:, 0:129], r=["pO%d" % a], w=["osb%d" % a])
                pend_epi[0] = (h, qg)
        epilogue(*pend_epi[0])
        epilogue_b(*pend_epi[0])
        b.dma("sp", scr["attnT_s"][:, :, :], attnT[:], r=["attnT"], w=["attnT_s"])
        S.flush()


def phase3(nc, S, b, io, scr, G):
    ident_f = G["ident_f"]
    dbg = G["dbg"]
    with ExitStack() as st:
        T = lambda name, shape, dt=F32: st.enter_context(nc.sbuf_tensor("s_" + name, list(shape), dt))
        cst = T("cst", [128, 512])
        selc = T("selc", [128, 8, 240])
        b.dma("sp", cst[:], io["cst"][:, :], w=["cst"])
        b.dma("sp", selc[:], io["selc"][:, :, :], w=["selc"])
        maskf, maskb = cst[:, 0:128], cst[:, 128:256]
        kka, kkd, kk8, kk1 = cst[0:64, 256:272], cst[0:64, 272:288], cst[0:64, 288:304], cst[0:64, 304:305]
        AR = T("AR", [64, 64]); AI = T("AI", [64, 64]); DT = T("DT", [64, 64])
        RHO = T("RHO", [64, 64]); TH = T("TH", [64, 64])
        FR = T("FR", [64, 64]); FI = T("FI", [64, 64])
        FBR = T("FBR", [64, 64, 16]); FBI = T("FBI", [64, 64, 16])
        CNR = T("CNR", [64, 64, 16]); CNI = T("CNI", [64, 64, 16])
        Dsq = T("Dsq", [128, 32])
        ygT = T("ygT", [128, 4, NOWN])
        b.dma("sp", AR[:], io["a_re"].rearrange("d g n -> n (d g)"), w=["AR"], slow=True)
        b.dma("sp", AI[:], io["a_im"].rearrange("d g n -> n (d g)"), w=["AI"], slow=True)
        b.dma("sp", DT[:], io["log_dt"].rearrange("d g -> (d g)").partition_broadcast(64), w=["DT"])
        for s_ in range(8):
            b.dma("sp", Dsq[s_ * 16:(s_ + 1) * 16, :], io["ssm_d"].rearrange("(g q) -> q g", q=16), w=["Dsq"], slow=True)
        b.act(DT[:], DT[:], AF.Exp, r=["DT"], w=["DT"])
        b.tt("dve", RHO[:], AR[:], DT[:], ALU.mult, r=["AR", "DT"], w=["RHO"])
        b.tt("dve", TH[:], AI[:], DT[:], ALU.mult, r=["AI", "DT"], w=["TH"])

        uid = [0]

        def cpow(st_, dre, dim, rho, th, kk, Gn, Kn, tag):
            uid[0] += 1
            u = "%s%d" % (tag, uid[0])
            T_ = lambda name, dt=F32: st_.enter_context(nc.sbuf_tensor("s_%s_%s" % (name, u), [64, Gn, Kn], dt))
            rk = T_("rk"); y = T_("y"); yi = T_("yi", I32); w_ = T_("w"); mg = T_("mg")
            kb_ = kk.unsqueeze(1).to_broadcast([64, Gn, Kn])
            b.tt("dve", rk[:], bcl(rho, Kn), kb_, ALU.mult, r=["RHO", "cst"], w=["rk" + u])
            b.act(mg[:], rk[:], AF.Exp, r=["rk" + u], w=["mg" + u])
            b.tt("dve", rk[:], bcl(th, Kn), kb_, ALU.mult, r=["TH", "cst", "mg" + u], w=["rk" + u])
            for dst, off in ((dim, 0.5), (dre, 0.75)):
                b.ts("dve", y[:], rk[:], 1.0 / (2 * PI), off, ALU.mult, ALU.add, r=["rk" + u], w=["y" + u])
                b.cp("dve", yi[:], y[:], r=["y" + u], w=["yi" + u])
                b.cp("dve", w_[:], yi[:], r=["yi" + u], w=["w" + u])
                b.tt("dve", y[:], y[:], w_[:], ALU.subtract, r=["y" + u, "w" + u], w=["y" + u])
                b.ts("dve", w_[:], y[:], 0.0, None, ALU.is_lt, r=["y" + u], w=["w" + u])
                b.tt("dve", y[:], y[:], w_[:], ALU.add, r=["y" + u, "w" + u], w=["y" + u])
                b.ts("dve", y[:], y[:], 2 * PI, -PI, ALU.mult, ALU.add, r=["y" + u], w=["y" + u])
                b.ts("dve", y[:], y[:], 3.1415925, -3.1415925, ALU.min, ALU.max, r=["y" + u], w=["y" + u])
                b.act(w_[:], y[:], AF.Sin, r=["y" + u], w=["w" + u])
                b.tt("dve", dst, w_[:], mg[:], ALU.mult, r=["w" + u, "mg" + u], w=[tag])

        with ExitStack() as st2:
            T2 = lambda name, shape, dt=F32: st2.enter_context(nc.sbuf_tensor("s_" + name, list(shape), dt))
            P2 = lambda name, shape, dt=F32: (S.excl.add(name), st2.enter_context(nc.psum_tensor("p_" + name, list(shape), dt)))[1]
            ABR = T2("ABR", [64, 64, 1]); ABI = T2("ABI", [64, 64, 1])
            BR = T2("BR", [64, 64, 16]); BI = T2("BI", [64, 64, 16])
            b.dma("sp", BR[:], io["b_re"].rearrange("d g n q -> n (d g) q"), w=["BR"])
            b.dma("act", BI[:], io["b_im"].rearrange("d g n q -> n (d g) q"), w=["BI"])
            cpow(st2, ABR[:], ABI[:], RHO[:], TH[:], kk1, 64, 1, "AB")
            den = T2("den", [64, 64]); t1 = T2("g_t1", [64, 64]); t2 = T2("g_t2", [64, 64]); nr = T2("nr", [64, 64])
            b.tt("dve", den[:], AR[:], AR[:], ALU.mult, r=["AR"], w=["den"])
            b.tt("dve", t1[:], AI[:], AI[:], ALU.mult, r=["AI"], w=["g_t1"])
            b.tt("dve", den[:], den[:], t1[:], ALU.add, r=["den", "g_t1"], w=["den"])
            b.recip(den[:], den[:], r=["den"], w=["den"])
            b.ts("dve", nr[:], ABR[:, :, 0], -1.0, None, ALU.add, r=["AB"], w=["nr"])
            b.tt("dve", t1[:], nr[:], AR[:], ALU.mult, r=["nr", "AR"], w=["g_t1"])
            b.tt("dve", t2[:], ABI[:, :, 0], AI[:], ALU.mult, r=["AB", "AI"], w=["g_t2"])
            b.tt("dve", t1[:], t1[:], t2[:], ALU.add, r=["g_t1", "g_t2"], w=["g_t1"])
            b.tt("dve", FR[:], t1[:], den[:], ALU.mult, r=["g_t1", "den"], w=["FR"])
            b.tt("dve", t1[:], ABI[:, :, 0], AR[:], ALU.mult, r=["AB", "AR", "FR"], w=["g_t1"])
            b.tt("dve", t2[:], nr[:], AI[:], ALU.mult, r=["nr", "AI"], w=["g_t2"])
            b.tt("dve", t1[:], t1[:], t2[:], ALU.subtract, r=["g_t1", "g_t2"], w=["g_t1"])
            b.tt("dve", FI[:], t1[:], den[:], ALU.mult, r=["g_t1", "den"], w=["FI"])
            ta = T2("g_ta", [64, 64, 16]); tb = T2("g_tb", [64, 64, 16])
            b.tt("dve", ta[:], BR[:], bcl(FR[:], 16), ALU.mult, r=["BR", "FR"], w=["g_ta"])
            b.tt("dve", tb[:], BI[:], bcl(FI[:], 16), ALU.mult, r=["BI", "FI"], w=["g_tb"])
            b.tt("dve", FBR[:], ta[:], tb[:], ALU.subtract, r=["g_ta", "g_tb"], w=["FBR"])
            b.tt("dve", ta[:], BI[:], bcl(FR[:], 16), ALU.mult, r=["BI", "FR", "FBR"], w=["g_ta"])
            b.tt("dve", tb[:], BR[:], bcl(FI[:], 16), ALU.mult, r=["BR", "FI", "FBR"], w=["g_tb"])
            b.tt("dve", FBI[:], ta[:], tb[:], ALU.add, r=["g_ta", "g_tb"], w=["FBI"])
            cn = [T2("cn%d" % i, [128, 64]) for i in range(2)]
            pC = [P2("pC%d" % i, [128, 512]) for i in range(2)]
            k_ = 0
            for (src, dstt, key) in ((io["c_re"], CNR, "CNR"), (io["c_im"], CNI, "CNI")):
                for d in range(2):
                    for gb in range(4):
                        i = k_ % 2
                        k_ += 1
                        b.dma("sp" if i == 0 else "act", cn[i][:], src[d, gb * 8:(gb + 1) * 8].rearrange("g p n -> (g p) n"), w=["cn%d" % i])
                        b.tr(pC[i][0:64, 0:128], cn[i][:], ident_f[:], r=["cn%d" % i, "ident_f"], w=["pC%d" % i])
                        b.cp("act", dstt[:, d * 32 + gb * 8: d * 32 + (gb + 1) * 8, :], pC[i][0:64, 0:128].rearrange("n (g p) -> n g p", g=8),
                             r=["pC%d" % i], w=[key])
            S.flush()

        def cmul(e, ore, oim, are, aim, bre, bim, t1, t2, kr, kw, neg_im=False):
            b.tt(e, t1, are, bre, ALU.mult, r=kr, w=[kw + "t1"])
            b.tt(e, t2, aim, bim, ALU.mult, r=kr, w=[kw + "t2"])
            b.tt(e, ore, t1, t2, ALU.subtract, r=[kw + "t1", kw + "t2"], w=[kw + "R"])
            b.tt(e, t1, are, bim, ALU.mult, r=kr + [kw + "R"], w=[kw + "t1"])
            b.tt(e, t2, aim, bre, ALU.mult, r=kr + [kw + "R"], w=[kw + "t2"])
            if neg_im:
                b.S.I(e, lambda: b.e[e].scalar_tensor_tensor(out=oim, in0=t1, scalar=-1.0, in1=t2, op0=ALU.mult, op1=ALU.subtract),
                      r=[kw + "t1", kw + "t2"], w=[kw + "I"]) if e == "dve" else None
            else:
                b.tt(e, oim, t1, t2, ALU.add, r=[kw + "t1", kw + "t2"], w=[kw + "I"])

        for gb in range(4):
            with ExitStack() as stb:
                Tb = lambda name, shape, dt=F32: stb.enter_context(nc.sbuf_tensor("s_%s_b%d" % (name, gb), list(shape), dt))
                EfR = Tb("EfR", [64, 8, 128]); EfI = Tb("EfI", [64, 8, 128]); EbR = Tb("EbR", [64, 8, 128]); EbI = Tb("EbI", [64, 8, 128])
                Mt = Tb("Mt", [128, 8, 128]); Wt = Tb("Wt", [128, 8, 4, 64])
                pwaR = Tb("pwaR", [64, 16, 16]); pwaI = Tb("pwaI", [64, 16, 16])
                p8R = Tb("p8R", [64, 16, 16]); p8I = Tb("p8I", [64, 16, 16])
                gsl = lambda d: slice(d * 32 + gb * 8, d * 32 + (gb + 1) * 8)
                rb = Tb("rb", [64, 16]); tb_ = Tb("tb", [64, 16])
                for d in range(2):
                    b.cp("dve", rb[:, d * 8:(d + 1) * 8], RHO[:, gsl(d)], r=["RHO"], w=["rb"])
                    b.cp("dve", tb_[:, d * 8:(d + 1) * 8], TH[:, gsl(d)], r=["TH"], w=["tb"])
                with ExitStack() as sa:
                    Ta = lambda name, shape, dt=F32: sa.enter_context(nc.sbuf_tensor("s_%s_a%d" % (name, gb), list(shape), dt))
                    Pa = lambda name, shape, dt=F32: (S.excl.add(name), sa.enter_context(nc.psum_tensor("p_%s_a%d" % (name, gb), list(shape), dt)))[1]
                    pwdR = Ta("pwdR", [64, 16, 16]); pwdI = Ta("pwdI", [64, 16, 16])
                    S.lastw["RHO"] = S.lastw.get("rb"); S.lastw["TH"] = S.lastw.get("tb")
                    cpow(sa, pwaR[:], pwaI[:], rb[:], tb_[:], kka, 16, 16, "pwa")
                    cpow(sa, pwdR[:], pwdI[:], rb[:], tb_[:], kkd, 16, 16, "pwd")
                    cpow(sa, p8R[:], p8I[:], rb[:], tb_[:], kk8, 16, 16, "p8")
                    X0R = Ta("X0R", [64, 8, 8, 16]); X0I = Ta("X0I", [64, 8, 8, 16])
                    XpR = Ta("XpR", [64, 8, 8, 16]); XpI = Ta("XpI", [64, 8, 8, 16])
                    X1R = Ta("X1R", [64, 8, 8, 16]); X1I = Ta("X1I", [64, 8, 8, 16])
                    Y0R = Ta("Y0R", [64, 8, 8, 16]); Y0I = Ta("Y0I", [64, 8, 8, 16])
                    Y1R = Ta("Y1R", [64, 8, 8, 16]); Y1I = Ta("Y1I", [64, 8, 8, 16])
                    c1 = Ta("c1", [64, 8, 8, 16]); c2 = Ta("c2", [64, 8, 8, 16])
                    v4 = lambda t: t[:].rearrange("n g (s q) -> n g s q", q=16)

                    def pws(R_, I_, d, a):
                        return bcl(R_[:, d * 8:(d + 1) * 8, a:a + 8], 16), bcl(I_[:, d * 8:(d + 1) * 8, a:a + 8], 16)

                    def fbs(R_, I_, d):
                        return (R_[:, gsl(d), :].unsqueeze(2).to_broadcast([64, 8, 8, 16]), I_[:, gsl(d), :].unsqueeze(2).to_broadcast([64, 8, 8, 16]))

                    jobs = [
                        (X0R[:], X0I[:], pws(pwdR, pwdI, 0, 8), fbs(FBR, FBI, 0), ["pwd", "FBR", "FBI"], "X0", False),
                        (XpR[:], XpI[:], pws(pwdR, pwdI, 0, 1), fbs(FBR, FBI, 0), ["pwd", "FBR", "FBI"], "Xp", False),
                        (X1R[:], X1I[:], pws(pwaR, pwaI, 1, 7), fbs(FBR, FBI, 1), ["pwa", "FBR", "FBI"], "X1", False),
                        (Y0R[:], Y0I[:], pws(pwaR, pwaI, 0, 7), fbs(CNR, CNI, 0), ["pwa", "CNR", "CNI"], "Y0", True),
                        (Y1R[:], Y1I[:], pws(pwdR, pwdI, 1, 8), fbs(CNR, CNI, 1), ["pwd", "CNR", "CNI"], "Y1", True),
                        (v4(EfR), v4(EfI), pws(pwaR, pwaI, 0, 8), fbs(CNR, CNI, 0), ["pwa", "CNR", "CNI"], "Ef", True),
                        (v4(EbR), v4(EbI), pws(pwdR, pwdI, 1, 0), fbs(CNR, CNI, 1), ["pwd", "CNR", "CNI"], "Eb", True),
                    ]
                    for (ore, oim, (are, aim), (bre, bim), kr, kw, neg) in jobs:
                        cmul("dve", ore, oim, are, aim, bre, bim, c1[:], c2[:], kr + ["c1", "c2"], kw, neg_im=neg)
                        S.lastw["c1"] = S.lastw.get(kw + "I"); S.lastw["c2"] = S.lastw.get(kw + "I")
                    pM = [Pa("pM%d" % i, [128, 512]) for i in range(2)]
                    pW = [Pa("pW%d" % i, [128, 512]) for i in range(2)]
                    mt = Ta("mtmp", [128, 128])
                    f2 = lambda t, g: t[:, g, :, :].rearrange("n s q -> n (s q)")
                    for g in range(8):
                        i = g % 2
                        b.mm(pM[i][:, 0:128], f2(X0R, g), f2(Y0R, g), start=True, stop=False, r=["X0R", "Y0R"], w=["pM%d" % i])
                        b.mm(pM[i][:, 0:128], f2(X0I, g), f2(Y0I, g), start=False, stop=True, r=["X0I", "Y0I"], w=["pM%d" % i])
                        b.mm(pM[i][:, 128:256], f2(X1R, g), f2(Y1R, g), start=True, stop=False, r=["X1R", "Y1R"], w=["pM%d" % i])
                        b.mm(pM[i][:, 128:256], f2(X1I, g), f2(Y1I, g), start=False, stop=True, r=["X1I", "Y1I"], w=["pM%d" % i])
                        b.tt("dve", Mt[:, g, :], pM[i][:, 0:128], maskf, ALU.mult, r=["pM%d" % i, "cst"], w=["Mt"])
                        b.tt("dve", mt[:], pM[i][:, 128:256], maskb, ALU.mult, r=["pM%d" % i, "cst"], w=["mtmp"])
                        b.tt("dve", Mt[:, g, :], Mt[:, g, :], mt[:], ALU.add, r=["Mt", "mtmp"], w=["Mt"])
                        b.stt(Mt[:, g, :], ident_f[:], Dsq[:, gb * 8 + g: gb * 8 + g + 1], Mt[:, g, :], ALU.mult, ALU.add, r=["ident_f", "Dsq", "Mt"], w=["Mt"])
                        for k_, (src, key) in enumerate(((XpR, "XpR"), (XpI, "XpI"), (X1R, "X1R"), (X1I, "X1I"))):
                            b.tr(pW[i][:, k_ * 64:(k_ + 1) * 64], f2(src, g), ident_f[0:64, 0:64], r=[key, "ident_f"], w=["pW%d" % i])
                        b.cp("act", Wt[:, g, :, :], pW[i][:, 0:256].rearrange("p (k n) -> p k n", k=4), r=["pW%d" % i], w=["Wt"])
                    S.flush()
                if "stop3a" in dbg:
                    return
                with ExitStack() as sb:
                    Tq = lambda name, shape, dt=F32: sb.enter_context(nc.sbuf_tensor("s_%s_q%d" % (name, gb), list(shape), dt))
                    Pq = lambda name, shape, dt=F32: (S.excl.add(name), sb.enter_context(nc.psum_tensor("p_%s_q%d" % (name, gb), list(shape), dt)))[1]
                    uTc = Tq("uTc", [128, NKEY])
                    U = Tq("U", [128, 8, 544])
                    SfR = Tq("SfR", [64, 8, 288]); SfI = Tq("SfI", [64, 8, 288])
                    SbR = Tq("SbR", [64, 8, 544]); SbI = Tq("SbI", [64, 8, 544])
                    Yg = Tq("Yg", [128, 8, 256])
                    ygx = [Tq("ygx%d" % i, [128, 256]) for i in range(2)]
                    ygt = [Tq("ygt%d" % i, [128, 256]) for i in range(2)]
                    pU = [Pq("pU%d" % i, [128, 512]) for i in range(2)]
                    pSt = [Pq("pSt%d" % i, [128, 512]) for i in range(2)]
                    pY = [Pq("pY%d" % i, [128, 512]) for i in range(2)]
                    b.dma("sp", uTc[:], scr["uT_s"][gb], w=["uTc"])
                    uv = uTc[:].rearrange("p (c s) -> p c s", s=8)
                    n_ = 0
                    for g in range(8):
                        for hh in range(2):
                            i = n_ % 2
                            n_ += 1
                            for s_ in range(8):
                                b.mm(pU[i][:, 0:272], selc[:, g, (7 - s_) * 16:(7 - s_) * 16 + 128], uv[:, hh * 272:(hh + 1) * 272, s_],
                                     start=(s_ == 0), stop=(s_ == 7), r=["selc", "uTc"], w=["pU%d" % i])
                            b.cp("act" if i == 0 else "dve", U[:, g, hh * 272:(hh + 1) * 272], pU[i][:, 0:272], r=["pU%d" % i], w=["U"])
                    n_ = 0
                    for g in range(8):
                        for (k_, dst, c0, nn, key) in ((0, SfR, 0, 288, "SfR"), (1, SfI, 0, 288, "SfI"), (2, SbR, 0, 272, "SbR"), (2, SbR, 272, 272, "SbR"),
                                                       (3, SbI, 0, 272, "SbI"), (3, SbI, 272, 272, "SbI")):
                            i = n_ % 2
                            n_ += 1
                            b.mm(pSt[i][0:64, 0:nn], Wt[:, g, k_, :], U[:, g, c0:c0 + nn], r=["Wt", "U"], w=["pSt%d" % i])
                            b.cp("act" if i == 0 else "dve", dst[:, g, c0:c0 + nn], pSt[i][0:64, 0:nn], r=["pSt%d" % i], w=[key])
                    tA = Tq("tA", [64, 8, 34]); tB = Tq("tB", [64, 8, 34])
                    tC = Tq("tC", [64, 8, 34]); tD = Tq("tD", [64, 8, 34])
                    CIfR = Tq("CIfR", [64, 8, 18]); CIfI = Tq("CIfI", [64, 8, 18])
                    CIbR = Tq("CIbR", [64, 8, 34]); CIbI = Tq("CIbI", [64, 8, 34])
                    carf = Tq("carf", [64, 2, 8]); carb = Tq("carb", [64, 2, 8])
                    b.memset("dve", carf[:], 0.0, w=["carf"])
                    b.memset("pool", carb[:], 0.0, w=["carb"])

                    def cmac(e, dR, dI, cR, cI, xR, xI, ta_, tb2_, keys, tk):
                        b.tt(e, ta_, cR, xR, ALU.mult, r=keys, w=[tk + "a"])
                        b.tt(e, dR, dR, ta_, ALU.add, r=keys + [tk + "a"], w=keys[:1])
                        b.tt(e, ta_, cI, xI, ALU.mult, r=keys, w=[tk + "a"])
                        b.tt(e, dR, dR, ta_, ALU.subtract, r=keys + [tk + "a"], w=keys[:1])
                        b.tt(e, tb2_, cR, xI, ALU.mult, r=keys, w=[tk + "b"])
                        b.tt(e, dI, dI, tb2_, ALU.add, r=keys + [tk + "b"], w=keys[1:2])
                        b.tt(e, tb2_, cI, xR, ALU.mult, r=keys, w=[tk + "b"])
                        b.tt(e, dI, dI, tb2_, ALU.add, r=keys + [tk + "b"], w=keys[1:2])

                    def bscan(e, SR, SI, kR, kI, c0, nb, asc, d, car, CIR, CII, ci0, ta_, tb2_, tk, blks=None):
                        VR = SR[:, :, c0:c0 + nb * 16].rearrange("n g (b j) -> n g b j", j=16)
                        VI = SI[:, :, c0:c0 + nb * 16].rearrange("n g (b j) -> n g b j", j=16)
                        a8R = bcl(pwaR[:, d * 8:(d + 1) * 8, 15], nb); a8I = bcl(pwaI[:, d * 8:(d + 1) * 8, 15], nb)
                        keys = [kR, kI, "pwa", "p8", tk + "car", tk + "CI"]
                        js = range(1, 16) if asc else range(14, -1, -1)
                        for j in js:
                            pj = j - 1 if asc else j + 1
                            cmac(e, VR[:, :, :, j], VI[:, :, :, j], a8R, a8I, VR[:, :, :, pj], VI[:, :, :, pj], ta_[:, :, 0:nb], tb2_[:, :, 0:nb], keys, tk)
                        last = 15 if asc else 0
                        a128R = p8R[:, d * 8:(d + 1) * 8, 15]; a128I = p8I[:, d * 8:(d + 1) * 8, 15]
                        if blks is None:
                            blks = range(nb) if asc else range(nb - 1, -1, -1)
                        for bl in blks:
                            b.cp(e, CIR[:, :, ci0 + bl], car[:, 0, :], r=keys, w=[tk + "CI"])
                            b.cp(e, CII[:, :, ci0 + bl], car[:, 1, :], r=keys, w=[tk + "CI"])
                            b.tt(e, ta_[:, :, 0], a128R, car[:, 0, :], ALU.mult, r=keys, w=[tk + "a"])
                            b.tt(e, ta_[:, :, 1], a128I, car[:, 1, :], ALU.mult, r=keys, w=[tk + "a"])
                            b.tt(e, tb2_[:, :, 0], a128R, car[:, 1, :], ALU.mult, r=keys, w=[tk + "b"])
                            b.tt(e, tb2_[:, :, 1], a128I, car[:, 0, :], ALU.mult, r=keys, w=[tk + "b"])
                            b.tt(e, car[:, 0, :], ta_[:, :, 0], ta_[:, :, 1], ALU.subtract, r=[tk + "a"] + keys, w=[tk + "car"])
                            b.tt(e, car[:, 1, :], tb2_[:, :, 0], tb2_[:, :, 1], ALU.add, r=[tk + "b"] + keys, w=[tk + "car"])
                            b.tt(e, car[:, 0, :], car[:, 0, :], VR[:, :, bl, last], ALU.add, r=keys, w=[tk + "car"])
                            b.tt(e, car[:, 1, :], car[:, 1, :], VI[:, :, bl, last], ALU.add, r=keys, w=[tk + "car"])
                        for j in range(16):
                            pj = j if asc else 15 - j
                            cR = bcl(p8R[:, d * 8:(d + 1) * 8, pj], nb); cI = bcl(p8I[:, d * 8:(d + 1) * 8, pj], nb)
                            cmac(e, VR[:, :, :, j], VI[:, :, :, j], cR, cI, CIR[:, :, ci0:ci0 + nb], CII[:, :, ci0:ci0 + nb], ta_[:, :, 0:nb], tb2_[:, :, 0:nb], keys, tk)

                    bscan("dve", SfR, SfI, "SfR", "SfI", 0, 18, True, 0, carf, CIfR, CIfI, 0, tA, tB, "sf")
                    bscan("pool", SbR, SbI, "SbR", "SbI", 0, 34, False, 1, carb, CIbR, CIbI, 0, tC, tD, "sb", blks=[1, 0] + list(range(33, 1, -1)))
                    for g in range(8):
                        i = g % 2
                        b.mm(pY[i][:, 0:256], Mt[:, g, :], U[:, g, 32:288], start=True, stop=False, r=["Mt", "U"], w=["pY%d" % i])
                        b.mm(pY[i][:, 0:256], EfR[:, g, :], SfR[:, g, 31:287], start=False, stop=False, r=["Ef", "EfR", "SfR"], w=["pY%d" % i])
                        b.mm(pY[i][:, 0:256], EfI[:, g, :], SfI[:, g, 31:287], start=False, stop=False, r=["Ef", "EfI", "SfI"], w=["pY%d" % i])
                        b.mm(pY[i][:, 0:256], EbR[:, g, :], SbR[:, g, 33:289], start=False, stop=False, r=["Eb", "EbR", "SbR"], w=["pY%d" % i])
                        b.mm(pY[i][:, 0:256], EbI[:, g, :], SbI[:, g, 33:289], start=False, stop=True, r=["Eb", "EbI", "SbI"], w=["pY%d" % i])
                        if "s5_nogelu" in dbg:
                            b.cp("act", Yg[:, g, :], pY[i][:, 0:256], r=["pY%d" % i], w=["Yg"])
                        else:
                            b.cp("act", ygx[i][:], pY[i][:, 0:256], r=["pY%d" % i], w=["ygx%d" % i])
                            gelu_tanh(b, Yg[:, g, :], ygx[i][:], ygt[i][:], "ygx%d" % i, "ygt%d" % i, "Yg")
                    yv = ygT[:, gb, :].rearrange("p (c s) -> p c s", s=8)
                    for t8 in range(8):
                        i = t8 % 2
                        for g in range(8):
                            b.mm(pU[i][:, 0:256], selc[:, t8, (7 - g) * 16:(7 - g) * 16 + 128], Yg[:, g, :], start=(g == 0), stop=(g == 7),
                                 r=["selc", "Yg"], w=["pU%d" % i])
                        b.cp("act" if i == 0 else "dve", yv[:, :, t8], pU[i][:, 0:256], r=["pU%d" % i], w=["ygT"])
                    S.flush()
        if "ygT_d" in dbg:
            b.dma("sp", G["ygT_d"], ygT[:], r=["ygT"], w=["ygT_d"])
            S.flush()
            return
        with ExitStack() as sg:
            Tg = lambda name, shape, dt=F32: sg.enter_context(nc.sbuf_tensor("s_" + name, list(shape), dt))
            Pg = lambda name, shape, dt=F32: (S.excl.add(name), sg.enter_context(nc.psum_tensor("p_" + name, list(shape), dt)))[1]
            ygb = Tg("ygb", [128, 4, NOWN], BF16)
            wgf = Tg("wgf", [128, 4, 512]); wgb = Tg("wgb", [128, 4, 512], BF16)
            bgl = Tg("bgl", [128, 4])
            sig = [Tg("sig%d" % i, [128, 512]) for i in range(2)]
            ssmT = Tg("ssmT", [128, 4, NOWN], BF16)
            pZ = [Pg("pZ%d" % i, [128, 512]) for i in range(2)]
            b.dma("sp", wgf[:], io["w_glu"].rearrange("(j p) n -> p j n", p=128), w=["wgf"])
            b.dma("sp", bgl[:], io["b_glu"].rearrange("(c p) -> p c", p=128), w=["bgl"], slow=True)
            b.cp("pool", wgb[:], wgf[:], r=["wgf"], w=["wgb"])
            b.cp("dve", ygb[:], ygT[:], r=["ygT"], w=["ygb"])
            n_ = 0
            for oc in range(4):
                for tb2 in range(4):
                    i = n_ % 2
                    n_ += 1
                    for kc in range(4):
                        b.mm(pZ[i][:], wgb[:, kc, oc * 128:(oc + 1) * 128], ygb[:, kc, tb2 * 512:(tb2 + 1) * 512], start=(kc == 0), stop=(kc == 3),
                             r=["wgb", "ygb"], w=["pZ%d" % i])
                    b.act(sig[i][:], pZ[i][:], AF.Sigmoid, r=["pZ%d" % i, "bgl"], w=["sig%d" % i], bias=bgl[:, oc:oc + 1])
                    b.tt("dve", ssmT[:, oc, tb2 * 512:(tb2 + 1) * 512], ygT[:, oc, tb2 * 512:(tb2 + 1) * 512], sig[i][:], ALU.mult,
                         r=["ygT", "sig%d" % i], w=["ssmT"])
            b.dma("sp", scr["ssmT_s"][:, :, :], ssmT[:], r=["ssmT"], w=["ssmT_s"])
            S.flush()


def phase4(nc, S, b, io, scr, G):
    ident_b = G["ident_b"]
    with ExitStack() as st:
        T = lambda name, shape, dt=F32: st.enter_context(nc.sbuf_tensor("s_" + name, list(shape), dt))
        P = lambda name, shape, dt=F32: (S.excl.add(name), st.enter_context(nc.psum_tensor("p_" + name, list(shape), dt)))[1]
        wa = T("wa", [128, 8, D], BF16); ws = T("ws", [128, 4, D], BF16); wo = T("wo", [128, 8, D], BF16); wg = T("wg", [128, 8, 2048], BF16)
        stg = [T("stg%d" % i, [128, 8, 512]) for i in range(2)]
        g1row = T("g1row", [128, D])
        b.dma("sp", g1row[:], scr["vec_s"][0].partition_broadcast(128), w=["g1row"])
        n_ = 0
        for (src, dst, nj, ncol, key) in ((io["w_attn_up"], wa, 8, D, "wa"), (io["w_ssm_up"], ws, 4, D, "ws"), (io["w_out"], wo, 8, D, "wo"),
                                          (io["w_in"][:, 3584:5632], wg, 8, 2048, "wg")):
            for cb in range(ncol // 512):
                i = n_ % 2
                n_ += 1
                b.dma("sp" if i == 0 else "act", stg[i][:, 0:nj, :], src[:, cb * 512:(cb + 1) * 512].rearrange("(j p) n -> p j n", p=128), w=["stg%d" % i])
                b.cp("pool" if i == 0 else "dve", dst[:, :, cb * 512:(cb + 1) * 512], stg[i][:, 0:nj, :], r=["stg%d" % i], w=[key])
        xs_t = [T("xs_t%d" % i, [128, 8, 128], BF16) for i in range(2)]
        at_t = [T("at_t%d" % i, [128, 8, 128], BF16) for i in range(2)]
        ss_t = [T("ss_t%d" % i, [128, 4, 128], BF16) for i in range(2)]
        x_t = [T("x_t%d" % i, [128, D]) for i in range(2)]
        gs = T("gs", [128, 2048])
        m1 = T("m1", [128, D]); m2 = T("m2", [128, D]); mb = T("mb", [128, D], BF16)
        mT = T("mT", [128, 8, 128], BF16)
        x1 = [T("x1_%d" % i, [128, D]) for i in range(2)]
        pG = [P("pG%d" % i, [128, 512]) for i in range(2)]
        pA = [P("pA4_%d" % i, [128, 512]) for i in range(2)]
        pS = [P("pS4_%d" % i, [128, 512]) for i in range(2)]
        pTr = P("pTr4", [128, 1024], BF16)
        for ti in range(16):
            i = ti % 2
            cs = slice(ti * 128, (ti + 1) * 128)
            b.dma("sp", xs_t[i][:], scr["xnT_s"][:, :, cs], w=["xs_t%d" % i])
            b.dma("act", at_t[i][:], scr["attnT_s"][:, :, cs], w=["at_t%d" % i])
            b.dma("sp", ss_t[i][:], scr["ssmT_s"][:, :, cs], w=["ss_t%d" % i])
            b.dma("act", x_t[i][:], io["x_seq"][cs, :], w=["x_t%d" % i])
            for blk in range(4):
                pg = pG[blk % 2]
                kg = "pG%d" % (blk % 2)
                for j in range(8):
                    b.mm(pg[:], xs_t[i][:, j, :], wg[:, j, blk * 512:(blk + 1) * 512], start=(j == 0), stop=(j == 7), r=["xs_t%d" % i, "wg"], w=[kg])
                b.act(gs[:, blk * 512:(blk + 1) * 512], pg[:], AF.Sigmoid, r=[kg], w=["gs"])
            for hf in range(2):
                for j in range(8):
                    b.mm(pA[hf][:], at_t[i][:, j, :], wa[:, j, hf * 512:(hf + 1) * 512], start=(j == 0), stop=(j == 7), r=["at_t%d" % i, "wa"], w=["pA4_%d" % hf])
                for j in range(4):
                    b.mm(pS[hf][:], ss_t[i][:, j, :], ws[:, j, hf * 512:(hf + 1) * 512], start=(j == 0), stop=(j == 3), r=["ss_t%d" % i, "ws"], w=["pS4_%d" % hf])
                b.tt("dve", m1[:, hf * 512:(hf + 1) * 512], pA[hf][:], gs[:, hf * 512:(hf + 1) * 512], ALU.mult, r=["pA4_%d" % hf, "gs"], w=["m1"])
                b.tt("dve", m2[:, hf * 512:(hf + 1) * 512], pS[hf][:], gs[:, 1024 + hf * 512:1024 + (hf + 1) * 512], ALU.mult, r=["pS4_%d" % hf, "gs"], w=["m2"])
            b.tt("pool", mb[:], m1[:], m2[:], ALU.add, r=["m1", "m2"], w=["mb"])
            for j in range(8):
                b.tr(pTr[:, j * 128:(j + 1) * 128], mb[:, j * 128:(j + 1) * 128], ident_b[:], r=["mb", "ident_b"], w=["pTr4"])
            b.cp("act", mT[:], pTr[:].rearrange("p (j t) -> p j t", j=8), r=["pTr4"], w=["mT"])
            for hf in range(2):
                for j in range(8):
                    b.mm(pA[hf][:], mT[:, j, :], wo[:, j, hf * 512:(hf + 1) * 512], start=(j == 0), stop=(j == 7), r=["mT", "wo"], w=["pA4_%d" % hf])
                b.tt("dve", m1[:, hf * 512:(hf + 1) * 512], pA[hf][:], g1row[:, hf * 512:(hf + 1) * 512], ALU.mult, r=["pA4_%d" % hf, "g1row"], w=["m1"])
            b.tt("pool", x1[i][:], m1[:], x_t[i][:], ALU.add, r=["m1", "x_t%d" % i], w=["x1_%d" % i])
            b.dma("sp", scr["x1_s"][cs, :], x1[i][:], r=["x1_%d" % i], w=["x1_s"])
        S.flush()


def phase5(nc, S, b, io, scr, G, out):
    ident_f = G["ident_f"]
    dbg = G["dbg"]
    with ExitStack() as st:
        T = lambda name, shape, dt=F32: st.enter_context(nc.sbuf_tensor("s_" + name, list(shape), dt))
        P = lambda name, shape, dt=F32: (S.excl.add(name), st.enter_context(nc.psum_tensor("p_" + name, list(shape), dt)))[1]
        wq = T("wq", [128, 8, D])
        rows = T("rows5", [128, 4, D])
        k1T = T("k1T", [64, 128]); k2T = T("k2T", [128, 128])
        kk1 = T("kk1", [128, 64]); kk2 = T("kk2", [128, 128])
        iota16 = T("iota16", [128, 16])
        pX = P("pX", [128, 1024])
        pTQ = P("pTQ", [128, 1024])
        pSc = P("pSc", [128, 2048])
        b.dma("sp", wq[:], io["w_query"].rearrange("(j p) n -> p j n", p=128), w=["wq"])
        b.dma("act", rows[:, 0, :], scr["vec_s"][1].partition_broadcast(128), w=["rows5"])
        b.dma("act", rows[:, 1, :], scr["vec_s"][2].partition_broadcast(128), w=["rows5"])
        b.dma("act", rows[:, 2, :], scr["vec_s"][3].partition_broadcast(128), w=["rows5"])
        b.dma("act", rows[:, 3, :], io["final_g"].partition_broadcast(128), w=["rows5"])
        b.dma("sp", kk1[:], io["sub_k1"][:, :], w=["kk1"])
        b.memset("pool", kk2[:], 0.0, w=["kk2"])
        b.dma("sp", kk2[:, 64:128], io["sub_k2"][:, :], r=["kk2"], w=["kk2"])
        b.dma("sp", iota16[:], io["cst"][:, 320:336], w=["iota16"])
        b.tr(pTQ[0:64, 0:128], kk1[:], ident_f[:], r=["kk1", "ident_f"], w=["pTQ"])
        b.cp("act", k1T[:], pTQ[0:64, 0:128], r=["pTQ"], w=["k1T"])
        b.tr(pTQ[:, 128:256], kk2[:], ident_f[:], r=["kk2", "ident_f"], w=["pTQ"])
        b.cp("act", k2T[:], pTQ[:, 128:256], r=["pTQ"], w=["k2T"])
        x1 = [T("x1t%d" % i, [128, D]) for i in range(2)]
        xn2 = T("xn2", [128, D]); tmpf = T("tmpf", [128, D]); junk = T("junk5", [128, D])
        st5 = T("st5", [128, 8])
        xn2T = T("xn2T", [128, 8, 128]); qTs = T("qTs", [128, 8, 128])
        sc = T("sc", [128, 2, 8, 128]); wk = T("wk", [128, 256])
        v12 = T("v12", [128, 2, 8, 16]); i12 = T("i12", [128, 2, 8, 16], U32); i12f = T("i12f", [128, 2, 8, 16])
        cand = T("cand", [128, 8, 256]); tv = T("tv", [128, 8, 16]); tj = T("tj", [128, 8, 16], U32)
        ta = T("ta5", [128, 8, 16], I32); taf = T("taf", [128, 8, 16]); tbf = T("tbf", [128, 8, 16])
        eq = T("eq", [128, 8, 16, 16]); sel1 = T("sel1", [128, 8, 16]); sel2 = T("sel2", [128, 8, 16])
        idxf = T("idxf", [128, 128]); idx32 = T("idx32", [128, 128], I32)
        ge = T("ge", [128, 8, 16]); gsum = T("gsum", [128, 8]); gate = T("gate", [128, 128])
        actv = T("actv", [128, 128]); wv = T("wv", [128, 128]); gtmp = T("gtmp", [128, 8])
        NS = 16
        uvb = [T("uvb%d" % i, [128, 2 * D], BF16) for i in range(NS)]
        vt = [T("vt%d" % i, [128, D], BF16) for i in range(3)]
        ident_b = G["ident_b"]
        osb = [T("osb5_%d" % i, [128, D]) for i in range(2)]
        acc = pSc[:, 0:1024]
        nu = 0
        nv = 0
        for ti in range(16):
            i = ti % 2
            cs = slice(ti * 128, (ti + 1) * 128)
            kx = "x1t%d" % i
            b.dma("sp", x1[i][:], scr["x1_s"][cs, :], w=[kx])
            b.act(junk[:], x1[i][:], AF.Square, r=[kx], w=["junk5", "st5a"], accum=st5[:, 0:1])
            b.ts("dve", st5[:, 1:2], st5[:, 0:1], 1.0 / D, EPS, ALU.mult, ALU.add, r=["st5a"], w=["st5b"])
            b.act(st5[:, 2:3], st5[:, 1:2], AF.Sqrt, r=["st5b"], w=["st5c"])
            b.recip(st5[:, 3:4], st5[:, 2:3], r=["st5c"], w=["st5d"])
            b.stt(tmpf[:], x1[i][:], st5[:, 3:4], rows[:, 1, :], ALU.mult, ALU.mult, r=[kx, "st5d", "rows5"], w=["tmpf"])
            b.tt("dve", xn2[:], tmpf[:], rows[:, 2, :], ALU.add, r=["tmpf", "rows5"], w=["xn2"])
            b.cp("act", pX[:], xn2[:], r=["xn2"], w=["pX"])
            for j in range(8):
                b.tr(pTQ[:, j * 128:(j + 1) * 128], xn2[:, j * 128:(j + 1) * 128], ident_f[:], r=["xn2", "ident_f"], w=["pTQ"])
            b.cp("act", xn2T[:], pTQ[:].rearrange("p (j t) -> p j t", j=8), r=["pTQ"], w=["xn2T"])
            for h in range(8):
                for j in range(8):
                    b.mm(pTQ[:, h * 128:(h + 1) * 128], wq[:, j, h * 128:(h + 1) * 128], xn2T[:, j, :], start=(j == 0), stop=(j == 7), r=["wq", "xn2T"], w=["pTQ"])
            b.cp("act", qTs[:], pTQ[:].rearrange("p (h t) -> p h t", h=8), r=["pTQ"], w=["qTs"])
            for h in range(8):
                b.mm(pSc[:, h * 128:(h + 1) * 128], qTs[0:64, h, :], k1T[:, :], r=["qTs", "k1T"], w=["pSc"])
            for h in range(8):
                b.mm(pSc[:, 1024 + h * 128:1024 + (h + 1) * 128], qTs[64:128, h, :], k2T[64:128, :], r=["qTs", "k2T"], w=["pSc"])
            b.cp("dve", sc[:, 0].rearrange("p h k -> p (h k)"), pSc[:, 0:1024], r=["pSc"], w=["sc"])
            b.cp("dve", sc[:, 1].rearrange("p h k -> p (h k)"), pSc[:, 1024:2048], r=["pSc"], w=["sc"])
            def route_head(h):
                for sd in range(2):
                    src = sc[:, sd, h, :]
                    b.S.I("dve", (lambda o=v12[:, sd, h, 0:8], s_=src: nc.vector.max(out=o, in_=s_)), r=["sc"], w=["v12"])
                    b.S.I("dve", (lambda o=i12[:, sd, h, 0:8], m=v12[:, sd, h, 0:8], s_=src: nc.vector.max_index(out=o, in_max=m, in_values=s_)), r=["sc", "v12"], w=["i12"])
                    b.S.I("dve", (lambda o=wk[:, 0:128], m=v12[:, sd, h, 0:8], s_=src: nc.vector.match_replace(out=o, in_to_replace=m, in_values=s_, imm_value=-1e30)), r=["sc", "v12"], w=["wk"])
                    b.S.I("dve", (lambda o=v12[:, sd, h, 8:16], s_=wk[:, 0:128]: nc.vector.max(out=o, in_=s_)), r=["wk"], w=["v12"])
                    b.S.I("dve", (lambda o=i12[:, sd, h, 8:16], m=v12[:, sd, h, 8:16], s_=wk[:, 0:128]: nc.vector.max_index(out=o, in_max=m, in_values=s_)), r=["wk", "v12"], w=["i12"])
                b.tt("dve", cand[:, h, :].rearrange("p (a c) -> p a c", a=16), bcl(v12[:, 0, h, :], 16), v12[:, 1, h, :].unsqueeze(1).to_broadcast([128, 16, 16]),
                     ALU.add, r=["v12"], w=["cand"])
                src = cand[:, h, :]
                b.S.I("dve", (lambda o=tv[:, h, 0:8], s_=src: nc.vector.max(out=o, in_=s_)), r=["cand"], w=["tv"])
                b.S.I("dve", (lambda o=tj[:, h, 0:8], m=tv[:, h, 0:8], s_=src: nc.vector.max_index(out=o, in_max=m, in_values=s_)), r=["cand", "tv"], w=["tj"])
                b.S.I("dve", (lambda o=wk[:, 0:256], m=tv[:, h, 0:8], s_=src: nc.vector.match_replace(out=o, in_to_replace=m, in_values=s_, imm_value=-1e30)), r=["cand", "tv"], w=["wk"])
                b.S.I("dve", (lambda o=tv[:, h, 8:16], s_=wk[:, 0:256]: nc.vector.max(out=o, in_=s_)), r=["wk"], w=["tv"])
                b.S.I("dve", (lambda o=tj[:, h, 8:16], m=tv[:, h, 8:16], s_=wk[:, 0:256]: nc.vector.max_index(out=o, in_max=m, in_values=s_)), r=["wk", "tv"], w=["tj"])
                b.cp("dve", tbf[:, h, :], tj[:, h, :], r=["tj"], w=["tbf"])
                b.ts("dve", taf[:, h, :], tbf[:, h, :], 1.0 / 16.0, None, ALU.mult, r=["tbf"], w=["taf"])
                b.cp("dve", ta[:, h, :], taf[:, h, :], r=["taf"], w=["ta5"])
                b.cp("dve", sel1[:, h, :], ta[:, h, :], r=["ta5"], w=["sel1"])
                b.tt("dve", sel2[:, h, :], taf[:, h, :], sel1[:, h, :], ALU.subtract, r=["taf", "sel1"], w=["sel2"])
                b.ts("dve", sel2[:, h, :], sel2[:, h, :], 0.0, None, ALU.is_lt, r=["sel2"], w=["sel2"])
                b.tt("dve", taf[:, h, :], sel1[:, h, :], sel2[:, h, :], ALU.subtract, r=["sel1", "sel2"], w=["taf"])
                b.stt(tbf[:, h, :], taf[:, h, :], -16.0, tbf[:, h, :], ALU.mult, ALU.add, r=["taf", "tbf"], w=["tbf"])
                b.cp("dve", i12f[:, 0, h, :], i12[:, 0, h, :], r=["i12"], w=["i12f"])
                b.cp("dve", i12f[:, 1, h, :], i12[:, 1, h, :], r=["i12"], w=["i12f"])
                io16 = iota16[:].unsqueeze(1).to_broadcast([128, 16, 16])
                for (pos, side, dst, key) in ((taf, 0, sel1, "sel1"), (tbf, 1, sel2, "sel2")):
                    b.tt("dve", eq[:, h], bcl(pos[:, h, :], 16), io16, ALU.is_equal, r=["taf", "tbf", "iota16"], w=["eq"])
                    b.tt("dve", eq[:, h], eq[:, h], i12f[:, side, h, :].unsqueeze(1).to_broadcast([128, 16, 16]), ALU.mult, r=["eq", "i12f"], w=["eq"])
                    b.red(dst[:, h, :], eq[:, h], ALU.add, r=["eq"], w=[key])
                hs = slice(h * 16, (h + 1) * 16)
                b.stt(idxf[:, hs], sel1[:, h, :], 128.0, sel2[:, h, :], ALU.mult, ALU.add, r=["sel1", "sel2"], w=["idxf"])
                b.cp("dve", idx32[:, hs], idxf[:, hs], r=["idxf"], w=["idx32_%d" % h])
                b.ts("dve", ge[:, h, :], tv[:, h, :], tv[:, h, 0:1], None, ALU.subtract, r=["tv"], w=["ge%d" % h])
                b.act(ge[:, h, :], ge[:, h, :], AF.Exp, r=["ge%d" % h], w=["ge%d" % h])
                b.red(gsum[:, h:h + 1], ge[:, h, :], ALU.add, r=["ge%d" % h], w=["gsum"])
                b.recip(gsum[:, h:h + 1], gsum[:, h:h + 1], r=["gsum"], w=["gsum"])
                b.ts("dve", gate[:, hs], ge[:, h, :], gsum[:, h:h + 1], None, ALU.mult, r=["ge%d" % h, "gsum"], w=["gate"])

            def experts(h):
                nonlocal nu, nv
                for g8 in (2 * h, 2 * h + 1):
                    sls = []
                    for k in range(8):
                        hk = g8 * 8 + k
                        sl = nu % NS
                        nu += 1
                        sls.append(sl)
                        S.D("pool", (lambda o=uvb[sl][:], ix=idx32[:, hk:hk + 1]: nc.gpsimd.indirect_dma_start(
                            out=o, out_offset=None, in_=scr["puv_b"].rearrange("e a d -> e (a d)"), in_offset=bass.IndirectOffsetOnAxis(ap=ix, axis=0))),
                            r=["idx32_%d" % h], w=["uvb%d" % sl])
                        b.stt(junk[:], uvb[sl][:, 0:D], 1.0, pX[:], ALU.mult, ALU.mult, r=["uvb%d" % sl, "pX"], w=["junk5", "actv"], accum=actv[:, hk:hk + 1])
                    cs8 = slice(g8 * 8, (g8 + 1) * 8)
                    gelu_tanh(b, wv[:, cs8], actv[:, cs8], gtmp[:, 0:8], "actv", "gtmp", "wv%d" % h)
                    b.tt("dve", wv[:, cs8], wv[:, cs8], gate[:, cs8], ALU.mult, r=["wv%d" % h, "gate"], w=["wv%d" % h])
                    for k in range(8):
                        hk = g8 * 8 + k
                        sl = sls[k]
                        s3 = nv % 3
                        nv += 1
                        b.act(vt[s3][:], uvb[sl][:, D:2 * D], AF.Copy, r=["uvb%d" % sl, "wv%d" % h], w=["vt%d" % s3], scale=wv[:, hk:hk + 1])
                        for hf in range(2):
                            b.mm(acc[:, hf * 512:(hf + 1) * 512], ident_b[:], vt[s3][:, hf * 512:(hf + 1) * 512], start=(hk == 0), stop=(hk == 127),
                                 r=["ident_b", "vt%d" % s3], w=["pSc"])

            route_head(0)
            for h in range(8):
                if h + 1 < 8:
                    route_head(h + 1)
                experts(h)
            b.tt("dve", tmpf[:], acc, rows[:, 0, :], ALU.mult, r=["pSc", "rows5"], w=["tmpf"])
            b.tt("dve", tmpf[:], tmpf[:], x1[i][:], ALU.add, r=["tmpf", kx], w=["tmpf"])
            b.act(junk[:], tmpf[:], AF.Square, r=["tmpf"], w=["junk5", "st5e"], accum=st5[:, 4:5])
            b.ts("dve", st5[:, 5:6], st5[:, 4:5], 1.0 / D, EPS, ALU.mult, ALU.add, r=["st5e"], w=["st5f"])
            b.act(st5[:, 6:7], st5[:, 5:6], AF.Sqrt, r=["st5f"], w=["st5g"])
            b.recip(st5[:, 7:8], st5[:, 6:7], r=["st5g"], w=["st5h"])
            b.stt(osb[i][:], tmpf[:], st5[:, 7:8], rows[:, 3, :], ALU.mult, ALU.mult, r=["tmpf", "st5h", "rows5"], w=["osb5_%d" % i])
            b.dma("sp", out[cs, :], osb[i][:], r=["osb5_%d" % i], w=["out"])
        S.flush()


def host_constants(half):
    t = np.arange(NT)
    if half == 1:
        t = t[::-1]
    posr = (t // 64).astype(np.float32)
    posc = (t % 64).astype(np.float32)
    p = np.arange(128)
    posT = np.where(((p % 64) < 32)[:, None], posr[None, :], posc[None, :]).astype(np.float32)
    fidx = (p % 16).astype(np.float32)[:, None]
    prot = np.zeros((128, 128), np.float32)
    for m in range(128):
        if (m % 32) < 16:
            prot[m + 16, m] = -1.0
        else:
            prot[m - 16, m] = 1.0
    selc = np.zeros((128, 8, 240), np.float32)
    for a in range(8):
        for q in range(16):
            selc[a * 16 + q, a, 112 + q] = 1.0
    cst = np.zeros((128, 512), np.float32)
    sidx = np.arange(128) // 16
    cst[:, 0:128] = (sidx[:, None] <= sidx[None, :]).astype(np.float32)
    cst[:, 128:256] = (sidx[:, None] >= sidx[None, :]).astype(np.float32)
    cst[:, 256:272] = np.arange(-7, 9, dtype=np.float32)[None, :]
    cst[:, 272:288] = (8 - np.arange(16, dtype=np.float32))[None, :]
    cst[:, 288:304] = (8.0 * (np.arange(16, dtype=np.float32) + 1))[None, :]
    cst[:, 304] = 1.0
    cst[:, 320:336] = np.arange(16, dtype=np.float32)[None, :]
    return dict(posT=np.ascontiguousarray(posT), fidx=fidx, prot=prot, selc=selc, cst=cst)


def make_in_maps(inputs):
    g = lambda k: np.asarray(inputs[k], dtype=np.float32)
    maps = []
    for core in range(8):
        bi, half = core // 2, core % 2
        xs = g("x")[bi]
        cs = g("ctx")[bi]
        dsel = [0, 1]
        if half == 1:
            xs = xs[::-1]
            cs = cs[::-1]
            dsel = [1, 0]
        m = dict(
            x_seq=np.ascontiguousarray(xs), ctx_seq=np.ascontiguousarray(cs), c_vec=np.ascontiguousarray(g("c")[bi]), c_ctx=g("c_ctx"),
            ada_w=g("ada_w")[0], ada_b=g("ada_b")[0], norm1_g=g("norm1_g")[0], norm2_g=g("norm2_g")[0], w_in=g("w_in")[0],
            lam4=np.ascontiguousarray(np.stack([g("lambda_q1")[0], g("lambda_k1")[0], g("lambda_q2")[0], g("lambda_k2")[0]])),
            subln_g=g("subln_g")[0], w_attn_up=g("w_attn_up")[0],
            a_re=np.ascontiguousarray(g("ssm_a_re")[0][dsel]), a_im=np.ascontiguousarray(g("ssm_a_im")[0][dsel]),
            log_dt=np.ascontiguousarray(g("ssm_log_dt")[0][dsel]), b_re=np.ascontiguousarray(g("ssm_b_re")[0][dsel]),
            b_im=np.ascontiguousarray(g("ssm_b_im")[0][dsel]), c_re=np.ascontiguousarray(g("ssm_c_re")[0][dsel]),
            c_im=np.ascontiguousarray(g("ssm_c_im")[0][dsel]), ssm_d=g("ssm_d")[0], w_glu=g("w_glu")[0], b_glu=g("b_glu")[0],
            w_ssm_up=g("w_ssm_up")[0], w_out=g("w_out")[0], w_query=g("peer_w_query")[0], sub_k1=g("peer_sub_k1")[0],
            sub_k2=g("peer_sub_k2")[0], peer_u=g("peer_u")[0], peer_v=g("peer_v")[0], final_g=g("final_norm_g"),
        )
        m.update(host_constants(half))
        maps.append(m)
    return maps


def kernel(**inputs):
    nc = build_program()
    maps = make_in_maps(inputs)
    res = run_bass_kernel_spmd(nc, maps, core_ids=list(range(8)))
    outp = np.zeros((4, NT, D), np.float32)
    for core in range(8):
        bi, half = core // 2, core % 2
        o = np.asarray(res.results[core]["out"], dtype=np.float32)
        if half == 0:
            outp[bi, :NOWN] = o
        else:
            outp[bi, NOWN:] = o[::-1]
    return outp
```

```python
import math
from contextlib import ExitStack

import numpy as np
import concourse.bass as bass
import concourse.mybir as mybir
from concourse.bass_utils import run_bass_kernel_spmd

F32 = mybir.dt.float32
BF16 = mybir.dt.bfloat16
I32 = mybir.dt.int32
U32 = mybir.dt.uint32
AF = mybir.ActivationFunctionType
ALU = mybir.AluOpType
AX = mybir.AxisListType

ENG = ("pe", "dve", "act", "pool", "sp")
D = 1024
NT = 4096
NOWN = 2048
NCTX = 256
NKEY = NCTX + NT
EPS = 1e-6
LAM_INIT = 0.2
PI = math.pi


class Sched:
    def __init__(self, nc, stack, n_dma_sems=32):
        self.nc = nc
        self.eobj = {"pe": nc.tensor, "dve": nc.vector, "act": nc.scalar, "pool": nc.gpsimd, "sp": nc.sync}
        self.sem = {e: stack.enter_context(nc.semaphore("sem_" + e)) for e in ENG if e != "sp"}
        self.cnt = {e: 0 for e in ENG}
        self.dsem = [stack.enter_context(nc.semaphore("dsem%d" % i)) for i in range(n_dma_sems)]
        self.dval = [0] * n_dma_sems
        self.dnext = 0
        self.waited = {e: {} for e in ENG}
        self.ops = {e: [] for e in ENG}
        self.lastw = {}
        self.readers = {}
        self.ninstr = 0
        self.excl = set()
        sems = list(self.sem.values()) + self.dsem
        with nc.Block() as block:
            @block.sync
            def _(eng):
                for h in sems:
                    nc.sync.sem_clear(h)

    def _semh(self, semkey):
        return self.sem[semkey] if isinstance(semkey, str) else self.dsem[semkey[1]]

    def _wait(self, e, tok):
        semkey, val, teng = tok
        if self.waited[e].get(semkey, 0) >= val:
            return
        self.waited[e][semkey] = val
        h = self._semh(semkey)
        eo = self.eobj[e]
        self.ops[e].append(lambda: eo.wait_ge(h, val))

    def _deps(self, e, r, w):
        toks = []
        for k in r:
            t = self.lastw.get(k)
            if t is not None and not (t[2] == e and e == "pe"):
                toks.append(t)
            if k in self.excl:
                for t in self.readers.get(k, ()):
                    if t[2] != e:
                        toks.append(t)
        for k in w:
            t = self.lastw.get(k)
            if t is not None and not (t[2] == e and t[0] == e):
                toks.append(t)
            for t in self.readers.get(k, ()):
                if t[2] == e and t[0] == e:
                    continue
                toks.append(t)
        return toks

    def _record(self, tok, r, w):
        for k in r:
            self.readers.setdefault(k, []).append(tok)
        for k in w:
            self.lastw[k] = tok
            self.readers[k] = []
        self.ninstr += 1

    def I(self, e, fn, r=(), w=()):
        for t in self._deps(e, r, w):
            self._wait(e, t)
        self.cnt[e] += 1
        val = self.cnt[e]
        h = self.sem[e]
        self.ops[e].append(lambda: fn().then_inc(h, 1))
        tok = (e, val, e)
        self._record(tok, r, w)
        return tok

    def D(self, e, fn, r=(), w=()):
        for t in self._deps(e, r, w):
            self._wait(e, t)
        i = self.dnext
        self.dnext = (self.dnext + 1) % len(self.dsem)
        if self.dval[i] > 0:
            self._wait(e, (("d", i), self.dval[i], "dma"))
        self.dval[i] += 16
        val = self.dval[i]
        h = self.dsem[i]
        self.ops[e].append(lambda: fn().then_inc(h, 16))
        tok = (("d", i), val, "dma")
        self._record(tok, r, w)
        return tok

    def wait_all(self, e="sp"):
        for i in range(len(self.dsem)):
            if self.dval[i] > 0:
                self._wait(e, (("d", i), self.dval[i], "dma"))
        for e2 in ENG:
            if e2 != "sp" and e2 != e and self.cnt[e2] > 0:
                self._wait(e, (e2, self.cnt[e2], e2))

    def flush(self):
        self.wait_all("sp")
        ops = self.ops
        self.ops = {e: [] for e in ENG}
        with self.nc.Block() as block:
            @block.tensor
            def _(eng):
                for f in ops["pe"]:
                    f()

            @block.vector
            def _(eng):
                for f in ops["dve"]:
                    f()

            @block.scalar
            def _(eng):
                for f in ops["act"]:
                    f()

            @block.gpsimd
            def _(eng):
                for f in ops["pool"]:
                    f()

            @block.sync
            def _(eng):
                for f in ops["sp"]:
                    f()
        self.lastw = {}
        self.readers = {}


class Bld:
    def __init__(self, nc, S):
        self.nc = nc
        self.S = S
        self.e = {"dve": nc.vector, "pool": nc.gpsimd, "act": nc.scalar, "sp": nc.sync, "pe": nc.tensor}

    def mm(self, out, lhsT, rhs, start=True, stop=True, r=(), w=()):
        nc = self.nc
        return self.S.I("pe", lambda: nc.tensor.matmul(out, lhsT=lhsT, rhs=rhs, start=start, stop=stop), r=r, w=w)

    def tr(self, out, in_, ident, r=(), w=()):
        nc = self.nc
        return self.S.I("pe", lambda: nc.tensor.transpose(out=out, in_=in_, identity=ident), r=r, w=w)

    def act(self, out, in_, func, r=(), w=(), scale=None, bias=None, accum=None):
        nc = self.nc
        kw = {}
        if scale is not None:
            kw["scale"] = scale
        if bias is not None:
            kw["bias"] = bias
        if accum is not None:
            kw["accum_out"] = accum
        return self.S.I("act", lambda: nc.scalar.activation(out=out, in_=in_, func=func, **kw), r=r, w=w)

    def tt(self, e, out, in0, in1, op, r=(), w=()):
        eo = self.e[e]
        return self.S.I(e, lambda: eo.tensor_tensor(out=out, in0=in0, in1=in1, op=op), r=r, w=w)

    def ts(self, e, out, in0, s1, s2, op0, op1=None, r=(), w=(), accum=None):
        eo = self.e[e]
        kw = {}
        if op1 is not None:
            kw["op1"] = op1
        if accum is not None:
            kw["accum_out"] = accum
        return self.S.I(e, lambda: eo.tensor_scalar(out=out, in0=in0, scalar1=s1, scalar2=s2, op0=op0, **kw), r=r, w=w)

    def stt(self, out, in0, scalar, in1, op0, op1, r=(), w=(), accum=None):
        nc = self.nc
        kw = {}
        if accum is not None:
            kw["accum_out"] = accum
        return self.S.I("dve", lambda: nc.vector.scalar_tensor_tensor(out=out, in0=in0, scalar=scalar, in1=in1, op0=op0, op1=op1, **kw), r=r, w=w)

    def cp(self, e, out, in_, r=(), w=()):
        if e == "act":
            nc = self.nc
            return self.S.I("act", lambda: nc.scalar.copy(out=out, in_=in_), r=r, w=w)
        eo = self.e[e]
        return self.S.I(e, lambda: eo.tensor_copy(out=out, in_=in_), r=r, w=w)

    def memset(self, e, ap, val, w=()):
        eo = self.e[e]
        return self.S.I(e, lambda: eo.memset(ap, val), w=w)

    def dma(self, e, out, in_, r=(), w=(), slow=False):
        eo = self.e[e]
        if slow:
            return self.S.D(e, lambda: eo.dma_start(out=out, in_=in_, allow_slow_non_contiguous=True), r=r, w=w)
        return self.S.D(e, lambda: eo.dma_start(out=out, in_=in_), r=r, w=w)

    def red(self, out, in_, op, axis=AX.X, r=(), w=()):
        nc = self.nc
        return self.S.I("dve", lambda: nc.vector.tensor_reduce(out=out, in_=in_, axis=axis, op=op), r=r, w=w)

    def recip(self, out, in_, r=(), w=()):
        nc = self.nc
        return self.S.I("dve", lambda: nc.vector.reciprocal(out=out, in_=in_), r=r, w=w)


GELU_C = 2.0 * math.sqrt(2.0 / math.pi)


def gelu_tanh(b, out, x, t, kx, kt, ko):
    b.tt("dve", t, x, x, ALU.mult, r=[kx], w=[kt])
    b.ts("dve", t, t, 0.044715, 1.0, ALU.mult, ALU.add, r=[kt], w=[kt])
    b.tt("dve", t, t, x, ALU.mult, r=[kt, kx], w=[kt])
    b.act(t, t, AF.Sigmoid, r=[kt], w=[kt], scale=GELU_C)
    b.tt("dve", out, x, t, ALU.mult, r=[kx, kt], w=[ko])


def bcl(ap, n):
    sh = list(ap.shape)
    return ap.unsqueeze(len(sh)).to_broadcast(sh + [n])


def build_program(debug=()):
    nc = bass.Bass("TRN2", target_bir_lowering=False)
    dbg = set(debug)

    def din(name, shape, dt=F32):
        return nc.dram_tensor(name, list(shape), dt, kind="ExternalInput").ap()

    def dscr(name, shape, dt=F32):
        kind = "ExternalOutput" if name in dbg else "Internal"
        return nc.dram_tensor(name, list(shape), dt, kind=kind).ap()

    io = dict(
        x_seq=din("x_seq", [NT, D]), ctx_seq=din("ctx_seq", [NCTX, D]), c_vec=din("c_vec", [D]), c_ctx=din("c_ctx", [D]),
        ada_w=din("ada_w", [D, 6 * D]), ada_b=din("ada_b", [6 * D]), norm1_g=din("norm1_g", [D]), norm2_g=din("norm2_g", [D]),
        w_in=din("w_in", [D, 5632]), lam4=din("lam4", [4, 64]), subln_g=din("subln_g", [128]),
        w_attn_up=din("w_attn_up", [D, D]), a_re=din("a_re", [2, 32, 64]), a_im=din("a_im", [2, 32, 64]),
        log_dt=din("log_dt", [2, 32]), b_re=din("b_re", [2, 32, 64, 16]), b_im=din("b_im", [2, 32, 64, 16]),
        c_re=din("c_re", [2, 32, 16, 64]), c_im=din("c_im", [2, 32, 16, 64]), ssm_d=din("ssm_d", [512]),
        w_glu=din("w_glu", [512, 512]), b_glu=din("b_glu", [512]), w_ssm_up=din("w_ssm_up", [512, D]),
        w_out=din("w_out", [D, D]), w_query=din("w_query", [D, D]), sub_k1=din("sub_k1", [128, 64]),
        sub_k2=din("sub_k2", [128, 64]), peer_u=din("peer_u", [16384, D]), peer_v=din("peer_v", [16384, D]),
        final_g=din("final_g", [D]), posT=din("posT", [128, NT]), fidx=din("fidx", [128, 1]), prot=din("prot", [128, 128]),
        selc=din("selc", [128, 8, 240]), cst=din("cst", [128, 512]),
    )
    out = nc.dram_tensor("out", [NOWN, D], F32, kind="ExternalOutput").ap()
    scr = dict(
        vec_s=dscr("vec_s", [4, D]),
        qT_s=dscr("qT_s", [8, 2, 65, NOWN], BF16),
        kT_s=dscr("kT_s", [8, 2, 65, NKEY], BF16),
        v_s=dscr("v_s", [34, 128, 8, 130], BF16),
        uT_s=dscr("uT_s", [4, 128, NKEY]),
        xnT_s=dscr("xnT_s", [128, 8, NOWN], BF16),
        attnT_s=dscr("attnT_s", [128, 8, NOWN], BF16),
        ssmT_s=dscr("ssmT_s", [128, 4, NOWN], BF16),
        x1_s=dscr("x1_s", [NOWN, D]),
        puv_b=dscr("puv_b", [16384, 2, D], BF16),
    )

    with ExitStack() as st:
        S = Sched(nc, st)
        b = Bld(nc, S)
        T = lambda name, shape, dt=F32: st.enter_context(nc.sbuf_tensor("s_" + name, list(shape), dt))
        G = {}
        G["ident_f"] = T("ident_f", [128, 128])
        G["ident_b"] = T("ident_b", [128, 128], BF16)
        G["vecT"] = T("vecT", [128, 10, 8])
        G["lam"] = T("lam", [128, 4])
        G["dbg"] = dbg
        if "stop5a" in dbg:
            G["idx_d"] = nc.dram_tensor("idx_d", [128, 128], I32, kind="ExternalOutput").ap()
            G["gate_d"] = nc.dram_tensor("gate_d", [128, 128], F32, kind="ExternalOutput").ap()
            G["sc_d"] = nc.dram_tensor("sc_d", [128, 2048], F32, kind="ExternalOutput").ap()
        if "ygT_d" in dbg:
            G["ygT_d"] = nc.dram_tensor("ygT_d", [128, 4, NOWN], F32, kind="ExternalOutput").ap()
        phase0(nc, S, b, io, scr, G)
        if "stop0" in dbg:
            return nc
        if "only5" in dbg:
            x1_in = nc.dram_tensor("x1_in", [NOWN, D], F32, kind="ExternalInput").ap()
            with nc.sbuf_tensor("s_cpy", [128, 16, D], F32) as cpy:
                b.dma("sp", cpy[:], x1_in.rearrange("(t p) d -> p t d", p=128), w=["cpy"])
                b.dma("sp", scr["x1_s"].rearrange("(t p) d -> p t d", p=128), cpy[:], r=["cpy"], w=["x1_s"])
                S.flush()
            phase5(nc, S, b, io, scr, G, out)
            return nc
        phase1(nc, S, b, io, scr, G)
        if "stop1" in dbg:
            return nc
        if "skip2" not in dbg:
            phase2(nc, S, b, io, scr, G)
        if "stop2" in dbg:
            return nc
        if "skip3" not in dbg:
            phase3(nc, S, b, io, scr, G)
        if "stop3" in dbg:
            return nc
        phase4(nc, S, b, io, scr, G)
        if "stop4" in dbg:
            return nc
        phase5(nc, S, b, io, scr, G, out)
    return nc


V_SCALE1, V_SHIFT1, V_SCALE1C, V_SHIFT1C, V_SCALE2, V_SHIFT2, V_G1, V_G2, V_N1G, V_N2G = range(10)
R_G1, R_G2, R_SCALE2, R_SHIFT2, R_FING = range(5)


def phase0(nc, S, b, io, scr, G):
    with ExitStack() as st:
        T = lambda name, shape, dt=F32: st.enter_context(nc.sbuf_tensor("s_" + name, list(shape), dt))
        P = lambda name, shape, dt=F32: (S.excl.add(name), st.enter_context(nc.psum_tensor("p_" + name, list(shape), dt)))[1]
        ident_f, ident_b, vecT, lam = G["ident_f"], G["ident_b"], G["vecT"], G["lam"]
        b.memset("pool", ident_f[:], 0.0, w=["ident_f"])
        S.I("pool", lambda: nc.gpsimd.affine_select(out=ident_f[:], in_=ident_f[:], pattern=[[-1, 128]], compare_op=ALU.not_equal,
                                                    fill=1.0, base=0, channel_multiplier=1), r=["ident_f"], w=["ident_f"])
        b.cp("pool", ident_b[:], ident_f[:], r=["ident_f"], w=["ident_b"])

        cT = T("cT", [128, 8, 2])
        b.dma("sp", cT[:, :, 0], io["c_vec"].rearrange("(j p) -> p j", p=128), w=["cT"], slow=True)
        b.dma("sp", cT[:, :, 1], io["c_ctx"].rearrange("(j p) -> p j", p=128), w=["cT"], slow=True)
        b.act(cT[:], cT[:], AF.Silu, r=["cT"], w=["cT"])
        adabT = T("adabT", [128, 48])
        b.dma("sp", adabT[:], io["ada_b"].rearrange("(c p) -> p c", p=128), w=["adabT"], slow=True)
        b.dma("sp", vecT[:, V_N1G, :], io["norm1_g"].rearrange("(j p) -> p j", p=128), w=["n1g"], slow=True)
        b.dma("sp", vecT[:, V_N2G, :], io["norm2_g"].rearrange("(j p) -> p j", p=128), w=["n2g"], slow=True)
        aw = [T("aw%d" % i, [128, 8, D]) for i in range(2)]
        modps = P("modps", [128, 48, 2])
        modT = T("modT", [128, 48, 2])
        for pc in range(6):
            sl = pc % 2
            b.dma("sp" if pc % 2 == 0 else "act", aw[sl][:], io["ada_w"][:, pc * D:(pc + 1) * D].rearrange("(j p) n -> p j n", p=128), w=["aw%d" % sl])
            for cc in range(8):
                for j in range(8):
                    b.mm(modps[:, pc * 8 + cc, :], aw[sl][:, j, cc * 128:(cc + 1) * 128], cT[:, j, :], start=(j == 0), stop=(j == 7),
                         r=["aw%d" % sl, "cT"], w=["modps"])
        b.tt("dve", modT[:], modps[:], bcl(adabT[:], 2), ALU.add, r=["modps", "adabT"], w=["modT"])
        b.stt(vecT[:, V_SCALE1, :], modT[:, 8:16, 0], 1.0, vecT[:, V_N1G, :], ALU.add, ALU.mult, r=["modT", "n1g"], w=["v_scale1"])
        b.stt(vecT[:, V_SCALE1C, :], modT[:, 8:16, 1], 1.0, vecT[:, V_N1G, :], ALU.add, ALU.mult, r=["modT", "n1g"], w=["v_scale1c"])
        b.stt(vecT[:, V_SCALE2, :], modT[:, 32:40, 0], 1.0, vecT[:, V_N2G, :], ALU.add, ALU.mult, r=["modT", "n2g"], w=["v_scale2"])
        b.cp("dve", vecT[:, V_SHIFT1, :], modT[:, 0:8, 0], r=["modT"], w=["v_shift1"])
        b.cp("dve", vecT[:, V_SHIFT1C, :], modT[:, 0:8, 1], r=["modT"], w=["v_shift1c"])
        b.cp("dve", vecT[:, V_SHIFT2, :], modT[:, 24:32, 0], r=["modT"], w=["v_shift2"])
        b.cp("dve", vecT[:, V_G1, :], modT[:, 16:24, 0], r=["modT"], w=["v_g1"])
        b.cp("dve", vecT[:, V_G2, :], modT[:, 40:48, 0], r=["modT"], w=["v_g2"])
        for i, (slot, key) in enumerate([(V_G1, "v_g1"), (V_G2, "v_g2"), (V_SCALE2, "v_scale2"), (V_SHIFT2, "v_shift2")]):
            b.dma("sp", scr["vec_s"][i].rearrange("(j p) -> p j", p=128), vecT[:, slot, :], r=[key], w=["vec_s%d" % i], slow=True)
        l4 = T("l4", [128, 4, 64])
        b.dma("sp", l4[:], io["lam4"].rearrange("a k -> (a k)").partition_broadcast(128).rearrange("p (a k) -> p a k", a=4), w=["l4"])
        lt = T("lt", [128, 2, 64])
        b.tt("dve", lt[:, 0, :], l4[:, 0, :], l4[:, 1, :], ALU.mult, r=["l4"], w=["lt"])
        b.tt("dve", lt[:, 1, :], l4[:, 2, :], l4[:, 3, :], ALU.mult, r=["l4"], w=["lt"])
        b.red(lam[:, 0:2], lt[:], ALU.add, r=["lt"], w=["lam"])
        b.act(lam[:, 0:2], lam[:, 0:2], AF.Exp, r=["lam"], w=["lam"])
        b.stt(lam[:, 2:3], lam[:, 0:1], LAM_INIT, lam[:, 1:2], ALU.add, ALU.subtract, r=["lam"], w=["lam2"])
        b.ts("dve", lam[:, 3:4], lam[:, 2:3], -1.0, None, ALU.mult, r=["lam2"], w=["lam3"])
        S.flush()


def phase1(nc, S, b, io, scr, G):
    ident_f, ident_b, vecT = G["ident_f"], G["ident_b"], G["vecT"]
    with ExitStack() as st:
        T = lambda name, shape, dt=F32: st.enter_context(nc.sbuf_tensor("s_" + name, list(shape), dt))
        P = lambda name, shape, dt=F32: (S.excl.add(name), st.enter_context(nc.psum_tensor("p_" + name, list(shape), dt)))[1]
        xnT = T("xnT", [128, 8, NKEY], BF16)
        with ExitStack() as st2:
            T2 = lambda name, shape, dt=F32: st2.enter_context(nc.sbuf_tensor("s_" + name, list(shape), dt))
            P2 = lambda name, shape, dt=F32: (S.excl.add(name), st2.enter_context(nc.psum_tensor("p_" + name, list(shape), dt)))[1]
            xt = [T2("xt%d" % i, [128, D]) for i in range(2)]
            xs = [T2("xs%d" % i, [128, D]) for i in range(2)]
            junk = T2("junk", [128, D])
            ss = [T2("ss%d" % i, [128, 4]) for i in range(2)]
            pT = [P2("pT%d" % i, [128, 8, 128]) for i in range(2)]
            tmp = [T2("tmp%d" % i, [128, 8, 128]) for i in range(2)]
            for ti in range(34):
                sl = ti % 2
                src = io["ctx_seq"][ti * 128:(ti + 1) * 128, :] if ti < 2 else io["x_seq"][(ti - 2) * 128:(ti - 1) * 128, :]
                vs, vh = (V_SCALE1C, V_SHIFT1C) if ti < 2 else (V_SCALE1, V_SHIFT1)
                ks, kh = ("v_scale1c", "v_shift1c") if ti < 2 else ("v_scale1", "v_shift1")
                b.dma("sp" if sl == 0 else "act", xt[sl][:], src, w=["xt%d" % sl])
                b.act(junk[:], xt[sl][:], AF.Square, r=["xt%d" % sl], w=["junk", "ss%d" % sl], accum=ss[sl][:, 0:1])
                b.ts("dve", ss[sl][:, 1:2], ss[sl][:, 0:1], 1.0 / D, EPS, ALU.mult, ALU.add, r=["ss%d" % sl], w=["ssb%d" % sl])
                b.act(ss[sl][:, 2:3], ss[sl][:, 1:2], AF.Sqrt, r=["ssb%d" % sl], w=["ssc%d" % sl])
                b.recip(ss[sl][:, 3:4], ss[sl][:, 2:3], r=["ssc%d" % sl], w=["ssd%d" % sl])
                b.act(xs[sl][:], xt[sl][:], AF.Copy, r=["xt%d" % sl, "ssd%d" % sl], w=["xs%d" % sl], scale=ss[sl][:, 3:4])
                for j in range(8):
                    b.tr(pT[sl][:, j, :], xs[sl][:, j * 128:(j + 1) * 128], ident_f[:], r=["xs%d" % sl], w=["pT%d" % sl])
                b.tt("dve", tmp[sl][:], pT[sl][:], bcl(vecT[:, vs, :], 128), ALU.mult, r=["pT%d" % sl, ks], w=["tmp%d" % sl])
                b.tt("pool", xnT[:, :, ti * 128:(ti + 1) * 128], tmp[sl][:], bcl(vecT[:, vh, :], 128), ALU.add, r=["tmp%d" % sl, kh], w=["xnT"])
            b.dma("sp", scr["xnT_s"][:, :, :], xnT[:, :, NCTX:NCTX + NOWN], r=["xnT"], w=["xnT_s"])
            S.flush()
        if "stop1a" in G["dbg"]:
            return
        cosT = T("cosT", [128, NT])
        sinT = T("sinT", [128, NT])
        with ExitStack() as st2:
            T2 = lambda name, shape, dt=F32: st2.enter_context(nc.sbuf_tensor("s_" + name, list(shape), dt))
            ang = T2("ang", [128, NT])
            y = T2("y", [128, NT])
            yi = T2("yi", [128, NT], I32)
            fi = T2("fi", [128, 2])
            b.dma("sp", ang[:], io["posT"][:, :], w=["ang"])
            b.dma("sp", fi[:, 0:1], io["fidx"][:, :], w=["fi"])
            b.act(fi[:, 1:2], fi[:, 0:1], AF.Exp, r=["fi"], w=["inv"], scale=-math.log(10000.0) / 16.0)
            b.ts("dve", ang[:], ang[:], fi[:, 1:2], None, ALU.mult, r=["ang", "inv"], w=["ang"])
            for tab, off, key in ((sinT, 0.5, "sinT"), (cosT, 0.75, "cosT")):
                b.ts("dve", y[:], ang[:], 1.0 / (2 * PI), off, ALU.mult, ALU.add, r=["ang"], w=["y"])
                b.cp("dve", yi[:], y[:], r=["y"], w=["yi"])
                b.cp("dve", tab[:], yi[:], r=["yi"], w=[key])
                b.tt("dve", y[:], y[:], tab[:], ALU.subtract, r=["y", key], w=["y"])
                b.ts("dve", tab[:], y[:], 0.0, None, ALU.is_lt, r=["y"], w=[key])
                b.tt("dve", y[:], y[:], tab[:], ALU.add, r=["y", key], w=["y"])
                b.ts("dve", y[:], y[:], 2 * PI, -PI, ALU.mult, ALU.add, r=["y"], w=["y"])
                b.ts("dve", y[:], y[:], 3.1415925, -3.1415925, ALU.min, ALU.max, r=["y"], w=["y"])
                b.act(tab[:], y[:], AF.Sin, r=["y"], w=[key])
            S.flush()
        if "stop1r" in G["dbg"]:
            return
        prot = T("prot", [128, 128], BF16)
        protf = T("protf", [128, 128])
        b.dma("sp", protf[:], io["prot"][:, :], w=["protf"])
        b.cp("dve", prot[:], protf[:], r=["protf"], w=["prot"])
        bones = T("bones", [128, 2], BF16)
        b.memset("pool", bones[:], 0.0, w=["bones"])
        b.memset("pool", bones[0:64, 0:1], 1.0, w=["bones"])
        b.memset("pool", bones[64:128, 1:2], 1.0, w=["bones"])
        onesrow = T("onesrow", [16, NKEY], BF16)
        b.memset("pool", onesrow[:], 1.0, w=["onesrow"])
        if "no_ones" not in G["dbg"]:
            b.dma("sp", scr["kT_s"][:, :, 64, :].rearrange("h m c -> (h m) c"), onesrow[:], r=["onesrow"], w=["kT_s64"])
        wf = [T("wf%d" % i, [128, 8, 512]) for i in range(2)]
        wb = [T("wb%d" % i, [128, 8, 512], BF16) for i in range(2)]
        pA = [P("pA%d" % i, [128, 512]) for i in range(2)]
        pB = [P("pB%d" % i, [128, 512]) for i in range(2)]
        pN = [P("pN%d" % i, [2, 512]) for i in range(2)]
        asb = [T("asb%d" % i, [128, 512], BF16) for i in range(2)]
        t1 = [T("t1_%d" % i, [128, 512]) for i in range(2)]
        t2 = [T("t2_%d" % i, [128, 512]) for i in range(2)]
        kr = [T("kr%d" % i, [128, 512], BF16) for i in range(3)]
        sq = [T("sq%d" % i, [128, 512], BF16) for i in range(2)]
        kmx = T("kmx", [2, 8, 10])
        negk = T("negk", [2, 8])
        nrow = [T("nrow%d" % i, [2, 512]) for i in range(2)]
        nrowb = [T("nrowb%d" % i, [2, 512], BF16) for i in range(2)]
        vsb = [T("vsb%d" % i, [128, 8, 130], BF16) for i in range(2)]
        usb = [T("usb%d" % i, [128, 512]) for i in range(2)]
        for i in range(2):
            b.memset("pool", vsb[i][:], 0.0, w=["vsb%d" % i])
            b.memset("pool", vsb[i][:, :, 128:129], 1.0, w=["vsb%d" % i])
        b.memset("pool", kmx[:], 0.0, w=["kmx"])
        cnt = {"w": 0, "t": 0, "v": 0, "u": 0, "kr": 0}
        if "stop1s" in G["dbg"]:
            S.flush()
            return

        def load_w(cb):
            sl = cnt["w"] % 2
            cnt["w"] += 1
            b.dma("sp", wf[sl][:], io["w_in"][:, cb * 512:(cb + 1) * 512].rearrange("(j p) n -> p j n", p=128), w=["wf%d" % sl])
            b.cp("dve" if "w_cast_dve" in G["dbg"] else "pool", wb[sl][:], wf[sl][:], r=["wf%d" % sl], w=["wb%d" % sl])
            return sl

        def qk_tile(wsl, ch, col0, ncol, tok0, rope, is_q, h):
            i = cnt["t"] % 2
            cnt["t"] += 1
            for j in range(8):
                b.mm(pA[i][:, :ncol], wb[wsl][:, j, ch * 128:(ch + 1) * 128], xnT[:, j, col0:col0 + ncol], start=(j == 0), stop=(j == 7),
                     r=["wb%d" % wsl, "xnT"], w=["pA%d" % i])
            ki = cnt["kr"] % 3
            cnt["kr"] += 1
            if rope and "no_rope_ops" not in G["dbg"]:
                b.cp("act", asb[i][:, :ncol], pA[i][:, :ncol], r=["pA%d" % i], w=["asb%d" % i])
                b.mm(pB[i][:, :ncol], prot[:], asb[i][:, :ncol], r=["prot", "asb%d" % i], w=["pB%d" % i])
                b.tt("dve", t1[i][:, :ncol], pA[i][:, :ncol], cosT[:, tok0:tok0 + ncol], ALU.mult, r=["pA%d" % i, "cosT"], w=["t1_%d" % i])
                b.tt("dve", t2[i][:, :ncol], pB[i][:, :ncol], sinT[:, tok0:tok0 + ncol], ALU.mult, r=["pB%d" % i, "sinT"], w=["t2_%d" % i])
                b.tt("pool", kr[ki][:, :ncol], t1[i][:, :ncol], t2[i][:, :ncol], ALU.add, r=["t1_%d" % i, "t2_%d" % i], w=["kr%d" % ki])
            else:
                b.cp("act", kr[ki][:, :ncol], pA[i][:, :ncol], r=["pA%d" % i], w=["kr%d" % ki])
            if "no_norm" not in G["dbg"]:
                b.act(sq[i][:, :ncol], kr[ki][:, :ncol], AF.Square, r=["kr%d" % ki], w=["sq%d" % i])
                b.mm(pN[i][:, :ncol], bones[:], sq[i][:, :ncol], r=["bones", "sq%d" % i], w=["pN%d" % i])
            return i, ki

        for cb in (2, 3):
            wsl = load_w(cb)
            for ch in range(4):
                h = (cb - 2) * 4 + ch
                blocks = [(0, NCTX, 0, False)] + [(NCTX + tb * 512, 512, tb * 512, True) for tb in range(8)]
                for bi, (col0, ncol, tok0, rope) in enumerate(blocks):
                    i, ki = qk_tile(wsl, ch, col0, ncol, tok0, rope, False, h)
                    if "no_norm" not in G["dbg"]:
                        b.red(kmx[:, h, bi:bi + 1], pN[i][:, :ncol], ALU.max, r=["pN%d" % i], w=["kmx"])
                    for m in range(2 if "no_kstore" not in G["dbg"] else 0):
                        b.dma("sp" if m == 0 else "act", scr["kT_s"][h, m, 0:64, col0:col0 + ncol], kr[ki][m * 64:(m + 1) * 64, :ncol], r=["kr%d" % ki], w=["kT_s"])
        if "stop1k" in G["dbg"]:
            S.flush()
            return
        b.red(negk[:], kmx[:], ALU.max, r=["kmx"], w=["negk"])
        b.act(negk[:], negk[:], AF.Sqrt, r=["negk"], w=["negk"])
        b.ts("dve", negk[:], negk[:], -1.0, None, ALU.mult, r=["negk"], w=["negk"])
        for cb in (0, 1):
            wsl = load_w(cb)
            for ch in range(4):
                h = cb * 4 + ch
                for tb in range(4):
                    i, ki = qk_tile(wsl, ch, NCTX + tb * 512, 512, tb * 512, True, True, h)
                    b.act(nrow[i][:], pN[i][:], AF.Sqrt, r=["pN%d" % i], w=["nrow%d" % i])
                    b.ts("dve", nrowb[i][:], nrow[i][:], negk[:, h:h + 1], None, ALU.mult, r=["nrow%d" % i, "negk"], w=["nrowb%d" % i])
                    b.dma("sp", scr["qT_s"][h, :, 64, tb * 512:(tb + 1) * 512], nrowb[i][:], r=["nrowb%d" % i], w=["qT_s"])
                    for m in range(2):
                        b.dma("sp" if m == 0 else "act", scr["qT_s"][h, m, 0:64, tb * 512:(tb + 1) * 512], kr[ki][m * 64:(m + 1) * 64, :], r=["kr%d" % ki], w=["qT_s"])
        if "stop1q" in G["dbg"]:
            S.flush()
            return
        wv = [load_w(4), load_w(5)]
        for ti in range(34):
            i = cnt["v"] % 2
            cnt["v"] += 1
            for half in range(2):
                pt = pA[half]
                for j in range(8):
                    b.mm(pt[:], xnT[:, j, ti * 128:(ti + 1) * 128], wb[wv[half]][:, j, :], start=(j == 0), stop=(j == 7),
                         r=["xnT", "wb%d" % wv[half]], w=["pA%d" % half])
                b.cp("act" if half == 0 else "dve", vsb[i][:, half * 4:(half + 1) * 4, 0:128], pt[:].rearrange("p (h e) -> p h e", h=4),
                     r=["pA%d" % half], w=["vsb%d" % i])
            b.dma("sp", scr["v_s"][ti], vsb[i][:], r=["vsb%d" % i], w=["v_s"])
        if "stop1v" in G["dbg"]:
            S.flush()
            return
        wsl = load_w(6)
        blocks = [(0, NCTX)] + [(NCTX + tb * 512, 512) for tb in range(8)]
        for ch in range(4):
            for (col0, ncol) in blocks:
                i = cnt["u"] % 2
                cnt["u"] += 1
                for j in range(8):
                    b.mm(pB[i][:, :ncol], wb[wsl][:, j, ch * 128:(ch + 1) * 128], xnT[:, j, col0:col0 + ncol], start=(j == 0), stop=(j == 7),
                         r=["wb%d" % wsl, "xnT"], w=["pB%d" % i])
                b.cp("act", usb[i][:, :ncol], pB[i][:, :ncol], r=["pB%d" % i], w=["usb%d" % i])
                b.dma("sp", scr["uT_s"][ch, :, col0:col0 + ncol], usb[i][:, :ncol], r=["usb%d" % i], w=["uT_s"])
        S.flush()


def phase2(nc, S, b, io, scr, G):
    ident_b, lam = G["ident_b"], G["lam"]
    with ExitStack() as st:
        T = lambda name, shape, dt=F32: st.enter_context(nc.sbuf_tensor("s_" + name, list(shape), dt))
        P = lambda name, shape, dt=F32: (S.excl.add(name), st.enter_context(nc.psum_tensor("p_" + name, list(shape), dt)))[1]
        kT = [T("kT%d" % i, [65, 2, NKEY], BF16) for i in range(2)]
        qT = [T("qT%d" % i, [65, 2, NOWN], BF16) for i in range(2)]
        vv = [T("vv%d" % i, [128, 34, 130], BF16) for i in range(2)]
        E = [T("E%d" % i, [128, 512], BF16) for i in range(4)]
        pS = [P("pS%d" % i, [128, 512]) for i in range(3)]
        pO = [P("pO%d" % i, [128, 512]) for i in range(4)]
        pTr = P("pTr", [128, 1024], BF16)
        osb = [T("osb%d" % i, [128, 130]) for i in range(4)]
        attnT = T("attnT", [128, 8, NOWN], BF16)
        gsub = T("gsub", [128, 128])
        sm = [T("sm%d" % i, [128, 8]) for i in range(2)]
        ot = [T("ot%d" % i, [128, 128]) for i in range(2)]
        ob = [T("ob%d" % i, [128, 128], BF16) for i in range(2)]
        junk = T("junk2", [128, 128])
        cvf = [T("cvf%d" % i, [128, 4, D]) for i in range(2)]
        cvb = [T("cvb%d" % i, [128, 4, D], BF16) for i in range(2)]
        cvc = [0]

        def convert_chunk():
            c = cvc[0]
            cvc[0] += 1
            if c >= 64:
                return
            i = c % 2
            src = io["peer_u"] if c < 32 else io["peer_v"]
            dst = scr["puv_b"][:, 0 if c < 32 else 1, :]
            r0 = (c % 32) * 512
            b.dma("sp", cvf[i][:], src[r0:r0 + 512, :].rearrange("(p j) d -> p j d", j=4), w=["cvf%d" % i])
            b.cp("pool", cvb[i][:], cvf[i][:], r=["cvf%d" % i], w=["cvb%d" % i])
            b.dma("act", dst[r0:r0 + 512, :].rearrange("(p j) d -> p j d", j=4), cvb[i][:], r=["cvb%d" % i], w=["pub"])

        b.dma("sp", gsub[:], io["subln_g"].partition_broadcast(128), w=["gsub"])
        b.ts("dve", gsub[:], gsub[:], 1.0 - LAM_INIT, None, ALU.mult, r=["gsub"], w=["gsub"])
        ecnt = 0

        def load_head(h):
            sl = h % 2
            b.dma("sp", kT[sl][:], scr["kT_s"][h].rearrange("m r c -> r m c"), w=["kT%d" % sl])
            b.dma("act", qT[sl][:], scr["qT_s"][h].rearrange("m r c -> r m c"), w=["qT%d" % sl])
            b.dma("sp", vv[sl][:], scr["v_s"][:, :, h, :].rearrange("k p e -> p k e"), w=["vv%d" % sl])

        pend_epi = [None]
        epic = [0]

        def epilogue(h, qg):
            for qb in range(2):
                e2 = epic[0] % 2
                epic[0] += 1
                o1, o2 = osb[qb], osb[2 + qb]
                k1, k2 = "osb%d" % qb, "osb%d" % (2 + qb)
                s_ = sm[e2]
                ks = "sm%d" % e2
                b.recip(s_[:, 0:1], o1[:, 128:129], r=[k1], w=[ks + "a"])
                b.recip(s_[:, 1:2], o2[:, 128:129], r=[k2], w=[ks + "b"])
                b.tt("dve", s_[:, 2:3], s_[:, 1:2], lam[:, 3:4], ALU.mult, r=[ks + "b", "lam3"], w=[ks + "c"])
                b.ts("dve", ot[e2][:], o1[:, 0:128], s_[:, 0:1], None, ALU.mult, r=[k1, ks + "a"], w=["ot%d" % e2])
                b.stt(ot[e2][:], o2[:, 0:128], s_[:, 2:3], ot[e2][:], ALU.mult, ALU.add, r=[k2, ks + "c", "ot%d" % e2], w=["ot%d" % e2])
                b.stt(junk[:], ot[e2][:], 1.0, ot[e2][:], ALU.mult, ALU.mult, r=["ot%d" % e2], w=["junk2", ks + "d"], accum=s_[:, 3:4])
                b.ts("dve", s_[:, 4:5], s_[:, 3:4], 1.0 / 128.0, EPS, ALU.mult, ALU.add, r=[ks + "d"], w=[ks + "e"])
                b.act(s_[:, 5:6], s_[:, 4:5], AF.Sqrt, r=[ks + "e"], w=[ks + "f"])
                b.recip(s_[:, 6:7], s_[:, 5:6], r=[ks + "f"], w=[ks + "g"])
                b.stt(ob[e2][:], ot[e2][:], s_[:, 6:7], gsub[:], ALU.mult, ALU.mult, r=["ot%d" % e2, ks + "g", "gsub"], w=["ob%d" % e2])

        def epilogue_b(h, qg):
            for qb in range(2):
                b.tr(pTr[:, qb * 128:(qb + 1) * 128], ob[qb][:], ident_b[:], r=["ob%d" % qb, "ident_b"], w=["pTr"])
            q0 = qg * 256
            b.cp("dve", attnT[:, h, q0:q0 + 256], pTr[:, 0:256], r=["pTr"], w=["attnT"])

        load_head(0)
        for h in range(8):
            sl = h % 2
            if h + 1 < 8:
                load_head(h + 1)
            for qg in range(8):
                eidx = {}
                for step in range(36):
                    kb = step
                    if kb < 34:
                        i = kb % 3
                        ei = ecnt % 4
                        ecnt += 1
                        eidx[kb] = ei
                        for m in range(2):
                            b.mm(pS[i][:, m * 256:(m + 1) * 256], kT[sl][:, m, kb * 128:(kb + 1) * 128], qT[sl][:, m, qg * 256:(qg + 1) * 256],
                                 r=["kT%d" % sl, "qT%d" % sl], w=["pS%d" % i])
                        b.act(E[ei][:], pS[i][:], AF.Exp, r=["pS%d" % i], w=["E%d" % ei], scale=0.125)
                    pk = step - 2
                    if pk >= 0:
                        pei = eidx[pk]
                        for m in range(2):
                            for qb in range(2):
                                a = m * 2 + qb
                                b.mm(pO[a][:, 0:129], E[pei][:, m * 256 + qb * 128: m * 256 + (qb + 1) * 128], vv[sl][:, pk, 0:129],
                                     start=(pk == 0), stop=(pk == 33), r=["E%d" % pei, "vv%d" % sl], w=["pO%d" % a])
                    if step == 10:
                        convert_chunk()
                    if step == 4 and pend_epi[0] is not None:
                        epilogue(*pend_epi[0])
                    if step == 20 and pend_epi[0] is not None:
                        epilogue_b(*pend_epi[0])
                        pend_epi[0] = None
                for a in range(4):
                    b.cp("act" if a % 2 == 0 else "dve", osb[a][:, 0:129], pO[a][:, 0:129], r=["pO%d" % a], w=["osb%d" % a])
                pend_epi[0] = (h, qg)
        epilogue(*pend_epi[0])
        epilogue_b(*pend_epi[0])
        b.dma("sp", scr["attnT_s"][:, :, :], attnT[:], r=["attnT"], w=["attnT_s"])
        S.flush()


def phase3(nc, S, b, io, scr, G):
    ident_f = G["ident_f"]
    dbg = G["dbg"]
    with ExitStack() as st:
        T = lambda name, shape, dt=F32: st.enter_context(nc.sbuf_tensor("s_" + name, list(shape), dt))
        cst = T("cst", [128, 512])
        selc = T("selc", [128, 8, 240])
        b.dma("sp", cst[:], io["cst"][:, :], w=["cst"])
        b.dma("sp", selc[:], io["selc"][:, :, :], w=["selc"])
        selcb = T("selcb", [128, 8, 240], BF16)
        b.cp("dve", selcb[:], selc[:], r=["selc"], w=["selcb"])
        maskf, maskb = cst[:, 0:128], cst[:, 128:256]
        kka, kkd, kk8, kk1 = cst[0:64, 256:272], cst[0:64, 272:288], cst[0:64, 288:304], cst[0:64, 304:305]
        AR = T("AR", [64, 64]); AI = T("AI", [64, 64]); DT = T("DT", [64, 64])
        RHO = T("RHO", [64, 64]); TH = T("TH", [64, 64])
        FR = T("FR", [64, 64]); FI = T("FI", [64, 64])
        FBR = T("FBR", [64, 64, 16]); FBI = T("FBI", [64, 64, 16])
        CNR = T("CNR", [64, 64, 16]); CNI = T("CNI", [64, 64, 16])
        Dsq = T("Dsq", [128, 32])
        ygT = T("ygT", [128, 4, NOWN])
        b.dma("sp", AR[:], io["a_re"].rearrange("d g n -> n (d g)"), w=["AR"], slow=True)
        b.dma("sp", AI[:], io["a_im"].rearrange("d g n -> n (d g)"), w=["AI"], slow=True)
        b.dma("sp", DT[:], io["log_dt"].rearrange("d g -> (d g)").partition_broadcast(64), w=["DT"])
        for s_ in range(8):
            b.dma("sp", Dsq[s_ * 16:(s_ + 1) * 16, :], io["ssm_d"].rearrange("(g q) -> q g", q=16), w=["Dsq"], slow=True)
        b.act(DT[:], DT[:], AF.Exp, r=["DT"], w=["DT"])
        b.tt("dve", RHO[:], AR[:], DT[:], ALU.mult, r=["AR", "DT"], w=["RHO"])
        b.tt("dve", TH[:], AI[:], DT[:], ALU.mult, r=["AI", "DT"], w=["TH"])

        uid = [0]

        def cpow(st_, dre, dim, rho, th, kk, Gn, Kn, tag):
            uid[0] += 1
            u = "%s%d" % (tag, uid[0])
            T_ = lambda name, dt=F32: st_.enter_context(nc.sbuf_tensor("s_%s_%s" % (name, u), [64, Gn, Kn], dt))
            rk = T_("rk"); y = T_("y"); yi = T_("yi", I32); w_ = T_("w"); mg = T_("mg")
            kb_ = kk.unsqueeze(1).to_broadcast([64, Gn, Kn])
            b.tt("dve", rk[:], bcl(rho, Kn), kb_, ALU.mult, r=["RHO", "cst"], w=["rk" + u])
            b.act(mg[:], rk[:], AF.Exp, r=["rk" + u], w=["mg" + u])
            b.tt("dve", rk[:], bcl(th, Kn), kb_, ALU.mult, r=["TH", "cst", "mg" + u], w=["rk" + u])
            for dst, off in ((dim, 0.5), (dre, 0.75)):
                b.ts("dve", y[:], rk[:], 1.0 / (2 * PI), off, ALU.mult, ALU.add, r=["rk" + u], w=["y" + u])
                b.cp("dve", yi[:], y[:], r=["y" + u], w=["yi" + u])
                b.cp("dve", w_[:], yi[:], r=["yi" + u], w=["w" + u])
                b.tt("dve", y[:], y[:], w_[:], ALU.subtract, r=["y" + u, "w" + u], w=["y" + u])
                b.ts("dve", w_[:], y[:], 0.0, None, ALU.is_lt, r=["y" + u], w=["w" + u])
                b.tt("dve", y[:], y[:], w_[:], ALU.add, r=["y" + u, "w" + u], w=["y" + u])
                b.ts("dve", y[:], y[:], 2 * PI, -PI, ALU.mult, ALU.add, r=["y" + u], w=["y" + u])
                b.ts("dve", y[:], y[:], 3.1415925, -3.1415925, ALU.min, ALU.max, r=["y" + u], w=["y" + u])
                b.act(w_[:], y[:], AF.Sin, r=["y" + u], w=["w" + u])
                b.tt("dve", dst, w_[:], mg[:], ALU.mult, r=["w" + u, "mg" + u], w=[tag])

        with ExitStack() as st2:
            T2 = lambda name, shape, dt=F32: st2.enter_context(nc.sbuf_tensor("s_" + name, list(shape), dt))
            P2 = lambda name, shape, dt=F32: (S.excl.add(name), st2.enter_context(nc.psum_tensor("p_" + name, list(shape), dt)))[1]
            ABR = T2("ABR", [64, 64, 1]); ABI = T2("ABI", [64, 64, 1])
            BR = T2("BR", [64, 64, 16]); BI = T2("BI", [64, 64, 16])
            b.dma("sp", BR[:], io["b_re"].rearrange("d g n q -> n (d g) q"), w=["BR"])
            b.dma("act", BI[:], io["b_im"].rearrange("d g n q -> n (d g) q"), w=["BI"])
            cpow(st2, ABR[:], ABI[:], RHO[:], TH[:], kk1, 64, 1, "AB")
            den = T2("den", [64, 64]); t1 = T2("g_t1", [64, 64]); t2 = T2("g_t2", [64, 64]); nr = T2("nr", [64, 64])
            b.tt("dve", den[:], AR[:], AR[:], ALU.mult, r=["AR"], w=["den"])
            b.tt("dve", t1[:], AI[:], AI[:], ALU.mult, r=["AI"], w=["g_t1"])
            b.tt("dve", den[:], den[:], t1[:], ALU.add, r=["den", "g_t1"], w=["den"])
            b.recip(den[:], den[:], r=["den"], w=["den"])
            b.ts("dve", nr[:], ABR[:, :, 0], -1.0, None, ALU.add, r=["AB"], w=["nr"])
            b.tt("dve", t1[:], nr[:], AR[:], ALU.mult, r=["nr", "AR"], w=["g_t1"])
            b.tt("dve", t2[:], ABI[:, :, 0], AI[:], ALU.mult, r=["AB", "AI"], w=["g_t2"])
            b.tt("dve", t1[:], t1[:], t2[:], ALU.add, r=["g_t1", "g_t2"], w=["g_t1"])
            b.tt("dve", FR[:], t1[:], den[:], ALU.mult, r=["g_t1", "den"], w=["FR"])
            b.tt("dve", t1[:], ABI[:, :, 0], AR[:], ALU.mult, r=["AB", "AR", "FR"], w=["g_t1"])
            b.tt("dve", t2[:], nr[:], AI[:], ALU.mult, r=["nr", "AI"], w=["g_t2"])
            b.tt("dve", t1[:], t1[:], t2[:], ALU.subtract, r=["g_t1", "g_t2"], w=["g_t1"])
            b.tt("dve", FI[:], t1[:], den[:], ALU.mult, r=["g_t1", "den"], w=["FI"])
            ta = T2("g_ta", [64, 64, 16]); tb = T2("g_tb", [64, 64, 16])
            b.tt("dve", ta[:], BR[:], bcl(FR[:], 16), ALU.mult, r=["BR", "FR"], w=["g_ta"])
            b.tt("dve", tb[:], BI[:], bcl(FI[:], 16), ALU.mult, r=["BI", "FI"], w=["g_tb"])
            b.tt("dve", FBR[:], ta[:], tb[:], ALU.subtract, r=["g_ta", "g_tb"], w=["FBR"])
            b.tt("dve", ta[:], BI[:], bcl(FR[:], 16), ALU.mult, r=["BI", "FR", "FBR"], w=["g_ta"])
            b.tt("dve", tb[:], BR[:], bcl(FI[:], 16), ALU.mult, r=["BR", "FI", "FBR"], w=["g_tb"])
            b.tt("dve", FBI[:], ta[:], tb[:], ALU.add, r=["g_ta", "g_tb"], w=["FBI"])
            cn = [T2("cn%d" % i, [128, 64]) for i in range(2)]
            pC = [P2("pC%d" % i, [128, 512]) for i in range(2)]
            k_ = 0
            for (src, dstt, key) in ((io["c_re"], CNR, "CNR"), (io["c_im"], CNI, "CNI")):
                for d in range(2):
                    for gb in range(4):
                        i = k_ % 2
                        k_ += 1
                        b.dma("sp" if i == 0 else "act", cn[i][:], src[d, gb * 8:(gb + 1) * 8].rearrange("g p n -> (g p) n"), w=["cn%d" % i])
                        b.tr(pC[i][0:64, 0:128], cn[i][:], ident_f[:], r=["cn%d" % i, "ident_f"], w=["pC%d" % i])
                        b.cp("act", dstt[:, d * 32 + gb * 8: d * 32 + (gb + 1) * 8, :], pC[i][0:64, 0:128].rearrange("n (g p) -> n g p", g=8),
                             r=["pC%d" % i], w=[key])
            S.flush()

        def cmul(e, ore, oim, are, aim, bre, bim, t1, t2, kr, kw, neg_im=False):
            b.tt(e, t1, are, bre, ALU.mult, r=kr, w=[kw + "t1"])
            b.tt(e, t2, aim, bim, ALU.mult, r=kr, w=[kw + "t2"])
            b.tt(e, ore, t1, t2, ALU.subtract, r=[kw + "t1", kw + "t2"], w=[kw + "R"])
            b.tt(e, t1, are, bim, ALU.mult, r=kr + [kw + "R"], w=[kw + "t1"])
            b.tt(e, t2, aim, bre, ALU.mult, r=kr + [kw + "R"], w=[kw + "t2"])
            if neg_im:
                b.S.I(e, lambda: b.e[e].scalar_tensor_tensor(out=oim, in0=t1, scalar=-1.0, in1=t2, op0=ALU.mult, op1=ALU.subtract),
                      r=[kw + "t1", kw + "t2"], w=[kw + "I"]) if e == "dve" else None
            else:
                b.tt(e, oim, t1, t2, ALU.add, r=[kw + "t1", kw + "t2"], w=[kw + "I"])

        for gb in range(4):
            with ExitStack() as stb:
                Tb = lambda name, shape, dt=F32: stb.enter_context(nc.sbuf_tensor("s_%s_b%d" % (name, gb), list(shape), dt))
                EfR = Tb("EfR", [64, 8, 128]); EfI = Tb("EfI", [64, 8, 128]); EbR = Tb("EbR", [64, 8, 128]); EbI = Tb("EbI", [64, 8, 128])
                Mt = Tb("Mt", [128, 8, 128]); Wt = Tb("Wt", [128, 8, 4, 64])
                pwaR = Tb("pwaR", [64, 16, 16]); pwaI = Tb("pwaI", [64, 16, 16])
                p8R = Tb("p8R", [64, 16, 16]); p8I = Tb("p8I", [64, 16, 16])
                gsl = lambda d: slice(d * 32 + gb * 8, d * 32 + (gb + 1) * 8)
                rb = Tb("rb", [64, 16]); tb_ = Tb("tb", [64, 16])
                for d in range(2):
                    b.cp("dve", rb[:, d * 8:(d + 1) * 8], RHO[:, gsl(d)], r=["RHO"], w=["rb"])
                    b.cp("dve", tb_[:, d * 8:(d + 1) * 8], TH[:, gsl(d)], r=["TH"], w=["tb"])
                with ExitStack() as sa:
                    Ta = lambda name, shape, dt=F32: sa.enter_context(nc.sbuf_tensor("s_%s_a%d" % (name, gb), list(shape), dt))
                    Pa = lambda name, shape, dt=F32: (S.excl.add(name), sa.enter_context(nc.psum_tensor("p_%s_a%d" % (name, gb), list(shape), dt)))[1]
                    pwdR = Ta("pwdR", [64, 16, 16]); pwdI = Ta("pwdI", [64, 16, 16])
                    S.lastw["RHO"] = S.lastw.get("rb"); S.lastw["TH"] = S.lastw.get("tb")
                    cpow(sa, pwaR[:], pwaI[:], rb[:], tb_[:], kka, 16, 16, "pwa")
                    cpow(sa, pwdR[:], pwdI[:], rb[:], tb_[:], kkd, 16, 16, "pwd")
                    cpow(sa, p8R[:], p8I[:], rb[:], tb_[:], kk8, 16, 16, "p8")
                    X0R = Ta("X0R", [64, 8, 8, 16]); X0I = Ta("X0I", [64, 8, 8, 16])
                    XpR = Ta("XpR", [64, 8, 8, 16]); XpI = Ta("XpI", [64, 8, 8, 16])
                    X1R = Ta("X1R", [64, 8, 8, 16]); X1I = Ta("X1I", [64, 8, 8, 16])
                    Y0R = Ta("Y0R", [64, 8, 8, 16]); Y0I = Ta("Y0I", [64, 8, 8, 16])
                    Y1R = Ta("Y1R", [64, 8, 8, 16]); Y1I = Ta("Y1I", [64, 8, 8, 16])
                    c1 = Ta("c1", [64, 8, 8, 16]); c2 = Ta("c2", [64, 8, 8, 16])
                    v4 = lambda t: t[:].rearrange("n g (s q) -> n g s q", q=16)

                    def pws(R_, I_, d, a):
                        return bcl(R_[:, d * 8:(d + 1) * 8, a:a + 8], 16), bcl(I_[:, d * 8:(d + 1) * 8, a:a + 8], 16)

                    def fbs(R_, I_, d):
                        return (R_[:, gsl(d), :].unsqueeze(2).to_broadcast([64, 8, 8, 16]), I_[:, gsl(d), :].unsqueeze(2).to_broadcast([64, 8, 8, 16]))

                    jobs = [
                        (X0R[:], X0I[:], pws(pwdR, pwdI, 0, 8), fbs(FBR, FBI, 0), ["pwd", "FBR", "FBI"], "X0", False),
                        (XpR[:], XpI[:], pws(pwdR, pwdI, 0, 1), fbs(FBR, FBI, 0), ["pwd", "FBR", "FBI"], "Xp", False),
                        (X1R[:], X1I[:], pws(pwaR, pwaI, 1, 7), fbs(FBR, FBI, 1), ["pwa", "FBR", "FBI"], "X1", False),
                        (Y0R[:], Y0I[:], pws(pwaR, pwaI, 0, 7), fbs(CNR, CNI, 0), ["pwa", "CNR", "CNI"], "Y0", True),
                        (Y1R[:], Y1I[:], pws(pwdR, pwdI, 1, 8), fbs(CNR, CNI, 1), ["pwd", "CNR", "CNI"], "Y1", True),
                        (v4(EfR), v4(EfI), pws(pwaR, pwaI, 0, 8), fbs(CNR, CNI, 0), ["pwa", "CNR", "CNI"], "Ef", True),
                        (v4(EbR), v4(EbI), pws(pwdR, pwdI, 1, 0), fbs(CNR, CNI, 1), ["pwd", "CNR", "CNI"], "Eb", True),
                    ]
                    for (ore, oim, (are, aim), (bre, bim), kr, kw, neg) in jobs:
                        cmul("dve", ore, oim, are, aim, bre, bim, c1[:], c2[:], kr + ["c1", "c2"], kw, neg_im=neg)
                        S.lastw["c1"] = S.lastw.get(kw + "I"); S.lastw["c2"] = S.lastw.get(kw + "I")
                    pM = [Pa("pM%d" % i, [128, 512]) for i in range(2)]
                    pW = [Pa("pW%d" % i, [128, 512]) for i in range(2)]
                    mt = Ta("mtmp", [128, 128])
                    f2 = lambda t, g: t[:, g, :, :].rearrange("n s q -> n (s q)")
                    for g in range(8):
                        i = g % 2
                        b.mm(pM[i][:, 0:128], f2(X0R, g), f2(Y0R, g), start=True, stop=False, r=["X0R", "Y0R"], w=["pM%d" % i])
                        b.mm(pM[i][:, 0:128], f2(X0I, g), f2(Y0I, g), start=False, stop=True, r=["X0I", "Y0I"], w=["pM%d" % i])
                        b.mm(pM[i][:, 128:256], f2(X1R, g), f2(Y1R, g), start=True, stop=False, r=["X1R", "Y1R"], w=["pM%d" % i])
                        b.mm(pM[i][:, 128:256], f2(X1I, g), f2(Y1I, g), start=False, stop=True, r=["X1I", "Y1I"], w=["pM%d" % i])
                        b.tt("dve", Mt[:, g, :], pM[i][:, 0:128], maskf, ALU.mult, r=["pM%d" % i, "cst"], w=["Mt"])
                        b.tt("dve", mt[:], pM[i][:, 128:256], maskb, ALU.mult, r=["pM%d" % i, "cst"], w=["mtmp"])
                        b.tt("dve", Mt[:, g, :], Mt[:, g, :], mt[:], ALU.add, r=["Mt", "mtmp"], w=["Mt"])
                        b.stt(Mt[:, g, :], ident_f[:], Dsq[:, gb * 8 + g: gb * 8 + g + 1], Mt[:, g, :], ALU.mult, ALU.add, r=["ident_f", "Dsq", "Mt"], w=["Mt"])
                        for k_, (src, key) in enumerate(((XpR, "XpR"), (XpI, "XpI"), (X1R, "X1R"), (X1I, "X1I"))):
                            b.tr(pW[i][:, k_ * 64:(k_ + 1) * 64], f2(src, g), ident_f[0:64, 0:64], r=[key, "ident_f"], w=["pW%d" % i])
                        b.cp("act", Wt[:, g, :, :], pW[i][:, 0:256].rearrange("p (k n) -> p k n", k=4), r=["pW%d" % i], w=["Wt"])
                    S.flush()
                if "stop3a" in dbg:
                    return
                with ExitStack() as sb:
                    Tq = lambda name, shape, dt=F32: sb.enter_context(nc.sbuf_tensor("s_%s_q%d" % (name, gb), list(shape), dt))
                    Pq = lambda name, shape, dt=F32: (S.excl.add(name), sb.enter_context(nc.psum_tensor("p_%s_q%d" % (name, gb), list(shape), dt)))[1]
                    uTc = Tq("uTc", [128, NKEY])
                    U = Tq("U", [128, 8, 544])
                    SfR = Tq("SfR", [64, 8, 288]); SfI = Tq("SfI", [64, 8, 288])
                    SbR = Tq("SbR", [64, 8, 544]); SbI = Tq("SbI", [64, 8, 544])
                    Yg = Tq("Yg", [128, 8, 256], BF16)
                    ygx = [Tq("ygx%d" % i, [128, 256]) for i in range(2)]
                    ygt = [Tq("ygt%d" % i, [128, 256]) for i in range(2)]
                    pU = [Pq("pU%d" % i, [128, 512]) for i in range(2)]
                    pSt = [Pq("pSt%d" % i, [128, 512]) for i in range(2)]
                    pY = [Pq("pY%d" % i, [128, 512]) for i in range(2)]
                    b.dma("sp", uTc[:], scr["uT_s"][gb], w=["uTc"])
                    uTb = Tq("uTb", [128, NKEY], BF16)
                    b.cp("act", uTb[:], uTc[:], r=["uTc"], w=["uTb"])
                    uv = uTb[:].rearrange("p (c s) -> p c s", s=8)
                    n_ = 0
                    for g in range(8):
                        for hh in range(2):
                            i = n_ % 2
                            n_ += 1
                            for s_ in range(8):
                                b.mm(pU[i][:, 0:272], selcb[:, g, (7 - s_) * 16:(7 - s_) * 16 + 128], uv[:, hh * 272:(hh + 1) * 272, s_],
                                     start=(s_ == 0), stop=(s_ == 7), r=["selcb", "uTb"], w=["pU%d" % i])
                            b.cp("act" if i == 0 else "dve", U[:, g, hh * 272:(hh + 1) * 272], pU[i][:, 0:272], r=["pU%d" % i], w=["U"])
                    n_ = 0
                    for g in range(8):
                        for (k_, dst, c0, nn, key) in ((0, SfR, 0, 288, "SfR"), (1, SfI, 0, 288, "SfI"), (2, SbR, 0, 272, "SbR"), (2, SbR, 272, 272, "SbR"),
                                                       (3, SbI, 0, 272, "SbI"), (3, SbI, 272, 272, "SbI")):
                            i = n_ % 2
                            n_ += 1
                            b.mm(pSt[i][0:64, 0:nn], Wt[:, g, k_, :], U[:, g, c0:c0 + nn], r=["Wt", "U"], w=["pSt%d" % i])
                            b.cp("act" if i == 0 else "dve", dst[:, g, c0:c0 + nn], pSt[i][0:64, 0:nn], r=["pSt%d" % i], w=[key])
                    tA = Tq("tA", [64, 8, 34]); tB = Tq("tB", [64, 8, 34])
                    tC = Tq("tC", [64, 8, 34]); tD = Tq("tD", [64, 8, 34])
                    CIfR = Tq("CIfR", [64, 8, 18]); CIfI = Tq("CIfI", [64, 8, 18])
                    CIbR = Tq("CIbR", [64, 8, 34]); CIbI = Tq("CIbI", [64, 8, 34])

                    def cmac(e, dR, dI, cR, cI, xR, xI, ta_, tb2_, keys, tk):
                        b.tt(e, ta_, cR, xR, ALU.mult, r=keys, w=[tk + "a"])
                        b.tt(e, dR, dR, ta_, ALU.add, r=keys + [tk + "a"], w=keys[:1])
                        b.tt(e, ta_, cI, xI, ALU.mult, r=keys, w=[tk + "a"])
                        b.tt(e, dR, dR, ta_, ALU.subtract, r=keys + [tk + "a"], w=keys[:1])
                        b.tt(e, tb2_, cR, xI, ALU.mult, r=keys, w=[tk + "b"])
                        b.tt(e, dI, dI, tb2_, ALU.add, r=keys + [tk + "b"], w=keys[1:2])
                        b.tt(e, tb2_, cI, xR, ALU.mult, r=keys, w=[tk + "b"])
                        b.tt(e, dI, dI, tb2_, ALU.add, r=keys + [tk + "b"], w=keys[1:2])

                    def views(SR, SI, nb):
                        VR = SR[:, :, 0:nb * 16].rearrange("n g (b j) -> n g b j", j=16)
                        VI = SI[:, :, 0:nb * 16].rearrange("n g (b j) -> n g b j", j=16)
                        return VR, VI

                    def scan1(e, SR, SI, kR, kI, nb, asc, d, ta_, tb2_, tk):
                        VR, VI = views(SR, SI, nb)
                        a8R = bcl(pwaR[:, d * 8:(d + 1) * 8, 15], nb); a8I = bcl(pwaI[:, d * 8:(d + 1) * 8, 15], nb)
                        keys = [kR, kI, "pwa", "p8"]
                        for j in (range(1, 16) if asc else range(14, -1, -1)):
                            pj = j - 1 if asc else j + 1
                            cmac(e, VR[:, :, :, j], VI[:, :, :, j], a8R, a8I, VR[:, :, :, pj], VI[:, :, :, pj], ta_[:, :, 0:nb], tb2_[:, :, 0:nb], keys, tk)

                    def scan2(e, SR, SI, kR, kI, nb, asc, d, CIR, CII, ta_, tb2_, tk, order, cik):
                        VR, VI = views(SR, SI, nb)
                        last = 15 if asc else 0
                        a128R = p8R[:, d * 8:(d + 1) * 8, 15]; a128I = p8I[:, d * 8:(d + 1) * 8, 15]
                        keys = [kR, kI, "pwa", "p8", cik]
                        b.memset(e, CIR[:], 0.0, w=[cik])
                        b.memset(e, CII[:], 0.0, w=[cik])
                        for q_ in range(len(order) - 1):
                            cur, nxt = order[q_], order[q_ + 1]
                            b.tt(e, ta_[:, :, 0], a128R, CIR[:, :, cur], ALU.mult, r=keys, w=[tk + "a"])
                            b.tt(e, ta_[:, :, 1], a128I, CII[:, :, cur], ALU.mult, r=keys, w=[tk + "a"])
                            b.tt(e, tb2_[:, :, 0], a128R, CII[:, :, cur], ALU.mult, r=keys, w=[tk + "b"])
                            b.tt(e, tb2_[:, :, 1], a128I, CIR[:, :, cur], ALU.mult, r=keys, w=[tk + "b"])
                            b.tt(e, CIR[:, :, nxt], ta_[:, :, 0], ta_[:, :, 1], ALU.subtract, r=[tk + "a"] + keys, w=[cik])
                            b.tt(e, CII[:, :, nxt], tb2_[:, :, 0], tb2_[:, :, 1], ALU.add, r=[tk + "b"] + keys, w=[cik])
                            b.tt(e, CIR[:, :, nxt], CIR[:, :, nxt], VR[:, :, cur, last], ALU.add, r=keys, w=[cik])
                            b.tt(e, CII[:, :, nxt], CII[:, :, nxt], VI[:, :, cur, last], ALU.add, r=keys, w=[cik])

                    def scan3(e, SR, SI, kR, kI, nb, asc, d, CIR, CII, ta_, tb2_, tk, cik):
                        VR, VI = views(SR, SI, nb)
                        keys = [kR, kI, "pwa", "p8", cik]
                        for j in range(16):
                            pj = j if asc else 15 - j
                            cR = bcl(p8R[:, d * 8:(d + 1) * 8, pj], nb); cI = bcl(p8I[:, d * 8:(d + 1) * 8, pj], nb)
                            cmac(e, VR[:, :, :, j], VI[:, :, :, j], cR, cI, CIR[:, :, 0:nb], CII[:, :, 0:nb], ta_[:, :, 0:nb], tb2_[:, :, 0:nb], keys, tk)

                    border = [1, 0] + list(range(33, 1, -1))
                    scan1("dve", SfR, SfI, "SfR", "SfI", 18, True, 0, tA, tB, "sf")
                    scan1("pool", SbR, SbI, "SbR", "SbI", 34, False, 1, tC, tD, "sb")
                    scan2("dve", SfR, SfI, "SfR", "SfI", 18, True, 0, CIfR, CIfI, tA, tB, "sf", list(range(18)), "sfCI")
                    scan3("dve", SfR, SfI, "SfR", "SfI", 18, True, 0, CIfR, CIfI, tA, tB, "sf", "sfCI")
                    scan2("pool", SbR, SbI, "SbR", "SbI", 34, False, 1, CIbR, CIbI, tC, tD, "sb", border, "sbCI")
                    scan3("pool", SbR, SbI, "SbR", "SbI", 34, False, 1, CIbR, CIbI, tC, tD, "sb", "sbCI")
                    for g in range(8):
                        i = g % 2
                        b.mm(pY[i][:, 0:256], Mt[:, g, :], U[:, g, 32:288], start=True, stop=False, r=["Mt", "U"], w=["pY%d" % i])
                        b.mm(pY[i][:, 0:256], EfR[:, g, :], SfR[:, g, 31:287], start=False, stop=False, r=["Ef", "EfR", "SfR"], w=["pY%d" % i])
                        b.mm(pY[i][:, 0:256], EfI[:, g, :], SfI[:, g, 31:287], start=False, stop=False, r=["Ef", "EfI", "SfI"], w=["pY%d" % i])
                        b.mm(pY[i][:, 0:256], EbR[:, g, :], SbR[:, g, 33:289], start=False, stop=False, r=["Eb", "EbR", "SbR"], w=["pY%d" % i])
                        b.mm(pY[i][:, 0:256], EbI[:, g, :], SbI[:, g, 33:289], start=False, stop=True, r=["Eb", "EbI", "SbI"], w=["pY%d" % i])
                        if "s5_nogelu" in dbg:
                            b.cp("act", Yg[:, g, :], pY[i][:, 0:256], r=["pY%d" % i], w=["Yg"])
                        else:
                            b.cp("act", ygx[i][:], pY[i][:, 0:256], r=["pY%d" % i], w=["ygx%d" % i])
                            gelu_tanh(b, Yg[:, g, :], ygx[i][:], ygt[i][:], "ygx%d" % i, "ygt%d" % i, "Yg")
                    yv = ygT[:, gb, :].rearrange("p (c s) -> p c s", s=8)
                    for t8 in range(8):
                        i = t8 % 2
                        for g in range(8):
                            b.mm(pU[i][:, 0:256], selcb[:, t8, (7 - g) * 16:(7 - g) * 16 + 128], Yg[:, g, :], start=(g == 0), stop=(g == 7),
                                 r=["selcb", "Yg"], w=["pU%d" % i])
                        b.cp("act" if i == 0 else "dve", yv[:, :, t8], pU[i][:, 0:256], r=["pU%d" % i], w=["ygT"])
                    S.flush()
        if "ygT_d" in dbg:
            b.dma("sp", G["ygT_d"], ygT[:], r=["ygT"], w=["ygT_d"])
            S.flush()
            return
        with ExitStack() as sg:
            Tg = lambda name, shape, dt=F32: sg.enter_context(nc.sbuf_tensor("s_" + name, list(shape), dt))
            Pg = lambda name, shape, dt=F32: (S.excl.add(name), sg.enter_context(nc.psum_tensor("p_" + name, list(shape), dt)))[1]
            ygb = Tg("ygb", [128, 4, NOWN], BF16)
            wgf = Tg("wgf", [128, 4, 512]); wgb = Tg("wgb", [128, 4, 512], BF16)
            bgl = Tg("bgl", [128, 4])
            sig = [Tg("sig%d" % i, [128, 512]) for i in range(2)]
            ssmT = Tg("ssmT", [128, 4, NOWN], BF16)
            pZ = [Pg("pZ%d" % i, [128, 512]) for i in range(2)]
            b.dma("sp", wgf[:], io["w_glu"].rearrange("(j p) n -> p j n", p=128), w=["wgf"])
            b.dma("sp", bgl[:], io["b_glu"].rearrange("(c p) -> p c", p=128), w=["bgl"], slow=True)
            b.cp("pool", wgb[:], wgf[:], r=["wgf"], w=["wgb"])
            b.cp("dve", ygb[:], ygT[:], r=["ygT"], w=["ygb"])
            n_ = 0
            for oc in range(4):
                for tb2 in range(4):
                    i = n_ % 2
                    n_ += 1
                    for kc in range(4):
                        b.mm(pZ[i][:], wgb[:, kc, oc * 128:(oc + 1) * 128], ygb[:, kc, tb2 * 512:(tb2 + 1) * 512], start=(kc == 0), stop=(kc == 3),
                             r=["wgb", "ygb"], w=["pZ%d" % i])
                    b.act(sig[i][:], pZ[i][:], AF.Sigmoid, r=["pZ%d" % i, "bgl"], w=["sig%d" % i], bias=bgl[:, oc:oc + 1])
                    b.tt("dve", ssmT[:, oc, tb2 * 512:(tb2 + 1) * 512], ygT[:, oc, tb2 * 512:(tb2 + 1) * 512], sig[i][:], ALU.mult,
                         r=["ygT", "sig%d" % i], w=["ssmT"])
            b.dma("sp", scr["ssmT_s"][:, :, :], ssmT[:], r=["ssmT"], w=["ssmT_s"])
            S.flush()


def phase4(nc, S, b, io, scr, G):
    ident_b = G["ident_b"]
    with ExitStack() as st:
        T = lambda name, shape, dt=F32: st.enter_context(nc.sbuf_tensor("s_" + name, list(shape), dt))
        P = lambda name, shape, dt=F32: (S.excl.add(name), st.enter_context(nc.psum_tensor("p_" + name, list(shape), dt)))[1]
        wa = T("wa", [128, 8, D], BF16); ws = T("ws", [128, 4, D], BF16); wo = T("wo", [128, 8, D], BF16); wg = T("wg", [128, 8, 2048], BF16)
        stg = [T("stg%d" % i, [128, 8, 512]) for i in range(2)]
        g1row = T("g1row", [128, D])
        b.dma("sp", g1row[:], scr["vec_s"][0].partition_broadcast(128), w=["g1row"])
        n_ = 0
        for (src, dst, nj, ncol, key) in ((io["w_attn_up"], wa, 8, D, "wa"), (io["w_ssm_up"], ws, 4, D, "ws"), (io["w_out"], wo, 8, D, "wo"),
                                          (io["w_in"][:, 3584:5632], wg, 8, 2048, "wg")):
            for cb in range(ncol // 512):
                i = n_ % 2
                n_ += 1
                b.dma("sp" if i == 0 else "act", stg[i][:, 0:nj, :], src[:, cb * 512:(cb + 1) * 512].rearrange("(j p) n -> p j n", p=128), w=["stg%d" % i])
                b.cp("pool" if i == 0 else "dve", dst[:, :, cb * 512:(cb + 1) * 512], stg[i][:, 0:nj, :], r=["stg%d" % i], w=[key])
        xs_t = [T("xs_t%d" % i, [128, 8, 128], BF16) for i in range(2)]
        at_t = [T("at_t%d" % i, [128, 8, 128], BF16) for i in range(2)]
        ss_t = [T("ss_t%d" % i, [128, 4, 128], BF16) for i in range(2)]
        x_t = [T("x_t%d" % i, [128, D]) for i in range(2)]
        gs = T("gs", [128, 2048])
        m1 = T("m1", [128, D]); m2 = T("m2", [128, D]); mb = T("mb", [128, D], BF16)
        mT = T("mT", [128, 8, 128], BF16)
        x1 = [T("x1_%d" % i, [128, D]) for i in range(2)]
        pG = [P("pG%d" % i, [128, 512]) for i in range(2)]
        pA = [P("pA4_%d" % i, [128, 512]) for i in range(2)]
        pS = [P("pS4_%d" % i, [128, 512]) for i in range(2)]
        pTr = P("pTr4", [128, 1024], BF16)
        for ti in range(16):
            i = ti % 2
            cs = slice(ti * 128, (ti + 1) * 128)
            b.dma("sp", xs_t[i][:], scr["xnT_s"][:, :, cs], w=["xs_t%d" % i])
            b.dma("act", at_t[i][:], scr["attnT_s"][:, :, cs], w=["at_t%d" % i])
            b.dma("sp", ss_t[i][:], scr["ssmT_s"][:, :, cs], w=["ss_t%d" % i])
            b.dma("act", x_t[i][:], io["x_seq"][cs, :], w=["x_t%d" % i])
            for blk in range(4):
                pg = pG[blk % 2]
                kg = "pG%d" % (blk % 2)
                for j in range(8):
                    b.mm(pg[:], xs_t[i][:, j, :], wg[:, j, blk * 512:(blk + 1) * 512], start=(j == 0), stop=(j == 7), r=["xs_t%d" % i, "wg"], w=[kg])
                b.act(gs[:, blk * 512:(blk + 1) * 512], pg[:], AF.Sigmoid, r=[kg], w=["gs"])
            for hf in range(2):
                for j in range(8):
                    b.mm(pA[hf][:], at_t[i][:, j, :], wa[:, j, hf * 512:(hf + 1) * 512], start=(j == 0), stop=(j == 7), r=["at_t%d" % i, "wa"], w=["pA4_%d" % hf])
                for j in range(4):
                    b.mm(pS[hf][:], ss_t[i][:, j, :], ws[:, j, hf * 512:(hf + 1) * 512], start=(j == 0), stop=(j == 3), r=["ss_t%d" % i, "ws"], w=["pS4_%d" % hf])
                b.tt("dve", m1[:, hf * 512:(hf + 1) * 512], pA[hf][:], gs[:, hf * 512:(hf + 1) * 512], ALU.mult, r=["pA4_%d" % hf, "gs"], w=["m1"])
                b.tt("dve", m2[:, hf * 512:(hf + 1) * 512], pS[hf][:], gs[:, 1024 + hf * 512:1024 + (hf + 1) * 512], ALU.mult, r=["pS4_%d" % hf, "gs"], w=["m2"])
            b.tt("pool", mb[:], m1[:], m2[:], ALU.add, r=["m1", "m2"], w=["mb"])
            for j in range(8):
                b.tr(pTr[:, j * 128:(j + 1) * 128], mb[:, j * 128:(j + 1) * 128], ident_b[:], r=["mb", "ident_b"], w=["pTr4"])
            b.cp("act", mT[:], pTr[:].rearrange("p (j t) -> p j t", j=8), r=["pTr4"], w=["mT"])
            for hf in range(2):
                for j in range(8):
                    b.mm(pA[hf][:], mT[:, j, :], wo[:, j, hf * 512:(hf + 1) * 512], start=(j == 0), stop=(j == 7), r=["mT", "wo"], w=["pA4_%d" % hf])
                b.tt("dve", m1[:, hf * 512:(hf + 1) * 512], pA[hf][:], g1row[:, hf * 512:(hf + 1) * 512], ALU.mult, r=["pA4_%d" % hf, "g1row"], w=["m1"])
            b.tt("pool", x1[i][:], m1[:], x_t[i][:], ALU.add, r=["m1", "x_t%d" % i], w=["x1_%d" % i])
            b.dma("sp", scr["x1_s"][cs, :], x1[i][:], r=["x1_%d" % i], w=["x1_s"])
        S.flush()


def phase5(nc, S, b, io, scr, G, out):
    ident_f = G["ident_f"]
    dbg = G["dbg"]
    with ExitStack() as st:
        T = lambda name, shape, dt=F32: st.enter_context(nc.sbuf_tensor("s_" + name, list(shape), dt))
        P = lambda name, shape, dt=F32: (S.excl.add(name), st.enter_context(nc.psum_tensor("p_" + name, list(shape), dt)))[1]
        wq = T("wq", [128, 8, D])
        rows = T("rows5", [128, 4, D])
        k1T = T("k1T", [64, 128]); k2T = T("k2T", [128, 128])
        kk1 = T("kk1", [128, 64]); kk2 = T("kk2", [128, 128])
        iota16 = T("iota16", [128, 16])
        pX = P("pX", [128, 1024])
        pTQ = P("pTQ", [128, 1024])
        pSc = P("pSc", [128, 2048])
        b.dma("sp", wq[:], io["w_query"].rearrange("(j p) n -> p j n", p=128), w=["wq"])
        b.dma("act", rows[:, 0, :], scr["vec_s"][1].partition_broadcast(128), w=["rows5"])
        b.dma("act", rows[:, 1, :], scr["vec_s"][2].partition_broadcast(128), w=["rows5"])
        b.dma("act", rows[:, 2, :], scr["vec_s"][3].partition_broadcast(128), w=["rows5"])
        b.dma("act", rows[:, 3, :], io["final_g"].partition_broadcast(128), w=["rows5"])
        b.dma("sp", kk1[:], io["sub_k1"][:, :], w=["kk1"])
        b.memset("pool", kk2[:], 0.0, w=["kk2"])
        b.dma("sp", kk2[:, 64:128], io["sub_k2"][:, :], r=["kk2"], w=["kk2"])
        b.dma("sp", iota16[:], io["cst"][:, 320:336], w=["iota16"])
        b.tr(pTQ[0:64, 0:128], kk1[:], ident_f[:], r=["kk1", "ident_f"], w=["pTQ"])
        b.cp("act", k1T[:], pTQ[0:64, 0:128], r=["pTQ"], w=["k1T"])
        b.tr(pTQ[:, 128:256], kk2[:], ident_f[:], r=["kk2", "ident_f"], w=["pTQ"])
        b.cp("act", k2T[:], pTQ[:, 128:256], r=["pTQ"], w=["k2T"])
        x1 = [T("x1t%d" % i, [128, D]) for i in range(2)]
        xn2 = T("xn2", [128, D]); tmpf = T("tmpf", [128, D]); junk = T("junk5", [128, D])
        st5 = T("st5", [128, 8])
        xn2T = T("xn2T", [128, 8, 128]); qTs = T("qTs", [128, 8, 128])
        sc = T("sc", [128, 2, 8, 128]); wk = T("wk", [128, 256])
        v12 = T("v12", [128, 2, 8, 16]); i12 = T("i12", [128, 2, 8, 16], U32); i12f = T("i12f", [128, 2, 8, 16])
        cand = T("cand", [128, 8, 256]); tv = T("tv", [128, 8, 16]); tj = T("tj", [128, 8, 16], U32)
        ta = T("ta5", [128, 8, 16], I32); taf = T("taf", [128, 8, 16]); tbf = T("tbf", [128, 8, 16])
        eq = T("eq", [128, 8, 16, 16]); sel1 = T("sel1", [128, 8, 16]); sel2 = T("sel2", [128, 8, 16])
        idxf = T("idxf", [128, 128]); idx32 = T("idx32", [128, 128], I32)
        ge = T("ge", [128, 8, 16]); gsum = T("gsum", [128, 8]); gate = T("gate", [128, 128])
        actv = T("actv", [128, 128]); wv = T("wv", [128, 128])
        NS = 16
        uvb = [T("uvb%d" % i, [128, 2 * D], BF16) for i in range(NS)]
        vt = [T("vt%d" % i, [128, D], BF16) for i in range(3)]
        ident_b = G["ident_b"]
        osb = [T("osb5_%d" % i, [128, D]) for i in range(2)]
        acc = pSc[:, 0:1024]
        nu = 0
        nv = 0
        for ti in range(16):
            i = ti % 2
            cs = slice(ti * 128, (ti + 1) * 128)
            kx = "x1t%d" % i
            b.dma("sp", x1[i][:], scr["x1_s"][cs, :], w=[kx])
            b.act(junk[:], x1[i][:], AF.Square, r=[kx], w=["junk5", "st5a"], accum=st5[:, 0:1])
            b.ts("dve", st5[:, 1:2], st5[:, 0:1], 1.0 / D, EPS, ALU.mult, ALU.add, r=["st5a"], w=["st5b"])
            b.act(st5[:, 2:3], st5[:, 1:2], AF.Sqrt, r=["st5b"], w=["st5c"])
            b.recip(st5[:, 3:4], st5[:, 2:3], r=["st5c"], w=["st5d"])
            b.stt(tmpf[:], x1[i][:], st5[:, 3:4], rows[:, 1, :], ALU.mult, ALU.mult, r=[kx, "st5d", "rows5"], w=["tmpf"])
            b.tt("dve", xn2[:], tmpf[:], rows[:, 2, :], ALU.add, r=["tmpf", "rows5"], w=["xn2"])
            b.cp("act", pX[:], xn2[:], r=["xn2"], w=["pX"])
            for j in range(8):
                b.tr(pTQ[:, j * 128:(j + 1) * 128], xn2[:, j * 128:(j + 1) * 128], ident_f[:], r=["xn2", "ident_f"], w=["pTQ"])
            b.cp("act", xn2T[:], pTQ[:].rearrange("p (j t) -> p j t", j=8), r=["pTQ"], w=["xn2T"])
            for h in range(8):
                for j in range(8):
                    b.mm(pTQ[:, h * 128:(h + 1) * 128], wq[:, j, h * 128:(h + 1) * 128], xn2T[:, j, :], start=(j == 0), stop=(j == 7), r=["wq", "xn2T"], w=["pTQ"])
            b.cp("act", qTs[:], pTQ[:].rearrange("p (h t) -> p h t", h=8), r=["pTQ"], w=["qTs"])
            for h in range(8):
                b.mm(pSc[:, h * 128:(h + 1) * 128], qTs[0:64, h, :], k1T[:, :], r=["qTs", "k1T"], w=["pSc"])
            for h in range(8):
                b.mm(pSc[:, 1024 + h * 128:1024 + (h + 1) * 128], qTs[64:128, h, :], k2T[64:128, :], r=["qTs", "k2T"], w=["pSc"])
            b.cp("dve", sc[:, 0].rearrange("p h k -> p (h k)"), pSc[:, 0:1024], r=["pSc"], w=["sc"])
            b.cp("dve", sc[:, 1].rearrange("p h k -> p (h k)"), pSc[:, 1024:2048], r=["pSc"], w=["sc"])
            for h in range(8):
                for sd in range(2):
                    src = sc[:, sd, h, :]
                    b.S.I("dve", (lambda o=v12[:, sd, h, 0:8], s_=src: nc.vector.max(out=o, in_=s_)), r=["sc"], w=["v12"])
                    b.S.I("dve", (lambda o=i12[:, sd, h, 0:8], m=v12[:, sd, h, 0:8], s_=src: nc.vector.max_index(out=o, in_max=m, in_values=s_)), r=["sc", "v12"], w=["i12"])
                    b.S.I("dve", (lambda o=wk[:, 0:128], m=v12[:, sd, h, 0:8], s_=src: nc.vector.match_replace(out=o, in_to_replace=m, in_values=s_, imm_value=-1e30)), r=["sc", "v12"], w=["wk"])
                    b.S.I("dve", (lambda o=v12[:, sd, h, 8:16], s_=wk[:, 0:128]: nc.vector.max(out=o, in_=s_)), r=["wk"], w=["v12"])
                    b.S.I("dve", (lambda o=i12[:, sd, h, 8:16], m=v12[:, sd, h, 8:16], s_=wk[:, 0:128]: nc.vector.max_index(out=o, in_max=m, in_values=s_)), r=["wk", "v12"], w=["i12"])
            b.tt("dve", cand[:].rearrange("p h (a c) -> p h a c", a=16), bcl(v12[:, 0, :, :], 16), v12[:, 1, :, :].unsqueeze(2).to_broadcast([128, 8, 16, 16]),
                 ALU.add, r=["v12"], w=["cand"])
            for h in range(8):
                src = cand[:, h, :]
                b.S.I("dve", (lambda o=tv[:, h, 0:8], s_=src: nc.vector.max(out=o, in_=s_)), r=["cand"], w=["tv"])
                b.S.I("dve", (lambda o=tj[:, h, 0:8], m=tv[:, h, 0:8], s_=src: nc.vector.max_index(out=o, in_max=m, in_values=s_)), r=["cand", "tv"], w=["tj"])
                b.S.I("dve", (lambda o=wk[:, 0:256], m=tv[:, h, 0:8], s_=src: nc.vector.match_replace(out=o, in_to_replace=m, in_values=s_, imm_value=-1e30)), r=["cand", "tv"], w=["wk"])
                b.S.I("dve", (lambda o=tv[:, h, 8:16], s_=wk[:, 0:256]: nc.vector.max(out=o, in_=s_)), r=["wk"], w=["tv"])
                b.S.I("dve", (lambda o=tj[:, h, 8:16], m=tv[:, h, 8:16], s_=wk[:, 0:256]: nc.vector.max_index(out=o, in_max=m, in_values=s_)), r=["wk", "tv"], w=["tj"])
            b.cp("dve", tbf[:], tj[:], r=["tj"], w=["tbf"])
            b.ts("dve", taf[:], tbf[:], 1.0 / 16.0, None, ALU.mult, r=["tbf"], w=["taf"])
            b.cp("dve", ta[:], taf[:], r=["taf"], w=["ta5"])
            b.cp("dve", sel1[:], ta[:], r=["ta5"], w=["sel1"])
            b.tt("dve", sel2[:], taf[:], sel1[:], ALU.subtract, r=["taf", "sel1"], w=["sel2"])
            b.ts("dve", sel2[:], sel2[:], 0.0, None, ALU.is_lt, r=["sel2"], w=["sel2"])
            b.tt("dve", taf[:], sel1[:], sel2[:], ALU.subtract, r=["sel1", "sel2"], w=["taf"])
            b.stt(tbf[:], taf[:], -16.0, tbf[:], ALU.mult, ALU.add, r=["taf", "tbf"], w=["tbf"])
            b.cp("dve", i12f[:], i12[:], r=["i12"], w=["i12f"])
            io16 = iota16[:].unsqueeze(1).unsqueeze(1).to_broadcast([128, 8, 16, 16])
            for (pos, side, dst, key) in ((taf, 0, sel1, "sel1"), (tbf, 1, sel2, "sel2")):
                b.tt("dve", eq[:], bcl(pos[:], 16), io16, ALU.is_equal, r=["taf", "tbf", "iota16"], w=["eq"])
                b.tt("dve", eq[:], eq[:], i12f[:, side, :, :].unsqueeze(2).to_broadcast([128, 8, 16, 16]), ALU.mult, r=["eq", "i12f"], w=["eq"])
                b.red(dst[:], eq[:], ALU.add, r=["eq"], w=[key])
            b.stt(idxf[:].rearrange("p (h k) -> p h k", h=8), sel1[:], 128.0, sel2[:], ALU.mult, ALU.add, r=["sel1", "sel2"], w=["idxf"])
            b.cp("dve", idx32[:], idxf[:], r=["idxf"], w=["idx32"])
            b.tt("dve", ge[:], tv[:], bcl(tv[:, :, 0], 16), ALU.subtract, r=["tv"], w=["ge"])
            b.act(ge[:], ge[:], AF.Exp, r=["ge"], w=["ge"])
            b.red(gsum[:], ge[:], ALU.add, r=["ge"], w=["gsum"])
            b.recip(gsum[:], gsum[:], r=["gsum"], w=["gsum"])
            b.tt("dve", gate[:].rearrange("p (h k) -> p h k", h=8), ge[:], bcl(gsum[:], 16), ALU.mult, r=["ge", "gsum"], w=["gate"])
            if "stop5a" in dbg:
                b.dma("sp", G["idx_d"], idx32[:], r=["idx32"], w=["idx_d"])
                b.dma("sp", G["gate_d"], gate[:], r=["gate"], w=["gate_d"])
                b.dma("sp", G["sc_d"], sc[:].rearrange("p s h k -> p (s h k)"), r=["sc"], w=["sc_d"])
                S.flush()
                return
            for g8 in range(16):
                sls = []
                for k in range(8):
                    hk = g8 * 8 + k
                    sl = nu % NS
                    nu += 1
                    sls.append(sl)
                    S.D("pool", (lambda o=uvb[sl][:], ix=idx32[:, hk:hk + 1]: nc.gpsimd.indirect_dma_start(
                        out=o, out_offset=None, in_=scr["puv_b"].rearrange("e a d -> e (a d)"), in_offset=bass.IndirectOffsetOnAxis(ap=ix, axis=0))),
                        r=["idx32"], w=["uvb%d" % sl])
                    b.stt(junk[:], uvb[sl][:, 0:D], 1.0, pX[:], ALU.mult, ALU.mult, r=["uvb%d" % sl, "pX"], w=["junk5", "actv"], accum=actv[:, hk:hk + 1])
                cs8 = slice(g8 * 8, (g8 + 1) * 8)
                gelu_tanh(b, wv[:, cs8], actv[:, cs8], idxf[:, cs8], "actv", "idxf", "wv")
                b.tt("dve", wv[:, cs8], wv[:, cs8], gate[:, cs8], ALU.mult, r=["wv", "gate"], w=["wv"])
                for k in range(8):
                    hk = g8 * 8 + k
                    sl = sls[k]
                    s3 = nv % 3
                    nv += 1
                    b.act(vt[s3][:], uvb[sl][:, D:2 * D], AF.Copy, r=["uvb%d" % sl, "wv"], w=["vt%d" % s3], scale=wv[:, hk:hk + 1])
                    for hf in range(2):
                        b.mm(acc[:, hf * 512:(hf + 1) * 512], ident_b[:], vt[s3][:, hf * 512:(hf + 1) * 512], start=(hk == 0), stop=(hk == 127),
                             r=["ident_b", "vt%d" % s3], w=["pSc"])
            b.tt("dve", tmpf[:], acc, rows[:, 0, :], ALU.mult, r=["pSc", "rows5"], w=["tmpf"])
            b.tt("dve", tmpf[:], tmpf[:], x1[i][:], ALU.add, r=["tmpf", kx], w=["tmpf"])
            b.act(junk[:], tmpf[:], AF.Square, r=["tmpf"], w=["junk5", "st5e"], accum=st5[:, 4:5])
            b.ts("dve", st5[:, 5:6], st5[:, 4:5], 1.0 / D, EPS, ALU.mult, ALU.add, r=["st5e"], w=["st5f"])
            b.act(st5[:, 6:7], st5[:, 5:6], AF.Sqrt, r=["st5f"], w=["st5g"])
            b.recip(st5[:, 7:8], st5[:, 6:7], r=["st5g"], w=["st5h"])
            b.stt(osb[i][:], tmpf[:], st5[:, 7:8], rows[:, 3, :], ALU.mult, ALU.mult, r=["tmpf", "st5h", "rows5"], w=["osb5_%d" % i])
            b.dma("sp", out[cs, :], osb[i][:], r=["osb5_%d" % i], w=["out"])
        S.flush()


def host_constants(half):
    t = np.arange(NT)
    if half == 1:
        t = t[::-1]
    posr = (t // 64).astype(np.float32)
    posc = (t % 64).astype(np.float32)
    p = np.arange(128)
    posT = np.where(((p % 64) < 32)[:, None], posr[None, :], posc[None, :]).astype(np.float32)
    fidx = (p % 16).astype(np.float32)[:, None]
    prot = np.zeros((128, 128), np.float32)
    for m in range(128):
        if (m % 32) < 16:
            prot[m + 16, m] = -1.0
        else:
            prot[m - 16, m] = 1.0
    selc = np.zeros((128, 8, 240), np.float32)
    for a in range(8):
        for q in range(16):
            selc[a * 16 + q, a, 112 + q] = 1.0
    cst = np.zeros((128, 512), np.float32)
    sidx = np.arange(128) // 16
    cst[:, 0:128] = (sidx[:, None] <= sidx[None, :]).astype(np.float32)
    cst[:, 128:256] = (sidx[:, None] >= sidx[None, :]).astype(np.float32)
    cst[:, 256:272] = np.arange(-7, 9, dtype=np.float32)[None, :]
    cst[:, 272:288] = (8 - np.arange(16, dtype=np.float32))[None, :]
    cst[:, 288:304] = (8.0 * (np.arange(16, dtype=np.float32) + 1))[None, :]
    cst[:, 304] = 1.0
    cst[:, 320:336] = np.arange(16, dtype=np.float32)[None, :]
    return dict(posT=np.ascontiguousarray(posT), fidx=fidx, prot=prot, selc=selc, cst=cst)


def make_in_maps(inputs):
    g = lambda k: np.asarray(inputs[k], dtype=np.float32)
    maps = []
    for core in range(8):
        bi, half = core // 2, core % 2
        xs = g("x")[bi]
        cs = g("ctx")[bi]
        dsel = [0, 1]
        if half == 1:
            xs = xs[::-1]
            cs = cs[::-1]
            dsel = [1, 0]
        m = dict(
            x_seq=np.ascontiguousarray(xs), ctx_seq=np.ascontiguousarray(cs), c_vec=np.ascontiguousarray(g("c")[bi]), c_ctx=g("c_ctx"),
            ada_w=g("ada_w")[0], ada_b=g("ada_b")[0], norm1_g=g("norm1_g")[0], norm2_g=g("norm2_g")[0], w_in=g("w_in")[0],
            lam4=np.ascontiguousarray(np.stack([g("lambda_q1")[0], g("lambda_k1")[0], g("lambda_q2")[0], g("lambda_k2")[0]])),
            subln_g=g("subln_g")[0], w_attn_up=g("w_attn_up")[0],
            a_re=np.ascontiguousarray(g("ssm_a_re")[0][dsel]), a_im=np.ascontiguousarray(g("ssm_a_im")[0][dsel]),
            log_dt=np.ascontiguousarray(g("ssm_log_dt")[0][dsel]), b_re=np.ascontiguousarray(g("ssm_b_re")[0][dsel]),
            b_im=np.ascontiguousarray(g("ssm_b_im")[0][dsel]), c_re=np.ascontiguousarray(g("ssm_c_re")[0][dsel]),
            c_im=np.ascontiguousarray(g("ssm_c_im")[0][dsel]), ssm_d=g("ssm_d")[0], w_glu=g("w_glu")[0], b_glu=g("b_glu")[0],
            w_ssm_up=g("w_ssm_up")[0], w_out=g("w_out")[0], w_query=g("peer_w_query")[0], sub_k1=g("peer_sub_k1")[0],
            sub_k2=g("peer_sub_k2")[0], peer_u=g("peer_u")[0], peer_v=g("peer_v")[0], final_g=g("final_norm_g"),
        )
        m.update(host_constants(half))
        maps.append(m)
    return maps


def kernel(**inputs):
    nc = build_program()
    maps = make_in_maps(inputs)
    res = run_bass_kernel_spmd(nc, maps, core_ids=list(range(8)))
    outp = np.zeros((4, NT, D), np.float32)
    for core in range(8):
        bi, half = core // 2, core % 2
        o = np.asarray(res.results[core]["out"], dtype=np.float32)
        if half == 0:
            outp[bi, :NOWN] = o
        else:
            outp[bi, NOWN:] = o[::-1]
    return outp
```

```python
import math
from contextlib import ExitStack

import numpy as np
import concourse.bass as bass
import concourse.mybir as mybir
from concourse.bass_utils import run_bass_kernel_spmd

F32 = mybir.dt.float32
BF16 = mybir.dt.bfloat16
I32 = mybir.dt.int32
U32 = mybir.dt.uint32
AF = mybir.ActivationFunctionType
ALU = mybir.AluOpType
AX = mybir.AxisListType

ENG = ("pe", "dve", "act", "pool", "sp")
D = 1024
NT = 4096
NOWN = 2048
NCTX = 256
NKEY = NCTX + NT
EPS = 1e-6
LAM_INIT = 0.2
PI = math.pi


class Sched:
    def __init__(self, nc, stack, n_dma_sems=32):
        self.nc = nc
        self.eobj = {"pe": nc.tensor, "dve": nc.vector, "act": nc.scalar, "pool": nc.gpsimd, "sp": nc.sync}
        self.sem = {e: stack.enter_context(nc.semaphore("sem_" + e)) for e in ENG if e != "sp"}
        self.cnt = {e: 0 for e in ENG}
        self.dsem = [stack.enter_context(nc.semaphore("dsem%d" % i)) for i in range(n_dma_sems)]
        self.dval = [0] * n_dma_sems
        self.dnext = 0
        self.waited = {e: {} for e in ENG}
        self.ops = {e: [] for e in ENG}
        self.lastw = {}
        self.readers = {}
        self.ninstr = 0
        self.excl = set()
        sems = list(self.sem.values()) + self.dsem
        with nc.Block() as block:
            @block.sync
            def _(eng):
                for h in sems:
                    nc.sync.sem_clear(h)

    def _semh(self, semkey):
        return self.sem[semkey] if isinstance(semkey, str) else self.dsem[semkey[1]]

    def _wait(self, e, tok):
        semkey, val, teng = tok
        if self.waited[e].get(semkey, 0) >= val:
            return
        self.waited[e][semkey] = val
        h = self._semh(semkey)
        eo = self.eobj[e]
        self.ops[e].append(lambda: eo.wait_ge(h, val))

    def _deps(self, e, r, w):
        toks = []
        for k in r:
            t = self.lastw.get(k)
            if t is not None and not (t[2] == e and e == "pe"):
                toks.append(t)
            if k in self.excl:
                for t in self.readers.get(k, ()):
                    if t[2] != e:
                        toks.append(t)
        for k in w:
            t = self.lastw.get(k)
            if t is not None and not (t[2] == e and t[0] == e):
                toks.append(t)
            for t in self.readers.get(k, ()):
                if t[2] == e and t[0] == e:
                    continue
                toks.append(t)
        return toks

    def _record(self, tok, r, w):
        for k in r:
            self.readers.setdefault(k, []).append(tok)
        for k in w:
            self.lastw[k] = tok
            self.readers[k] = []
        self.ninstr += 1

    def I(self, e, fn, r=(), w=()):
        for t in self._deps(e, r, w):
            self._wait(e, t)
        self.cnt[e] += 1
        val = self.cnt[e]
        h = self.sem[e]
        self.ops[e].append(lambda: fn().then_inc(h, 1))
        tok = (e, val, e)
        self._record(tok, r, w)
        return tok

    def D(self, e, fn, r=(), w=()):
        for t in self._deps(e, r, w):
            self._wait(e, t)
        i = self.dnext
        self.dnext = (self.dnext + 1) % len(self.dsem)
        if self.dval[i] > 0:
            self._wait(e, (("d", i), self.dval[i], "dma"))
        self.dval[i] += 16
        val = self.dval[i]
        h = self.dsem[i]
        self.ops[e].append(lambda: fn().then_inc(h, 16))
        tok = (("d", i), val, "dma")
        self._record(tok, r, w)
        return tok

    def wait_all(self, e="sp"):
        for i in range(len(self.dsem)):
            if self.dval[i] > 0:
                self._wait(e, (("d", i), self.dval[i], "dma"))
        for e2 in ENG:
            if e2 != "sp" and e2 != e and self.cnt[e2] > 0:
                self._wait(e, (e2, self.cnt[e2], e2))

    def flush(self):
        self.wait_all("sp")
        ops = self.ops
        self.ops = {e: [] for e in ENG}
        with self.nc.Block() as block:
            @block.tensor
            def _(eng):
                for f in ops["pe"]:
                    f()

            @block.vector
            def _(eng):
                for f in ops["dve"]:
                    f()

            @block.scalar
            def _(eng):
                for f in ops["act"]:
                    f()

            @block.gpsimd
            def _(eng):
                for f in ops["pool"]:
                    f()

            @block.sync
            def _(eng):
                for f in ops["sp"]:
                    f()
        self.lastw = {}
        self.readers = {}


class Bld:
    def __init__(self, nc, S):
        self.nc = nc
        self.S = S
        self.e = {"dve": nc.vector, "pool": nc.gpsimd, "act": nc.scalar, "sp": nc.sync, "pe": nc.tensor}

    def mm(self, out, lhsT, rhs, start=True, stop=True, r=(), w=()):
        nc = self.nc
        return self.S.I("pe", lambda: nc.tensor.matmul(out, lhsT=lhsT, rhs=rhs, start=start, stop=stop), r=r, w=w)

    def tr(self, out, in_, ident, r=(), w=()):
        nc = self.nc
        return self.S.I("pe", lambda: nc.tensor.transpose(out=out, in_=in_, identity=ident), r=r, w=w)

    def act(self, out, in_, func, r=(), w=(), scale=None, bias=None, accum=None):
        nc = self.nc
        kw = {}
        if scale is not None:
            kw["scale"] = scale
        if bias is not None:
            kw["bias"] = bias
        if accum is not None:
            kw["accum_out"] = accum
        return self.S.I("act", lambda: nc.scalar.activation(out=out, in_=in_, func=func, **kw), r=r, w=w)

    def tt(self, e, out, in0, in1, op, r=(), w=()):
        eo = self.e[e]
        return self.S.I(e, lambda: eo.tensor_tensor(out=out, in0=in0, in1=in1, op=op), r=r, w=w)

    def ts(self, e, out, in0, s1, s2, op0, op1=None, r=(), w=(), accum=None):
        eo = self.e[e]
        kw = {}
        if op1 is not None:
            kw["op1"] = op1
        if accum is not None:
            kw["accum_out"] = accum
        return self.S.I(e, lambda: eo.tensor_scalar(out=out, in0=in0, scalar1=s1, scalar2=s2, op0=op0, **kw), r=r, w=w)

    def stt(self, out, in0, scalar, in1, op0, op1, r=(), w=(), accum=None):
        nc = self.nc
        kw = {}
        if accum is not None:
            kw["accum_out"] = accum
        return self.S.I("dve", lambda: nc.vector.scalar_tensor_tensor(out=out, in0=in0, scalar=scalar, in1=in1, op0=op0, op1=op1, **kw), r=r, w=w)

    def cp(self, e, out, in_, r=(), w=()):
        if e == "act":
            nc = self.nc
            return self.S.I("act", lambda: nc.scalar.copy(out=out, in_=in_), r=r, w=w)
        eo = self.e[e]
        return self.S.I(e, lambda: eo.tensor_copy(out=out, in_=in_), r=r, w=w)

    def memset(self, e, ap, val, w=()):
        eo = self.e[e]
        return self.S.I(e, lambda: eo.memset(ap, val), w=w)

    def dma(self, e, out, in_, r=(), w=(), slow=False):
        eo = self.e[e]
        if slow:
            return self.S.D(e, lambda: eo.dma_start(out=out, in_=in_, allow_slow_non_contiguous=True), r=r, w=w)
        return self.S.D(e, lambda: eo.dma_start(out=out, in_=in_), r=r, w=w)

    def red(self, out, in_, op, axis=AX.X, r=(), w=()):
        nc = self.nc
        return self.S.I("dve", lambda: nc.vector.tensor_reduce(out=out, in_=in_, axis=axis, op=op), r=r, w=w)

    def recip(self, out, in_, r=(), w=()):
        nc = self.nc
        return self.S.I("dve", lambda: nc.vector.reciprocal(out=out, in_=in_), r=r, w=w)


GELU_C = 2.0 * math.sqrt(2.0 / math.pi)


def gelu_tanh(b, out, x, t, kx, kt, ko):
    b.tt("dve", t, x, x, ALU.mult, r=[kx], w=[kt])
    b.ts("dve", t, t, 0.044715, 1.0, ALU.mult, ALU.add, r=[kt], w=[kt])
    b.tt("dve", t, t, x, ALU.mult, r=[kt, kx], w=[kt])
    b.act(t, t, AF.Sigmoid, r=[kt], w=[kt], scale=GELU_C)
    b.tt("dve", out, x, t, ALU.mult, r=[kx, kt], w=[ko])


def bcl(ap, n):
    sh = list(ap.shape)
    return ap.unsqueeze(len(sh)).to_broadcast(sh + [n])


def build_program(debug=()):
    nc = bass.Bass("TRN2", target_bir_lowering=False)
    dbg = set(debug)

    def din(name, shape, dt=F32):
        return nc.dram_tensor(name, list(shape), dt, kind="ExternalInput").ap()

    def dscr(name, shape, dt=F32):
        kind = "ExternalOutput" if name in dbg else "Internal"
        return nc.dram_tensor(name, list(shape), dt, kind=kind).ap()

    io = dict(
        x_seq=din("x_seq", [NT, D]), ctx_seq=din("ctx_seq", [NCTX, D]), c_vec=din("c_vec", [D]), c_ctx=din("c_ctx", [D]),
        ada_w=din("ada_w", [D, 6 * D]), ada_b=din("ada_b", [6 * D]), norm1_g=din("norm1_g", [D]), norm2_g=din("norm2_g", [D]),
        w_in=din("w_in", [D, 5632]), lam4=din("lam4", [4, 64]), subln_g=din("subln_g", [128]),
        w_attn_up=din("w_attn_up", [D, D]), a_re=din("a_re", [2, 32, 64]), a_im=din("a_im", [2, 32, 64]),
        log_dt=din("log_dt", [2, 32]), b_re=din("b_re", [2, 32, 64, 16]), b_im=din("b_im", [2, 32, 64, 16]),
        c_re=din("c_re", [2, 32, 16, 64]), c_im=din("c_im", [2, 32, 16, 64]), ssm_d=din("ssm_d", [512]),
        w_glu=din("w_glu", [512, 512]), b_glu=din("b_glu", [512]), w_ssm_up=din("w_ssm_up", [512, D]),
        w_out=din("w_out", [D, D]), w_query=din("w_query", [D, D]), sub_k1=din("sub_k1", [128, 64]),
        sub_k2=din("sub_k2", [128, 64]), peer_u=din("peer_u", [16384, D]), peer_v=din("peer_v", [16384, D]),
        final_g=din("final_g", [D]), posT=din("posT", [128, NT]), fidx=din("fidx", [128, 1]), prot=din("prot", [128, 128]),
        selc=din("selc", [128, 8, 240]), cst=din("cst", [128, 512]),
    )
    out = nc.dram_tensor("out", [NOWN, D], F32, kind="ExternalOutput").ap()
    scr = dict(
        vec_s=dscr("vec_s", [4, D]),
        qT_s=dscr("qT_s", [8, 2, 65, NOWN], BF16),
        kT_s=dscr("kT_s", [8, 2, 65, NKEY], BF16),
        v_s=dscr("v_s", [34, 128, 8, 130], BF16),
        uT_s=dscr("uT_s", [4, 128, NKEY]),
        xnT_s=dscr("xnT_s", [128, 8, NOWN], BF16),
        attnT_s=dscr("attnT_s", [128, 8, NOWN], BF16),
        ssmT_s=dscr("ssmT_s", [128, 4, NOWN], BF16),
        x1_s=dscr("x1_s", [NOWN, D]),
        puv_b=dscr("puv_b", [16384, 2, D], BF16),
    )

    with ExitStack() as st:
        S = Sched(nc, st)
        b = Bld(nc, S)
        T = lambda name, shape, dt=F32: st.enter_context(nc.sbuf_tensor("s_" + name, list(shape), dt))
        G = {}
        G["ident_f"] = T("ident_f", [128, 128])
        G["ident_b"] = T("ident_b", [128, 128], BF16)
        G["vecT"] = T("vecT", [128, 10, 8])
        G["lam"] = T("lam", [128, 4])
        G["dbg"] = dbg
        if "stop5a" in dbg:
            G["idx_d"] = nc.dram_tensor("idx_d", [128, 128], I32, kind="ExternalOutput").ap()
            G["gate_d"] = nc.dram_tensor("gate_d", [128, 128], F32, kind="ExternalOutput").ap()
            G["sc_d"] = nc.dram_tensor("sc_d", [128, 2048], F32, kind="ExternalOutput").ap()
        if "ygT_d" in dbg:
            G["ygT_d"] = nc.dram_tensor("ygT_d", [128, 4, NOWN], F32, kind="ExternalOutput").ap()
        phase0(nc, S, b, io, scr, G)
        if "stop0" in dbg:
            return nc
        if "only5" in dbg:
            x1_in = nc.dram_tensor("x1_in", [NOWN, D], F32, kind="ExternalInput").ap()
            with nc.sbuf_tensor("s_cpy", [128, 16, D], F32) as cpy:
                b.dma("sp", cpy[:], x1_in.rearrange("(t p) d -> p t d", p=128), w=["cpy"])
                b.dma("sp", scr["x1_s"].rearrange("(t p) d -> p t d", p=128), cpy[:], r=["cpy"], w=["x1_s"])
                S.flush()
            phase5(nc, S, b, io, scr, G, out)
            return nc
        phase1(nc, S, b, io, scr, G)
        if "stop1" in dbg:
            return nc
        if "skip2" not in dbg:
            phase2(nc, S, b, io, scr, G)
        if "stop2" in dbg:
            return nc
        if "skip3" not in dbg:
            phase3(nc, S, b, io, scr, G)
        if "stop3" in dbg:
            return nc
        phase4(nc, S, b, io, scr, G)
        if "stop4" in dbg:
            return nc
        phase5(nc, S, b, io, scr, G, out)
    return nc


V_SCALE1, V_SHIFT1, V_SCALE1C, V_SHIFT1C, V_SCALE2, V_SHIFT2, V_G1, V_G2, V_N1G, V_N2G = range(10)
R_G1, R_G2, R_SCALE2, R_SHIFT2, R_FING = range(5)


def phase0(nc, S, b, io, scr, G):
    with ExitStack() as st:
        T = lambda name, shape, dt=F32: st.enter_context(nc.sbuf_tensor("s_" + name, list(shape), dt))
        P = lambda name, shape, dt=F32: (S.excl.add(name), st.enter_context(nc.psum_tensor("p_" + name, list(shape), dt)))[1]
        ident_f, ident_b, vecT, lam = G["ident_f"], G["ident_b"], G["vecT"], G["lam"]
        b.memset("pool", ident_f[:], 0.0, w=["ident_f"])
        S.I("pool", lambda: nc.gpsimd.affine_select(out=ident_f[:], in_=ident_f[:], pattern=[[-1, 128]], compare_op=ALU.not_equal,
                                                    fill=1.0, base=0, channel_multiplier=1), r=["ident_f"], w=["ident_f"])
        b.cp("pool", ident_b[:], ident_f[:], r=["ident_f"], w=["ident_b"])

        cT = T("cT", [128, 8, 2])
        b.dma("sp", cT[:, :, 0], io["c_vec"].rearrange("(j p) -> p j", p=128), w=["cT"], slow=True)
        b.dma("sp", cT[:, :, 1], io["c_ctx"].rearrange("(j p) -> p j", p=128), w=["cT"], slow=True)
        b.act(cT[:], cT[:], AF.Silu, r=["cT"], w=["cT"])
        adabT = T("adabT", [128, 48])
        b.dma("sp", adabT[:], io["ada_b"].rearrange("(c p) -> p c", p=128), w=["adabT"], slow=True)
        b.dma("sp", vecT[:, V_N1G, :], io["norm1_g"].rearrange("(j p) -> p j", p=128), w=["n1g"], slow=True)
        b.dma("sp", vecT[:, V_N2G, :], io["norm2_g"].rearrange("(j p) -> p j", p=128), w=["n2g"], slow=True)
        aw = [T("aw%d" % i, [128, 8, D]) for i in range(2)]
        modps = P("modps", [128, 48, 2])
        modT = T("modT", [128, 48, 2])
        for pc in range(6):
            sl = pc % 2
            b.dma("sp" if pc % 2 == 0 else "act", aw[sl][:], io["ada_w"][:, pc * D:(pc + 1) * D].rearrange("(j p) n -> p j n", p=128), w=["aw%d" % sl])
            for cc in range(8):
                for j in range(8):
                    b.mm(modps[:, pc * 8 + cc, :], aw[sl][:, j, cc * 128:(cc + 1) * 128], cT[:, j, :], start=(j == 0), stop=(j == 7),
                         r=["aw%d" % sl, "cT"], w=["modps"])
        b.tt("dve", modT[:], modps[:], bcl(adabT[:], 2), ALU.add, r=["modps", "adabT"], w=["modT"])
        b.stt(vecT[:, V_SCALE1, :], modT[:, 8:16, 0], 1.0, vecT[:, V_N1G, :], ALU.add, ALU.mult, r=["modT", "n1g"], w=["v_scale1"])
        b.stt(vecT[:, V_SCALE1C, :], modT[:, 8:16, 1], 1.0, vecT[:, V_N1G, :], ALU.add, ALU.mult, r=["modT", "n1g"], w=["v_scale1c"])
        b.stt(vecT[:, V_SCALE2, :], modT[:, 32:40, 0], 1.0, vecT[:, V_N2G, :], ALU.add, ALU.mult, r=["modT", "n2g"], w=["v_scale2"])
        b.cp("dve", vecT[:, V_SHIFT1, :], modT[:, 0:8, 0], r=["modT"], w=["v_shift1"])
        b.cp("dve", vecT[:, V_SHIFT1C, :], modT[:, 0:8, 1], r=["modT"], w=["v_shift1c"])
        b.cp("dve", vecT[:, V_SHIFT2, :], modT[:, 24:32, 0], r=["modT"], w=["v_shift2"])
        b.cp("dve", vecT[:, V_G1, :], modT[:, 16:24, 0], r=["modT"], w=["v_g1"])
        b.cp("dve", vecT[:, V_G2, :], modT[:, 40:48, 0], r=["modT"], w=["v_g2"])
        for i, (slot, key) in enumerate([(V_G1, "v_g1"), (V_G2, "v_g2"), (V_SCALE2, "v_scale2"), (V_SHIFT2, "v_shift2")]):
            b.dma("sp", scr["vec_s"][i].rearrange("(j p) -> p j", p=128), vecT[:, slot, :], r=[key], w=["vec_s%d" % i], slow=True)
        l4 = T("l4", [128, 4, 64])
        b.dma("sp", l4[:], io["lam4"].rearrange("a k -> (a k)").partition_broadcast(128).rearrange("p (a k) -> p a k", a=4), w=["l4"])
        lt = T("lt", [128, 2, 64])
        b.tt("dve", lt[:, 0, :], l4[:, 0, :], l4[:, 1, :], ALU.mult, r=["l4"], w=["lt"])
        b.tt("dve", lt[:, 1, :], l4[:, 2, :], l4[:, 3, :], ALU.mult, r=["l4"], w=["lt"])
        b.red(lam[:, 0:2], lt[:], ALU.add, r=["lt"], w=["lam"])
        b.act(lam[:, 0:2], lam[:, 0:2], AF.Exp, r=["lam"], w=["lam"])
        b.stt(lam[:, 2:3], lam[:, 0:1], LAM_INIT, lam[:, 1:2], ALU.add, ALU.subtract, r=["lam"], w=["lam2"])
        b.ts("dve", lam[:, 3:4], lam[:, 2:3], -1.0, None, ALU.mult, r=["lam2"], w=["lam3"])
        S.flush()


def phase1(nc, S, b, io, scr, G):
    ident_f, ident_b, vecT = G["ident_f"], G["ident_b"], G["vecT"]
    with ExitStack() as st:
        T = lambda name, shape, dt=F32: st.enter_context(nc.sbuf_tensor("s_" + name, list(shape), dt))
        P = lambda name, shape, dt=F32: (S.excl.add(name), st.enter_context(nc.psum_tensor("p_" + name, list(shape), dt)))[1]
        xnT = T("xnT", [128, 8, NKEY], BF16)
        with ExitStack() as st2:
            T2 = lambda name, shape, dt=F32: st2.enter_context(nc.sbuf_tensor("s_" + name, list(shape), dt))
            P2 = lambda name, shape, dt=F32: (S.excl.add(name), st2.enter_context(nc.psum_tensor("p_" + name, list(shape), dt)))[1]
            xt = [T2("xt%d" % i, [128, D]) for i in range(2)]
            xs = [T2("xs%d" % i, [128, D]) for i in range(2)]
            junk = T2("junk", [128, D])
            ss = [T2("ss%d" % i, [128, 4]) for i in range(2)]
            pT = [P2("pT%d" % i, [128, 8, 128]) for i in range(2)]
            tmp = [T2("tmp%d" % i, [128, 8, 128]) for i in range(2)]
            for ti in range(34):
                sl = ti % 2
                src = io["ctx_seq"][ti * 128:(ti + 1) * 128, :] if ti < 2 else io["x_seq"][(ti - 2) * 128:(ti - 1) * 128, :]
                vs, vh = (V_SCALE1C, V_SHIFT1C) if ti < 2 else (V_SCALE1, V_SHIFT1)
                ks, kh = ("v_scale1c", "v_shift1c") if ti < 2 else ("v_scale1", "v_shift1")
                b.dma("sp" if sl == 0 else "act", xt[sl][:], src, w=["xt%d" % sl])
                b.act(junk[:], xt[sl][:], AF.Square, r=["xt%d" % sl], w=["junk", "ss%d" % sl], accum=ss[sl][:, 0:1])
                b.ts("dve", ss[sl][:, 1:2], ss[sl][:, 0:1], 1.0 / D, EPS, ALU.mult, ALU.add, r=["ss%d" % sl], w=["ssb%d" % sl])
                b.act(ss[sl][:, 2:3], ss[sl][:, 1:2], AF.Sqrt, r=["ssb%d" % sl], w=["ssc%d" % sl])
                b.recip(ss[sl][:, 3:4], ss[sl][:, 2:3], r=["ssc%d" % sl], w=["ssd%d" % sl])
                b.act(xs[sl][:], xt[sl][:], AF.Copy, r=["xt%d" % sl, "ssd%d" % sl], w=["xs%d" % sl], scale=ss[sl][:, 3:4])
                for j in range(8):
                    b.tr(pT[sl][:, j, :], xs[sl][:, j * 128:(j + 1) * 128], ident_f[:], r=["xs%d" % sl], w=["pT%d" % sl])
                b.tt("dve", tmp[sl][:], pT[sl][:], bcl(vecT[:, vs, :], 128), ALU.mult, r=["pT%d" % sl, ks], w=["tmp%d" % sl])
                b.tt("pool", xnT[:, :, ti * 128:(ti + 1) * 128], tmp[sl][:], bcl(vecT[:, vh, :], 128), ALU.add, r=["tmp%d" % sl, kh], w=["xnT"])
            b.dma("sp", scr["xnT_s"][:, :, :], xnT[:, :, NCTX:NCTX + NOWN], r=["xnT"], w=["xnT_s"])
            S.flush()
        if "stop1a" in G["dbg"]:
            return
        cosT = T("cosT", [128, NT])
        sinT = T("sinT", [128, NT])
        with ExitStack() as st2:
            T2 = lambda name, shape, dt=F32: st2.enter_context(nc.sbuf_tensor("s_" + name, list(shape), dt))
            ang = T2("ang", [128, NT])
            y = T2("y", [128, NT])
            yi = T2("yi", [128, NT], I32)
            fi = T2("fi", [128, 2])
            b.dma("sp", ang[:], io["posT"][:, :], w=["ang"])
            b.dma("sp", fi[:, 0:1], io["fidx"][:, :], w=["fi"])
            b.act(fi[:, 1:2], fi[:, 0:1], AF.Exp, r=["fi"], w=["inv"], scale=-math.log(10000.0) / 16.0)
            b.ts("dve", ang[:], ang[:], fi[:, 1:2], None, ALU.mult, r=["ang", "inv"], w=["ang"])
            for tab, off, key in ((sinT, 0.5, "sinT"), (cosT, 0.75, "cosT")):
                b.ts("dve", y[:], ang[:], 1.0 / (2 * PI), off, ALU.mult, ALU.add, r=["ang"], w=["y"])
                b.cp("dve", yi[:], y[:], r=["y"], w=["yi"])
                b.cp("dve", tab[:], yi[:], r=["yi"], w=[key])
                b.tt("dve", y[:], y[:], tab[:], ALU.subtract, r=["y", key], w=["y"])
                b.ts("dve", tab[:], y[:], 0.0, None, ALU.is_lt, r=["y"], w=[key])
                b.tt("dve", y[:], y[:], tab[:], ALU.add, r=["y", key], w=["y"])
                b.ts("dve", y[:], y[:], 2 * PI, -PI, ALU.mult, ALU.add, r=["y"], w=["y"])
                b.ts("dve", y[:], y[:], 3.1415925, -3.1415925, ALU.min, ALU.max, r=["y"], w=["y"])
                b.act(tab[:], y[:], AF.Sin, r=["y"], w=[key])
            S.flush()
        if "stop1r" in G["dbg"]:
            return
        prot = T("prot", [128, 128], BF16)
        protf = T("protf", [128, 128])
        b.dma("sp", protf[:], io["prot"][:, :], w=["protf"])
        b.cp("dve", prot[:], protf[:], r=["protf"], w=["prot"])
        bones = T("bones", [128, 2], BF16)
        b.memset("pool", bones[:], 0.0, w=["bones"])
        b.memset("pool", bones[0:64, 0:1], 1.0, w=["bones"])
        b.memset("pool", bones[64:128, 1:2], 1.0, w=["bones"])
        onesrow = T("onesrow", [16, NKEY], BF16)
        b.memset("pool", onesrow[:], 1.0, w=["onesrow"])
        if "no_ones" not in G["dbg"]:
            b.dma("sp", scr["kT_s"][:, :, 64, :].rearrange("h m c -> (h m) c"), onesrow[:], r=["onesrow"], w=["kT_s64"])
        wf = [T("wf%d" % i, [128, 8, 512]) for i in range(2)]
        wb = [T("wb%d" % i, [128, 8, 512], BF16) for i in range(2)]
        pA = [P("pA%d" % i, [128, 512]) for i in range(2)]
        pB = [P("pB%d" % i, [128, 512]) for i in range(2)]
        pN = [P("pN%d" % i, [2, 512]) for i in range(2)]
        asb = [T("asb%d" % i, [128, 512], BF16) for i in range(2)]
        t1 = [T("t1_%d" % i, [128, 512]) for i in range(2)]
        t2 = [T("t2_%d" % i, [128, 512]) for i in range(2)]
        kr = [T("kr%d" % i, [128, 512], BF16) for i in range(3)]
        sq = [T("sq%d" % i, [128, 512], BF16) for i in range(2)]
        kmx = T("kmx", [2, 8, 10])
        negk = T("negk", [2, 8])
        nrow = [T("nrow%d" % i, [2, 512]) for i in range(2)]
        nrowb = [T("nrowb%d" % i, [2, 512], BF16) for i in range(2)]
        vsb = [T("vsb%d" % i, [128, 8, 130], BF16) for i in range(2)]
        usb = [T("usb%d" % i, [128, 512]) for i in range(2)]
        for i in range(2):
            b.memset("pool", vsb[i][:], 0.0, w=["vsb%d" % i])
            b.memset("pool", vsb[i][:, :, 128:129], 1.0, w=["vsb%d" % i])
        b.memset("pool", kmx[:], 0.0, w=["kmx"])
        cnt = {"w": 0, "t": 0, "v": 0, "u": 0, "kr": 0}
        if "stop1s" in G["dbg"]:
            S.flush()
            return

        def load_w(cb):
            sl = cnt["w"] % 2
            cnt["w"] += 1
            b.dma("sp", wf[sl][:], io["w_in"][:, cb * 512:(cb + 1) * 512].rearrange("(j p) n -> p j n", p=128), w=["wf%d" % sl])
            b.cp("dve" if "w_cast_dve" in G["dbg"] else "pool", wb[sl][:], wf[sl][:], r=["wf%d" % sl], w=["wb%d" % sl])
            return sl

        def qk_tile(wsl, ch, col0, ncol, tok0, rope, is_q, h):
            i = cnt["t"] % 2
            cnt["t"] += 1
            for j in range(8):
                b.mm(pA[i][:, :ncol], wb[wsl][:, j, ch * 128:(ch + 1) * 128], xnT[:, j, col0:col0 + ncol], start=(j == 0), stop=(j == 7),
                     r=["wb%d" % wsl, "xnT"], w=["pA%d" % i])
            ki = cnt["kr"] % 3
            cnt["kr"] += 1
            if rope and "no_rope_ops" not in G["dbg"]:
                b.cp("act", asb[i][:, :ncol], pA[i][:, :ncol], r=["pA%d" % i], w=["asb%d" % i])
                b.mm(pB[i][:, :ncol], prot[:], asb[i][:, :ncol], r=["prot", "asb%d" % i], w=["pB%d" % i])
                b.tt("dve", t1[i][:, :ncol], pA[i][:, :ncol], cosT[:, tok0:tok0 + ncol], ALU.mult, r=["pA%d" % i, "cosT"], w=["t1_%d" % i])
                b.tt("dve", t2[i][:, :ncol], pB[i][:, :ncol], sinT[:, tok0:tok0 + ncol], ALU.mult, r=["pB%d" % i, "sinT"], w=["t2_%d" % i])
                b.tt("pool", kr[ki][:, :ncol], t1[i][:, :ncol], t2[i][:, :ncol], ALU.add, r=["t1_%d" % i, "t2_%d" % i], w=["kr%d" % ki])
            else:
                b.cp("act", kr[ki][:, :ncol], pA[i][:, :ncol], r=["pA%d" % i], w=["kr%d" % ki])
            if "no_norm" not in G["dbg"]:
                b.act(sq[i][:, :ncol], kr[ki][:, :ncol], AF.Square, r=["kr%d" % ki], w=["sq%d" % i])
                b.mm(pN[i][:, :ncol], bones[:], sq[i][:, :ncol], r=["bones", "sq%d" % i], w=["pN%d" % i])
            return i, ki

        for cb in (2, 3):
            wsl = load_w(cb)
            for ch in range(4):
                h = (cb - 2) * 4 + ch
                blocks = [(0, NCTX, 0, False)] + [(NCTX + tb * 512, 512, tb * 512, True) for tb in range(8)]
                for bi, (col0, ncol, tok0, rope) in enumerate(blocks):
                    i, ki = qk_tile(wsl, ch, col0, ncol, tok0, rope, False, h)
                    if "no_norm" not in G["dbg"]:
                        b.red(kmx[:, h, bi:bi + 1], pN[i][:, :ncol], ALU.max, r=["pN%d" % i], w=["kmx"])
                    for m in range(2 if "no_kstore" not in G["dbg"] else 0):
                        b.dma("sp" if m == 0 else "act", scr["kT_s"][h, m, 0:64, col0:col0 + ncol], kr[ki][m * 64:(m + 1) * 64, :ncol], r=["kr%d" % ki], w=["kT_s"])
        if "stop1k" in G["dbg"]:
            S.flush()
            return
        b.red(negk[:], kmx[:], ALU.max, r=["kmx"], w=["negk"])
        b.act(negk[:], negk[:], AF.Sqrt, r=["negk"], w=["negk"])
        b.ts("dve", negk[:], negk[:], -1.0, None, ALU.mult, r=["negk"], w=["negk"])
        for cb in (0, 1):
            wsl = load_w(cb)
            for ch in range(4):
                h = cb * 4 + ch
                for tb in range(4):
                    i, ki = qk_tile(wsl, ch, NCTX + tb * 512, 512, tb * 512, True, True, h)
                    b.act(nrow[i][:], pN[i][:], AF.Sqrt, r=["pN%d" % i], w=["nrow%d" % i])
                    b.ts("dve", nrowb[i][:], nrow[i][:], negk[:, h:h + 1], None, ALU.mult, r=["nrow%d" % i, "negk"], w=["nrowb%d" % i])
                    b.dma("sp", scr["qT_s"][h, :, 64, tb * 512:(tb + 1) * 512], nrowb[i][:], r=["nrowb%d" % i], w=["qT_s"])
                    for m in range(2):
                        b.dma("sp" if m == 0 else "act", scr["qT_s"][h, m, 0:64, tb * 512:(tb + 1) * 512], kr[ki][m * 64:(m + 1) * 64, :], r=["kr%d" % ki], w=["qT_s"])
        if "stop1q" in G["dbg"]:
            S.flush()
            return
        wv = [load_w(4), load_w(5)]
        for ti in range(34):
            i = cnt["v"] % 2
            cnt["v"] += 1
            for half in range(2):
                pt = pA[half]
                for j in range(8):
                    b.mm(pt[:], xnT[:, j, ti * 128:(ti + 1) * 128], wb[wv[half]][:, j, :], start=(j == 0), stop=(j == 7),
                         r=["xnT", "wb%d" % wv[half]], w=["pA%d" % half])
                b.cp("act" if half == 0 else "dve", vsb[i][:, half * 4:(half + 1) * 4, 0:128], pt[:].rearrange("p (h e) -> p h e", h=4),
                     r=["pA%d" % half], w=["vsb%d" % i])
            b.dma("sp", scr["v_s"][ti], vsb[i][:], r=["vsb%d" % i], w=["v_s"])
        if "stop1v" in G["dbg"]:
            S.flush()
            return
        wsl = load_w(6)
        blocks = [(0, NCTX)] + [(NCTX + tb * 512, 512) for tb in range(8)]
        for ch in range(4):
            for (col0, ncol) in blocks:
                i = cnt["u"] % 2
                cnt["u"] += 1
                for j in range(8):
                    b.mm(pB[i][:, :ncol], wb[wsl][:, j, ch * 128:(ch + 1) * 128], xnT[:, j, col0:col0 + ncol], start=(j == 0), stop=(j == 7),
                         r=["wb%d" % wsl, "xnT"], w=["pB%d" % i])
                b.cp("act", usb[i][:, :ncol], pB[i][:, :ncol], r=["pB%d" % i], w=["usb%d" % i])
                b.dma("sp", scr["uT_s"][ch, :, col0:col0 + ncol], usb[i][:, :ncol], r=["usb%d" % i], w=["uT_s"])
        S.flush()


def phase2(nc, S, b, io, scr, G):
    ident_b, lam = G["ident_b"], G["lam"]
    with ExitStack() as st:
        T = lambda name, shape, dt=F32: st.enter_context(nc.sbuf_tensor("s_" + name, list(shape), dt))
        P = lambda name, shape, dt=F32: (S.excl.add(name), st.enter_context(nc.psum_tensor("p_" + name, list(shape), dt)))[1]
        kT = [T("kT%d" % i, [65, 2, NKEY], BF16) for i in range(2)]
        qT = [T("qT%d" % i, [65, 2, NOWN], BF16) for i in range(2)]
        vv = [T("vv%d" % i, [128, 34, 130], BF16) for i in range(2)]
        E = [T("E%d" % i, [128, 512], BF16) for i in range(4)]
        pS = [P("pS%d" % i, [128, 512]) for i in range(3)]
        pO = [P("pO%d" % i, [128, 512]) for i in range(4)]
        pTr = P("pTr", [128, 1024], BF16)
        osb = [T("osb%d" % i, [128, 130]) for i in range(4)]
        attnT = T("attnT", [128, 8, NOWN], BF16)
        gsub = T("gsub", [128, 128])
        sm = [T("sm%d" % i, [128, 8]) for i in range(2)]
        ot = [T("ot%d" % i, [128, 128]) for i in range(2)]
        ob = [T("ob%d" % i, [128, 128], BF16) for i in range(2)]
        junk = T("junk2", [128, 128])
        cvf = [T("cvf%d" % i, [128, 4, D]) for i in range(2)]
        cvb = [T("cvb%d" % i, [128, 4, D], BF16) for i in range(2)]
        cvc = [0]

        def convert_chunk():
            c = cvc[0]
            cvc[0] += 1
            if c >= 64:
                return
            i = c % 2
            src = io["peer_u"] if c < 32 else io["peer_v"]
            dst = scr["puv_b"][:, 0 if c < 32 else 1, :]
            r0 = (c % 32) * 512
            b.dma("sp", cvf[i][:], src[r0:r0 + 512, :].rearrange("(p j) d -> p j d", j=4), w=["cvf%d" % i])
            b.cp("pool", cvb[i][:], cvf[i][:], r=["cvf%d" % i], w=["cvb%d" % i])
            b.dma("sp", dst[r0:r0 + 512, :].rearrange("(p j) d -> p j d", j=4), cvb[i][:], r=["cvb%d" % i], w=["pub"])

        b.dma("sp", gsub[:], io["subln_g"].partition_broadcast(128), w=["gsub"])
        b.ts("dve", gsub[:], gsub[:], 1.0 - LAM_INIT, None, ALU.mult, r=["gsub"], w=["gsub"])
        ecnt = 0

        def load_head(h):
            sl = h % 2
            b.dma("sp", kT[sl][:], scr["kT_s"][h].rearrange("m r c -> r m c"), w=["kT%d" % sl])
            b.dma("act", qT[sl][:], scr["qT_s"][h].rearrange("m r c -> r m c"), w=["qT%d" % sl])
            b.dma("sp", vv[sl][:], scr["v_s"][:, :, h, :].rearrange("k p e -> p k e"), w=["vv%d" % sl])

        pend_epi = [None]
        epic = [0]

        def epilogue(h, qg):
            for qb in range(2):
                e2 = epic[0] % 2
                epic[0] += 1
                o1, o2 = osb[qb], osb[2 + qb]
                k1, k2 = "osb%d" % qb, "osb%d" % (2 + qb)
                s_ = sm[e2]
                ks = "sm%d" % e2
                b.recip(s_[:, 0:1], o1[:, 128:129], r=[k1], w=[ks + "a"])
                b.recip(s_[:, 1:2], o2[:, 128:129], r=[k2], w=[ks + "b"])
                b.tt("dve", s_[:, 2:3], s_[:, 1:2], lam[:, 3:4], ALU.mult, r=[ks + "b", "lam3"], w=[ks + "c"])
                b.ts("dve", ot[e2][:], o1[:, 0:128], s_[:, 0:1], None, ALU.mult, r=[k1, ks + "a"], w=["ot%d" % e2])
                b.stt(ot[e2][:], o2[:, 0:128], s_[:, 2:3], ot[e2][:], ALU.mult, ALU.add, r=[k2, ks + "c", "ot%d" % e2], w=["ot%d" % e2])
                b.stt(junk[:], ot[e2][:], 1.0, ot[e2][:], ALU.mult, ALU.mult, r=["ot%d" % e2], w=["junk2", ks + "d"], accum=s_[:, 3:4])
                b.ts("dve", s_[:, 4:5], s_[:, 3:4], 1.0 / 128.0, EPS, ALU.mult, ALU.add, r=[ks + "d"], w=[ks + "e"])
                b.act(s_[:, 5:6], s_[:, 4:5], AF.Sqrt, r=[ks + "e"], w=[ks + "f"])
                b.recip(s_[:, 6:7], s_[:, 5:6], r=[ks + "f"], w=[ks + "g"])
                b.stt(ob[e2][:], ot[e2][:], s_[:, 6:7], gsub[:], ALU.mult, ALU.mult, r=["ot%d" % e2, ks + "g", "gsub"], w=["ob%d" % e2])

        def epilogue_b(h, qg):
            for qb in range(2):
                b.tr(pTr[:, qb * 128:(qb + 1) * 128], ob[qb][:], ident_b[:], r=["ob%d" % qb, "ident_b"], w=["pTr"])
            q0 = qg * 256
            b.cp("dve", attnT[:, h, q0:q0 + 256], pTr[:, 0:256], r=["pTr"], w=["attnT"])

        load_head(0)
        for h in range(8):
            sl = h % 2
            if h + 1 < 8:
                load_head(h + 1)
            for qg in range(8):
                eidx = {}
                for step in range(36):
                    kb = step
                    if kb < 34:
                        i = kb % 3
                        ei = ecnt % 4
                        ecnt += 1
                        eidx[kb] = ei
                        for m in range(2):
                            b.mm(pS[i][:, m * 256:(m + 1) * 256], kT[sl][:, m, kb * 128:(kb + 1) * 128], qT[sl][:, m, qg * 256:(qg + 1) * 256],
                                 r=["kT%d" % sl, "qT%d" % sl], w=["pS%d" % i])
                        b.act(E[ei][:], pS[i][:], AF.Exp, r=["pS%d" % i], w=["E%d" % ei], scale=0.125)
                    pk = step - 2
                    if pk >= 0:
                        pei = eidx[pk]
                        for m in range(2):
                            for qb in range(2):
                                a = m * 2 + qb
                                b.mm(pO[a][:, 0:129], E[pei][:, m * 256 + qb * 128: m * 256 + (qb + 1) * 128], vv[sl][:, pk, 0:129],
                                     start=(pk == 0), stop=(pk == 33), r=["E%d" % pei, "vv%d" % sl], w=["pO%d" % a])
                    if step == 10:
                        convert_chunk()
                    if step == 4 and pend_epi[0] is not None:
                        epilogue(*pend_epi[0])
                    if step == 20 and pend_epi[0] is not None:
                        epilogue_b(*pend_epi[0])
                        pend_epi[0] = None
                for a in range(4):
                    b.cp("act" if a % 2 == 0 else "dve", osb[a][:, 0:129], pO[a][:, 0:129], r=["pO%d" % a], w=["osb%d" % a])
                pend_epi[0] = (h, qg)
        epilogue(*pend_epi[0])
        epilogue_b(*pend_epi[0])
        b.dma("sp", scr["attnT_s"][:, :, :], attnT[:], r=["attnT"], w=["attnT_s"])
        S.flush()


def phase3(nc, S, b, io, scr, G):
    ident_f = G["ident_f"]
    dbg = G["dbg"]
    with ExitStack() as st:
        T = lambda name, shape, dt=F32: st.enter_context(nc.sbuf_tensor("s_" + name, list(shape), dt))
        cst = T("cst", [128, 512])
        selc = T("selc", [128, 8, 240])
        b.dma("sp", cst[:], io["cst"][:, :], w=["cst"])
        b.dma("sp", selc[:], io["selc"][:, :, :], w=["selc"])
        selcb = T("selcb", [128, 8, 240], BF16)
        b.cp("dve", selcb[:], selc[:], r=["selc"], w=["selcb"])
        maskf, maskb = cst[:, 0:128], cst[:, 128:256]
        kka, kkd, kk8, kk1 = cst[0:64, 256:272], cst[0:64, 272:288], cst[0:64, 288:304], cst[0:64, 304:305]
        AR = T("AR", [64, 64]); AI = T("AI", [64, 64]); DT = T("DT", [64, 64])
        RHO = T("RHO", [64, 64]); TH = T("TH", [64, 64])
        FR = T("FR", [64, 64]); FI = T("FI", [64, 64])
        FBR = T("FBR", [64, 64, 16]); FBI = T("FBI", [64, 64, 16])
        CNR = T("CNR", [64, 64, 16]); CNI = T("CNI", [64, 64, 16])
        Dsq = T("Dsq", [128, 32])
        ygT = T("ygT", [128, 4, NOWN])
        b.dma("sp", AR[:], io["a_re"].rearrange("d g n -> n (d g)"), w=["AR"], slow=True)
        b.dma("sp", AI[:], io["a_im"].rearrange("d g n -> n (d g)"), w=["AI"], slow=True)
        b.dma("sp", DT[:], io["log_dt"].rearrange("d g -> (d g)").partition_broadcast(64), w=["DT"])
        for s_ in range(8):
            b.dma("sp", Dsq[s_ * 16:(s_ + 1) * 16, :], io["ssm_d"].rearrange("(g q) -> q g", q=16), w=["Dsq"], slow=True)
        b.act(DT[:], DT[:], AF.Exp, r=["DT"], w=["DT"])
        b.tt("dve", RHO[:], AR[:], DT[:], ALU.mult, r=["AR", "DT"], w=["RHO"])
        b.tt("dve", TH[:], AI[:], DT[:], ALU.mult, r=["AI", "DT"], w=["TH"])

        uid = [0]

        def cpow(st_, dre, dim, rho, th, kk, Gn, Kn, tag):
            uid[0] += 1
            u = "%s%d" % (tag, uid[0])
            T_ = lambda name, dt=F32: st_.enter_context(nc.sbuf_tensor("s_%s_%s" % (name, u), [64, Gn, Kn], dt))
            rk = T_("rk"); y = T_("y"); yi = T_("yi", I32); w_ = T_("w"); mg = T_("mg")
            kb_ = kk.unsqueeze(1).to_broadcast([64, Gn, Kn])
            b.tt("dve", rk[:], bcl(rho, Kn), kb_, ALU.mult, r=["RHO", "cst"], w=["rk" + u])
            b.act(mg[:], rk[:], AF.Exp, r=["rk" + u], w=["mg" + u])
            b.tt("dve", rk[:], bcl(th, Kn), kb_, ALU.mult, r=["TH", "cst", "mg" + u], w=["rk" + u])
            for dst, off in ((dim, 0.5), (dre, 0.75)):
                b.ts("dve", y[:], rk[:], 1.0 / (2 * PI), off, ALU.mult, ALU.add, r=["rk" + u], w=["y" + u])
                b.cp("dve", yi[:], y[:], r=["y" + u], w=["yi" + u])
                b.cp("dve", w_[:], yi[:], r=["yi" + u], w=["w" + u])
                b.tt("dve", y[:], y[:], w_[:], ALU.subtract, r=["y" + u, "w" + u], w=["y" + u])
                b.ts("dve", w_[:], y[:], 0.0, None, ALU.is_lt, r=["y" + u], w=["w" + u])
                b.tt("dve", y[:], y[:], w_[:], ALU.add, r=["y" + u, "w" + u], w=["y" + u])
                b.ts("dve", y[:], y[:], 2 * PI, -PI, ALU.mult, ALU.add, r=["y" + u], w=["y" + u])
                b.ts("dve", y[:], y[:], 3.1415925, -3.1415925, ALU.min, ALU.max, r=["y" + u], w=["y" + u])
                b.act(w_[:], y[:], AF.Sin, r=["y" + u], w=["w" + u])
                b.tt("dve", dst, w_[:], mg[:], ALU.mult, r=["w" + u, "mg" + u], w=[tag])

        with ExitStack() as st2:
            T2 = lambda name, shape, dt=F32: st2.enter_context(nc.sbuf_tensor("s_" + name, list(shape), dt))
            P2 = lambda name, shape, dt=F32: (S.excl.add(name), st2.enter_context(nc.psum_tensor("p_" + name, list(shape), dt)))[1]
            ABR = T2("ABR", [64, 64, 1]); ABI = T2("ABI", [64, 64, 1])
            BR = T2("BR", [64, 64, 16]); BI = T2("BI", [64, 64, 16])
            b.dma("sp", BR[:], io["b_re"].rearrange("d g n q -> n (d g) q"), w=["BR"])
            b.dma("act", BI[:], io["b_im"].rearrange("d g n q -> n (d g) q"), w=["BI"])
            cpow(st2, ABR[:], ABI[:], RHO[:], TH[:], kk1, 64, 1, "AB")
            den = T2("den", [64, 64]); t1 = T2("g_t1", [64, 64]); t2 = T2("g_t2", [64, 64]); nr = T2("nr", [64, 64])
            b.tt("dve", den[:], AR[:], AR[:], ALU.mult, r=["AR"], w=["den"])
            b.tt("dve", t1[:], AI[:], AI[:], ALU.mult, r=["AI"], w=["g_t1"])
            b.tt("dve", den[:], den[:], t1[:], ALU.add, r=["den", "g_t1"], w=["den"])
            b.recip(den[:], den[:], r=["den"], w=["den"])
            b.ts("dve", nr[:], ABR[:, :, 0], -1.0, None, ALU.add, r=["AB"], w=["nr"])
            b.tt("dve", t1[:], nr[:], AR[:], ALU.mult, r=["nr", "AR"], w=["g_t1"])
            b.tt("dve", t2[:], ABI[:, :, 0], AI[:], ALU.mult, r=["AB", "AI"], w=["g_t2"])
            b.tt("dve", t1[:], t1[:], t2[:], ALU.add, r=["g_t1", "g_t2"], w=["g_t1"])
            b.tt("dve", FR[:], t1[:], den[:], ALU.mult, r=["g_t1", "den"], w=["FR"])
            b.tt("dve", t1[:], ABI[:, :, 0], AR[:], ALU.mult, r=["AB", "AR", "FR"], w=["g_t1"])
            b.tt("dve", t2[:], nr[:], AI[:], ALU.mult, r=["nr", "AI"], w=["g_t2"])
            b.tt("dve", t1[:], t1[:], t2[:], ALU.subtract, r=["g_t1", "g_t2"], w=["g_t1"])
            b.tt("dve", FI[:], t1[:], den[:], ALU.mult, r=["g_t1", "den"], w=["FI"])
            ta = T2("g_ta", [64, 64, 16]); tb = T2("g_tb", [64, 64, 16])
            b.tt("dve", ta[:], BR[:], bcl(FR[:], 16), ALU.mult, r=["BR", "FR"], w=["g_ta"])
            b.tt("dve", tb[:], BI[:], bcl(FI[:], 16), ALU.mult, r=["BI", "FI"], w=["g_tb"])
            b.tt("dve", FBR[:], ta[:], tb[:], ALU.subtract, r=["g_ta", "g_tb"], w=["FBR"])
            b.tt("dve", ta[:], BI[:], bcl(FR[:], 16), ALU.mult, r=["BI", "FR", "FBR"], w=["g_ta"])
            b.tt("dve", tb[:], BR[:], bcl(FI[:], 16), ALU.mult, r=["BR", "FI", "FBR"], w=["g_tb"])
            b.tt("dve", FBI[:], ta[:], tb[:], ALU.add, r=["g_ta", "g_tb"], w=["FBI"])
            cn = [T2("cn%d" % i, [128, 64]) for i in range(2)]
            pC = [P2("pC%d" % i, [128, 512]) for i in range(2)]
            k_ = 0
            for (src, dstt, key) in ((io["c_re"], CNR, "CNR"), (io["c_im"], CNI, "CNI")):
                for d in range(2):
                    for gb in range(4):
                        i = k_ % 2
                        k_ += 1
                        b.dma("sp" if i == 0 else "act", cn[i][:], src[d, gb * 8:(gb + 1) * 8].rearrange("g p n -> (g p) n"), w=["cn%d" % i])
                        b.tr(pC[i][0:64, 0:128], cn[i][:], ident_f[:], r=["cn%d" % i, "ident_f"], w=["pC%d" % i])
                        b.cp("act", dstt[:, d * 32 + gb * 8: d * 32 + (gb + 1) * 8, :], pC[i][0:64, 0:128].rearrange("n (g p) -> n g p", g=8),
                             r=["pC%d" % i], w=[key])
            S.flush()

        def cmul(e, ore, oim, are, aim, bre, bim, t1, t2, kr, kw, neg_im=False):
            b.tt(e, t1, are, bre, ALU.mult, r=kr, w=[kw + "t1"])
            b.tt(e, t2, aim, bim, ALU.mult, r=kr, w=[kw + "t2"])
            b.tt(e, ore, t1, t2, ALU.subtract, r=[kw + "t1", kw + "t2"], w=[kw + "R"])
            b.tt(e, t1, are, bim, ALU.mult, r=kr + [kw + "R"], w=[kw + "t1"])
            b.tt(e, t2, aim, bre, ALU.mult, r=kr + [kw + "R"], w=[kw + "t2"])
            if neg_im:
                b.S.I(e, lambda: b.e[e].scalar_tensor_tensor(out=oim, in0=t1, scalar=-1.0, in1=t2, op0=ALU.mult, op1=ALU.subtract),
                      r=[kw + "t1", kw + "t2"], w=[kw + "I"]) if e == "dve" else None
            else:
                b.tt(e, oim, t1, t2, ALU.add, r=[kw + "t1", kw + "t2"], w=[kw + "I"])

        for gb in range(4):
            with ExitStack() as stb:
                Tb = lambda name, shape, dt=F32: stb.enter_context(nc.sbuf_tensor("s_%s_b%d" % (name, gb), list(shape), dt))
                EfR = Tb("EfR", [64, 8, 128]); EfI = Tb("EfI", [64, 8, 128]); EbR = Tb("EbR", [64, 8, 128]); EbI = Tb("EbI", [64, 8, 128])
                Mt = Tb("Mt", [128, 8, 128]); Wt = Tb("Wt", [128, 8, 4, 64])
                pwaR = Tb("pwaR", [64, 16, 16]); pwaI = Tb("pwaI", [64, 16, 16])
                p8R = Tb("p8R", [64, 16, 16]); p8I = Tb("p8I", [64, 16, 16])
                gsl = lambda d: slice(d * 32 + gb * 8, d * 32 + (gb + 1) * 8)
                rb = Tb("rb", [64, 16]); tb_ = Tb("tb", [64, 16])
                for d in range(2):
                    b.cp("dve", rb[:, d * 8:(d + 1) * 8], RHO[:, gsl(d)], r=["RHO"], w=["rb"])
                    b.cp("dve", tb_[:, d * 8:(d + 1) * 8], TH[:, gsl(d)], r=["TH"], w=["tb"])
                with ExitStack() as sa:
                    Ta = lambda name, shape, dt=F32: sa.enter_context(nc.sbuf_tensor("s_%s_a%d" % (name, gb), list(shape), dt))
                    Pa = lambda name, shape, dt=F32: (S.excl.add(name), sa.enter_context(nc.psum_tensor("p_%s_a%d" % (name, gb), list(shape), dt)))[1]
                    pwdR = Ta("pwdR", [64, 16, 16]); pwdI = Ta("pwdI", [64, 16, 16])
                    S.lastw["RHO"] = S.lastw.get("rb"); S.lastw["TH"] = S.lastw.get("tb")
                    cpow(sa, pwaR[:], pwaI[:], rb[:], tb_[:], kka, 16, 16, "pwa")
                    cpow(sa, pwdR[:], pwdI[:], rb[:], tb_[:], kkd, 16, 16, "pwd")
                    cpow(sa, p8R[:], p8I[:], rb[:], tb_[:], kk8, 16, 16, "p8")
                    X0R = Ta("X0R", [64, 8, 8, 16]); X0I = Ta("X0I", [64, 8, 8, 16])
                    XpR = Ta("XpR", [64, 8, 8, 16]); XpI = Ta("XpI", [64, 8, 8, 16])
                    X1R = Ta("X1R", [64, 8, 8, 16]); X1I = Ta("X1I", [64, 8, 8, 16])
                    Y0R = Ta("Y0R", [64, 8, 8, 16]); Y0I = Ta("Y0I", [64, 8, 8, 16])
                    Y1R = Ta("Y1R", [64, 8, 8, 16]); Y1I = Ta("Y1I", [64, 8, 8, 16])
                    c1 = Ta("c1", [64, 8, 8, 16]); c2 = Ta("c2", [64, 8, 8, 16])
                    v4 = lambda t: t[:].rearrange("n g (s q) -> n g s q", q=16)

                    def pws(R_, I_, d, a):
                        return bcl(R_[:, d * 8:(d + 1) * 8, a:a + 8], 16), bcl(I_[:, d * 8:(d + 1) * 8, a:a + 8], 16)

                    def fbs(R_, I_, d):
                        return (R_[:, gsl(d), :].unsqueeze(2).to_broadcast([64, 8, 8, 16]), I_[:, gsl(d), :].unsqueeze(2).to_broadcast([64, 8, 8, 16]))

                    jobs = [
                        (X0R[:], X0I[:], pws(pwdR, pwdI, 0, 8), fbs(FBR, FBI, 0), ["pwd", "FBR", "FBI"], "X0", False),
                        (XpR[:], XpI[:], pws(pwdR, pwdI, 0, 1), fbs(FBR, FBI, 0), ["pwd", "FBR", "FBI"], "Xp", False),
                        (X1R[:], X1I[:], pws(pwaR, pwaI, 1, 7), fbs(FBR, FBI, 1), ["pwa", "FBR", "FBI"], "X1", False),
                        (Y0R[:], Y0I[:], pws(pwaR, pwaI, 0, 7), fbs(CNR, CNI, 0), ["pwa", "CNR", "CNI"], "Y0", True),
                        (Y1R[:], Y1I[:], pws(pwdR, pwdI, 1, 8), fbs(CNR, CNI, 1), ["pwd", "CNR", "CNI"], "Y1", True),
                        (v4(EfR), v4(EfI), pws(pwaR, pwaI, 0, 8), fbs(CNR, CNI, 0), ["pwa", "CNR", "CNI"], "Ef", True),
                        (v4(EbR), v4(EbI), pws(pwdR, pwdI, 1, 0), fbs(CNR, CNI, 1), ["pwd", "CNR", "CNI"], "Eb", True),
                    ]
                    for (ore, oim, (are, aim), (bre, bim), kr, kw, neg) in jobs:
                        cmul("dve", ore, oim, are, aim, bre, bim, c1[:], c2[:], kr + ["c1", "c2"], kw, neg_im=neg)
                        S.lastw["c1"] = S.lastw.get(kw + "I"); S.lastw["c2"] = S.lastw.get(kw + "I")
                    pM = [Pa("pM%d" % i, [128, 512]) for i in range(2)]
                    pW = [Pa("pW%d" % i, [128, 512]) for i in range(2)]
                    mt = Ta("mtmp", [128, 128])
                    f2 = lambda t, g: t[:, g, :, :].rearrange("n s q -> n (s q)")
                    for g in range(8):
                        i = g % 2
                        b.mm(pM[i][:, 0:128], f2(X0R, g), f2(Y0R, g), start=True, stop=False, r=["X0R", "Y0R"], w=["pM%d" % i])
                        b.mm(pM[i][:, 0:128], f2(X0I, g), f2(Y0I, g), start=False, stop=True, r=["X0I", "Y0I"], w=["pM%d" % i])
                        b.mm(pM[i][:, 128:256], f2(X1R, g), f2(Y1R, g), start=True, stop=False, r=["X1R", "Y1R"], w=["pM%d" % i])
                        b.mm(pM[i][:, 128:256], f2(X1I, g), f2(Y1I, g), start=False, stop=True, r=["X1I", "Y1I"], w=["pM%d" % i])
                        b.tt("dve", Mt[:, g, :], pM[i][:, 0:128], maskf, ALU.mult, r=["pM%d" % i, "cst"], w=["Mt"])
                        b.tt("dve", mt[:], pM[i][:, 128:256], maskb, ALU.mult, r=["pM%d" % i, "cst"], w=["mtmp"])
                        b.tt("dve", Mt[:, g, :], Mt[:, g, :], mt[:], ALU.add, r=["Mt", "mtmp"], w=["Mt"])
                        b.stt(Mt[:, g, :], ident_f[:], Dsq[:, gb * 8 + g: gb * 8 + g + 1], Mt[:, g, :], ALU.mult, ALU.add, r=["ident_f", "Dsq", "Mt"], w=["Mt"])
                        for k_, (src, key) in enumerate(((XpR, "XpR"), (XpI, "XpI"), (X1R, "X1R"), (X1I, "X1I"))):
                            b.tr(pW[i][:, k_ * 64:(k_ + 1) * 64], f2(src, g), ident_f[0:64, 0:64], r=[key, "ident_f"], w=["pW%d" % i])
                        b.cp("act", Wt[:, g, :, :], pW[i][:, 0:256].rearrange("p (k n) -> p k n", k=4), r=["pW%d" % i], w=["Wt"])
                    S.flush()
                if "stop3a" in dbg:
                    return
                with ExitStack() as sb:
                    Tq = lambda name, shape, dt=F32: sb.enter_context(nc.sbuf_tensor("s_%s_q%d" % (name, gb), list(shape), dt))
                    Pq = lambda name, shape, dt=F32: (S.excl.add(name), sb.enter_context(nc.psum_tensor("p_%s_q%d" % (name, gb), list(shape), dt)))[1]
                    uTc = Tq("uTc", [128, NKEY])
                    U = Tq("U", [128, 8, 544])
                    SfR = Tq("SfR", [64, 8, 288]); SfI = Tq("SfI", [64, 8, 288])
                    SbR = Tq("SbR", [64, 8, 544]); SbI = Tq("SbI", [64, 8, 544])
                    Yg = Tq("Yg", [128, 8, 256], BF16)
                    ygx = [Tq("ygx%d" % i, [128, 256]) for i in range(2)]
                    ygt = [Tq("ygt%d" % i, [128, 256]) for i in range(2)]
                    pU = [Pq("pU%d" % i, [128, 512]) for i in range(2)]
                    pSt = [Pq("pSt%d" % i, [128, 512]) for i in range(2)]
                    pY = [Pq("pY%d" % i, [128, 512]) for i in range(2)]
                    b.dma("sp", uTc[:], scr["uT_s"][gb], w=["uTc"])
                    uTb = Tq("uTb", [128, NKEY], BF16)
                    b.cp("act", uTb[:], uTc[:], r=["uTc"], w=["uTb"])
                    uv = uTb[:].rearrange("p (c s) -> p c s", s=8)
                    n_ = 0
                    for g in range(8):
                        for hh in range(2):
                            i = n_ % 2
                            n_ += 1
                            for s_ in range(8):
                                b.mm(pU[i][:, 0:272], selcb[:, g, (7 - s_) * 16:(7 - s_) * 16 + 128], uv[:, hh * 272:(hh + 1) * 272, s_],
                                     start=(s_ == 0), stop=(s_ == 7), r=["selcb", "uTb"], w=["pU%d" % i])
                            b.cp("act" if i == 0 else "dve", U[:, g, hh * 272:(hh + 1) * 272], pU[i][:, 0:272], r=["pU%d" % i], w=["U"])
                    n_ = 0
                    for g in range(8):
                        for (k_, dst, c0, nn, key) in ((0, SfR, 0, 288, "SfR"), (1, SfI, 0, 288, "SfI"), (2, SbR, 0, 272, "SbR"), (2, SbR, 272, 272, "SbR"),
                                                       (3, SbI, 0, 272, "SbI"), (3, SbI, 272, 272, "SbI")):
                            i = n_ % 2
                            n_ += 1
                            b.mm(pSt[i][0:64, 0:nn], Wt[:, g, k_, :], U[:, g, c0:c0 + nn], r=["Wt", "U"], w=["pSt%d" % i])
                            b.cp("act" if i == 0 else "dve", dst[:, g, c0:c0 + nn], pSt[i][0:64, 0:nn], r=["pSt%d" % i], w=[key])
                    tA = Tq("tA", [64, 8, 34]); tB = Tq("tB", [64, 8, 34])
                    tC = Tq("tC", [64, 8, 34]); tD = Tq("tD", [64, 8, 34])
                    CIfR = Tq("CIfR", [64, 8, 18]); CIfI = Tq("CIfI", [64, 8, 18])
                    CIbR = Tq("CIbR", [64, 8, 34]); CIbI = Tq("CIbI", [64, 8, 34])

                    def cmac(e, dR, dI, cR, cI, xR, xI, ta_, tb2_, keys, tk):
                        b.tt(e, ta_, cR, xR, ALU.mult, r=keys, w=[tk + "a"])
                        b.tt(e, dR, dR, ta_, ALU.add, r=keys + [tk + "a"], w=keys[:1])
                        b.tt(e, ta_, cI, xI, ALU.mult, r=keys, w=[tk + "a"])
                        b.tt(e, dR, dR, ta_, ALU.subtract, r=keys + [tk + "a"], w=keys[:1])
                        b.tt(e, tb2_, cR, xI, ALU.mult, r=keys, w=[tk + "b"])
                        b.tt(e, dI, dI, tb2_, ALU.add, r=keys + [tk + "b"], w=keys[1:2])
                        b.tt(e, tb2_, cI, xR, ALU.mult, r=keys, w=[tk + "b"])
                        b.tt(e, dI, dI, tb2_, ALU.add, r=keys + [tk + "b"], w=keys[1:2])

                    def views(SR, SI, nb):
                        VR = SR[:, :, 0:nb * 16].rearrange("n g (b j) -> n g b j", j=16)
                        VI = SI[:, :, 0:nb * 16].rearrange("n g (b j) -> n g b j", j=16)
                        return VR, VI

                    def scan1(e, SR, SI, kR, kI, nb, asc, d, ta_, tb2_, tk):
                        VR, VI = views(SR, SI, nb)
                        a8R = bcl(pwaR[:, d * 8:(d + 1) * 8, 15], nb); a8I = bcl(pwaI[:, d * 8:(d + 1) * 8, 15], nb)
                        keys = [kR, kI, "pwa", "p8"]
                        for j in (range(1, 16) if asc else range(14, -1, -1)):
                            pj = j - 1 if asc else j + 1
                            cmac(e, VR[:, :, :, j], VI[:, :, :, j], a8R, a8I, VR[:, :, :, pj], VI[:, :, :, pj], ta_[:, :, 0:nb], tb2_[:, :, 0:nb], keys, tk)

                    def scan2(e, SR, SI, kR, kI, nb, asc, d, CIR, CII, ta_, tb2_, tk, order, cik):
                        VR, VI = views(SR, SI, nb)
                        last = 15 if asc else 0
                        a128R = p8R[:, d * 8:(d + 1) * 8, 15]; a128I = p8I[:, d * 8:(d + 1) * 8, 15]
                        keys = [kR, kI, "pwa", "p8", cik]
                        b.memset(e, CIR[:], 0.0, w=[cik])
                        b.memset(e, CII[:], 0.0, w=[cik])
                        for q_ in range(len(order) - 1):
                            cur, nxt = order[q_], order[q_ + 1]
                            b.tt(e, ta_[:, :, 0], a128R, CIR[:, :, cur], ALU.mult, r=keys, w=[tk + "a"])
                            b.tt(e, ta_[:, :, 1], a128I, CII[:, :, cur], ALU.mult, r=keys, w=[tk + "a"])
                            b.tt(e, tb2_[:, :, 0], a128R, CII[:, :, cur], ALU.mult, r=keys, w=[tk + "b"])
                            b.tt(e, tb2_[:, :, 1], a128I, CIR[:, :, cur], ALU.mult, r=keys, w=[tk + "b"])
                            b.tt(e, CIR[:, :, nxt], ta_[:, :, 0], ta_[:, :, 1], ALU.subtract, r=[tk + "a"] + keys, w=[cik])
                            b.tt(e, CII[:, :, nxt], tb2_[:, :, 0], tb2_[:, :, 1], ALU.add, r=[tk + "b"] + keys, w=[cik])
                            b.tt(e, CIR[:, :, nxt], CIR[:, :, nxt], VR[:, :, cur, last], ALU.add, r=keys, w=[cik])
                            b.tt(e, CII[:, :, nxt], CII[:, :, nxt], VI[:, :, cur, last], ALU.add, r=keys, w=[cik])

                    def scan3(e, SR, SI, kR, kI, nb, asc, d, CIR, CII, ta_, tb2_, tk, cik):
                        VR, VI = views(SR, SI, nb)
                        keys = [kR, kI, "pwa", "p8", cik]
                        for j in range(16):
                            pj = j if asc else 15 - j
                            cR = bcl(p8R[:, d * 8:(d + 1) * 8, pj], nb); cI = bcl(p8I[:, d * 8:(d + 1) * 8, pj], nb)
                            cmac(e, VR[:, :, :, j], VI[:, :, :, j], cR, cI, CIR[:, :, 0:nb], CII[:, :, 0:nb], ta_[:, :, 0:nb], tb2_[:, :, 0:nb], keys, tk)

                    border = [1, 0] + list(range(33, 1, -1))
                    scan1("dve", SfR, SfI, "SfR", "SfI", 18, True, 0, tA, tB, "sf")
                    scan1("pool", SbR, SbI, "SbR", "SbI", 34, False, 1, tC, tD, "sb")
                    scan2("dve", SfR, SfI, "SfR", "SfI", 18, True, 0, CIfR, CIfI, tA, tB, "sf", list(range(18)), "sfCI")
                    scan3("dve", SfR, SfI, "SfR", "SfI", 18, True, 0, CIfR, CIfI, tA, tB, "sf", "sfCI")
                    scan2("pool", SbR, SbI, "SbR", "SbI", 34, False, 1, CIbR, CIbI, tC, tD, "sb", border, "sbCI")
                    scan3("pool", SbR, SbI, "SbR", "SbI", 34, False, 1, CIbR, CIbI, tC, tD, "sb", "sbCI")
                    for g in range(8):
                        i = g % 2
                        b.mm(pY[i][:, 0:256], Mt[:, g, :], U[:, g, 32:288], start=True, stop=False, r=["Mt", "U"], w=["pY%d" % i])
                        b.mm(pY[i][:, 0:256], EfR[:, g, :], SfR[:, g, 31:287], start=False, stop=False, r=["Ef", "EfR", "SfR"], w=["pY%d" % i])
                        b.mm(pY[i][:, 0:256], EfI[:, g, :], SfI[:, g, 31:287], start=False, stop=False, r=["Ef", "EfI", "SfI"], w=["pY%d" % i])
                        b.mm(pY[i][:, 0:256], EbR[:, g, :], SbR[:, g, 33:289], start=False, stop=False, r=["Eb", "EbR", "SbR"], w=["pY%d" % i])
                        b.mm(pY[i][:, 0:256], EbI[:, g, :], SbI[:, g, 33:289], start=False, stop=True, r=["Eb", "EbI", "SbI"], w=["pY%d" % i])
                        if "s5_nogelu" in dbg:
                            b.cp("act", Yg[:, g, :], pY[i][:, 0:256], r=["pY%d" % i], w=["Yg"])
                        else:
                            b.cp("act", ygx[i][:], pY[i][:, 0:256], r=["pY%d" % i], w=["ygx%d" % i])
                            gelu_tanh(b, Yg[:, g, :], ygx[i][:], ygt[i][:], "ygx%d" % i, "ygt%d" % i, "Yg")
                    yv = ygT[:, gb, :].rearrange("p (c s) -> p c s", s=8)
                    for t8 in range(8):
                        i = t8 % 2
                        for g in range(8):
                            b.mm(pU[i][:, 0:256], selcb[:, t8, (7 - g) * 16:(7 - g) * 16 + 128], Yg[:, g, :], start=(g == 0), stop=(g == 7),
                                 r=["selcb", "Yg"], w=["pU%d" % i])
                        b.cp("act" if i == 0 else "dve", yv[:, :, t8], pU[i][:, 0:256], r=["pU%d" % i], w=["ygT"])
                    S.flush()
        if "ygT_d" in dbg:
            b.dma("sp", G["ygT_d"], ygT[:], r=["ygT"], w=["ygT_d"])
            S.flush()
            return
        with ExitStack() as sg:
            Tg = lambda name, shape, dt=F32: sg.enter_context(nc.sbuf_tensor("s_" + name, list(shape), dt))
            Pg = lambda name, shape, dt=F32: (S.excl.add(name), sg.enter_context(nc.psum_tensor("p_" + name, list(shape), dt)))[1]
            ygb = Tg("ygb", [128, 4, NOWN], BF16)
            wgf = Tg("wgf", [128, 4, 512]); wgb = Tg("wgb", [128, 4, 512], BF16)
            bgl = Tg("bgl", [128, 4])
            sig = [Tg("sig%d" % i, [128, 512]) for i in range(2)]
            ssmT = Tg("ssmT", [128, 4, NOWN], BF16)
            pZ = [Pg("pZ%d" % i, [128, 512]) for i in range(2)]
            b.dma("sp", wgf[:], io["w_glu"].rearrange("(j p) n -> p j n", p=128), w=["wgf"])
            b.dma("sp", bgl[:], io["b_glu"].rearrange("(c p) -> p c", p=128), w=["bgl"], slow=True)
            b.cp("pool", wgb[:], wgf[:], r=["wgf"], w=["wgb"])
            b.cp("dve", ygb[:], ygT[:], r=["ygT"], w=["ygb"])
            n_ = 0
            for oc in range(4):
                for tb2 in range(4):
                    i = n_ % 2
                    n_ += 1
                    for kc in range(4):
                        b.mm(pZ[i][:], wgb[:, kc, oc * 128:(oc + 1) * 128], ygb[:, kc, tb2 * 512:(tb2 + 1) * 512], start=(kc == 0), stop=(kc == 3),
                             r=["wgb", "ygb"], w=["pZ%d" % i])
                    b.act(sig[i][:], pZ[i][:], AF.Sigmoid, r=["pZ%d" % i, "bgl"], w=["sig%d" % i], bias=bgl[:, oc:oc + 1])
                    b.tt("dve", ssmT[:, oc, tb2 * 512:(tb2 + 1) * 512], ygT[:, oc, tb2 * 512:(tb2 + 1) * 512], sig[i][:], ALU.mult,
                         r=["ygT", "sig%d" % i], w=["ssmT"])
            b.dma("sp", scr["ssmT_s"][:, :, :], ssmT[:], r=["ssmT"], w=["ssmT_s"])
            S.flush()


def phase4(nc, S, b, io, scr, G):
    ident_b = G["ident_b"]
    with ExitStack() as st:
        T = lambda name, shape, dt=F32: st.enter_context(nc.sbuf_tensor("s_" + name, list(shape), dt))
        P = lambda name, shape, dt=F32: (S.excl.add(name), st.enter_context(nc.psum_tensor("p_" + name, list(shape), dt)))[1]
        wa = T("wa", [128, 8, D], BF16); ws = T("ws", [128, 4, D], BF16); wo = T("wo", [128, 8, D], BF16); wg = T("wg", [128, 8, 2048], BF16)
        stg = [T("stg%d" % i, [128, 8, 512]) for i in range(2)]
        g1row = T("g1row", [128, D])
        b.dma("sp", g1row[:], scr["vec_s"][0].partition_broadcast(128), w=["g1row"])
        n_ = 0
        for (src, dst, nj, ncol, key) in ((io["w_attn_up"], wa, 8, D, "wa"), (io["w_ssm_up"], ws, 4, D, "ws"), (io["w_out"], wo, 8, D, "wo"),
                                          (io["w_in"][:, 3584:5632], wg, 8, 2048, "wg")):
            for cb in range(ncol // 512):
                i = n_ % 2
                n_ += 1
                b.dma("sp" if i == 0 else "act", stg[i][:, 0:nj, :], src[:, cb * 512:(cb + 1) * 512].rearrange("(j p) n -> p j n", p=128), w=["stg%d" % i])
                b.cp("pool" if i == 0 else "dve", dst[:, :, cb * 512:(cb + 1) * 512], stg[i][:, 0:nj, :], r=["stg%d" % i], w=[key])
        xs_t = [T("xs_t%d" % i, [128, 8, 128], BF16) for i in range(2)]
        at_t = [T("at_t%d" % i, [128, 8, 128], BF16) for i in range(2)]
        ss_t = [T("ss_t%d" % i, [128, 4, 128], BF16) for i in range(2)]
        x_t = [T("x_t%d" % i, [128, D]) for i in range(2)]
        gs = T("gs", [128, 2048])
        m1 = T("m1", [128, D]); m2 = T("m2", [128, D]); mb = T("mb", [128, D], BF16)
        mT = T("mT", [128, 8, 128], BF16)
        x1 = [T("x1_%d" % i, [128, D]) for i in range(2)]
        pG = [P("pG%d" % i, [128, 512]) for i in range(2)]
        pA = [P("pA4_%d" % i, [128, 512]) for i in range(2)]
        pS = [P("pS4_%d" % i, [128, 512]) for i in range(2)]
        pTr = P("pTr4", [128, 1024], BF16)
        for ti in range(16):
            i = ti % 2
            cs = slice(ti * 128, (ti + 1) * 128)
            b.dma("sp", xs_t[i][:], scr["xnT_s"][:, :, cs], w=["xs_t%d" % i])
            b.dma("act", at_t[i][:], scr["attnT_s"][:, :, cs], w=["at_t%d" % i])
            b.dma("sp", ss_t[i][:], scr["ssmT_s"][:, :, cs], w=["ss_t%d" % i])
            b.dma("act", x_t[i][:], io["x_seq"][cs, :], w=["x_t%d" % i])
            for blk in range(4):
                pg = pG[blk % 2]
                kg = "pG%d" % (blk % 2)
                for j in range(8):
                    b.mm(pg[:], xs_t[i][:, j, :], wg[:, j, blk * 512:(blk + 1) * 512], start=(j == 0), stop=(j == 7), r=["xs_t%d" % i, "wg"], w=[kg])
                b.act(gs[:, blk * 512:(blk + 1) * 512], pg[:], AF.Sigmoid, r=[kg], w=["gs"])
            for hf in range(2):
                for j in range(8):
                    b.mm(pA[hf][:], at_t[i][:, j, :], wa[:, j, hf * 512:(hf + 1) * 512], start=(j == 0), stop=(j == 7), r=["at_t%d" % i, "wa"], w=["pA4_%d" % hf])
                for j in range(4):
                    b.mm(pS[hf][:], ss_t[i][:, j, :], ws[:, j, hf * 512:(hf + 1) * 512], start=(j == 0), stop=(j == 3), r=["ss_t%d" % i, "ws"], w=["pS4_%d" % hf])
                b.tt("dve", m1[:, hf * 512:(hf + 1) * 512], pA[hf][:], gs[:, hf * 512:(hf + 1) * 512], ALU.mult, r=["pA4_%d" % hf, "gs"], w=["m1"])
                b.tt("dve", m2[:, hf * 512:(hf + 1) * 512], pS[hf][:], gs[:, 1024 + hf * 512:1024 + (hf + 1) * 512], ALU.mult, r=["pS4_%d" % hf, "gs"], w=["m2"])
            b.tt("pool", mb[:], m1[:], m2[:], ALU.add, r=["m1", "m2"], w=["mb"])
            for j in range(8):
                b.tr(pTr[:, j * 128:(j + 1) * 128], mb[:, j * 128:(j + 1) * 128], ident_b[:], r=["mb", "ident_b"], w=["pTr4"])
            b.cp("act", mT[:], pTr[:].rearrange("p (j t) -> p j t", j=8), r=["pTr4"], w=["mT"])
            for hf in range(2):
                for j in range(8):
                    b.mm(pA[hf][:], mT[:, j, :], wo[:, j, hf * 512:(hf + 1) * 512], start=(j == 0), stop=(j == 7), r=["mT", "wo"], w=["pA4_%d" % hf])
                b.tt("dve", m1[:, hf * 512:(hf + 1) * 512], pA[hf][:], g1row[:, hf * 512:(hf + 1) * 512], ALU.mult, r=["pA4_%d" % hf, "g1row"], w=["m1"])
            b.tt("pool", x1[i][:], m1[:], x_t[i][:], ALU.add, r=["m1", "x_t%d" % i], w=["x1_%d" % i])
            b.dma("sp", scr["x1_s"][cs, :], x1[i][:], r=["x1_%d" % i], w=["x1_s"])
        S.flush()


def phase5(nc, S, b, io, scr, G, out):
    ident_f = G["ident_f"]
    dbg = G["dbg"]
    with ExitStack() as st:
        T = lambda name, shape, dt=F32: st.enter_context(nc.sbuf_tensor("s_" + name, list(shape), dt))
        P = lambda name, shape, dt=F32: (S.excl.add(name), st.enter_context(nc.psum_tensor("p_" + name, list(shape), dt)))[1]
        wq = T("wq", [128, 8, D])
        rows = T("rows5", [128, 4, D])
        k1T = T("k1T", [64, 128]); k2T = T("k2T", [128, 128])
        kk1 = T("kk1", [128, 64]); kk2 = T("kk2", [128, 128])
        iota16 = T("iota16", [128, 16])
        pX = P("pX", [128, 1024])
        pTQ = P("pTQ", [128, 1024])
        pSc = P("pSc", [128, 2048])
        b.dma("sp", wq[:], io["w_query"].rearrange("(j p) n -> p j n", p=128), w=["wq"])
        b.dma("act", rows[:, 0, :], scr["vec_s"][1].partition_broadcast(128), w=["rows5"])
        b.dma("act", rows[:, 1, :], scr["vec_s"][2].partition_broadcast(128), w=["rows5"])
        b.dma("act", rows[:, 2, :], scr["vec_s"][3].partition_broadcast(128), w=["rows5"])
        b.dma("act", rows[:, 3, :], io["final_g"].partition_broadcast(128), w=["rows5"])
        b.dma("sp", kk1[:], io["sub_k1"][:, :], w=["kk1"])
        b.memset("pool", kk2[:], 0.0, w=["kk2"])
        b.dma("sp", kk2[:, 64:128], io["sub_k2"][:, :], r=["kk2"], w=["kk2"])
        b.dma("sp", iota16[:], io["cst"][:, 320:336], w=["iota16"])
        b.tr(pTQ[0:64, 0:128], kk1[:], ident_f[:], r=["kk1", "ident_f"], w=["pTQ"])
        b.cp("act", k1T[:], pTQ[0:64, 0:128], r=["pTQ"], w=["k1T"])
        b.tr(pTQ[:, 128:256], kk2[:], ident_f[:], r=["kk2", "ident_f"], w=["pTQ"])
        b.cp("act", k2T[:], pTQ[:, 128:256], r=["pTQ"], w=["k2T"])
        x1 = [T("x1t%d" % i, [128, D]) for i in range(2)]
        xn2 = T("xn2", [128, D]); tmpf = T("tmpf", [128, D]); junk = T("junk5", [128, D])
        st5 = T("st5", [128, 8])
        xn2T = T("xn2T", [128, 8, 128]); qTs = T("qTs", [128, 8, 128])
        sc = T("sc", [128, 2, 8, 128]); wk = T("wk", [128, 256])
        v12 = T("v12", [128, 2, 8, 16]); i12 = T("i12", [128, 2, 8, 16], U32); i12f = T("i12f", [128, 2, 8, 16])
        cand = T("cand", [128, 8, 256]); tv = T("tv", [128, 8, 16]); tj = T("tj", [128, 8, 16], U32)
        ta = T("ta5", [128, 8, 16], I32); taf = T("taf", [128, 8, 16]); tbf = T("tbf", [128, 8, 16])
        eq = T("eq", [128, 8, 16, 16]); sel1 = T("sel1", [128, 8, 16]); sel2 = T("sel2", [128, 8, 16])
        idxf = T("idxf", [128, 128]); idx32 = T("idx32", [128, 128], I32)
        ge = T("ge", [128, 8, 16]); gsum = T("gsum", [128, 8]); gate = T("gate", [128, 128])
        actv = T("actv", [128, 128]); wv = T("wv", [128, 128])
        NS = 16
        uvb = [T("uvb%d" % i, [128, 2 * D], BF16) for i in range(NS)]
        vt = [T("vt%d" % i, [128, D], BF16) for i in range(3)]
        ident_b = G["ident_b"]
        osb = [T("osb5_%d" % i, [128, D]) for i in range(2)]
        acc = pSc[:, 0:1024]
        nu = 0
        nv = 0
        for ti in range(16):
            i = ti % 2
            cs = slice(ti * 128, (ti + 1) * 128)
            kx = "x1t%d" % i
            b.dma("sp", x1[i][:], scr["x1_s"][cs, :], w=[kx])
            b.act(junk[:], x1[i][:], AF.Square, r=[kx], w=["junk5", "st5a"], accum=st5[:, 0:1])
            b.ts("dve", st5[:, 1:2], st5[:, 0:1], 1.0 / D, EPS, ALU.mult, ALU.add, r=["st5a"], w=["st5b"])
            b.act(st5[:, 2:3], st5[:, 1:2], AF.Sqrt, r=["st5b"], w=["st5c"])
            b.recip(st5[:, 3:4], st5[:, 2:3], r=["st5c"], w=["st5d"])
            b.stt(tmpf[:], x1[i][:], st5[:, 3:4], rows[:, 1, :], ALU.mult, ALU.mult, r=[kx, "st5d", "rows5"], w=["tmpf"])
            b.tt("dve", xn2[:], tmpf[:], rows[:, 2, :], ALU.add, r=["tmpf", "rows5"], w=["xn2"])
            b.cp("act", pX[:], xn2[:], r=["xn2"], w=["pX"])
            for j in range(8):
                b.tr(pTQ[:, j * 128:(j + 1) * 128], xn2[:, j * 128:(j + 1) * 128], ident_f[:], r=["xn2", "ident_f"], w=["pTQ"])
            b.cp("act", xn2T[:], pTQ[:].rearrange("p (j t) -> p j t", j=8), r=["pTQ"], w=["xn2T"])
            for h in range(8):
                for j in range(8):
                    b.mm(pTQ[:, h * 128:(h + 1) * 128], wq[:, j, h * 128:(h + 1) * 128], xn2T[:, j, :], start=(j == 0), stop=(j == 7), r=["wq", "xn2T"], w=["pTQ"])
            b.cp("act", qTs[:], pTQ[:].rearrange("p (h t) -> p h t", h=8), r=["pTQ"], w=["qTs"])
            for h in range(8):
                b.mm(pSc[:, h * 128:(h + 1) * 128], qTs[0:64, h, :], k1T[:, :], r=["qTs", "k1T"], w=["pSc"])
            for h in range(8):
                b.mm(pSc[:, 1024 + h * 128:1024 + (h + 1) * 128], qTs[64:128, h, :], k2T[64:128, :], r=["qTs", "k2T"], w=["pSc"])
            b.cp("dve", sc[:, 0].rearrange("p h k -> p (h k)"), pSc[:, 0:1024], r=["pSc"], w=["sc"])
            b.cp("dve", sc[:, 1].rearrange("p h k -> p (h k)"), pSc[:, 1024:2048], r=["pSc"], w=["sc"])
            for h in range(8):
                for sd in range(2):
                    src = sc[:, sd, h, :]
                    b.S.I("dve", (lambda o=v12[:, sd, h, 0:8], s_=src: nc.vector.max(out=o, in_=s_)), r=["sc"], w=["v12"])
                    b.S.I("dve", (lambda o=i12[:, sd, h, 0:8], m=v12[:, sd, h, 0:8], s_=src: nc.vector.max_index(out=o, in_max=m, in_values=s_)), r=["sc", "v12"], w=["i12"])
                    b.S.I("dve", (lambda o=wk[:, 0:128], m=v12[:, sd, h, 0:8], s_=src: nc.vector.match_replace(out=o, in_to_replace=m, in_values=s_, imm_value=-1e30)), r=["sc", "v12"], w=["wk"])
                    b.S.I("dve", (lambda o=v12[:, sd, h, 8:16], s_=wk[:, 0:128]: nc.vector.max(out=o, in_=s_)), r=["wk"], w=["v12"])
                    b.S.I("dve", (lambda o=i12[:, sd, h, 8:16], m=v12[:, sd, h, 8:16], s_=wk[:, 0:128]: nc.vector.max_index(out=o, in_max=m, in_values=s_)), r=["wk", "v12"], w=["i12"])
            b.tt("dve", cand[:].rearrange("p h (a c) -> p h a c", a=16), bcl(v12[:, 0, :, :], 16), v12[:, 1, :, :].unsqueeze(2).to_broadcast([128, 8, 16, 16]),
                 ALU.add, r=["v12"], w=["cand"])
            for h in range(8):
                src = cand[:, h, :]
                b.S.I("dve", (lambda o=tv[:, h, 0:8], s_=src: nc.vector.max(out=o, in_=s_)), r=["cand"], w=["tv"])
                b.S.I("dve", (lambda o=tj[:, h, 0:8], m=tv[:, h, 0:8], s_=src: nc.vector.max_index(out=o, in_max=m, in_values=s_)), r=["cand", "tv"], w=["tj"])
                b.S.I("dve", (lambda o=wk[:, 0:256], m=tv[:, h, 0:8], s_=src: nc.vector.match_replace(out=o, in_to_replace=m, in_values=s_, imm_value=-1e30)), r=["cand", "tv"], w=["wk"])
                b.S.I("dve", (lambda o=tv[:, h, 8:16], s_=wk[:, 0:256]: nc.vector.max(out=o, in_=s_)), r=["wk"], w=["tv"])
                b.S.I("dve", (lambda o=tj[:, h, 8:16], m=tv[:, h, 8:16], s_=wk[:, 0:256]: nc.vector.max_index(out=o, in_max=m, in_values=s_)), r=["wk", "tv"], w=["tj"])
            b.cp("dve", tbf[:], tj[:], r=["tj"], w=["tbf"])
            b.ts("dve", taf[:], tbf[:], 1.0 / 16.0, None, ALU.mult, r=["tbf"], w=["taf"])
            b.cp("dve", ta[:], taf[:], r=["taf"], w=["ta5"])
            b.cp("dve", sel1[:], ta[:], r=["ta5"], w=["sel1"])
            b.tt("dve", sel2[:], taf[:], sel1[:], ALU.subtract, r=["taf", "sel1"], w=["sel2"])
            b.ts("dve", sel2[:], sel2[:], 0.0, None, ALU.is_lt, r=["sel2"], w=["sel2"])
            b.tt("dve", taf[:], sel1[:], sel2[:], ALU.subtract, r=["sel1", "sel2"], w=["taf"])
            b.stt(tbf[:], taf[:], -16.0, tbf[:], ALU.mult, ALU.add, r=["taf", "tbf"], w=["tbf"])
            b.cp("dve", i12f[:], i12[:], r=["i12"], w=["i12f"])
            io16 = iota16[:].unsqueeze(1).unsqueeze(1).to_broadcast([128, 8, 16, 16])
            for (pos, side, dst, key) in ((taf, 0, sel1, "sel1"), (tbf, 1, sel2, "sel2")):
                b.tt("dve", eq[:], bcl(pos[:], 16), io16, ALU.is_equal, r=["taf", "tbf", "iota16"], w=["eq"])
                b.tt("dve", eq[:], eq[:], i12f[:, side, :, :].unsqueeze(2).to_broadcast([128, 8, 16, 16]), ALU.mult, r=["eq", "i12f"], w=["eq"])
                b.red(dst[:], eq[:], ALU.add, r=["eq"], w=[key])
            b.stt(idxf[:].rearrange("p (h k) -> p h k", h=8), sel1[:], 128.0, sel2[:], ALU.mult, ALU.add, r=["sel1", "sel2"], w=["idxf"])
            b.cp("dve", idx32[:], idxf[:], r=["idxf"], w=["idx32"])
            b.tt("dve", ge[:], tv[:], bcl(tv[:, :, 0], 16), ALU.subtract, r=["tv"], w=["ge"])
            b.act(ge[:], ge[:], AF.Exp, r=["ge"], w=["ge"])
            b.red(gsum[:], ge[:], ALU.add, r=["ge"], w=["gsum"])
            b.recip(gsum[:], gsum[:], r=["gsum"], w=["gsum"])
            b.tt("dve", gate[:].rearrange("p (h k) -> p h k", h=8), ge[:], bcl(gsum[:], 16), ALU.mult, r=["ge", "gsum"], w=["gate"])
            if "stop5a" in dbg:
                b.dma("sp", G["idx_d"], idx32[:], r=["idx32"], w=["idx_d"])
                b.dma("sp", G["gate_d"], gate[:], r=["gate"], w=["gate_d"])
                b.dma("sp", G["sc_d"], sc[:].rearrange("p s h k -> p (s h k)"), r=["sc"], w=["sc_d"])
                S.flush()
                return
            for g8 in range(16):
                sls = []
                for k in range(8):
                    hk = g8 * 8 + k
                    sl = nu % NS
                    nu += 1
                    sls.append(sl)
                    S.D("pool", (lambda o=uvb[sl][:], ix=idx32[:, hk:hk + 1]: nc.gpsimd.indirect_dma_start(
                        out=o, out_offset=None, in_=scr["puv_b"].rearrange("e a d -> e (a d)"), in_offset=bass.IndirectOffsetOnAxis(ap=ix, axis=0))),
                        r=["idx32"], w=["uvb%d" % sl])
                    b.stt(junk[:], uvb[sl][:, 0:D], 1.0, pX[:], ALU.mult, ALU.mult, r=["uvb%d" % sl, "pX"], w=["junk5", "actv"], accum=actv[:, hk:hk + 1])
                cs8 = slice(g8 * 8, (g8 + 1) * 8)
                gelu_tanh(b, wv[:, cs8], actv[:, cs8], idxf[:, cs8], "actv", "idxf", "wv")
                b.tt("dve", wv[:, cs8], wv[:, cs8], gate[:, cs8], ALU.mult, r=["wv", "gate"], w=["wv"])
                for k in range(8):
                    hk = g8 * 8 + k
                    sl = sls[k]
                    s3 = nv % 3
                    nv += 1
                    b.act(vt[s3][:], uvb[sl][:, D:2 * D], AF.Copy, r=["uvb%d" % sl, "wv"], w=["vt%d" % s3], scale=wv[:, hk:hk + 1])
                    for hf in range(2):
                        b.mm(acc[:, hf * 512:(hf + 1) * 512], ident_b[:], vt[s3][:, hf * 512:(hf + 1) * 512], start=(hk == 0), stop=(hk == 127),
                             r=["ident_b", "vt%d" % s3], w=["pSc"])
            b.tt("dve", tmpf[:], acc, rows[:, 0, :], ALU.mult, r=["pSc", "rows5"], w=["tmpf"])
            b.tt("dve", tmpf[:], tmpf[:], x1[i][:], ALU.add, r=["tmpf", kx], w=["tmpf"])
            b.act(junk[:], tmpf[:], AF.Square, r=["tmpf"], w=["junk5", "st5e"], accum=st5[:, 4:5])
            b.ts("dve", st5[:, 5:6], st5[:, 4:5], 1.0 / D, EPS, ALU.mult, ALU.add, r=["st5e"], w=["st5f"])
            b.act(st5[:, 6:7], st5[:, 5:6], AF.Sqrt, r=["st5f"], w=["st5g"])
            b.recip(st5[:, 7:8], st5[:, 6:7], r=["st5g"], w=["st5h"])
            b.stt(osb[i][:], tmpf[:], st5[:, 7:8], rows[:, 3, :], ALU.mult, ALU.mult, r=["tmpf", "st5h", "rows5"], w=["osb5_%d" % i])
            b.dma("sp", out[cs, :], osb[i][:], r=["osb5_%d" % i], w=["out"])
        S.flush()


def host_constants(half):
    t = np.arange(NT)
    if half == 1:
        t = t[::-1]
    posr = (t // 64).astype(np.float32)
    posc = (t % 64).astype(np.float32)
    p = np.arange(128)
    posT = np.where(((p % 64) < 32)[:, None], posr[None, :], posc[None, :]).astype(np.float32)
    fidx = (p % 16).astype(np.float32)[:, None]
    prot = np.zeros((128, 128), np.float32)
    for m in range(128):
        if (m % 32) < 16:
            prot[m + 16, m] = -1.0
        else:
            prot[m - 16, m] = 1.0
    selc = np.zeros((128, 8, 240), np.float32)
    for a in range(8):
        for q in range(16):
            selc[a * 16 + q, a, 112 + q] = 1.0
    cst = np.zeros((128, 512), np.float32)
    sidx = np.arange(128) // 16
    cst[:, 0:128] = (sidx[:, None] <= sidx[None, :]).astype(np.float32)
    cst[:, 128:256] = (sidx[:, None] >= sidx[None, :]).astype(np.float32)
    cst[:, 256:272] = np.arange(-7, 9, dtype=np.float32)[None, :]
    cst[:, 272:288] = (8 - np.arange(16, dtype=np.float32))[None, :]
    cst[:, 288:304] = (8.0 * (np.arange(16, dtype=np.float32) + 1))[None, :]
    cst[:, 304] = 1.0
    cst[:, 320:336] = np.arange(16, dtype=np.float32)[None, :]
    return dict(posT=np.ascontiguousarray(posT), fidx=fidx, prot=prot, selc=selc, cst=cst)


def make_in_maps(inputs):
    g = lambda k: np.asarray(inputs[k], dtype=np.float32)
    maps = []
    for core in range(8):
        bi, half = core // 2, core % 2
        xs = g("x")[bi]
        cs = g("ctx")[bi]
        dsel = [0, 1]
        if half == 1:
            xs = xs[::-1]
            cs = cs[::-1]
            dsel = [1, 0]
        m = dict(
            x_seq=np.ascontiguousarray(xs), ctx_seq=np.ascontiguousarray(cs), c_vec=np.ascontiguousarray(g("c")[bi]), c_ctx=g("c_ctx"),
            ada_w=g("ada_w")[0], ada_b=g("ada_b")[0], norm1_g=g("norm1_g")[0], norm2_g=g("norm2_g")[0], w_in=g("w_in")[0],
            lam4=np.ascontiguousarray(np.stack([g("lambda_q1")[0], g("lambda_k1")[0], g("lambda_q2")[0], g("lambda_k2")[0]])),
            subln_g=g("subln_g")[0], w_attn_up=g("w_attn_up")[0],
            a_re=np.ascontiguousarray(g("ssm_a_re")[0][dsel]), a_im=np.ascontiguousarray(g("ssm_a_im")[0][dsel]),
            log_dt=np.ascontiguousarray(g("ssm_log_dt")[0][dsel]), b_re=np.ascontiguousarray(g("ssm_b_re")[0][dsel]),
            b_im=np.ascontiguousarray(g("ssm_b_im")[0][dsel]), c_re=np.ascontiguousarray(g("ssm_c_re")[0][dsel]),
            c_im=np.ascontiguousarray(g("ssm_c_im")[0][dsel]), ssm_d=g("ssm_d")[0], w_glu=g("w_glu")[0], b_glu=g("b_glu")[0],
            w_ssm_up=g("w_ssm_up")[0], w_out=g("w_out")[0], w_query=g("peer_w_query")[0], sub_k1=g("peer_sub_k1")[0],
            sub_k2=g("peer_sub_k2")[0], peer_u=g("peer_u")[0], peer_v=g("peer_v")[0], final_g=g("final_norm_g"),
        )
        m.update(host_constants(half))
        maps.append(m)
    return maps


def kernel(**inputs):
    nc = build_program()
    maps = make_in_maps(inputs)
    res = run_bass_kernel_spmd(nc, maps, core_ids=list(range(8)))
    outp = np.zeros((4, NT, D), np.float32)
    for core in range(8):
        bi, half = core // 2, core % 2
        o = np.asarray(res.results[core]["out"], dtype=np.float32)
        if half == 0:
            outp[bi, :NOWN] = o
        else:
            outp[bi, NOWN:] = o[::-1]
    return outp
```

```python
import math
from contextlib import ExitStack

import numpy as np
import concourse.bass as bass
import concourse.mybir as mybir
from concourse.bass_utils import run_bass_kernel_spmd

F32 = mybir.dt.float32
BF16 = mybir.dt.bfloat16
I32 = mybir.dt.int32
U32 = mybir.dt.uint32
AF = mybir.ActivationFunctionType
ALU = mybir.AluOpType
AX = mybir.AxisListType

ENG = ("pe", "dve", "act", "pool", "sp")
D = 1024
NT = 4096
NOWN = 2048
NCTX = 256
NKEY = NCTX + NT
EPS = 1e-6
LAM_INIT = 0.2
PI = math.pi


class Sched:
    def __init__(self, nc, stack, n_dma_sems=32):
        self.nc = nc
        self.eobj = {"pe": nc.tensor, "dve": nc.vector, "act": nc.scalar, "pool": nc.gpsimd, "sp": nc.sync}
        self.sem = {e: stack.enter_context(nc.semaphore("sem_" + e)) for e in ENG if e != "sp"}
        self.cnt = {e: 0 for e in ENG}
        self.dsem = [stack.enter_context(nc.semaphore("dsem%d" % i)) for i in range(n_dma_sems)]
        self.dval = [0] * n_dma_sems
        self.dnext = 0
        self.waited = {e: {} for e in ENG}
        self.ops = {e: [] for e in ENG}
        self.lastw = {}
        self.readers = {}
        self.ninstr = 0
        self.excl = set()
        sems = list(self.sem.values()) + self.dsem
        with nc.Block() as block:
            @block.sync
            def _(eng):
                for h in sems:
                    nc.sync.sem_clear(h)

    def _semh(self, semkey):
        return self.sem[semkey] if isinstance(semkey, str) else self.dsem[semkey[1]]

    def _wait(self, e, tok):
        semkey, val, teng = tok
        if self.waited[e].get(semkey, 0) >= val:
            return
        self.waited[e][semkey] = val
        h = self._semh(semkey)
        eo = self.eobj[e]
        self.ops[e].append(lambda: eo.wait_ge(h, val))

    def _deps(self, e, r, w):
        toks = []
        for k in r:
            t = self.lastw.get(k)
            if t is not None and not (t[2] == e and e == "pe"):
                toks.append(t)
            if k in self.excl:
                for t in self.readers.get(k, ()):
                    if t[2] != e:
                        toks.append(t)
        for k in w:
            t = self.lastw.get(k)
            if t is not None and not (t[2] == e and t[0] == e):
                toks.append(t)
            for t in self.readers.get(k, ()):
                if t[2] == e and t[0] == e:
                    continue
                toks.append(t)
        return toks

    def _record(self, tok, r, w):
        for k in r:
            self.readers.setdefault(k, []).append(tok)
        for k in w:
            self.lastw[k] = tok
            self.readers[k] = []
        self.ninstr += 1

    def I(self, e, fn, r=(), w=()):
        for t in self._deps(e, r, w):
            self._wait(e, t)
        self.cnt[e] += 1
        val = self.cnt[e]
        h = self.sem[e]
        self.ops[e].append(lambda: fn().then_inc(h, 1))
        tok = (e, val, e)
        self._record(tok, r, w)
        return tok

    def D(self, e, fn, r=(), w=()):
        for t in self._deps(e, r, w):
            self._wait(e, t)
        i = self.dnext
        self.dnext = (self.dnext + 1) % len(self.dsem)
        if self.dval[i] > 0:
            self._wait(e, (("d", i), self.dval[i], "dma"))
        self.dval[i] += 16
        val = self.dval[i]
        h = self.dsem[i]
        self.ops[e].append(lambda: fn().then_inc(h, 16))
        tok = (("d", i), val, "dma")
        self._record(tok, r, w)
        return tok

    def wait_all(self, e="sp"):
        for i in range(len(self.dsem)):
            if self.dval[i] > 0:
                self._wait(e, (("d", i), self.dval[i], "dma"))
        for e2 in ENG:
            if e2 != "sp" and e2 != e and self.cnt[e2] > 0:
                self._wait(e, (e2, self.cnt[e2], e2))

    def flush(self):
        self.wait_all("sp")
        ops = self.ops
        self.ops = {e: [] for e in ENG}
        with self.nc.Block() as block:
            @block.tensor
            def _(eng):
                for f in ops["pe"]:
                    f()

            @block.vector
            def _(eng):
                for f in ops["dve"]:
                    f()

            @block.scalar
            def _(eng):
                for f in ops["act"]:
                    f()

            @block.gpsimd
            def _(eng):
                for f in ops["pool"]:
                    f()

            @block.sync
            def _(eng):
                for f in ops["sp"]:
                    f()
        self.lastw = {}
        self.readers = {}


class Bld:
    def __init__(self, nc, S):
        self.nc = nc
        self.S = S
        self.e = {"dve": nc.vector, "pool": nc.gpsimd, "act": nc.scalar, "sp": nc.sync, "pe": nc.tensor}

    def mm(self, out, lhsT, rhs, start=True, stop=True, r=(), w=()):
        nc = self.nc
        return self.S.I("pe", lambda: nc.tensor.matmul(out, lhsT=lhsT, rhs=rhs, start=start, stop=stop), r=r, w=w)

    def tr(self, out, in_, ident, r=(), w=()):
        nc = self.nc
        return self.S.I("pe", lambda: nc.tensor.transpose(out=out, in_=in_, identity=ident), r=r, w=w)

    def act(self, out, in_, func, r=(), w=(), scale=None, bias=None, accum=None):
        nc = self.nc
        kw = {}
        if scale is not None:
            kw["scale"] = scale
        if bias is not None:
            kw["bias"] = bias
        if accum is not None:
            kw["accum_out"] = accum
        return self.S.I("act", lambda: nc.scalar.activation(out=out, in_=in_, func=func, **kw), r=r, w=w)

    def tt(self, e, out, in0, in1, op, r=(), w=()):
        eo = self.e[e]
        return self.S.I(e, lambda: eo.tensor_tensor(out=out, in0=in0, in1=in1, op=op), r=r, w=w)

    def ts(self, e, out, in0, s1, s2, op0, op1=None, r=(), w=(), accum=None):
        eo = self.e[e]
        kw = {}
        if op1 is not None:
            kw["op1"] = op1
        if accum is not None:
            kw["accum_out"] = accum
        return self.S.I(e, lambda: eo.tensor_scalar(out=out, in0=in0, scalar1=s1, scalar2=s2, op0=op0, **kw), r=r, w=w)

    def stt(self, out, in0, scalar, in1, op0, op1, r=(), w=(), accum=None):
        nc = self.nc
        kw = {}
        if accum is not None:
            kw["accum_out"] = accum
        return self.S.I("dve", lambda: nc.vector.scalar_tensor_tensor(out=out, in0=in0, scalar=scalar, in1=in1, op0=op0, op1=op1, **kw), r=r, w=w)

    def cp(self, e, out, in_, r=(), w=()):
        if e == "act":
            nc = self.nc
            return self.S.I("act", lambda: nc.scalar.copy(out=out, in_=in_), r=r, w=w)
        eo = self.e[e]
        return self.S.I(e, lambda: eo.tensor_copy(out=out, in_=in_), r=r, w=w)

    def memset(self, e, ap, val, w=()):
        eo = self.e[e]
        return self.S.I(e, lambda: eo.memset(ap, val), w=w)

    def dma(self, e, out, in_, r=(), w=(), slow=False):
        eo = self.e[e]
        if slow:
            return self.S.D(e, lambda: eo.dma_start(out=out, in_=in_, allow_slow_non_contiguous=True), r=r, w=w)
        return self.S.D(e, lambda: eo.dma_start(out=out, in_=in_), r=r, w=w)

    def red(self, out, in_, op, axis=AX.X, r=(), w=()):
        nc = self.nc
        return self.S.I("dve", lambda: nc.vector.tensor_reduce(out=out, in_=in_, axis=axis, op=op), r=r, w=w)

    def recip(self, out, in_, r=(), w=()):
        nc = self.nc
        return self.S.I("dve", lambda: nc.vector.reciprocal(out=out, in_=in_), r=r, w=w)


GELU_C = 2.0 * math.sqrt(2.0 / math.pi)


def gelu_tanh(b, out, x, t, kx, kt, ko):
    b.tt("dve", t, x, x, ALU.mult, r=[kx], w=[kt])
    b.ts("dve", t, t, 0.044715, 1.0, ALU.mult, ALU.add, r=[kt], w=[kt])
    b.tt("dve", t, t, x, ALU.mult, r=[kt, kx], w=[kt])
    b.act(t, t, AF.Sigmoid, r=[kt], w=[kt], scale=GELU_C)
    b.tt("dve", out, x, t, ALU.mult, r=[kx, kt], w=[ko])


def bcl(ap, n):
    sh = list(ap.shape)
    return ap.unsqueeze(len(sh)).to_broadcast(sh + [n])


def build_program(debug=()):
    nc = bass.Bass("TRN2", target_bir_lowering=False)
    dbg = set(debug)

    def din(name, shape, dt=F32):
        return nc.dram_tensor(name, list(shape), dt, kind="ExternalInput").ap()

    def dscr(name, shape, dt=F32):
        kind = "ExternalOutput" if name in dbg else "Internal"
        return nc.dram_tensor(name, list(shape), dt, kind=kind).ap()

    io = dict(
        x_seq=din("x_seq", [NT, D]), ctx_seq=din("ctx_seq", [NCTX, D]), c_vec=din("c_vec", [D]), c_ctx=din("c_ctx", [D]),
        ada_w=din("ada_w", [D, 6 * D]), ada_b=din("ada_b", [6 * D]), norm1_g=din("norm1_g", [D]), norm2_g=din("norm2_g", [D]),
        w_in=din("w_in", [D, 5632]), lam4=din("lam4", [4, 64]), subln_g=din("subln_g", [128]),
        w_attn_up=din("w_attn_up", [D, D]), a_re=din("a_re", [2, 32, 64]), a_im=din("a_im", [2, 32, 64]),
        log_dt=din("log_dt", [2, 32]), b_re=din("b_re", [2, 32, 64, 16]), b_im=din("b_im", [2, 32, 64, 16]),
        c_re=din("c_re", [2, 32, 16, 64]), c_im=din("c_im", [2, 32, 16, 64]), ssm_d=din("ssm_d", [512]),
        w_glu=din("w_glu", [512, 512]), b_glu=din("b_glu", [512]), w_ssm_up=din("w_ssm_up", [512, D]),
        w_out=din("w_out", [D, D]), w_query=din("w_query", [D, D]), sub_k1=din("sub_k1", [128, 64]),
        sub_k2=din("sub_k2", [128, 64]), peer_u=din("peer_u", [16384, D]), peer_v=din("peer_v", [16384, D]),
        final_g=din("final_g", [D]), posT=din("posT", [128, NT]), fidx=din("fidx", [128, 1]), prot=din("prot", [128, 128]),
        selc=din("selc", [128, 8, 240]), cst=din("cst", [128, 512]),
    )
    out = nc.dram_tensor("out", [NOWN, D], F32, kind="ExternalOutput").ap()
    scr = dict(
        vec_s=dscr("vec_s", [4, D]),
        qT_s=dscr("qT_s", [8, 2, 65, NOWN], BF16),
        kT_s=dscr("kT_s", [8, 2, 65, NKEY], BF16),
        v_s=dscr("v_s", [34, 128, 8, 130], BF16),
        uT_s=dscr("uT_s", [4, 128, NKEY]),
        xnT_s=dscr("xnT_s", [128, 8, NOWN], BF16),
        attnT_s=dscr("attnT_s", [128, 8, NOWN], BF16),
        ssmT_s=dscr("ssmT_s", [128, 4, NOWN], BF16),
        x1_s=dscr("x1_s", [NOWN, D]),
        puv_b=dscr("puv_b", [16384, 2, D], BF16),
    )

    with ExitStack() as st:
        S = Sched(nc, st)
        b = Bld(nc, S)
        T = lambda name, shape, dt=F32: st.enter_context(nc.sbuf_tensor("s_" + name, list(shape), dt))
        G = {}
        G["ident_f"] = T("ident_f", [128, 128])
        G["ident_b"] = T("ident_b", [128, 128], BF16)
        G["vecT"] = T("vecT", [128, 10, 8])
        G["lam"] = T("lam", [128, 4])
        G["dbg"] = dbg
        if "stop5a" in dbg:
            G["idx_d"] = nc.dram_tensor("idx_d", [128, 128], I32, kind="ExternalOutput").ap()
            G["gate_d"] = nc.dram_tensor("gate_d", [128, 128], F32, kind="ExternalOutput").ap()
            G["sc_d"] = nc.dram_tensor("sc_d", [128, 2048], F32, kind="ExternalOutput").ap()
        if "ygT_d" in dbg:
            G["ygT_d"] = nc.dram_tensor("ygT_d", [128, 4, NOWN], F32, kind="ExternalOutput").ap()
        phase0(nc, S, b, io, scr, G)
        if "stop0" in dbg:
            return nc
        if "only5" in dbg:
            x1_in = nc.dram_tensor("x1_in", [NOWN, D], F32, kind="ExternalInput").ap()
            with nc.sbuf_tensor("s_cpy", [128, 16, D], F32) as cpy:
                b.dma("sp", cpy[:], x1_in.rearrange("(t p) d -> p t d", p=128), w=["cpy"])
                b.dma("sp", scr["x1_s"].rearrange("(t p) d -> p t d", p=128), cpy[:], r=["cpy"], w=["x1_s"])
                S.flush()
            phase5(nc, S, b, io, scr, G, out)
            return nc
        phase1(nc, S, b, io, scr, G)
        if "stop1" in dbg:
            return nc
        if "skip2" not in dbg:
            phase2(nc, S, b, io, scr, G)
        if "stop2" in dbg:
            return nc
        if "skip3" not in dbg:
            phase3(nc, S, b, io, scr, G)
        if "stop3" in dbg:
            return nc
        phase4(nc, S, b, io, scr, G)
        if "stop4" in dbg:
            return nc
        phase5(nc, S, b, io, scr, G, out)
    return nc


V_SCALE1, V_SHIFT1, V_SCALE1C, V_SHIFT1C, V_SCALE2, V_SHIFT2, V_G1, V_G2, V_N1G, V_N2G = range(10)
R_G1, R_G2, R_SCALE2, R_SHIFT2, R_FING = range(5)


def phase0(nc, S, b, io, scr, G):
    with ExitStack() as st:
        T = lambda name, shape, dt=F32: st.enter_context(nc.sbuf_tensor("s_" + name, list(shape), dt))
        P = lambda name, shape, dt=F32: (S.excl.add(name), st.enter_context(nc.psum_tensor("p_" + name, list(shape), dt)))[1]
        ident_f, ident_b, vecT, lam = G["ident_f"], G["ident_b"], G["vecT"], G["lam"]
        b.memset("pool", ident_f[:], 0.0, w=["ident_f"])
        S.I("pool", lambda: nc.gpsimd.affine_select(out=ident_f[:], in_=ident_f[:], pattern=[[-1, 128]], compare_op=ALU.not_equal,
                                                    fill=1.0, base=0, channel_multiplier=1), r=["ident_f"], w=["ident_f"])
        b.cp("pool", ident_b[:], ident_f[:], r=["ident_f"], w=["ident_b"])

        cT = T("cT", [128, 8, 2])
        b.dma("sp", cT[:, :, 0], io["c_vec"].rearrange("(j p) -> p j", p=128), w=["cT"], slow=True)
        b.dma("sp", cT[:, :, 1], io["c_ctx"].rearrange("(j p) -> p j", p=128), w=["cT"], slow=True)
        b.act(cT[:], cT[:], AF.Silu, r=["cT"], w=["cT"])
        adabT = T("adabT", [128, 48])
        b.dma("sp", adabT[:], io["ada_b"].rearrange("(c p) -> p c", p=128), w=["adabT"], slow=True)
        b.dma("sp", vecT[:, V_N1G, :], io["norm1_g"].rearrange("(j p) -> p j", p=128), w=["n1g"], slow=True)
        b.dma("sp", vecT[:, V_N2G, :], io["norm2_g"].rearrange("(j p) -> p j", p=128), w=["n2g"], slow=True)
        aw = [T("aw%d" % i, [128, 8, D]) for i in range(2)]
        modps = P("modps", [128, 48, 2])
        modT = T("modT", [128, 48, 2])
        for pc in range(6):
            sl = pc % 2
            b.dma("sp" if pc % 2 == 0 else "act", aw[sl][:], io["ada_w"][:, pc * D:(pc + 1) * D].rearrange("(j p) n -> p j n", p=128), w=["aw%d" % sl])
            for cc in range(8):
                for j in range(8):
                    b.mm(modps[:, pc * 8 + cc, :], aw[sl][:, j, cc * 128:(cc + 1) * 128], cT[:, j, :], start=(j == 0), stop=(j == 7),
                         r=["aw%d" % sl, "cT"], w=["modps"])
        b.tt("dve", modT[:], modps[:], bcl(adabT[:], 2), ALU.add, r=["modps", "adabT"], w=["modT"])
        b.stt(vecT[:, V_SCALE1, :], modT[:, 8:16, 0], 1.0, vecT[:, V_N1G, :], ALU.add, ALU.mult, r=["modT", "n1g"], w=["v_scale1"])
        b.stt(vecT[:, V_SCALE1C, :], modT[:, 8:16, 1], 1.0, vecT[:, V_N1G, :], ALU.add, ALU.mult, r=["modT", "n1g"], w=["v_scale1c"])
        b.stt(vecT[:, V_SCALE2, :], modT[:, 32:40, 0], 1.0, vecT[:, V_N2G, :], ALU.add, ALU.mult, r=["modT", "n2g"], w=["v_scale2"])
        b.cp("dve", vecT[:, V_SHIFT1, :], modT[:, 0:8, 0], r=["modT"], w=["v_shift1"])
        b.cp("dve", vecT[:, V_SHIFT1C, :], modT[:, 0:8, 1], r=["modT"], w=["v_shift1c"])
        b.cp("dve", vecT[:, V_SHIFT2, :], modT[:, 24:32, 0], r=["modT"], w=["v_shift2"])
        b.cp("dve", vecT[:, V_G1, :], modT[:, 16:24, 0], r=["modT"], w=["v_g1"])
        b.cp("dve", vecT[:, V_G2, :], modT[:, 40:48, 0], r=["modT"], w=["v_g2"])
        for i, (slot, key) in enumerate([(V_G1, "v_g1"), (V_G2, "v_g2"), (V_SCALE2, "v_scale2"), (V_SHIFT2, "v_shift2")]):
            b.dma("sp", scr["vec_s"][i].rearrange("(j p) -> p j", p=128), vecT[:, slot, :], r=[key], w=["vec_s%d" % i], slow=True)
        l4 = T("l4", [128, 4, 64])
        b.dma("sp", l4[:], io["lam4"].rearrange("a k -> (a k)").partition_broadcast(128).rearrange("p (a k) -> p a k", a=4), w=["l4"])
        lt = T("lt", [128, 2, 64])
        b.tt("dve", lt[:, 0, :], l4[:, 0, :], l4[:, 1, :], ALU.mult, r=["l4"], w=["lt"])
        b.tt("dve", lt[:, 1, :], l4[:, 2, :], l4[:, 3, :], ALU.mult, r=["l4"], w=["lt"])
        b.red(lam[:, 0:2], lt[:], ALU.add, r=["lt"], w=["lam"])
        b.act(lam[:, 0:2], lam[:, 0:2], AF.Exp, r=["lam"], w=["lam"])
        b.stt(lam[:, 2:3], lam[:, 0:1], LAM_INIT, lam[:, 1:2], ALU.add, ALU.subtract, r=["lam"], w=["lam2"])
        b.ts("dve", lam[:, 3:4], lam[:, 2:3], -1.0, None, ALU.mult, r=["lam2"], w=["lam3"])
        S.flush()


def phase1(nc, S, b, io, scr, G):
    ident_f, ident_b, vecT = G["ident_f"], G["ident_b"], G["vecT"]
    with ExitStack() as st:
        T = lambda name, shape, dt=F32: st.enter_context(nc.sbuf_tensor("s_" + name, list(shape), dt))
        P = lambda name, shape, dt=F32: (S.excl.add(name), st.enter_context(nc.psum_tensor("p_" + name, list(shape), dt)))[1]
        xnT = T("xnT", [128, 8, NKEY], BF16)
        with ExitStack() as st2:
            T2 = lambda name, shape, dt=F32: st2.enter_context(nc.sbuf_tensor("s_" + name, list(shape), dt))
            P2 = lambda name, shape, dt=F32: (S.excl.add(name), st2.enter_context(nc.psum_tensor("p_" + name, list(shape), dt)))[1]
            xt = [T2("xt%d" % i, [128, D]) for i in range(2)]
            xs = [T2("xs%d" % i, [128, D]) for i in range(2)]
            junk = T2("junk", [128, D])
            ss = [T2("ss%d" % i, [128, 4]) for i in range(2)]
            pT = [P2("pT%d" % i, [128, 8, 128]) for i in range(2)]
            tmp = [T2("tmp%d" % i, [128, 8, 128]) for i in range(2)]
            for ti in range(34):
                sl = ti % 2
                src = io["ctx_seq"][ti * 128:(ti + 1) * 128, :] if ti < 2 else io["x_seq"][(ti - 2) * 128:(ti - 1) * 128, :]
                vs, vh = (V_SCALE1C, V_SHIFT1C) if ti < 2 else (V_SCALE1, V_SHIFT1)
                ks, kh = ("v_scale1c", "v_shift1c") if ti < 2 else ("v_scale1", "v_shift1")
                b.dma("sp" if sl == 0 else "act", xt[sl][:], src, w=["xt%d" % sl])
                b.act(junk[:], xt[sl][:], AF.Square, r=["xt%d" % sl], w=["junk", "ss%d" % sl], accum=ss[sl][:, 0:1])
                b.ts("dve", ss[sl][:, 1:2], ss[sl][:, 0:1], 1.0 / D, EPS, ALU.mult, ALU.add, r=["ss%d" % sl], w=["ssb%d" % sl])
                b.act(ss[sl][:, 2:3], ss[sl][:, 1:2], AF.Sqrt, r=["ssb%d" % sl], w=["ssc%d" % sl])
                b.recip(ss[sl][:, 3:4], ss[sl][:, 2:3], r=["ssc%d" % sl], w=["ssd%d" % sl])
                b.act(xs[sl][:], xt[sl][:], AF.Copy, r=["xt%d" % sl, "ssd%d" % sl], w=["xs%d" % sl], scale=ss[sl][:, 3:4])
                for j in range(8):
                    b.tr(pT[sl][:, j, :], xs[sl][:, j * 128:(j + 1) * 128], ident_f[:], r=["xs%d" % sl], w=["pT%d" % sl])
                b.tt("dve", tmp[sl][:], pT[sl][:], bcl(vecT[:, vs, :], 128), ALU.mult, r=["pT%d" % sl, ks], w=["tmp%d" % sl])
                b.tt("pool", xnT[:, :, ti * 128:(ti + 1) * 128], tmp[sl][:], bcl(vecT[:, vh, :], 128), ALU.add, r=["tmp%d" % sl, kh], w=["xnT"])
            b.dma("sp", scr["xnT_s"][:, :, :], xnT[:, :, NCTX:NCTX + NOWN], r=["xnT"], w=["xnT_s"])
            S.flush()
        if "stop1a" in G["dbg"]:
            return
        cosT = T("cosT", [128, NT])
        sinT = T("sinT", [128, NT])
        with ExitStack() as st2:
            T2 = lambda name, shape, dt=F32: st2.enter_context(nc.sbuf_tensor("s_" + name, list(shape), dt))
            ang = T2("ang", [128, NT])
            y = T2("y", [128, NT])
            yi = T2("yi", [128, NT], I32)
            fi = T2("fi", [128, 2])
            b.dma("sp", ang[:], io["posT"][:, :], w=["ang"])
            b.dma("sp", fi[:, 0:1], io["fidx"][:, :], w=["fi"])
            b.act(fi[:, 1:2], fi[:, 0:1], AF.Exp, r=["fi"], w=["inv"], scale=-math.log(10000.0) / 16.0)
            b.ts("dve", ang[:], ang[:], fi[:, 1:2], None, ALU.mult, r=["ang", "inv"], w=["ang"])
            for tab, off, key in ((sinT, 0.5, "sinT"), (cosT, 0.75, "cosT")):
                b.ts("dve", y[:], ang[:], 1.0 / (2 * PI), off, ALU.mult, ALU.add, r=["ang"], w=["y"])
                b.cp("dve", yi[:], y[:], r=["y"], w=["yi"])
                b.cp("dve", tab[:], yi[:], r=["yi"], w=[key])
                b.tt("dve", y[:], y[:], tab[:], ALU.subtract, r=["y", key], w=["y"])
                b.ts("dve", tab[:], y[:], 0.0, None, ALU.is_lt, r=["y"], w=[key])
                b.tt("dve", y[:], y[:], tab[:], ALU.add, r=["y", key], w=["y"])
                b.ts("dve", y[:], y[:], 2 * PI, -PI, ALU.mult, ALU.add, r=["y"], w=["y"])
                b.ts("dve", y[:], y[:], 3.1415925, -3.1415925, ALU.min, ALU.max, r=["y"], w=["y"])
                b.act(tab[:], y[:], AF.Sin, r=["y"], w=[key])
            S.flush()
        if "stop1r" in G["dbg"]:
            return
        prot = T("prot", [128, 128], BF16)
        protf = T("protf", [128, 128])
        b.dma("sp", protf[:], io["prot"][:, :], w=["protf"])
        b.cp("dve", prot[:], protf[:], r=["protf"], w=["prot"])
        bones = T("bones", [128, 2], BF16)
        b.memset("pool", bones[:], 0.0, w=["bones"])
        b.memset("pool", bones[0:64, 0:1], 1.0, w=["bones"])
        b.memset("pool", bones[64:128, 1:2], 1.0, w=["bones"])
        onesrow = T("onesrow", [16, NKEY], BF16)
        b.memset("pool", onesrow[:], 1.0, w=["onesrow"])
        if "no_ones" not in G["dbg"]:
            b.dma("sp", scr["kT_s"][:, :, 64, :].rearrange("h m c -> (h m) c"), onesrow[:], r=["onesrow"], w=["kT_s64"])
        wf = [T("wf%d" % i, [128, 8, 512]) for i in range(2)]
        wb = [T("wb%d" % i, [128, 8, 512], BF16) for i in range(2)]
        pA = [P("pA%d" % i, [128, 512]) for i in range(2)]
        pB = [P("pB%d" % i, [128, 512]) for i in range(2)]
        pN = [P("pN%d" % i, [2, 512]) for i in range(2)]
        asb = [T("asb%d" % i, [128, 512], BF16) for i in range(2)]
        t1 = [T("t1_%d" % i, [128, 512]) for i in range(2)]
        t2 = [T("t2_%d" % i, [128, 512]) for i in range(2)]
        kr = [T("kr%d" % i, [128, 512], BF16) for i in range(3)]
        sq = [T("sq%d" % i, [128, 512], BF16) for i in range(2)]
        kmx = T("kmx", [2, 8, 10])
        negk = T("negk", [2, 8])
        nrow = [T("nrow%d" % i, [2, 512]) for i in range(2)]
        nrowb = [T("nrowb%d" % i, [2, 512], BF16) for i in range(2)]
        vsb = [T("vsb%d" % i, [128, 8, 130], BF16) for i in range(2)]
        usb = [T("usb%d" % i, [128, 512]) for i in range(2)]
        for i in range(2):
            b.memset("pool", vsb[i][:], 0.0, w=["vsb%d" % i])
            b.memset("pool", vsb[i][:, :, 128:129], 1.0, w=["vsb%d" % i])
        b.memset("pool", kmx[:], 0.0, w=["kmx"])
        cnt = {"w": 0, "t": 0, "v": 0, "u": 0, "kr": 0}
        if "stop1s" in G["dbg"]:
            S.flush()
            return

        def load_w(cb):
            sl = cnt["w"] % 2
            cnt["w"] += 1
            b.dma("sp", wf[sl][:], io["w_in"][:, cb * 512:(cb + 1) * 512].rearrange("(j p) n -> p j n", p=128), w=["wf%d" % sl])
            b.cp("dve" if "w_cast_dve" in G["dbg"] else "pool", wb[sl][:], wf[sl][:], r=["wf%d" % sl], w=["wb%d" % sl])
            return sl

        def qk_tile(wsl, ch, col0, ncol, tok0, rope, is_q, h):
            i = cnt["t"] % 2
            cnt["t"] += 1
            for j in range(8):
                b.mm(pA[i][:, :ncol], wb[wsl][:, j, ch * 128:(ch + 1) * 128], xnT[:, j, col0:col0 + ncol], start=(j == 0), stop=(j == 7),
                     r=["wb%d" % wsl, "xnT"], w=["pA%d" % i])
            ki = cnt["kr"] % 3
            cnt["kr"] += 1
            if rope and "no_rope_ops" not in G["dbg"]:
                b.cp("act", asb[i][:, :ncol], pA[i][:, :ncol], r=["pA%d" % i], w=["asb%d" % i])
                b.mm(pB[i][:, :ncol], prot[:], asb[i][:, :ncol], r=["prot", "asb%d" % i], w=["pB%d" % i])
                b.tt("dve", t1[i][:, :ncol], pA[i][:, :ncol], cosT[:, tok0:tok0 + ncol], ALU.mult, r=["pA%d" % i, "cosT"], w=["t1_%d" % i])
                b.tt("dve", t2[i][:, :ncol], pB[i][:, :ncol], sinT[:, tok0:tok0 + ncol], ALU.mult, r=["pB%d" % i, "sinT"], w=["t2_%d" % i])
                b.tt("pool", kr[ki][:, :ncol], t1[i][:, :ncol], t2[i][:, :ncol], ALU.add, r=["t1_%d" % i, "t2_%d" % i], w=["kr%d" % ki])
            else:
                b.cp("act", kr[ki][:, :ncol], pA[i][:, :ncol], r=["pA%d" % i], w=["kr%d" % ki])
            if "no_norm" not in G["dbg"]:
                b.act(sq[i][:, :ncol], kr[ki][:, :ncol], AF.Square, r=["kr%d" % ki], w=["sq%d" % i])
                b.mm(pN[i][:, :ncol], bones[:], sq[i][:, :ncol], r=["bones", "sq%d" % i], w=["pN%d" % i])
            return i, ki

        for cb in (2, 3):
            wsl = load_w(cb)
            for ch in range(4):
                h = (cb - 2) * 4 + ch
                blocks = [(0, NCTX, 0, False)] + [(NCTX + tb * 512, 512, tb * 512, True) for tb in range(8)]
                for bi, (col0, ncol, tok0, rope) in enumerate(blocks):
                    i, ki = qk_tile(wsl, ch, col0, ncol, tok0, rope, False, h)
                    if "no_norm" not in G["dbg"]:
                        b.red(kmx[:, h, bi:bi + 1], pN[i][:, :ncol], ALU.max, r=["pN%d" % i], w=["kmx"])
                    for m in range(2 if "no_kstore" not in G["dbg"] else 0):
                        b.dma("sp", scr["kT_s"][h, m, 0:64, col0:col0 + ncol], kr[ki][m * 64:(m + 1) * 64, :ncol], r=["kr%d" % ki], w=["kT_s"])
        if "stop1k" in G["dbg"]:
            S.flush()
            return
        b.red(negk[:], kmx[:], ALU.max, r=["kmx"], w=["negk"])
        b.act(negk[:], negk[:], AF.Sqrt, r=["negk"], w=["negk"])
        b.ts("dve", negk[:], negk[:], -1.0, None, ALU.mult, r=["negk"], w=["negk"])
        for cb in (0, 1):
            wsl = load_w(cb)
            for ch in range(4):
                h = cb * 4 + ch
                for tb in range(4):
                    i, ki = qk_tile(wsl, ch, NCTX + tb * 512, 512, tb * 512, True, True, h)
                    b.act(nrow[i][:], pN[i][:], AF.Sqrt, r=["pN%d" % i], w=["nrow%d" % i])
                    b.ts("dve", nrowb[i][:], nrow[i][:], negk[:, h:h + 1], None, ALU.mult, r=["nrow%d" % i, "negk"], w=["nrowb%d" % i])
                    b.dma("sp", scr["qT_s"][h, :, 64, tb * 512:(tb + 1) * 512], nrowb[i][:], r=["nrowb%d" % i], w=["qT_s"])
                    for m in range(2):
                        b.dma("sp", scr["qT_s"][h, m, 0:64, tb * 512:(tb + 1) * 512], kr[ki][m * 64:(m + 1) * 64, :], r=["kr%d" % ki], w=["qT_s"])
        if "stop1q" in G["dbg"]:
            S.flush()
            return
        wv = [load_w(4), load_w(5)]
        for ti in range(34):
            i = cnt["v"] % 2
            cnt["v"] += 1
            for half in range(2):
                pt = pA[half]
                for j in range(8):
                    b.mm(pt[:], xnT[:, j, ti * 128:(ti + 1) * 128], wb[wv[half]][:, j, :], start=(j == 0), stop=(j == 7),
                         r=["xnT", "wb%d" % wv[half]], w=["pA%d" % half])
                b.cp("act" if half == 0 else "dve", vsb[i][:, half * 4:(half + 1) * 4, 0:128], pt[:].rearrange("p (h e) -> p h e", h=4),
                     r=["pA%d" % half], w=["vsb%d" % i])
            b.dma("sp", scr["v_s"][ti], vsb[i][:], r=["vsb%d" % i], w=["v_s"])
        if "stop1v" in G["dbg"]:
            S.flush()
            return
        wsl = load_w(6)
        blocks = [(0, NCTX)] + [(NCTX + tb * 512, 512) for tb in range(8)]
        for ch in range(4):
            for (col0, ncol) in blocks:
                i = cnt["u"] % 2
                cnt["u"] += 1
                for j in range(8):
                    b.mm(pB[i][:, :ncol], wb[wsl][:, j, ch * 128:(ch + 1) * 128], xnT[:, j, col0:col0 + ncol], start=(j == 0), stop=(j == 7),
                         r=["wb%d" % wsl, "xnT"], w=["pB%d" % i])
                b.cp("act", usb[i][:, :ncol], pB[i][:, :ncol], r=["pB%d" % i], w=["usb%d" % i])
                b.dma("sp", scr["uT_s"][ch, :, col0:col0 + ncol], usb[i][:, :ncol], r=["usb%d" % i], w=["uT_s"])
        S.flush()


def phase2(nc, S, b, io, scr, G):
    ident_b, lam = G["ident_b"], G["lam"]
    with ExitStack() as st:
        T = lambda name, shape, dt=F32: st.enter_context(nc.sbuf_tensor("s_" + name, list(shape), dt))
        P = lambda name, shape, dt=F32: (S.excl.add(name), st.enter_context(nc.psum_tensor("p_" + name, list(shape), dt)))[1]
        kT = [T("kT%d" % i, [65, 2, NKEY], BF16) for i in range(2)]
        qT = [T("qT%d" % i, [65, 2, NOWN], BF16) for i in range(2)]
        vv = [T("vv%d" % i, [128, 34, 130], BF16) for i in range(2)]
        E = [T("E%d" % i, [128, 512], BF16) for i in range(4)]
        pS = [P("pS%d" % i, [128, 512]) for i in range(3)]
        pO = [P("pO%d" % i, [128, 512]) for i in range(4)]
        pTr = P("pTr", [128, 1024], BF16)
        osb = [T("osb%d" % i, [128, 130]) for i in range(4)]
        attnT = T("attnT", [128, 8, NOWN], BF16)
        gsub = T("gsub", [128, 128])
        sm = [T("sm%d" % i, [128, 8]) for i in range(2)]
        ot = [T("ot%d" % i, [128, 128]) for i in range(2)]
        ob = [T("ob%d" % i, [128, 128], BF16) for i in range(2)]
        junk = T("junk2", [128, 128])
        cvf = [T("cvf%d" % i, [128, 4, D]) for i in range(2)]
        cvb = [T("cvb%d" % i, [128, 4, D], BF16) for i in range(2)]
        cvc = [0]

        def convert_chunk():
            c = cvc[0]
            cvc[0] += 1
            if c >= 64:
                return
            i = c % 2
            src = io["peer_u"] if c < 32 else io["peer_v"]
            dst = scr["puv_b"][:, 0 if c < 32 else 1, :]
            r0 = (c % 32) * 512
            b.dma("sp", cvf[i][:], src[r0:r0 + 512, :].rearrange("(p j) d -> p j d", j=4), w=["cvf%d" % i])
            b.cp("pool", cvb[i][:], cvf[i][:], r=["cvf%d" % i], w=["cvb%d" % i])
            b.dma("sp", dst[r0:r0 + 512, :].rearrange("(p j) d -> p j d", j=4), cvb[i][:], r=["cvb%d" % i], w=["pub"])

        b.dma("sp", gsub[:], io["subln_g"].partition_broadcast(128), w=["gsub"])
        b.ts("dve", gsub[:], gsub[:], 1.0 - LAM_INIT, None, ALU.mult, r=["gsub"], w=["gsub"])
        ecnt = 0

        def load_head(h):
            sl = h % 2
            b.dma("sp", kT[sl][:], scr["kT_s"][h].rearrange("m r c -> r m c"), w=["kT%d" % sl])
            b.dma("act", qT[sl][:], scr["qT_s"][h].rearrange("m r c -> r m c"), w=["qT%d" % sl])
            b.dma("sp", vv[sl][:], scr["v_s"][:, :, h, :].rearrange("k p e -> p k e"), w=["vv%d" % sl])

        pend_epi = [None]
        epic = [0]

        def epilogue(h, qg):
            for qb in range(2):
                e2 = epic[0] % 2
                epic[0] += 1
                o1, o2 = osb[qb], osb[2 + qb]
                k1, k2 = "osb%d" % qb, "osb%d" % (2 + qb)
                s_ = sm[e2]
                ks = "sm%d" % e2
                b.recip(s_[:, 0:1], o1[:, 128:129], r=[k1], w=[ks + "a"])
                b.recip(s_[:, 1:2], o2[:, 128:129], r=[k2], w=[ks + "b"])
                b.tt("dve", s_[:, 2:3], s_[:, 1:2], lam[:, 3:4], ALU.mult, r=[ks + "b", "lam3"], w=[ks + "c"])
                b.ts("dve", ot[e2][:], o1[:, 0:128], s_[:, 0:1], None, ALU.mult, r=[k1, ks + "a"], w=["ot%d" % e2])
                b.stt(ot[e2][:], o2[:, 0:128], s_[:, 2:3], ot[e2][:], ALU.mult, ALU.add, r=[k2, ks + "c", "ot%d" % e2], w=["ot%d" % e2])
                b.stt(junk[:], ot[e2][:], 1.0, ot[e2][:], ALU.mult, ALU.mult, r=["ot%d" % e2], w=["junk2", ks + "d"], accum=s_[:, 3:4])
                b.ts("dve", s_[:, 4:5], s_[:, 3:4], 1.0 / 128.0, EPS, ALU.mult, ALU.add, r=[ks + "d"], w=[ks + "e"])
                b.act(s_[:, 5:6], s_[:, 4:5], AF.Sqrt, r=[ks + "e"], w=[ks + "f"])
                b.recip(s_[:, 6:7], s_[:, 5:6], r=[ks + "f"], w=[ks + "g"])
                b.stt(ob[e2][:], ot[e2][:], s_[:, 6:7], gsub[:], ALU.mult, ALU.mult, r=["ot%d" % e2, ks + "g", "gsub"], w=["ob%d" % e2])

        def epilogue_b(h, qg):
            for qb in range(2):
                b.tr(pTr[:, qb * 128:(qb + 1) * 128], ob[qb][:], ident_b[:], r=["ob%d" % qb, "ident_b"], w=["pTr"])
            q0 = qg * 256
            b.cp("dve", attnT[:, h, q0:q0 + 256], pTr[:, 0:256], r=["pTr"], w=["attnT"])

        load_head(0)
        for h in range(8):
            sl = h % 2
            if h + 1 < 8:
                load_head(h + 1)
            for qg in range(8):
                eidx = {}
                for step in range(36):
                    kb = step
                    if kb < 34:
                        i = kb % 3
                        ei = ecnt % 4
                        ecnt += 1
                        eidx[kb] = ei
                        for m in range(2):
                            b.mm(pS[i][:, m * 256:(m + 1) * 256], kT[sl][:, m, kb * 128:(kb + 1) * 128], qT[sl][:, m, qg * 256:(qg + 1) * 256],
                                 r=["kT%d" % sl, "qT%d" % sl], w=["pS%d" % i])
                        b.act(E[ei][:], pS[i][:], AF.Exp, r=["pS%d" % i], w=["E%d" % ei], scale=0.125)
                    pk = step - 2
                    if pk >= 0:
                        pei = eidx[pk]
                        for m in range(2):
                            for qb in range(2):
                                a = m * 2 + qb
                                b.mm(pO[a][:, 0:129], E[pei][:, m * 256 + qb * 128: m * 256 + (qb + 1) * 128], vv[sl][:, pk, 0:129],
                                     start=(pk == 0), stop=(pk == 33), r=["E%d" % pei, "vv%d" % sl], w=["pO%d" % a])
                    if step == 10:
                        convert_chunk()
                    if step == 4 and pend_epi[0] is not None:
                        epilogue(*pend_epi[0])
                    if step == 20 and pend_epi[0] is not None:
                        epilogue_b(*pend_epi[0])
                        pend_epi[0] = None
                for a in range(4):
                    b.cp("act" if a % 2 == 0 else "dve", osb[a][:, 0:129], pO[a][:, 0:129], r=["pO%d" % a], w=["osb%d" % a])
                pend_epi[0] = (h, qg)
        epilogue(*pend_epi[0])
        epilogue_b(*pend_epi[0])
        b.dma("sp", scr["attnT_s"][:, :, :], attnT[:], r=["attnT"], w=["attnT_s"])
        S.flush()


def phase3(nc, S, b, io, scr, G):
    ident_f = G["ident_f"]
    dbg = G["dbg"]
    with ExitStack() as st:
        T = lambda name, shape, dt=F32: st.enter_context(nc.sbuf_tensor("s_" + name, list(shape), dt))
        cst = T("cst", [128, 512])
        selc = T("selc", [128, 8, 240])
        b.dma("sp", cst[:], io["cst"][:, :], w=["cst"])
        b.dma("sp", selc[:], io["selc"][:, :, :], w=["selc"])
        selcb = T("selcb", [128, 8, 240], BF16)
        b.cp("dve", selcb[:], selc[:], r=["selc"], w=["selcb"])
        maskf, maskb = cst[:, 0:128], cst[:, 128:256]
        kka, kkd, kk8, kk1 = cst[0:64, 256:272], cst[0:64, 272:288], cst[0:64, 288:304], cst[0:64, 304:305]
        AR = T("AR", [64, 64]); AI = T("AI", [64, 64]); DT = T("DT", [64, 64])
        RHO = T("RHO", [64, 64]); TH = T("TH", [64, 64])
        FR = T("FR", [64, 64]); FI = T("FI", [64, 64])
        FBR = T("FBR", [64, 64, 16]); FBI = T("FBI", [64, 64, 16])
        CNR = T("CNR", [64, 64, 16]); CNI = T("CNI", [64, 64, 16])
        Dsq = T("Dsq", [128, 32])
        ygT = T("ygT", [128, 4, NOWN])
        b.dma("sp", AR[:], io["a_re"].rearrange("d g n -> n (d g)"), w=["AR"], slow=True)
        b.dma("sp", AI[:], io["a_im"].rearrange("d g n -> n (d g)"), w=["AI"], slow=True)
        b.dma("sp", DT[:], io["log_dt"].rearrange("d g -> (d g)").partition_broadcast(64), w=["DT"])
        for s_ in range(8):
            b.dma("sp", Dsq[s_ * 16:(s_ + 1) * 16, :], io["ssm_d"].rearrange("(g q) -> q g", q=16), w=["Dsq"], slow=True)
        b.act(DT[:], DT[:], AF.Exp, r=["DT"], w=["DT"])
        b.tt("dve", RHO[:], AR[:], DT[:], ALU.mult, r=["AR", "DT"], w=["RHO"])
        b.tt("dve", TH[:], AI[:], DT[:], ALU.mult, r=["AI", "DT"], w=["TH"])

        uid = [0]

        def cpow(st_, dre, dim, rho, th, kk, Gn, Kn, tag):
            uid[0] += 1
            u = "%s%d" % (tag, uid[0])
            T_ = lambda name, dt=F32: st_.enter_context(nc.sbuf_tensor("s_%s_%s" % (name, u), [64, Gn, Kn], dt))
            rk = T_("rk"); y = T_("y"); yi = T_("yi", I32); w_ = T_("w"); mg = T_("mg")
            kb_ = kk.unsqueeze(1).to_broadcast([64, Gn, Kn])
            b.tt("dve", rk[:], bcl(rho, Kn), kb_, ALU.mult, r=["RHO", "cst"], w=["rk" + u])
            b.act(mg[:], rk[:], AF.Exp, r=["rk" + u], w=["mg" + u])
            b.tt("dve", rk[:], bcl(th, Kn), kb_, ALU.mult, r=["TH", "cst", "mg" + u], w=["rk" + u])
            for dst, off in ((dim, 0.5), (dre, 0.75)):
                b.ts("dve", y[:], rk[:], 1.0 / (2 * PI), off, ALU.mult, ALU.add, r=["rk" + u], w=["y" + u])
                b.cp("dve", yi[:], y[:], r=["y" + u], w=["yi" + u])
                b.cp("dve", w_[:], yi[:], r=["yi" + u], w=["w" + u])
                b.tt("dve", y[:], y[:], w_[:], ALU.subtract, r=["y" + u, "w" + u], w=["y" + u])
                b.ts("dve", w_[:], y[:], 0.0, None, ALU.is_lt, r=["y" + u], w=["w" + u])
                b.tt("dve", y[:], y[:], w_[:], ALU.add, r=["y" + u, "w" + u], w=["y" + u])
                b.ts("dve", y[:], y[:], 2 * PI, -PI, ALU.mult, ALU.add, r=["y" + u], w=["y" + u])
                b.ts("dve", y[:], y[:], 3.1415925, -3.1415925, ALU.min, ALU.max, r=["y" + u], w=["y" + u])
                b.act(w_[:], y[:], AF.Sin, r=["y" + u], w=["w" + u])
                b.tt("dve", dst, w_[:], mg[:], ALU.mult, r=["w" + u, "mg" + u], w=[tag])

        with ExitStack() as st2:
            T2 = lambda name, shape, dt=F32: st2.enter_context(nc.sbuf_tensor("s_" + name, list(shape), dt))
            P2 = lambda name, shape, dt=F32: (S.excl.add(name), st2.enter_context(nc.psum_tensor("p_" + name, list(shape), dt)))[1]
            ABR = T2("ABR", [64, 64, 1]); ABI = T2("ABI", [64, 64, 1])
            BR = T2("BR", [64, 64, 16]); BI = T2("BI", [64, 64, 16])
            b.dma("sp", BR[:], io["b_re"].rearrange("d g n q -> n (d g) q"), w=["BR"])
            b.dma("act", BI[:], io["b_im"].rearrange("d g n q -> n (d g) q"), w=["BI"])
            cpow(st2, ABR[:], ABI[:], RHO[:], TH[:], kk1, 64, 1, "AB")
            den = T2("den", [64, 64]); t1 = T2("g_t1", [64, 64]); t2 = T2("g_t2", [64, 64]); nr = T2("nr", [64, 64])
            b.tt("dve", den[:], AR[:], AR[:], ALU.mult, r=["AR"], w=["den"])
            b.tt("dve", t1[:], AI[:], AI[:], ALU.mult, r=["AI"], w=["g_t1"])
            b.tt("dve", den[:], den[:], t1[:], ALU.add, r=["den", "g_t1"], w=["den"])
            b.recip(den[:], den[:], r=["den"], w=["den"])
            b.ts("dve", nr[:], ABR[:, :, 0], -1.0, None, ALU.add, r=["AB"], w=["nr"])
            b.tt("dve", t1[:], nr[:], AR[:], ALU.mult, r=["nr", "AR"], w=["g_t1"])
            b.tt("dve", t2[:], ABI[:, :, 0], AI[:], ALU.mult, r=["AB", "AI"], w=["g_t2"])
            b.tt("dve", t1[:], t1[:], t2[:], ALU.add, r=["g_t1", "g_t2"], w=["g_t1"])
            b.tt("dve", FR[:], t1[:], den[:], ALU.mult, r=["g_t1", "den"], w=["FR"])
            b.tt("dve", t1[:], ABI[:, :, 0], AR[:], ALU.mult, r=["AB", "AR", "FR"], w=["g_t1"])
            b.tt("dve", t2[:], nr[:], AI[:], ALU.mult, r=["nr", "AI"], w=["g_t2"])
            b.tt("dve", t1[:], t1[:], t2[:], ALU.subtract, r=["g_t1", "g_t2"], w=["g_t1"])
            b.tt("dve", FI[:], t1[:], den[:], ALU.mult, r=["g_t1", "den"], w=["FI"])
            ta = T2("g_ta", [64, 64, 16]); tb = T2("g_tb", [64, 64, 16])
            b.tt("dve", ta[:], BR[:], bcl(FR[:], 16), ALU.mult, r=["BR", "FR"], w=["g_ta"])
            b.tt("dve", tb[:], BI[:], bcl(FI[:], 16), ALU.mult, r=["BI", "FI"], w=["g_tb"])
            b.tt("dve", FBR[:], ta[:], tb[:], ALU.subtract, r=["g_ta", "g_tb"], w=["FBR"])
            b.tt("dve", ta[:], BI[:], bcl(FR[:], 16), ALU.mult, r=["BI", "FR", "FBR"], w=["g_ta"])
            b.tt("dve", tb[:], BR[:], bcl(FI[:], 16), ALU.mult, r=["BR", "FI", "FBR"], w=["g_tb"])
            b.tt("dve", FBI[:], ta[:], tb[:], ALU.add, r=["g_ta", "g_tb"], w=["FBI"])
            cn = [T2("cn%d" % i, [128, 64]) for i in range(2)]
            pC = [P2("pC%d" % i, [128, 512]) for i in range(2)]
            k_ = 0
            for (src, dstt, key) in ((io["c_re"], CNR, "CNR"), (io["c_im"], CNI, "CNI")):
                for d in range(2):
                    for gb in range(4):
                        i = k_ % 2
                        k_ += 1
                        b.dma("sp" if i == 0 else "act", cn[i][:], src[d, gb * 8:(gb + 1) * 8].rearrange("g p n -> (g p) n"), w=["cn%d" % i])
                        b.tr(pC[i][0:64, 0:128], cn[i][:], ident_f[:], r=["cn%d" % i, "ident_f"], w=["pC%d" % i])
                        b.cp("act", dstt[:, d * 32 + gb * 8: d * 32 + (gb + 1) * 8, :], pC[i][0:64, 0:128].rearrange("n (g p) -> n g p", g=8),
                             r=["pC%d" % i], w=[key])
            S.flush()

        def cmul(e, ore, oim, are, aim, bre, bim, t1, t2, kr, kw, neg_im=False):
            b.tt(e, t1, are, bre, ALU.mult, r=kr, w=[kw + "t1"])
            b.tt(e, t2, aim, bim, ALU.mult, r=kr, w=[kw + "t2"])
            b.tt(e, ore, t1, t2, ALU.subtract, r=[kw + "t1", kw + "t2"], w=[kw + "R"])
            b.tt(e, t1, are, bim, ALU.mult, r=kr + [kw + "R"], w=[kw + "t1"])
            b.tt(e, t2, aim, bre, ALU.mult, r=kr + [kw + "R"], w=[kw + "t2"])
            if neg_im:
                b.S.I(e, lambda: b.e[e].scalar_tensor_tensor(out=oim, in0=t1, scalar=-1.0, in1=t2, op0=ALU.mult, op1=ALU.subtract),
                      r=[kw + "t1", kw + "t2"], w=[kw + "I"]) if e == "dve" else None
            else:
                b.tt(e, oim, t1, t2, ALU.add, r=[kw + "t1", kw + "t2"], w=[kw + "I"])

        for gb in range(4):
            with ExitStack() as stb:
                Tb = lambda name, shape, dt=F32: stb.enter_context(nc.sbuf_tensor("s_%s_b%d" % (name, gb), list(shape), dt))
                EfR = Tb("EfR", [64, 8, 128]); EfI = Tb("EfI", [64, 8, 128]); EbR = Tb("EbR", [64, 8, 128]); EbI = Tb("EbI", [64, 8, 128])
                Mt = Tb("Mt", [128, 8, 128]); Wt = Tb("Wt", [128, 8, 4, 64])
                pwaR = Tb("pwaR", [64, 16, 16]); pwaI = Tb("pwaI", [64, 16, 16])
                p8R = Tb("p8R", [64, 16, 16]); p8I = Tb("p8I", [64, 16, 16])
                gsl = lambda d: slice(d * 32 + gb * 8, d * 32 + (gb + 1) * 8)
                rb = Tb("rb", [64, 16]); tb_ = Tb("tb", [64, 16])
                for d in range(2):
                    b.cp("dve", rb[:, d * 8:(d + 1) * 8], RHO[:, gsl(d)], r=["RHO"], w=["rb"])
                    b.cp("dve", tb_[:, d * 8:(d + 1) * 8], TH[:, gsl(d)], r=["TH"], w=["tb"])
                with ExitStack() as sa:
                    Ta = lambda name, shape, dt=F32: sa.enter_context(nc.sbuf_tensor("s_%s_a%d" % (name, gb), list(shape), dt))
                    Pa = lambda name, shape, dt=F32: (S.excl.add(name), sa.enter_context(nc.psum_tensor("p_%s_a%d" % (name, gb), list(shape), dt)))[1]
                    pwdR = Ta("pwdR", [64, 16, 16]); pwdI = Ta("pwdI", [64, 16, 16])
                    S.lastw["RHO"] = S.lastw.get("rb"); S.lastw["TH"] = S.lastw.get("tb")
                    cpow(sa, pwaR[:], pwaI[:], rb[:], tb_[:], kka, 16, 16, "pwa")
                    cpow(sa, pwdR[:], pwdI[:], rb[:], tb_[:], kkd, 16, 16, "pwd")
                    cpow(sa, p8R[:], p8I[:], rb[:], tb_[:], kk8, 16, 16, "p8")
                    X0R = Ta("X0R", [64, 8, 8, 16]); X0I = Ta("X0I", [64, 8, 8, 16])
                    XpR = Ta("XpR", [64, 8, 8, 16]); XpI = Ta("XpI", [64, 8, 8, 16])
                    X1R = Ta("X1R", [64, 8, 8, 16]); X1I = Ta("X1I", [64, 8, 8, 16])
                    Y0R = Ta("Y0R", [64, 8, 8, 16]); Y0I = Ta("Y0I", [64, 8, 8, 16])
                    Y1R = Ta("Y1R", [64, 8, 8, 16]); Y1I = Ta("Y1I", [64, 8, 8, 16])
                    c1 = Ta("c1", [64, 8, 8, 16]); c2 = Ta("c2", [64, 8, 8, 16])
                    v4 = lambda t: t[:].rearrange("n g (s q) -> n g s q", q=16)

                    def pws(R_, I_, d, a):
                        return bcl(R_[:, d * 8:(d + 1) * 8, a:a + 8], 16), bcl(I_[:, d * 8:(d + 1) * 8, a:a + 8], 16)

                    def fbs(R_, I_, d):
                        return (R_[:, gsl(d), :].unsqueeze(2).to_broadcast([64, 8, 8, 16]), I_[:, gsl(d), :].unsqueeze(2).to_broadcast([64, 8, 8, 16]))

                    jobs = [
                        (X0R[:], X0I[:], pws(pwdR, pwdI, 0, 8), fbs(FBR, FBI, 0), ["pwd", "FBR", "FBI"], "X0", False),
                        (XpR[:], XpI[:], pws(pwdR, pwdI, 0, 1), fbs(FBR, FBI, 0), ["pwd", "FBR", "FBI"], "Xp", False),
                        (X1R[:], X1I[:], pws(pwaR, pwaI, 1, 7), fbs(FBR, FBI, 1), ["pwa", "FBR", "FBI"], "X1", False),
                        (Y0R[:], Y0I[:], pws(pwaR, pwaI, 0, 7), fbs(CNR, CNI, 0), ["pwa", "CNR", "CNI"], "Y0", True),
                        (Y1R[:], Y1I[:], pws(pwdR, pwdI, 1, 8), fbs(CNR, CNI, 1), ["pwd", "CNR", "CNI"], "Y1", True),
                        (v4(EfR), v4(EfI), pws(pwaR, pwaI, 0, 8), fbs(CNR, CNI, 0), ["pwa", "CNR", "CNI"], "Ef", True),
                        (v4(EbR), v4(EbI), pws(pwdR, pwdI, 1, 0), fbs(CNR, CNI, 1), ["pwd", "CNR", "CNI"], "Eb", True),
                    ]
                    for (ore, oim, (are, aim), (bre, bim), kr, kw, neg) in jobs:
                        cmul("dve", ore, oim, are, aim, bre, bim, c1[:], c2[:], kr + ["c1", "c2"], kw, neg_im=neg)
                        S.lastw["c1"] = S.lastw.get(kw + "I"); S.lastw["c2"] = S.lastw.get(kw + "I")
                    pM = [Pa("pM%d" % i, [128, 512]) for i in range(2)]
                    pW = [Pa("pW%d" % i, [128, 512]) for i in range(2)]
                    mt = Ta("mtmp", [128, 128])
                    f2 = lambda t, g: t[:, g, :, :].rearrange("n s q -> n (s q)")
                    for g in range(8):
                        i = g % 2
                        b.mm(pM[i][:, 0:128], f2(X0R, g), f2(Y0R, g), start=True, stop=False, r=["X0R", "Y0R"], w=["pM%d" % i])
                        b.mm(pM[i][:, 0:128], f2(X0I, g), f2(Y0I, g), start=False, stop=True, r=["X0I", "Y0I"], w=["pM%d" % i])
                        b.mm(pM[i][:, 128:256], f2(X1R, g), f2(Y1R, g), start=True, stop=False, r=["X1R", "Y1R"], w=["pM%d" % i])
                        b.mm(pM[i][:, 128:256], f2(X1I, g), f2(Y1I, g), start=False, stop=True, r=["X1I", "Y1I"], w=["pM%d" % i])
                        b.tt("dve", Mt[:, g, :], pM[i][:, 0:128], maskf, ALU.mult, r=["pM%d" % i, "cst"], w=["Mt"])
                        b.tt("dve", mt[:], pM[i][:, 128:256], maskb, ALU.mult, r=["pM%d" % i, "cst"], w=["mtmp"])
                        b.tt("dve", Mt[:, g, :], Mt[:, g, :], mt[:], ALU.add, r=["Mt", "mtmp"], w=["Mt"])
                        b.stt(Mt[:, g, :], ident_f[:], Dsq[:, gb * 8 + g: gb * 8 + g + 1], Mt[:, g, :], ALU.mult, ALU.add, r=["ident_f", "Dsq", "Mt"], w=["Mt"])
                        for k_, (src, key) in enumerate(((XpR, "XpR"), (XpI, "XpI"), (X1R, "X1R"), (X1I, "X1I"))):
                            b.tr(pW[i][:, k_ * 64:(k_ + 1) * 64], f2(src, g), ident_f[0:64, 0:64], r=[key, "ident_f"], w=["pW%d" % i])
                        b.cp("act", Wt[:, g, :, :], pW[i][:, 0:256].rearrange("p (k n) -> p k n", k=4), r=["pW%d" % i], w=["Wt"])
                    S.flush()
                if "stop3a" in dbg:
                    return
                with ExitStack() as sb:
                    Tq = lambda name, shape, dt=F32: sb.enter_context(nc.sbuf_tensor("s_%s_q%d" % (name, gb), list(shape), dt))
                    Pq = lambda name, shape, dt=F32: (S.excl.add(name), sb.enter_context(nc.psum_tensor("p_%s_q%d" % (name, gb), list(shape), dt)))[1]
                    uTc = Tq("uTc", [128, NKEY])
                    U = Tq("U", [128, 8, 544])
                    SfR = Tq("SfR", [64, 8, 288]); SfI = Tq("SfI", [64, 8, 288])
                    SbR = Tq("SbR", [64, 8, 544]); SbI = Tq("SbI", [64, 8, 544])
                    Yg = Tq("Yg", [128, 8, 256], BF16)
                    ygx = [Tq("ygx%d" % i, [128, 256]) for i in range(2)]
                    ygt = [Tq("ygt%d" % i, [128, 256]) for i in range(2)]
                    pU = [Pq("pU%d" % i, [128, 512]) for i in range(2)]
                    pSt = [Pq("pSt%d" % i, [128, 512]) for i in range(2)]
                    pY = [Pq("pY%d" % i, [128, 512]) for i in range(2)]
                    b.dma("sp", uTc[:], scr["uT_s"][gb], w=["uTc"])
                    uTb = Tq("uTb", [128, NKEY], BF16)
                    b.cp("act", uTb[:], uTc[:], r=["uTc"], w=["uTb"])
                    uv = uTb[:].rearrange("p (c s) -> p c s", s=8)
                    n_ = 0
                    for g in range(8):
                        for hh in range(2):
                            i = n_ % 2
                            n_ += 1
                            for s_ in range(8):
                                b.mm(pU[i][:, 0:272], selcb[:, g, (7 - s_) * 16:(7 - s_) * 16 + 128], uv[:, hh * 272:(hh + 1) * 272, s_],
                                     start=(s_ == 0), stop=(s_ == 7), r=["selcb", "uTb"], w=["pU%d" % i])
                            b.cp("act" if i == 0 else "dve", U[:, g, hh * 272:(hh + 1) * 272], pU[i][:, 0:272], r=["pU%d" % i], w=["U"])
                    n_ = 0
                    for g in range(8):
                        for (k_, dst, c0, nn, key) in ((0, SfR, 0, 288, "SfR"), (1, SfI, 0, 288, "SfI"), (2, SbR, 0, 272, "SbR"), (2, SbR, 272, 272, "SbR"),
                                                       (3, SbI, 0, 272, "SbI"), (3, SbI, 272, 272, "SbI")):
                            i = n_ % 2
                            n_ += 1
                            b.mm(pSt[i][0:64, 0:nn], Wt[:, g, k_, :], U[:, g, c0:c0 + nn], r=["Wt", "U"], w=["pSt%d" % i])
                            b.cp("act" if i == 0 else "dve", dst[:, g, c0:c0 + nn], pSt[i][0:64, 0:nn], r=["pSt%d" % i], w=[key])
                    tA = Tq("tA", [64, 8, 34]); tB = Tq("tB", [64, 8, 34])
                    tC = Tq("tC", [64, 8, 34]); tD = Tq("tD", [64, 8, 34])
                    CIfR = Tq("CIfR", [64, 8, 18]); CIfI = Tq("CIfI", [64, 8, 18])
                    CIbR = Tq("CIbR", [64, 8, 34]); CIbI = Tq("CIbI", [64, 8, 34])

                    def cmac(e, dR, dI, cR, cI, xR, xI, ta_, tb2_, keys, tk):
                        b.tt(e, ta_, cR, xR, ALU.mult, r=keys, w=[tk + "a"])
                        b.tt(e, dR, dR, ta_, ALU.add, r=keys + [tk + "a"], w=keys[:1])
                        b.tt(e, ta_, cI, xI, ALU.mult, r=keys, w=[tk + "a"])
                        b.tt(e, dR, dR, ta_, ALU.subtract, r=keys + [tk + "a"], w=keys[:1])
                        b.tt(e, tb2_, cR, xI, ALU.mult, r=keys, w=[tk + "b"])
                        b.tt(e, dI, dI, tb2_, ALU.add, r=keys + [tk + "b"], w=keys[1:2])
                        b.tt(e, tb2_, cI, xR, ALU.mult, r=keys, w=[tk + "b"])
                        b.tt(e, dI, dI, tb2_, ALU.add, r=keys + [tk + "b"], w=keys[1:2])

                    def views(SR, SI, nb):
                        VR = SR[:, :, 0:nb * 16].rearrange("n g (b j) -> n g b j", j=16)
                        VI = SI[:, :, 0:nb * 16].rearrange("n g (b j) -> n g b j", j=16)
                        return VR, VI

                    def scan1(e, SR, SI, kR, kI, nb, asc, d, ta_, tb2_, tk):
                        VR, VI = views(SR, SI, nb)
                        a8R = bcl(pwaR[:, d * 8:(d + 1) * 8, 15], nb); a8I = bcl(pwaI[:, d * 8:(d + 1) * 8, 15], nb)
                        keys = [kR, kI, "pwa", "p8"]
                        for j in (range(1, 16) if asc else range(14, -1, -1)):
                            pj = j - 1 if asc else j + 1
                            cmac(e, VR[:, :, :, j], VI[:, :, :, j], a8R, a8I, VR[:, :, :, pj], VI[:, :, :, pj], ta_[:, :, 0:nb], tb2_[:, :, 0:nb], keys, tk)

                    def scan2(e, SR, SI, kR, kI, nb, asc, d, CIR, CII, ta_, tb2_, tk, order, cik):
                        VR, VI = views(SR, SI, nb)
                        last = 15 if asc else 0
                        a128R = p8R[:, d * 8:(d + 1) * 8, 15]; a128I = p8I[:, d * 8:(d + 1) * 8, 15]
                        keys = [kR, kI, "pwa", "p8", cik]
                        b.memset(e, CIR[:], 0.0, w=[cik])
                        b.memset(e, CII[:], 0.0, w=[cik])
                        for q_ in range(len(order) - 1):
                            cur, nxt = order[q_], order[q_ + 1]
                            b.tt(e, ta_[:, :, 0], a128R, CIR[:, :, cur], ALU.mult, r=keys, w=[tk + "a"])
                            b.tt(e, ta_[:, :, 1], a128I, CII[:, :, cur], ALU.mult, r=keys, w=[tk + "a"])
                            b.tt(e, tb2_[:, :, 0], a128R, CII[:, :, cur], ALU.mult, r=keys, w=[tk + "b"])
                            b.tt(e, tb2_[:, :, 1], a128I, CIR[:, :, cur], ALU.mult, r=keys, w=[tk + "b"])
                            b.tt(e, CIR[:, :, nxt], ta_[:, :, 0], ta_[:, :, 1], ALU.subtract, r=[tk + "a"] + keys, w=[cik])
                            b.tt(e, CII[:, :, nxt], tb2_[:, :, 0], tb2_[:, :, 1], ALU.add, r=[tk + "b"] + keys, w=[cik])
                            b.tt(e, CIR[:, :, nxt], CIR[:, :, nxt], VR[:, :, cur, last], ALU.add, r=keys, w=[cik])
                            b.tt(e, CII[:, :, nxt], CII[:, :, nxt], VI[:, :, cur, last], ALU.add, r=keys, w=[cik])

                    def scan3(e, SR, SI, kR, kI, nb, asc, d, CIR, CII, ta_, tb2_, tk, cik):
                        VR, VI = views(SR, SI, nb)
                        keys = [kR, kI, "pwa", "p8", cik]
                        for j in range(16):
                            pj = j if asc else 15 - j
                            cR = bcl(p8R[:, d * 8:(d + 1) * 8, pj], nb); cI = bcl(p8I[:, d * 8:(d + 1) * 8, pj], nb)
                            cmac(e, VR[:, :, :, j], VI[:, :, :, j], cR, cI, CIR[:, :, 0:nb], CII[:, :, 0:nb], ta_[:, :, 0:nb], tb2_[:, :, 0:nb], keys, tk)

                    border = [1, 0] + list(range(33, 1, -1))
                    scan1("dve", SfR, SfI, "SfR", "SfI", 18, True, 0, tA, tB, "sf")
                    scan1("pool", SbR, SbI, "SbR", "SbI", 34, False, 1, tC, tD, "sb")
                    scan2("dve", SfR, SfI, "SfR", "SfI", 18, True, 0, CIfR, CIfI, tA, tB, "sf", list(range(18)), "sfCI")
                    scan3("dve", SfR, SfI, "SfR", "SfI", 18, True, 0, CIfR, CIfI, tA, tB, "sf", "sfCI")
                    scan2("pool", SbR, SbI, "SbR", "SbI", 34, False, 1, CIbR, CIbI, tC, tD, "sb", border, "sbCI")
                    scan3("pool", SbR, SbI, "SbR", "SbI", 34, False, 1, CIbR, CIbI, tC, tD, "sb", "sbCI")
                    for g in range(8):
                        i = g % 2
                        b.mm(pY[i][:, 0:256], Mt[:, g, :], U[:, g, 32:288], start=True, stop=False, r=["Mt", "U"], w=["pY%d" % i])
                        b.mm(pY[i][:, 0:256], EfR[:, g, :], SfR[:, g, 31:287], start=False, stop=False, r=["Ef", "EfR", "SfR"], w=["pY%d" % i])
                        b.mm(pY[i][:, 0:256], EfI[:, g, :], SfI[:, g, 31:287], start=False, stop=False, r=["Ef", "EfI", "SfI"], w=["pY%d" % i])
                        b.mm(pY[i][:, 0:256], EbR[:, g, :], SbR[:, g, 33:289], start=False, stop=False, r=["Eb", "EbR", "SbR"], w=["pY%d" % i])
                        b.mm(pY[i][:, 0:256], EbI[:, g, :], SbI[:, g, 33:289], start=False, stop=True, r=["Eb", "EbI", "SbI"], w=["pY%d" % i])
                        if "s5_nogelu" in dbg:
                            b.cp("act", Yg[:, g, :], pY[i][:, 0:256], r=["pY%d" % i], w=["Yg"])
                        else:
                            b.cp("act", ygx[i][:], pY[i][:, 0:256], r=["pY%d" % i], w=["ygx%d" % i])
                            gelu_tanh(b, Yg[:, g, :], ygx[i][:], ygt[i][:], "ygx%d" % i, "ygt%d" % i, "Yg")
                    yv = ygT[:, gb, :].rearrange("p (c s) -> p c s", s=8)
                    for t8 in range(8):
                        i = t8 % 2
                        for g in range(8):
                            b.mm(pU[i][:, 0:256], selcb[:, t8, (7 - g) * 16:(7 - g) * 16 + 128], Yg[:, g, :], start=(g == 0), stop=(g == 7),
                                 r=["selcb", "Yg"], w=["pU%d" % i])
                        b.cp("act" if i == 0 else "dve", yv[:, :, t8], pU[i][:, 0:256], r=["pU%d" % i], w=["ygT"])
                    S.flush()
        if "ygT_d" in dbg:
            b.dma("sp", G["ygT_d"], ygT[:], r=["ygT"], w=["ygT_d"])
            S.flush()
            return
        with ExitStack() as sg:
            Tg = lambda name, shape, dt=F32: sg.enter_context(nc.sbuf_tensor("s_" + name, list(shape), dt))
            Pg = lambda name, shape, dt=F32: (S.excl.add(name), sg.enter_context(nc.psum_tensor("p_" + name, list(shape), dt)))[1]
            ygb = Tg("ygb", [128, 4, NOWN], BF16)
            wgf = Tg("wgf", [128, 4, 512]); wgb = Tg("wgb", [128, 4, 512], BF16)
            bgl = Tg("bgl", [128, 4])
            sig = [Tg("sig%d" % i, [128, 512]) for i in range(2)]
            ssmT = Tg("ssmT", [128, 4, NOWN], BF16)
            pZ = [Pg("pZ%d" % i, [128, 512]) for i in range(2)]
            b.dma("sp", wgf[:], io["w_glu"].rearrange("(j p) n -> p j n", p=128), w=["wgf"])
            b.dma("sp", bgl[:], io["b_glu"].rearrange("(c p) -> p c", p=128), w=["bgl"], slow=True)
            b.cp("pool", wgb[:], wgf[:], r=["wgf"], w=["wgb"])
            b.cp("dve", ygb[:], ygT[:], r=["ygT"], w=["ygb"])
            n_ = 0
            for oc in range(4):
                for tb2 in range(4):
                    i = n_ % 2
                    n_ += 1
                    for kc in range(4):
                        b.mm(pZ[i][:], wgb[:, kc, oc * 128:(oc + 1) * 128], ygb[:, kc, tb2 * 512:(tb2 + 1) * 512], start=(kc == 0), stop=(kc == 3),
                             r=["wgb", "ygb"], w=["pZ%d" % i])
                    b.act(sig[i][:], pZ[i][:], AF.Sigmoid, r=["pZ%d" % i, "bgl"], w=["sig%d" % i], bias=bgl[:, oc:oc + 1])
                    b.tt("dve", ssmT[:, oc, tb2 * 512:(tb2 + 1) * 512], ygT[:, oc, tb2 * 512:(tb2 + 1) * 512], sig[i][:], ALU.mult,
                         r=["ygT", "sig%d" % i], w=["ssmT"])
            b.dma("sp", scr["ssmT_s"][:, :, :], ssmT[:], r=["ssmT"], w=["ssmT_s"])
            S.flush()


def phase4(nc, S, b, io, scr, G):
    ident_b = G["ident_b"]
    with ExitStack() as st:
        T = lambda name, shape, dt=F32: st.enter_context(nc.sbuf_tensor("s_" + name, list(shape), dt))
        P = lambda name, shape, dt=F32: (S.excl.add(name), st.enter_context(nc.psum_tensor("p_" + name, list(shape), dt)))[1]
        wa = T("wa", [128, 8, D], BF16); ws = T("ws", [128, 4, D], BF16); wo = T("wo", [128, 8, D], BF16); wg = T("wg", [128, 8, 2048], BF16)
        stg = [T("stg%d" % i, [128, 8, 512]) for i in range(2)]
        g1row = T("g1row", [128, D])
        b.dma("sp", g1row[:], scr["vec_s"][0].partition_broadcast(128), w=["g1row"])
        n_ = 0
        for (src, dst, nj, ncol, key) in ((io["w_attn_up"], wa, 8, D, "wa"), (io["w_ssm_up"], ws, 4, D, "ws"), (io["w_out"], wo, 8, D, "wo"),
                                          (io["w_in"][:, 3584:5632], wg, 8, 2048, "wg")):
            for cb in range(ncol // 512):
                i = n_ % 2
                n_ += 1
                b.dma("sp" if i == 0 else "act", stg[i][:, 0:nj, :], src[:, cb * 512:(cb + 1) * 512].rearrange("(j p) n -> p j n", p=128), w=["stg%d" % i])
                b.cp("pool" if i == 0 else "dve", dst[:, :, cb * 512:(cb + 1) * 512], stg[i][:, 0:nj, :], r=["stg%d" % i], w=[key])
        xs_t = [T("xs_t%d" % i, [128, 8, 128], BF16) for i in range(2)]
        at_t = [T("at_t%d" % i, [128, 8, 128], BF16) for i in range(2)]
        ss_t = [T("ss_t%d" % i, [128, 4, 128], BF16) for i in range(2)]
        x_t = [T("x_t%d" % i, [128, D]) for i in range(2)]
        gs = T("gs", [128, 2048])
        m1 = T("m1", [128, D]); m2 = T("m2", [128, D]); mb = T("mb", [128, D], BF16)
        mT = T("mT", [128, 8, 128], BF16)
        x1 = [T("x1_%d" % i, [128, D]) for i in range(2)]
        pG = [P("pG%d" % i, [128, 512]) for i in range(2)]
        pA = [P("pA4_%d" % i, [128, 512]) for i in range(2)]
        pS = [P("pS4_%d" % i, [128, 512]) for i in range(2)]
        pTr = P("pTr4", [128, 1024], BF16)
        for ti in range(16):
            i = ti % 2
            cs = slice(ti * 128, (ti + 1) * 128)
            b.dma("sp", xs_t[i][:], scr["xnT_s"][:, :, cs], w=["xs_t%d" % i])
            b.dma("act", at_t[i][:], scr["attnT_s"][:, :, cs], w=["at_t%d" % i])
            b.dma("sp", ss_t[i][:], scr["ssmT_s"][:, :, cs], w=["ss_t%d" % i])
            b.dma("act", x_t[i][:], io["x_seq"][cs, :], w=["x_t%d" % i])
            for blk in range(4):
                pg = pG[blk % 2]
                kg = "pG%d" % (blk % 2)
                for j in range(8):
                    b.mm(pg[:], xs_t[i][:, j, :], wg[:, j, blk * 512:(blk + 1) * 512], start=(j == 0), stop=(j == 7), r=["xs_t%d" % i, "wg"], w=[kg])
                b.act(gs[:, blk * 512:(blk + 1) * 512], pg[:], AF.Sigmoid, r=[kg], w=["gs"])
            for hf in range(2):
                for j in range(8):
                    b.mm(pA[hf][:], at_t[i][:, j, :], wa[:, j, hf * 512:(hf + 1) * 512], start=(j == 0), stop=(j == 7), r=["at_t%d" % i, "wa"], w=["pA4_%d" % hf])
                for j in range(4):
                    b.mm(pS[hf][:], ss_t[i][:, j, :], ws[:, j, hf * 512:(hf + 1) * 512], start=(j == 0), stop=(j == 3), r=["ss_t%d" % i, "ws"], w=["pS4_%d" % hf])
                b.tt("dve", m1[:, hf * 512:(hf + 1) * 512], pA[hf][:], gs[:, hf * 512:(hf + 1) * 512], ALU.mult, r=["pA4_%d" % hf, "gs"], w=["m1"])
                b.tt("dve", m2[:, hf * 512:(hf + 1) * 512], pS[hf][:], gs[:, 1024 + hf * 512:1024 + (hf + 1) * 512], ALU.mult, r=["pS4_%d" % hf, "gs"], w=["m2"])
            b.tt("pool", mb[:], m1[:], m2[:], ALU.add, r=["m1", "m2"], w=["mb"])
            for j in range(8):
                b.tr(pTr[:, j * 128:(j + 1) * 128], mb[:, j * 128:(j + 1) * 128], ident_b[:], r=["mb", "ident_b"], w=["pTr4"])
            b.cp("act", mT[:], pTr[:].rearrange("p (j t) -> p j t", j=8), r=["pTr4"], w=["mT"])
            for hf in range(2):
                for j in range(8):
                    b.mm(pA[hf][:], mT[:, j, :], wo[:, j, hf * 512:(hf + 1) * 512], start=(j == 0), stop=(j == 7), r=["mT", "wo"], w=["pA4_%d" % hf])
                b.tt("dve", m1[:, hf * 512:(hf + 1) * 512], pA[hf][:], g1row[:, hf * 512:(hf + 1) * 512], ALU.mult, r=["pA4_%d" % hf, "g1row"], w=["m1"])
            b.tt("pool", x1[i][:], m1[:], x_t[i][:], ALU.add, r=["m1", "x_t%d" % i], w=["x1_%d" % i])
            b.dma("sp", scr["x1_s"][cs, :], x1[i][:], r=["x1_%d" % i], w=["x1_s"])
        S.flush()


def phase5(nc, S, b, io, scr, G, out):
    ident_f = G["ident_f"]
    dbg = G["dbg"]
    with ExitStack() as st:
        T = lambda name, shape, dt=F32: st.enter_context(nc.sbuf_tensor("s_" + name, list(shape), dt))
        P = lambda name, shape, dt=F32: (S.excl.add(name), st.enter_context(nc.psum_tensor("p_" + name, list(shape), dt)))[1]
        wq = T("wq", [128, 8, D])
        rows = T("rows5", [128, 4, D])
        k1T = T("k1T", [64, 128]); k2T = T("k2T", [128, 128])
        kk1 = T("kk1", [128, 64]); kk2 = T("kk2", [128, 128])
        iota16 = T("iota16", [128, 16])
        pX = P("pX", [128, 1024])
        pTQ = P("pTQ", [128, 1024])
        pSc = P("pSc", [128, 2048])
        b.dma("sp", wq[:], io["w_query"].rearrange("(j p) n -> p j n", p=128), w=["wq"])
        b.dma("act", rows[:, 0, :], scr["vec_s"][1].partition_broadcast(128), w=["rows5"])
        b.dma("act", rows[:, 1, :], scr["vec_s"][2].partition_broadcast(128), w=["rows5"])
        b.dma("act", rows[:, 2, :], scr["vec_s"][3].partition_broadcast(128), w=["rows5"])
        b.dma("act", rows[:, 3, :], io["final_g"].partition_broadcast(128), w=["rows5"])
        b.dma("sp", kk1[:], io["sub_k1"][:, :], w=["kk1"])
        b.memset("pool", kk2[:], 0.0, w=["kk2"])
        b.dma("sp", kk2[:, 64:128], io["sub_k2"][:, :], r=["kk2"], w=["kk2"])
        b.dma("sp", iota16[:], io["cst"][:, 320:336], w=["iota16"])
        b.tr(pTQ[0:64, 0:128], kk1[:], ident_f[:], r=["kk1", "ident_f"], w=["pTQ"])
        b.cp("act", k1T[:], pTQ[0:64, 0:128], r=["pTQ"], w=["k1T"])
        b.tr(pTQ[:, 128:256], kk2[:], ident_f[:], r=["kk2", "ident_f"], w=["pTQ"])
        b.cp("act", k2T[:], pTQ[:, 128:256], r=["pTQ"], w=["k2T"])
        x1 = [T("x1t%d" % i, [128, D]) for i in range(2)]
        xn2 = T("xn2", [128, D]); tmpf = T("tmpf", [128, D]); junk = T("junk5", [128, D])
        st5 = T("st5", [128, 8])
        xn2T = T("xn2T", [128, 8, 128]); qTs = T("qTs", [128, 8, 128])
        sc = T("sc", [128, 2, 8, 128]); wk = T("wk", [128, 256])
        v12 = T("v12", [128, 2, 8, 16]); i12 = T("i12", [128, 2, 8, 16], U32); i12f = T("i12f", [128, 2, 8, 16])
        cand = T("cand", [128, 8, 256]); tv = T("tv", [128, 8, 16]); tj = T("tj", [128, 8, 16], U32)
        ta = T("ta5", [128, 8, 16], I32); taf = T("taf", [128, 8, 16]); tbf = T("tbf", [128, 8, 16])
        eq = T("eq", [128, 8, 16, 16]); sel1 = T("sel1", [128, 8, 16]); sel2 = T("sel2", [128, 8, 16])
        idxf = T("idxf", [128, 128]); idx32 = T("idx32", [128, 128], I32)
        ge = T("ge", [128, 8, 16]); gsum = T("gsum", [128, 8]); gate = T("gate", [128, 128])
        actv = T("actv", [128, 128]); wv = T("wv", [128, 128])
        NS = 16
        uvb = [T("uvb%d" % i, [128, 2 * D], BF16) for i in range(NS)]
        vt = [T("vt%d" % i, [128, D], BF16) for i in range(3)]
        ident_b = G["ident_b"]
        osb = [T("osb5_%d" % i, [128, D]) for i in range(2)]
        acc = pSc[:, 0:1024]
        nu = 0
        nv = 0
        for ti in range(16):
            i = ti % 2
            cs = slice(ti * 128, (ti + 1) * 128)
            kx = "x1t%d" % i
            b.dma("sp", x1[i][:], scr["x1_s"][cs, :], w=[kx])
            b.act(junk[:], x1[i][:], AF.Square, r=[kx], w=["junk5", "st5a"], accum=st5[:, 0:1])
            b.ts("dve", st5[:, 1:2], st5[:, 0:1], 1.0 / D, EPS, ALU.mult, ALU.add, r=["st5a"], w=["st5b"])
            b.act(st5[:, 2:3], st5[:, 1:2], AF.Sqrt, r=["st5b"], w=["st5c"])
            b.recip(st5[:, 3:4], st5[:, 2:3], r=["st5c"], w=["st5d"])
            b.stt(tmpf[:], x1[i][:], st5[:, 3:4], rows[:, 1, :], ALU.mult, ALU.mult, r=[kx, "st5d", "rows5"], w=["tmpf"])
            b.tt("dve", xn2[:], tmpf[:], rows[:, 2, :], ALU.add, r=["tmpf", "rows5"], w=["xn2"])
            b.cp("act", pX[:], xn2[:], r=["xn2"], w=["pX"])
            for j in range(8):
                b.tr(pTQ[:, j * 128:(j + 1) * 128], xn2[:, j * 128:(j + 1) * 128], ident_f[:], r=["xn2", "ident_f"], w=["pTQ"])
            b.cp("act", xn2T[:], pTQ[:].rearrange("p (j t) -> p j t", j=8), r=["pTQ"], w=["xn2T"])
            for h in range(8):
                for j in range(8):
                    b.mm(pTQ[:, h * 128:(h + 1) * 128], wq[:, j, h * 128:(h + 1) * 128], xn2T[:, j, :], start=(j == 0), stop=(j == 7), r=["wq", "xn2T"], w=["pTQ"])
            b.cp("act", qTs[:], pTQ[:].rearrange("p (h t) -> p h t", h=8), r=["pTQ"], w=["qTs"])
            for h in range(8):
                b.mm(pSc[:, h * 128:(h + 1) * 128], qTs[0:64, h, :], k1T[:, :], r=["qTs", "k1T"], w=["pSc"])
            for h in range(8):
                b.mm(pSc[:, 1024 + h * 128:1024 + (h + 1) * 128], qTs[64:128, h, :], k2T[64:128, :], r=["qTs", "k2T"], w=["pSc"])
            b.cp("dve", sc[:, 0].rearrange("p h k -> p (h k)"), pSc[:, 0:1024], r=["pSc"], w=["sc"])
            b.cp("dve", sc[:, 1].rearrange("p h k -> p (h k)"), pSc[:, 1024:2048], r=["pSc"], w=["sc"])
            for h in range(8):
                for sd in range(2):
                    src = sc[:, sd, h, :]
                    b.S.I("dve", (lambda o=v12[:, sd, h, 0:8], s_=src: nc.vector.max(out=o, in_=s_)), r=["sc"], w=["v12"])
                    b.S.I("dve", (lambda o=i12[:, sd, h, 0:8], m=v12[:, sd, h, 0:8], s_=src: nc.vector.max_index(out=o, in_max=m, in_values=s_)), r=["sc", "v12"], w=["i12"])
                    b.S.I("dve", (lambda o=wk[:, 0:128], m=v12[:, sd, h, 0:8], s_=src: nc.vector.match_replace(out=o, in_to_replace=m, in_values=s_, imm_value=-1e30)), r=["sc", "v12"], w=["wk"])
                    b.S.I("dve", (lambda o=v12[:, sd, h, 8:16], s_=wk[:, 0:128]: nc.vector.max(out=o, in_=s_)), r=["wk"], w=["v12"])
                    b.S.I("dve", (lambda o=i12[:, sd, h, 8:16], m=v12[:, sd, h, 8:16], s_=wk[:, 0:128]: nc.vector.max_index(out=o, in_max=m, in_values=s_)), r=["wk", "v12"], w=["i12"])
            b.tt("dve", cand[:].rearrange("p h (a c) -> p h a c", a=16), bcl(v12[:, 0, :, :], 16), v12[:, 1, :, :].unsqueeze(2).to_broadcast([128, 8, 16, 16]),
                 ALU.add, r=["v12"], w=["cand"])
            for h in range(8):
                src = cand[:, h, :]
                b.S.I("dve", (lambda o=tv[:, h, 0:8], s_=src: nc.vector.max(out=o, in_=s_)), r=["cand"], w=["tv"])
                b.S.I("dve", (lambda o=tj[:, h, 0:8], m=tv[:, h, 0:8], s_=src: nc.vector.max_index(out=o, in_max=m, in_values=s_)), r=["cand", "tv"], w=["tj"])
                b.S.I("dve", (lambda o=wk[:, 0:256], m=tv[:, h, 0:8], s_=src: nc.vector.match_replace(out=o, in_to_replace=m, in_values=s_, imm_value=-1e30)), r=["cand", "tv"], w=["wk"])
                b.S.I("dve", (lambda o=tv[:, h, 8:16], s_=wk[:, 0:256]: nc.vector.max(out=o, in_=s_)), r=["wk"], w=["tv"])
                b.S.I("dve", (lambda o=tj[:, h, 8:16], m=tv[:, h, 8:16], s_=wk[:, 0:256]: nc.vector.max_index(out=o, in_max=m, in_values=s_)), r=["wk", "tv"], w=["tj"])
            b.cp("dve", tbf[:], tj[:], r=["tj"], w=["tbf"])
            b.ts("dve", taf[:], tbf[:], 1.0 / 16.0, None, ALU.mult, r=["tbf"], w=["taf"])
            b.cp("dve", ta[:], taf[:], r=["taf"], w=["ta5"])
            b.cp("dve", sel1[:], ta[:], r=["ta5"], w=["sel1"])
            b.tt("dve", sel2[:], taf[:], sel1[:], ALU.subtract, r=["taf", "sel1"], w=["sel2"])
            b.ts("dve", sel2[:], sel2[:], 0.0, None, ALU.is_lt, r=["sel2"], w=["sel2"])
            b.tt("dve", taf[:], sel1[:], sel2[:], ALU.subtract, r=["sel1", "sel2"], w=["taf"])
            b.stt(tbf[:], taf[:], -16.0, tbf[:], ALU.mult, ALU.add, r=["taf", "tbf"], w=["tbf"])
            b.cp("dve", i12f[:], i12[:], r=["i12"], w=["i12f"])
            io16 = iota16[:].unsqueeze(1).unsqueeze(1).to_broadcast([128, 8, 16, 16])
            for (pos, side, dst, key) in ((taf, 0, sel1, "sel1"), (tbf, 1, sel2, "sel2")):
                b.tt("dve", eq[:], bcl(pos[:], 16), io16, ALU.is_equal, r=["taf", "tbf", "iota16"], w=["eq"])
                b.tt("dve", eq[:], eq[:], i12f[:, side, :, :].unsqueeze(2).to_broadcast([128, 8, 16, 16]), ALU.mult, r=["eq", "i12f"], w=["eq"])
                b.red(dst[:], eq[:], ALU.add, r=["eq"], w=[key])
            b.stt(idxf[:].rearrange("p (h k) -> p h k", h=8), sel1[:], 128.0, sel2[:], ALU.mult, ALU.add, r=["sel1", "sel2"], w=["idxf"])
            b.cp("dve", idx32[:], idxf[:], r=["idxf"], w=["idx32"])
            b.tt("dve", ge[:], tv[:], bcl(tv[:, :, 0], 16), ALU.subtract, r=["tv"], w=["ge"])
            b.act(ge[:], ge[:], AF.Exp, r=["ge"], w=["ge"])
            b.red(gsum[:], ge[:], ALU.add, r=["ge"], w=["gsum"])
            b.recip(gsum[:], gsum[:], r=["gsum"], w=["gsum"])
            b.tt("dve", gate[:].rearrange("p (h k) -> p h k", h=8), ge[:], bcl(gsum[:], 16), ALU.mult, r=["ge", "gsum"], w=["gate"])
            if "stop5a" in dbg:
                b.dma("sp", G["idx_d"], idx32[:], r=["idx32"], w=["idx_d"])
                b.dma("sp", G["gate_d"], gate[:], r=["gate"], w=["gate_d"])
                b.dma("sp", G["sc_d"], sc[:].rearrange("p s h k -> p (s h k)"), r=["sc"], w=["sc_d"])
                S.flush()
                return
            for g8 in range(16):
                sls = []
                for k in range(8):
                    hk = g8 * 8 + k
                    sl = nu % NS
                    nu += 1
                    sls.append(sl)
                    S.D("pool", (lambda o=uvb[sl][:], ix=idx32[:, hk:hk + 1]: nc.gpsimd.indirect_dma_start(
                        out=o, out_offset=None, in_=scr["puv_b"].rearrange("e a d -> e (a d)"), in_offset=bass.IndirectOffsetOnAxis(ap=ix, axis=0))),
                        r=["idx32"], w=["uvb%d" % sl])
                    b.stt(junk[:], uvb[sl][:, 0:D], 1.0, pX[:], ALU.mult, ALU.mult, r=["uvb%d" % sl, "pX"], w=["junk5", "actv"], accum=actv[:, hk:hk + 1])
                cs8 = slice(g8 * 8, (g8 + 1) * 8)
                gelu_tanh(b, wv[:, cs8], actv[:, cs8], idxf[:, cs8], "actv", "idxf", "wv")
                b.tt("dve", wv[:, cs8], wv[:, cs8], gate[:, cs8], ALU.mult, r=["wv", "gate"], w=["wv"])
                for k in range(8):
                    hk = g8 * 8 + k
                    sl = sls[k]
                    s3 = nv % 3
                    nv += 1
                    b.act(vt[s3][:], uvb[sl][:, D:2 * D], AF.Copy, r=["uvb%d" % sl, "wv"], w=["vt%d" % s3], scale=wv[:, hk:hk + 1])
                    for hf in range(2):
                        b.mm(acc[:, hf * 512:(hf + 1) * 512], ident_b[:], vt[s3][:, hf * 512:(hf + 1) * 512], start=(hk == 0), stop=(hk == 127),
                             r=["ident_b", "vt%d" % s3], w=["pSc"])
            b.tt("dve", tmpf[:], acc, rows[:, 0, :], ALU.mult, r=["pSc", "rows5"], w=["tmpf"])
            b.tt("dve", tmpf[:], tmpf[:], x1[i][:], ALU.add, r=["tmpf", kx], w=["tmpf"])
            b.act(junk[:], tmpf[:], AF.Square, r=["tmpf"], w=["junk5", "st5e"], accum=st5[:, 4:5])
            b.ts("dve", st5[:, 5:6], st5[:, 4:5], 1.0 / D, EPS, ALU.mult, ALU.add, r=["st5e"], w=["st5f"])
            b.act(st5[:, 6:7], st5[:, 5:6], AF.Sqrt, r=["st5f"], w=["st5g"])
            b.recip(st5[:, 7:8], st5[:, 6:7], r=["st5g"], w=["st5h"])
            b.stt(osb[i][:], tmpf[:], st5[:, 7:8], rows[:, 3, :], ALU.mult, ALU.mult, r=["tmpf", "st5h", "rows5"], w=["osb5_%d" % i])
            b.dma("sp", out[cs, :], osb[i][:], r=["osb5_%d" % i], w=["out"])
        S.flush()


def host_constants(half):
    t = np.arange(NT)
    if half == 1:
        t = t[::-1]
    posr = (t // 64).astype(np.float32)
    posc = (t % 64).astype(np.float32)
    p = np.arange(128)
    posT = np.where(((p % 64) < 32)[:, None], posr[None, :], posc[None, :]).astype(np.float32)
    fidx = (p % 16).astype(np.float32)[:, None]
    prot = np.zeros((128, 128), np.float32)
    for m in range(128):
        if (m % 32) < 16:
            prot[m + 16, m] = -1.0
        else:
            prot[m - 16, m] = 1.0
    selc = np.zeros((128, 8, 240), np.float32)
    for a in range(8):
        for q in range(16):
            selc[a * 16 + q, a, 112 + q] = 1.0
    cst = np.zeros((128, 512), np.float32)
    sidx = np.arange(128) // 16
    cst[:, 0:128] = (sidx[:, None] <= sidx[None, :]).astype(np.float32)
    cst[:, 128:256] = (sidx[:, None] >= sidx[None, :]).astype(np.float32)
    cst[:, 256:272] = np.arange(-7, 9, dtype=np.float32)[None, :]
    cst[:, 272:288] = (8 - np.arange(16, dtype=np.float32))[None, :]
    cst[:, 288:304] = (8.0 * (np.arange(16, dtype=np.float32) + 1))[None, :]
    cst[:, 304] = 1.0
    cst[:, 320:336] = np.arange(16, dtype=np.float32)[None, :]
    return dict(posT=np.ascontiguousarray(posT), fidx=fidx, prot=prot, selc=selc, cst=cst)


def make_in_maps(inputs):
    g = lambda k: np.asarray(inputs[k], dtype=np.float32)
    maps = []
    for core in range(8):
        bi, half = core // 2, core % 2
        xs = g("x")[bi]
        cs = g("ctx")[bi]
        dsel = [0, 1]
        if half == 1:
            xs = xs[::-1]
            cs = cs[::-1]
            dsel = [1, 0]
        m = dict(
            x_seq=np.ascontiguousarray(xs), ctx_seq=np.ascontiguousarray(cs), c_vec=np.ascontiguousarray(g("c")[bi]), c_ctx=g("c_ctx"),
            ada_w=g("ada_w")[0], ada_b=g("ada_b")[0], norm1_g=g("norm1_g")[0], norm2_g=g("norm2_g")[0], w_in=g("w_in")[0],
            lam4=np.ascontiguousarray(np.stack([g("lambda_q1")[0], g("lambda_k1")[0], g("lambda_q2")[0], g("lambda_k2")[0]])),
            subln_g=g("subln_g")[0], w_attn_up=g("w_attn_up")[0],
            a_re=np.ascontiguousarray(g("ssm_a_re")[0][dsel]), a_im=np.ascontiguousarray(g("ssm_a_im")[0][dsel]),
            log_dt=np.ascontiguousarray(g("ssm_log_dt")[0][dsel]), b_re=np.ascontiguousarray(g("ssm_b_re")[0][dsel]),
            b_im=np.ascontiguousarray(g("ssm_b_im")[0][dsel]), c_re=np.ascontiguousarray(g("ssm_c_re")[0][dsel]),
            c_im=np.ascontiguousarray(g("ssm_c_im")[0][dsel]), ssm_d=g("ssm_d")[0], w_glu=g("w_glu")[0], b_glu=g("b_glu")[0],
            w_ssm_up=g("w_ssm_up")[0], w_out=g("w_out")[0], w_query=g("peer_w_query")[0], sub_k1=g("peer_sub_k1")[0],
            sub_k2=g("peer_sub_k2")[0], peer_u=g("peer_u")[0], peer_v=g("peer_v")[0], final_g=g("final_norm_g"),
        )
        m.update(host_constants(half))
        maps.append(m)
    return maps


def kernel(**inputs):
    nc = build_program()
    maps = make_in_maps(inputs)
    res = run_bass_kernel_spmd(nc, maps, core_ids=list(range(8)))
    outp = np.zeros((4, NT, D), np.float32)
    for core in range(8):
        bi, half = core // 2, core % 2
        o = np.asarray(res.results[core]["out"], dtype=np.float32)
        if half == 0:
            outp[bi, :NOWN] = o
        else:
            outp[bi, NOWN:] = o[::-1]
    return outp
```
